# Optimizing a Trainium2 kernel written in Bass

```python
import math
import jax
import jax.numpy as jnp
from jax import lax
import numpy as np

D_MODEL = 1024
BATCH = 16
SEQ = 4096
DEPTH = 1

CTX_LEN = 256
GRID_W = 64
H_M = 8
D_HM = 64
W_M = H_M * D_HM
H_R = 8
D_HR = 64
W_R = H_R * D_HR
MIX_W = W_M + W_R
W_LORA = 64
A_LORA = 64
G_LORA = 128
CONV_K = 3
CONV_CH = 2 * W_M + 3 * W_R
SECTION_WIDTHS = (W_M, W_M, W_R, W_R, W_R, W_M, W_M, 4 * H_M, W_LORA, W_LORA, A_LORA, G_LORA)
IN_COLS = sum(SECTION_WIDTHS)
SPLIT_POINTS = tuple(int(v) for v in np.cumsum(SECTION_WIDTHS)[:-1])
CHUNK = 64
N_GROUPS = 4
E_PER_GROUP = 8
N_EXPERTS = N_GROUPS * E_PER_GROUP
TOP_K_IN_GROUP = 2
D_EXPERT = 512
MOE_BLOCK = 256
ALPHA = (2.0 * DEPTH) ** 0.25
BETA = (8.0 * DEPTH) ** -0.25
DECAY_SCALE = math.exp(-0.5)
LN_EPS = 1e-6
GN_EPS = 64e-5

kernel_name = 'hybrid_mlstm_rwkv7_hmoe_dit'


def _layernorm(z, gain=None, bias=None):
    zf = z.astype(jnp.float32)
    mu = jnp.mean(zf, -1, keepdims=True)
    var = jnp.mean(jnp.square(zf - mu), -1, keepdims=True)
    y = (zf - mu) * lax.rsqrt(var + LN_EPS)
    if gain is not None:
        y = y * gain + bias
    return y.astype(z.dtype)


def _headnorm(z, eps):
    zf = z.astype(jnp.float32)
    mu = jnp.mean(zf, -1, keepdims=True)
    var = jnp.mean(jnp.square(zf - mu), -1, keepdims=True)
    return (zf - mu) * lax.rsqrt(var + eps)


def _modulate(z, shift, scale):
    return _layernorm(z) * (1 + scale) + shift


def _rev_segments(u):
    return jnp.concatenate([jnp.flip(u[:, :CTX_LEN], 1), jnp.flip(u[:, CTX_LEN:], 1)], axis=1)


def _dir_stack(u_fwd, u_bwd):
    return jnp.concatenate([u_fwd, _rev_segments(u_bwd)], axis=0)


def _dir_merge(y):
    b = y.shape[0] // 2
    return y[:b] + _rev_segments(y[b:])


def _conv_grid(u, w, rows):
    b, l, ch = u.shape
    img = u.reshape(b, rows, l // rows, ch)
    out = lax.conv_general_dilated(img, w[:, :, None, :].astype(u.dtype), (1, 1), 'SAME',
                                   dimension_numbers=('NHWC', 'HWIO', 'NHWC'), feature_group_count=ch)
    return out.reshape(b, l, ch)


def _conv_seq(u, w_row):
    ch = u.shape[-1]
    return lax.conv_general_dilated(u, w_row[:, None, :].astype(u.dtype), (1,), 'SAME',
                                    dimension_numbers=('NWC', 'WIO', 'NWC'), feature_group_count=ch)


def _mlstm_chunkwise(q, k, v, log_i, log_f):
    z, t, h, dh = q.shape
    nc = t // CHUNK
    def chunks(a):
        a = a.astype(jnp.float32).reshape((z, nc, CHUNK, h) + a.shape[3:])
        return jnp.swapaxes(jnp.swapaxes(a, 0, 1), 2, 3)
    qc, kc, vc, lic, lfc = chunks(q), chunks(k), chunks(v), chunks(log_i), chunks(log_f)
    tri = jnp.tril(jnp.ones((CHUNK, CHUNK), bool))

    def step(carry, inp):
        c_mat, n_vec, m = carry
        qb, kb, vb, li, lf = inp
        b = jnp.cumsum(lf, -1)
        dmat = jnp.where(tri, b[..., :, None] - b[..., None, :] + li[..., None, :], -jnp.inf)
        m_inter = b + m[..., None]
        m_t = jnp.maximum(m_inter, jnp.max(dmat, -1))
        s = jnp.einsum('zhtd,zhsd->zhts', qb, kb) * jnp.exp(dmat - m_t[..., None])
        inter = jnp.exp(m_inter - m_t)
        num = inter[..., None] * jnp.einsum('zhtd,zhde->zhte', qb, c_mat) + jnp.einsum('zhts,zhse->zhte', s, vb)
        den = inter * jnp.einsum('zhtd,zhd->zht', qb, n_vec) + jnp.sum(s, -1)
        h_out = num / jnp.maximum(jnp.abs(den), jnp.exp(-m_t))[..., None]
        b_last = b[..., -1]
        g = b_last[..., None] - b + li
        m_new = jnp.maximum(b_last + m, jnp.max(g, -1))
        wts = jnp.exp(g - m_new[..., None])
        decay = jnp.exp(b_last + m - m_new)
        c_mat = decay[..., None, None] * c_mat + jnp.einsum('zhs,zhsd,zhse->zhde', wts, kb, vb)
        n_vec = decay[..., None] * n_vec + jnp.einsum('zhs,zhsd->zhd', wts, kb)
        return (c_mat, n_vec, m_new), h_out

    init = (jnp.zeros((z, h, dh, dh), jnp.float32), jnp.zeros((z, h, dh), jnp.float32), jnp.zeros((z, h), jnp.float32))
    _, hs = lax.scan(step, init, (qc, kc, vc, lic, lfc))
    return jnp.swapaxes(jnp.swapaxes(hs, 2, 3), 0, 1).reshape(z, t, h, dh)


def _mlstm_group(mq, mk, mv, mo, gates, b_i, b_f, norm_w):
    b, t, _ = mq.shape
    q = jax.nn.silu(mq).reshape(b, t, H_M, D_HM)
    k = jax.nn.silu(mk).reshape(b, t, H_M, D_HM) * (D_HM ** -0.5)
    v = mv.reshape(b, t, H_M, D_HM)
    gt = gates.astype(jnp.float32).reshape(b, t, 4, H_M)
    li_f = gt[:, :, 0] + b_i[0]
    lf_f = jax.nn.log_sigmoid(gt[:, :, 1] + b_f[0])
    li_b = gt[:, :, 2] + b_i[1]
    lf_b = jax.nn.log_sigmoid(gt[:, :, 3] + b_f[1])
    h = _mlstm_chunkwise(_dir_stack(q, q), _dir_stack(k, k), _dir_stack(v, v),
                         _dir_stack(li_f, li_b), _dir_stack(lf_f, lf_b))
    h = _headnorm(_dir_merge(h), LN_EPS) * norm_w.reshape(H_M, D_HM)
    return jax.nn.sigmoid(mo) * h.reshape(b, t, W_M)


def _rwkv7_scan(r, w, k, v, kh, a):
    z, t, h, n = r.shape
    seq = tuple(jnp.moveaxis(s.astype(jnp.float32), 1, 0) for s in (r, w, k, v, kh, a))

    def step(state, inp):
        r_t, w_t, k_t, v_t, kh_t, a_t = inp
        removed = jnp.einsum('zhvk,zhk->zhv', state, kh_t)
        state = (state * w_t[:, :, None, :] - removed[..., None] * (kh_t * a_t)[:, :, None, :]
                 + v_t[..., None] * k_t[:, :, None, :])
        return state, jnp.einsum('zhvk,zhk->zhv', state, r_t)

    _, y = lax.scan(step, jnp.zeros((z, h, n, n), jnp.float32), seq)
    return jnp.moveaxis(y, 0, 1)


def _rwkv7_group(rr, rk, rv, lw_f, lw_b, la, lg, w0, w_b_mat, a0, a_b_mat, g_b_mat, kk, ka, bonus_w, gn_w, gn_b):
    b, t, _ = rr.shape
    shp = (b, t, H_R, D_HR)
    w_fwd = jnp.exp(-DECAY_SCALE * jax.nn.sigmoid((w0[0] + jnp.tanh(lw_f) @ w_b_mat[0]).astype(jnp.float32)))
    w_bwd = jnp.exp(-DECAY_SCALE * jax.nn.sigmoid((w0[1] + jnp.tanh(lw_b) @ w_b_mat[1]).astype(jnp.float32)))
    a = jax.nn.sigmoid(a0 + la @ a_b_mat)
    g = jax.nn.sigmoid(lg) @ g_b_mat
    kap = (rk * kk).reshape(shp).astype(jnp.float32)
    kh = kap / jnp.maximum(jnp.sqrt(jnp.sum(jnp.square(kap), -1, keepdims=True)), 1e-12)
    kmod = (rk * (1 + (a - 1) * ka)).reshape(shp)
    r = rr.reshape(shp)
    v = rv.reshape(shp)
    a_h = a.reshape(shp)
    y = _rwkv7_scan(_dir_stack(r, r), _dir_stack(w_fwd.reshape(shp), w_bwd.reshape(shp)), _dir_stack(kmod, kmod),
                    _dir_stack(v, v), _dir_stack(kh, kh), _dir_stack(a_h, a_h))
    y = _headnorm(_dir_merge(y), GN_EPS) * gn_w.reshape(H_R, D_HR) + gn_b.reshape(H_R, D_HR)
    bonus = jnp.sum(r * kmod * bonus_w, -1, keepdims=True) * v
    return (y + bonus).reshape(b, t, W_R) * g


def _mixer(h_ctx, h_lat, rows, w_in, conv_w, m_bias_i, m_bias_f, m_norm_w, r_w0, r_wB, r_a0, r_aB, r_gB,
           r_kk, r_ka, r_bonus, r_norm_w, r_norm_b):
    p_ctx = h_ctx @ w_in
    p_lat = h_lat @ w_in
    p_ctx = jnp.concatenate([_conv_seq(p_ctx[..., :CONV_CH], conv_w[CONV_K // 2]), p_ctx[..., CONV_CH:]], -1)
    p_lat = jnp.concatenate([_conv_grid(p_lat[..., :CONV_CH], conv_w, rows), p_lat[..., CONV_CH:]], -1)
    u = jnp.concatenate([p_ctx, p_lat], axis=1)
    mq, mk, rr, rk, rv, mv, mo, gates, lw_f, lw_b, la, lg = jnp.split(u, SPLIT_POINTS, axis=-1)
    out_m = _mlstm_group(mq, mk, mv, mo, gates, m_bias_i, m_bias_f, m_norm_w)
    out_r = _rwkv7_group(rr, rk, rv, lw_f, lw_b, la, lg, r_w0, r_wB, r_a0, r_aB, r_gB, r_kk, r_ka, r_bonus,
                         r_norm_w, r_norm_b)
    return jnp.concatenate([out_m.astype(out_r.dtype), out_r], -1)


def _hier_moe(h, rt_g, rt_g_b, rt_e, rt_e_b, ex_gate, ex_up, ex_down):
    b, t, d = h.shape
    tok = h.reshape(b * t, d)
    n = tok.shape[0]
    lg = (tok @ rt_g + rt_g_b).astype(jnp.float32)
    p_grp = jax.nn.softmax(lg, axis=-1)
    grp = jnp.argmax(lg, axis=-1).astype(jnp.int32)
    p_top = jnp.take_along_axis(p_grp, grp[:, None], axis=-1)
    le = (tok @ rt_e + rt_e_b).astype(jnp.float32).reshape(n, N_GROUPS, E_PER_GROUP)
    le_grp = jnp.take_along_axis(le, jnp.broadcast_to(grp[:, None, None], (n, 1, E_PER_GROUP)), axis=1)[:, 0]
    top_val, top_idx = lax.top_k(le_grp, TOP_K_IN_GROUP)
    wts = jax.nn.softmax(top_val, axis=-1) * p_top
    e_flat = (grp[:, None] * E_PER_GROUP + top_idx.astype(jnp.int32)).reshape(-1)
    w_flat = wts.reshape(-1)
    t_flat = jnp.repeat(jnp.arange(n, dtype=jnp.int32), TOP_K_IN_GROUP)
    order = jnp.argsort(e_flat)
    e_s, t_s, w_s = e_flat[order], t_flat[order], w_flat[order]
    counts = jnp.zeros((N_EXPERTS,), jnp.int32).at[e_flat].add(1)
    starts = jnp.cumsum(counts) - counts
    padded = (counts + MOE_BLOCK - 1) // MOE_BLOCK * MOE_BLOCK
    pad_ends = jnp.cumsum(padded)
    pad_starts = pad_ends - padded
    dest = pad_starts[e_s] + jnp.arange(e_s.shape[0], dtype=jnp.int32) - starts[e_s]
    n_pairs = n * TOP_K_IN_GROUP
    buf = (-(-n_pairs // MOE_BLOCK) + N_EXPERTS) * MOE_BLOCK
    tok_buf = jnp.full((buf,), n, jnp.int32).at[dest].set(t_s)
    w_buf = jnp.zeros((buf,), w_s.dtype).at[dest].set(w_s)
    n_blk = buf // MOE_BLOCK
    blk_exp = jnp.minimum(jnp.searchsorted(pad_ends, jnp.arange(n_blk, dtype=jnp.int32) * MOE_BLOCK, side='right'),
                          N_EXPERTS - 1)
    tok_pad = jnp.concatenate([tok, jnp.zeros((1, d), tok.dtype)], 0)

    def expert_block(args):
        idx, w, e = args
        xb = tok_pad[idx]
        hb = jax.nn.silu(xb @ ex_gate[e]) * (xb @ ex_up[e])
        return (hb @ ex_down[e]) * w[:, None]

    y = lax.map(expert_block, (tok_buf.reshape(n_blk, MOE_BLOCK), w_buf.reshape(n_blk, MOE_BLOCK), blk_exp))
    out = jnp.zeros((n + 1, d), y.dtype).at[tok_buf].add(y.reshape(buf, d))
    return out[:n].reshape(b, t, d).astype(h.dtype)


def setup_inputs(seed: int = 0) -> dict:
    key = jax.random.key(seed)
    ks = jax.random.split(key, 40)
    def nrm(k, shape, s):
        return jax.random.normal(k, shape, jnp.float32) * s
    L = DEPTH
    return {
        'x': nrm(ks[0], (BATCH, SEQ, D_MODEL), 1.0),
        'c': nrm(ks[1], (BATCH, D_MODEL), 1.0),
        'ctx': nrm(ks[2], (BATCH, CTX_LEN, D_MODEL), 1.0),
        'c_ctx': nrm(ks[3], (D_MODEL,), 1.0),
        'w_ada': nrm(ks[4], (L, D_MODEL, 6 * D_MODEL), D_MODEL ** -0.5),
        'b_ada': nrm(ks[5], (L, 6 * D_MODEL), 0.02),
        'w_in': nrm(ks[6], (L, D_MODEL, IN_COLS), D_MODEL ** -0.5),
        'conv_w': nrm(ks[7], (L, CONV_K, CONV_K, CONV_CH), 0.3),
        'm_bias_i': nrm(ks[8], (L, 2, H_M), 0.5),
        'm_bias_f': 3.0 + nrm(ks[9], (L, 2, H_M), 0.5),
        'm_norm_w': 1.0 + nrm(ks[10], (L, W_M), 0.05),
        'r_w0': nrm(ks[11], (L, 2, W_R), 0.5),
        'r_wB': nrm(ks[12], (L, 2, W_LORA, W_R), W_LORA ** -0.5),
        'r_a0': nrm(ks[13], (L, W_R), 0.5),
        'r_aB': nrm(ks[14], (L, A_LORA, W_R), A_LORA ** -0.5),
        'r_gB': nrm(ks[15], (L, G_LORA, W_R), G_LORA ** -0.5),
        'r_kk': 0.85 + nrm(ks[16], (L, W_R), 0.05),
        'r_ka': 1.0 + nrm(ks[17], (L, W_R), 0.05),
        'r_bonus': nrm(ks[18], (L, H_R, D_HR), 0.1),
        'r_norm_w': 1.0 + nrm(ks[19], (L, W_R), 0.05),
        'r_norm_b': nrm(ks[20], (L, W_R), 0.02),
        'w_out': nrm(ks[21], (L, MIX_W, D_MODEL), BETA * MIX_W ** -0.5),
        'ln1_g': 1.0 + nrm(ks[22], (L, D_MODEL), 0.05),
        'ln1_b': nrm(ks[23], (L, D_MODEL), 0.02),
        'ln2_g': 1.0 + nrm(ks[24], (L, D_MODEL), 0.05),
        'ln2_b': nrm(ks[25], (L, D_MODEL), 0.02),
        'rt_g': nrm(ks[26], (L, D_MODEL, N_GROUPS), D_MODEL ** -0.5),
        'rt_g_b': nrm(ks[27], (L, N_GROUPS), 0.01),
        'rt_e': nrm(ks[28], (L, D_MODEL, N_EXPERTS), D_MODEL ** -0.5),
        'rt_e_b': nrm(ks[29], (L, N_EXPERTS), 0.01),
        'ex_gate': nrm(ks[30], (L, N_EXPERTS, D_MODEL, D_EXPERT), D_MODEL ** -0.5),
        'ex_up': nrm(ks[31], (L, N_EXPERTS, D_MODEL, D_EXPERT), D_MODEL ** -0.5),
        'ex_down': nrm(ks[32], (L, N_EXPERTS, D_EXPERT, D_MODEL), BETA * D_EXPERT ** -0.5),
    }


def reference(x, c, ctx, c_ctx, w_ada, b_ada, w_in, conv_w, m_bias_i, m_bias_f, m_norm_w, r_w0, r_wB, r_a0, r_aB,
              r_gB, r_kk, r_ka, r_bonus, r_norm_w, r_norm_b, w_out, ln1_g, ln1_b, ln2_g, ln2_b, rt_g, rt_g_b, rt_e,
              rt_e_b, ex_gate, ex_up, ex_down):
    rows = x.shape[1] // GRID_W
    for l in range(DEPTH):
        mod = jax.nn.silu(c) @ w_ada[l] + b_ada[l]
        mod_c = jax.nn.silu(c_ctx) @ w_ada[l] + b_ada[l]
        sh1, sc1, g1, sh2, sc2, g2 = jnp.split(mod[:, None, :], 6, axis=-1)
        sh1c, sc1c, g1c, sh2c, sc2c, g2c = jnp.split(mod_c, 6)
        mixed = _mixer(_modulate(ctx, sh1c, sc1c), _modulate(x, sh1, sc1), rows, w_in[l], conv_w[l], m_bias_i[l],
                       m_bias_f[l], m_norm_w[l], r_w0[l], r_wB[l], r_a0[l], r_aB[l], r_gB[l], r_kk[l], r_ka[l],
                       r_bonus[l], r_norm_w[l], r_norm_b[l])
        x = _layernorm(ALPHA * x + g1 * (mixed[:, CTX_LEN:] @ w_out[l]), ln1_g[l], ln1_b[l])
        ffn = _hier_moe(_modulate(x, sh2, sc2), rt_g[l], rt_g_b[l], rt_e[l], rt_e_b[l], ex_gate[l], ex_up[l], ex_down[l])
        x = _layernorm(ALPHA * x + g2 * ffn, ln2_g[l], ln2_b[l])
        if l + 1 < DEPTH:
            ctx = _layernorm(ALPHA * ctx + g1c * (mixed[:, :CTX_LEN] @ w_out[l]), ln1_g[l], ln1_b[l])
            ffn_c = _hier_moe(_modulate(ctx, sh2c, sc2c), rt_g[l], rt_g_b[l], rt_e[l], rt_e_b[l], ex_gate[l],
                              ex_up[l], ex_down[l])
            ctx = _layernorm(ALPHA * ctx + g2c * ffn_c, ln2_g[l], ln2_b[l])
    return x
```

```python
import math, os
from contextlib import ExitStack
import numpy as np
import concourse.bass as bass
import concourse.mybir as mybir
from concourse.bass_utils import run_bass_kernel_spmd

F32 = mybir.dt.float32
BF16 = mybir.dt.bfloat16
I32 = mybir.dt.int32
AF = mybir.ActivationFunctionType
ALU = mybir.AluOpType
AX = mybir.AxisListType

D = 1024
SEQ = 4096
CTX = 256
T = SEQ + CTX
NCH = T // 64
INC = 3936
DS = math.exp(-0.5)
ALPHA = 2.0 ** 0.25
LN_EPS = 1e-6
GN_EPS = 64e-5
SEC = dict(mq=0, mk=512, rr=1024, rk=1536, rv=2048, mv=2560, mo=3072, gates=3584,
           lwf=3616, lwb=3680, la=3744, lg=3808)
NDS = 40


class _Stop(Exception):
    pass


class Buf:
    def __init__(self, t):
        self.t = t
        self.w = {}
        self.r = {}

    def __getitem__(self, k):
        return self.t[k]


def _merge(d, s):
    for k, v in s.items():
        if d.get(k, 0) < v:
            d[k] = v


class KB:
    def __init__(self, nc):
        self.nc = nc
        self.engs = {'pe': nc.tensor, 'dve': nc.vector, 'act': nc.scalar, 'pool': nc.gpsimd, 'sp': nc.sync}
        self.esem = {e: nc.alloc_semaphore('es_' + e) for e in self.engs}
        self.ecnt = {e: 0 for e in self.engs}
        self.pending = {e: False for e in self.engs}
        self.seen = {e: {} for e in self.engs}
        self.dsem = [nc.alloc_semaphore('ds%d' % i) for i in range(NDS)]
        self.dcnt = [0] * NDS
        self.dnext = 0
        self.es = None
        self.uid = 0

    def semh(self, key):
        return self.esem[key] if isinstance(key, str) else self.dsem[key[1]]

    def _wait(self, eng, need):
        for key, cnt in need.items():
            if self.seen[eng].get(key, 0) >= cnt:
                continue
            if key == eng and eng in ('pe',):
                continue
            self.engs[eng].wait_ge(self.semh(key), cnt)
            self.seen[eng][key] = cnt

    def op(self, eng, fn, reads=(), writes=(), inc=True):
        need = {}
        for b in reads:
            _merge(need, b.w)
        for b in writes:
            _merge(need, b.w)
            _merge(need, b.r)
        self._wait(eng, need)
        ins = fn(self.engs[eng])
        cnt = self.ecnt[eng] + 1
        if inc:
            ins.then_inc(self.esem[eng], 1)
            self.ecnt[eng] = cnt
        for b in reads:
            b.r[eng] = cnt
        for b in writes:
            b.w = {eng: cnt}
            b.r = {}
        return ins

    def dma(self, q, out, in_, reads=(), writes=(), pw=(), indirect=None, **kw):
        i = self.dnext
        self.dnext = (i + 1) % NDS
        need = {}
        if self.dcnt[i]:
            need[('d', i)] = self.dcnt[i]
        for b in reads:
            _merge(need, b.w)
        for b in writes:
            _merge(need, b.w)
            _merge(need, b.r)
        for b in pw:
            _merge(need, b.r)
        self._wait(q, need)
        if indirect is None:
            ins = self.engs[q].dma_start(out=out, in_=in_, **kw)
        else:
            ins = self.engs[q].indirect_dma_start(out, indirect[0], in_, indirect[1], **kw)
        self.dcnt[i] += 16
        ins.then_inc(self.dsem[i], 16)
        key = ('d', i)
        cnt = self.dcnt[i]
        for b in reads:
            b.r[key] = cnt
        for b in writes:
            b.w = {key: cnt}
            b.r = {}
        for b in pw:
            b.w[key] = cnt
        return ins

    def barrier(self):
        need = {e: c for e, c in self.ecnt.items() if c}
        for i in range(NDS):
            if self.dcnt[i]:
                need[('d', i)] = self.dcnt[i]
        for e in self.engs:
            self._wait(e, need)

    def sb(self, name, shape, dt):
        self.uid += 1
        return Buf(self.es.enter_context(self.nc.sbuf_tensor("s%d_%s" % (self.uid, name), list(shape), dt)))

    def ps(self, name, shape, dt=F32):
        self.uid += 1
        return Buf(self.es.enter_context(self.nc.psum_tensor("p%d_%s" % (self.uid, name), list(shape), dt)))


def build(NB=2, debug=None, phases=(0, 1, 2, 3, 4, 5, 6)):
    nc = bass.Bass("TRN2", target_bir_lowering=False)
    k = KB(nc)

    def din(name, shape):
        return nc.dram_tensor(name, list(shape), F32, kind="ExternalInput").ap()

    x_in = din("x", [NB, SEQ, D])
    ctx_in = din("ctx", [NB, CTX, D])
    cc_in = din("cc", [3, D])
    w_ada = din("w_ada", [D, 6 * D])
    b_ada = din("b_ada", [1, 6 * D])
    w_in = din("w_in", [D, INC])
    conv_w = din("conv_w", [9, 2560])
    m_bias = din("m_bias", [32, 1])
    ident_in = din("ident", [128, 128])
    gmask_in = din("gmask", [32, 2])
    cmask_in = din("cmask", [64, 2, 64])
    m_norm_w = din("m_norm_w", [1, 512])
    rp_in = din("rp", [64, 8, 7])
    smask_in = din("smask", [64, 1088])
    rmask_in = din("rmask", [128, 2, 128])
    nmask_in = din("nmask", [64, 2, 64])
    r_norm_w = din("r_norm_w", [1, 512])
    r_norm_b = din("r_norm_b", [1, 512])
    r_wB = din("r_wB", [2, 64, 512])
    r_aB = din("r_aB", [64, 512])
    r_gB = din("r_gB", [128, 512])
    w_out = din("w_out", [D, D])
    ln1_g = din("ln1_g", [1, D]); ln1_b = din("ln1_b", [1, D]); ln2_g = din("ln2_g", [1, D]); ln2_b = din("ln2_b", [1, D])
    rt_in = din("rt", [D, 36]); rtb_in = din("rtb", [1, 36])
    ex_gate = din("ex_gate", [32 * D, 512]); ex_up = din("ex_up", [32 * D, 512]); ex_down = din("ex_down", [32 * 512, D])
    tris_in = din("tris", [128, 128]); thr_in = din("thr", [128, 128]); blki_in = din("blki", [128, 160]); kcp_in = din("kcp", [128, 12])
    tokid_in = nc.dram_tensor("tokid", [128, 64, 16], I32, kind="ExternalInput").ap()
    out_d = nc.dram_tensor("out", [NB, SEQ, D], F32, kind="ExternalOutput").ap()

    def dscr(name, shape, dt=F32):
        kind = "ExternalOutput" if (debug and name in debug) else "Internal"
        return Buf(nc.dram_tensor(name, list(shape), dt, kind=kind).ap())

    MODD = dscr("MODD", [3, 6 * D])
    FM = [dscr("FM%d" % b, [INC, T]) for b in range(NB)]
    MIX = [dscr("MIX%d" % b, [SEQ, D]) for b in range(NB)]
    NBLK_ = NB * SEQ * 2 // 128 + 32
    X1 = dscr("X1", [NB * SEQ, D])
    H2 = dscr("H2", [NB * SEQ, D], BF16)
    TOKB = dscr("TOKB", [NBLK_ * 128, 16], I32)
    YB = dscr("YB", [NBLK_ * 128, D])

    with ExitStack() as es0:
        k.es = es0
        ident = k.sb("ident", [128, 128], F32)
        identb = k.sb("identb", [128, 128], BF16)
        ones_f = k.sb("ones_f", [128, 128], F32)
        modT = k.sb("modT", [128, 48, 3], F32)
        k.dma('sp', ident[:], ident_in[:, :], writes=[ident])
        k.op('dve', lambda e: e.tensor_copy(identb[:], ident[:]), reads=[ident], writes=[identb])

        with ExitStack() as es:
            k.es = es
            cc = k.sb("cc", [3, D], F32)
            scT = k.sb("scT", [128, 8, 3], F32)
            bada = k.sb("bada", [3, 6 * D], F32)
            mods = k.sb("mods", [3, 6 * D], F32)
            wa = [k.sb("wa%d" % i, [128, 8, 512], F32) for i in range(2)]
            pst = k.ps("p0t", [128, 8, 3])
            psm = [k.ps("p0m%d" % i, [3, 512]) for i in range(2)]
            pmt = k.ps("p0mt", [128, 48, 3])
            k.dma('sp', cc[:], cc_in[:, :], writes=[cc])
            k.dma('sp', bada[:], b_ada[0:1, :].partition_broadcast(3), writes=[bada])
            k.op('act', lambda e: e.activation(cc[:], cc[:], AF.Silu), reads=[cc], writes=[cc])
            for kc in range(8):
                k.op('pe', lambda e: e.transpose(pst[:, kc, :], cc[:, kc * 128:(kc + 1) * 128], ident[0:3, 0:3]),
                     reads=[cc, ident], writes=[pst], inc=(kc == 7))
            k.op('dve', lambda e: e.tensor_copy(scT[:], pst[:]), reads=[pst], writes=[scT])
            for n in range(12):
                wb = wa[n % 2]
                k.dma('sp', wb[:], w_ada[:, n * 512:(n + 1) * 512].rearrange("(kc p) n -> p kc n", p=128), writes=[wb])
                pm = psm[n % 2]
                for kc in range(8):
                    k.op('pe', lambda e: e.matmul(pm[:], scT[:, kc, :], wb[:, kc, :], start=(kc == 0), stop=(kc == 7)),
                         reads=[scT, wb], writes=[pm] if kc == 0 else [], inc=(kc == 7))
                k.op('dve', lambda e: e.tensor_tensor(mods[:, n * 512:(n + 1) * 512], pm[:], bada[:, n * 512:(n + 1) * 512], ALU.add),
                     reads=[pm, bada], writes=[mods])
            k.dma('sp', MODD[:, :], mods[:], reads=[mods], writes=[MODD])
            for j in range(48):
                k.op('pe', lambda e: e.transpose(pmt[:, j, :], mods[:, j * 128:(j + 1) * 128], ident[0:3, 0:3]),
                     reads=[mods, ident], writes=[pmt], inc=(j == 47))
            k.op('dve', lambda e: e.tensor_copy(modT[:], pmt[:]), reads=[pmt], writes=[modT])
            for j0 in (8, 32):
                k.op('dve', lambda e: e.tensor_scalar(modT[:, j0:j0 + 8, :], modT[:, j0:j0 + 8, :], 1.0, None, ALU.add),
                     reads=[modT], writes=[modT])
        k.barrier()

        with ExitStack() as es:
          if 1 in phases:
              k.es = es
              wbf = k.sb("wbf", [128, 8, INC], BF16)
              hT = k.sb("hT", [128, 8, T], BF16)
              xt = [k.sb("xt%d" % i, [128, D], F32) for i in range(2)]
              xn = [k.sb("xn%d" % i, [128, D], BF16) for i in range(2)]
              st = k.sb("st", [128, 2, 6], F32)
              mv = k.sb("mv", [128, 2], F32)
              rstd = k.sb("rstd", [128, 1], F32)
              pT = [k.sb("pT%d" % i, [128, T], F32) for i in range(2)]
              acc = k.sb("acc", [128, T], F32)
              cw = k.sb("cw", [128, 20, 9], F32)
              mb = k.sb("mb", [32, 4], F32)
              ptr = [k.ps("p1t%d" % i, [128, 8, 128], BF16) for i in range(2)]
              pmm = [k.ps("p1m%d" % i, [128, 512]) for i in range(3)]
              pcw = k.ps("p1cw", [128, 20, 9])
              for kc in range(8):
                  for hf in range(2):
                      k.dma('pool', wbf[:, kc, hf * 1968:(hf + 1) * 1968],
                            w_in[kc * 128:(kc + 1) * 128, hf * 1968:(hf + 1) * 1968], pw=[wbf])
              crow = k.sb("crow", [9, 2560], F32)
              k.dma('sp', crow[:], conv_w[:, :], writes=[crow])
              for c in range(20):
                  k.op('pe', lambda e: e.transpose(pcw[:, c, :], crow[:, c * 128:(c + 1) * 128], ident[0:9, 0:9]),
                       reads=[crow, ident], writes=[pcw], inc=(c == 19))
              k.op('dve', lambda e: e.tensor_copy(cw[:], pcw[:]), reads=[pcw], writes=[cw])
              k.dma('sp', mb[:, 0:1], m_bias[:, :], writes=[mb])
              k.op('dve', lambda e: e.tensor_scalar(mb[:, 1:2], mb[:, 0:1], -1.0, None, ALU.mult), reads=[mb], writes=[mb])
              k.dma('sp', mb[:, 2:4], gmask_in[:, :], pw=[mb])
              for b in range(NB):
                  for i in range(int(os.environ.get('P1A', T // 128))):
                      xb, xnb, pt = xt[i % 2], xn[i % 2], ptr[i % 2]
                      src = ctx_in[b, i * 128:(i + 1) * 128, :] if i < 2 else x_in[b, (i - 2) * 128:(i - 1) * 128, :]
                      r = 2 if i < 2 else b
                      k.dma('sp', xb[:], src, writes=[xb])
                      S1 = int(os.environ.get('P1S', 9))
                      for hf in range(2):
                          k.op('dve', lambda e: e.bn_stats(st[:, hf, :], xb[:, hf * 512:(hf + 1) * 512]), reads=[xb], writes=[st])
                      if S1 >= 2: k.op('dve', lambda e: e.bn_aggr(mv[:], st[:].rearrange("p a b -> p (a b)")), reads=[st], writes=[mv])
                      if S1 >= 3: k.op('act', lambda e: e.activation(rstd[:], mv[:, 1:2], AF.Sqrt, bias=LN_EPS), reads=[mv], writes=[rstd])
                      if S1 >= 4: k.op('dve', lambda e: e.reciprocal(rstd[:], rstd[:]), reads=[rstd], writes=[rstd])
                      if S1 >= 5: k.op('dve', lambda e: e.tensor_scalar(xnb[:], xb[:], mv[:, 0:1], rstd[:, 0:1], ALU.subtract, ALU.mult),
                           reads=[xb, mv, rstd], writes=[xnb])
                      for kc in range(8 if S1 >= 6 else 0):
                          k.op('pe', lambda e: e.transpose(pt[:, kc, :], xnb[:, kc * 128:(kc + 1) * 128], identb[:]),
                               reads=[xnb, identb], writes=[pt] if kc == 0 else [], inc=(kc == 7))
                      for kc in range(8 if S1 >= 7 else 0):
                          if i % 2 == 0:
                              k.op('act', lambda e: e.activation(hT[:, kc, i * 128:(i + 1) * 128], pt[:, kc, :], AF.Identity,
                                                                 bias=modT[:, kc, r:r + 1], scale=modT[:, 8 + kc, r:r + 1]),
                                   reads=[pt, modT], writes=[] if (i or kc) else [hT])
                          else:
                              k.op('dve', lambda e: e.tensor_scalar(hT[:, kc, i * 128:(i + 1) * 128], pt[:, kc, :],
                                                                    modT[:, 8 + kc, r:r + 1], modT[:, kc, r:r + 1], ALU.mult, ALU.add),
                                   reads=[pt, modT], writes=[])
                      hT.w['act'] = k.ecnt['act']
                      hT.w['dve'] = k.ecnt['dve']
                  chunks = [(c * 128, 128) for c in range(28)] + [(3584, 32), (3616, 64), (3680, 64), (3744, 64), (3808, 128)]
                  for ci, (c0, M) in enumerate(chunks[:int(os.environ.get('P1C', 99))]):
                      pb = pT[ci % 2]
                      for g in range(9):
                          t0 = g * 512
                          n = min(512, T - t0)
                          pm = pmm[(ci * 9 + g) % 3]
                          for kc in range(8):
                              k.op('pe', lambda e: e.matmul(pm[0:M, 0:n], wbf[:, kc, c0:c0 + M], hT[:, kc, t0:t0 + n],
                                                            start=(kc == 0), stop=(kc == 7)),
                                   reads=[wbf, hT], writes=[pm] if kc == 0 else [], inc=(kc == 7))
                          k.op('act', lambda e: e.activation(pb[0:M, t0:t0 + n], pm[0:M, 0:n], AF.Identity),
                               reads=[pm], writes=[pb] if g == 0 else [])
                          pb.w['act'] = k.ecnt['act']
                      src = pb
                      if c0 < 2560:
                          c = c0 // 128
                          k.op('act', lambda e: e.activation(acc[:, :], pb[:, :], AF.Identity, scale=cw[:, c, 4:5]),
                               reads=[pb, cw], writes=[acc])
                          k.op('dve', lambda e: e.scalar_tensor_tensor(acc[:, 1:CTX], pb[:, 0:CTX - 1], cw[:, c, 3:4], acc[:, 1:CTX], ALU.mult, ALU.add),
                               reads=[pb, cw, acc], writes=[acc])
                          k.op('dve', lambda e: e.scalar_tensor_tensor(acc[:, 0:CTX - 1], pb[:, 1:CTX], cw[:, c, 5:6], acc[:, 0:CTX - 1], ALU.mult, ALU.add),
                               reads=[pb, cw, acc], writes=[acc])
                          a3 = acc[:, CTX:T].rearrange("p (r c) -> p r c", c=64)
                          p3 = pb[:, CTX:T].rearrange("p (r c) -> p r c", c=64)
                          for ky in range(3):
                              for kx in range(3):
                                  if ky == 1 and kx == 1:
                                      continue
                                  dy, dx = ky - 1, kx - 1
                                  oy0, oy1 = max(0, -dy), 64 - max(0, dy)
                                  ox0, ox1 = max(0, -dx), 64 - max(0, dx)
                                  k.op('dve', lambda e: e.scalar_tensor_tensor(
                                      a3[:, oy0:oy1, ox0:ox1], p3[:, oy0 + dy:oy1 + dy, ox0 + dx:ox1 + dx],
                                      cw[:, c, ky * 3 + kx:ky * 3 + kx + 1], a3[:, oy0:oy1, ox0:ox1], ALU.mult, ALU.add),
                                      reads=[pb, cw, acc], writes=[acc])
                          src = acc
                      sec = [s for s, v in SEC.items() if v <= c0][-1]
                      if sec in ('mq', 'mk'):
                          k.op('act', lambda e: e.activation(acc[:, :], src[:, :], AF.Silu), reads=[src], writes=[acc])
                          if sec == 'mk':
                              k.op('dve', lambda e: e.tensor_scalar(acc[:, :], acc[:, :], 0.125, None, ALU.mult), reads=[acc], writes=[acc])
                          src = acc
                      elif sec in ('mo', 'lg'):
                          k.op('act', lambda e: e.activation(acc[0:M, :], src[0:M, :], AF.Sigmoid), reads=[src], writes=[acc])
                          src = acc
                      elif sec in ('lwf', 'lwb'):
                          k.op('act', lambda e: e.activation(acc[0:M, :], src[0:M, :], AF.Tanh), reads=[src], writes=[acc])
                          src = acc
                      elif sec == 'gates':
                          tmp = pT[1 - ci % 2]
                          k.op('act', lambda e: e.activation(tmp[0:32, :], pb[0:32, :], AF.Exp, bias=mb[:, 1:2], scale=-1.0),
                               reads=[pb, mb], writes=[tmp])
                          k.op('act', lambda e: e.activation(tmp[0:32, :], tmp[0:32, :], AF.Ln, bias=1.0), reads=[tmp], writes=[tmp])
                          k.op('dve', lambda e: e.tensor_scalar(tmp[0:32, :], tmp[0:32, :], mb[:, 3:4], None, ALU.mult), reads=[tmp, mb], writes=[tmp])
                          k.op('dve', lambda e: e.tensor_scalar(acc[0:32, :], pb[0:32, :], mb[:, 0:1], mb[:, 2:3], ALU.add, ALU.mult),
                               reads=[pb, mb], writes=[acc])
                          k.op('dve', lambda e: e.tensor_tensor(acc[0:32, :], acc[0:32, :], tmp[0:32, :], ALU.add), reads=[acc, tmp], writes=[acc])
                          src = acc
                      k.dma('sp', FM[b][c0:c0 + M, :], src[0:M, :], reads=[src], pw=[FM[b]])
        k.barrier()


        with ExitStack() as es:
          if 2 in phases:
            k.es = es
            cm = k.sb("cm", [64, 2, 64], F32)
            k.dma('sp', cm[:], cmask_in[:, :, :], writes=[cm])
            nw = k.sb("nw", [64, 512], F32)
            k.dma('sp', nw[:], m_norm_w[0:1, :].partition_broadcast(64), writes=[nw])
            k.op('pool', lambda e: e.memset(ones_f[:], 1.0), writes=[ones_f])
            GA = k.sb("GA", [64, NCH, 48], F32)
            gT = k.sb("gT", [32, T], F32)
            G = k.sb("G", [64, 32], F32)
            qh = k.sb("qh", [64, 2, T], BF16)
            kh = k.sb("kh", [64, 2, T], BF16)
            vT = k.sb("vT", [128, T], BF16)
            moT = k.sb("moT", [128, T], F32)
            Hf = k.sb("Hf", [64, NCH, 2, 64], F32)
            Ktm = k.sb("Ktm", [64, 2, 64], BF16)
            Vaug = k.sb("Vaug", [64, 2, 66], BF16)
            PTm = k.sb("PTm", [64, 2, 64], BF16)
            Cst = k.sb("Cst", [64, 2, 66], F32)
            Cbf = k.sb("Cbf", [64, 2, 66], BF16)
            dn = k.sb("dn", [64, 2], F32)
            ff = k.sb("ff", [64, 2], F32)
            hs = k.sb("hs", [64, 2, 64], F32)
            st2 = k.sb("st2", [64, 2, 6], F32)
            mv2 = k.sb("mv2", [64, 2, 2], F32)
            rs2 = k.sb("rs2", [64, 2], F32)
            om = k.sb("om", [64, 128], F32)
            pg = k.ps("p2g", [64, 32])
            pbb = k.ps("p2b", [64, 32])
            pk = k.ps("p2k", [64, 2, 64], BF16)
            pv = k.ps("p2v", [64, 128], BF16)
            pp = k.ps("p2p", [64, 2, 64])
            po = k.ps("p2o", [64, 2, 66])
            pc = k.ps("p2c", [64, 2, 66])
            pmo = k.ps("p2mo", [64, 128])
            for b in range(NB):
                k.dma('sp', gT[:], FM[b][3584:3616, :], reads=[FM[b]], writes=[gT])
                for c in range(NCH):
                    k.op('pe', lambda e: e.transpose(pg[:], gT[:, c * 64:(c + 1) * 64], ident[0:32, 0:32]), reads=[gT, ident], writes=[pg])
                    k.op('dve', lambda e: e.tensor_copy(G[:], pg[:]), reads=[pg], writes=[G])
                    k.op('pe', lambda e: e.matmul(pbb[:, 0:8], cm[:, 0, :], G[:, 8:16], start=True, stop=True), reads=[cm, G], writes=[pbb], inc=False)
                    k.op('pe', lambda e: e.matmul(pbb[:, 8:16], cm[:, 1, :], G[:, 24:32], start=True, stop=True), reads=[cm, G], inc=False)
                    k.op('pe', lambda e: e.matmul(pbb[:, 16:24], ones_f[0:64, 0:64], G[:, 8:16], start=True, stop=True), reads=[ones_f, G], inc=False)
                    k.op('pe', lambda e: e.matmul(pbb[:, 24:32], ones_f[0:64, 0:64], G[:, 24:32], start=True, stop=True), reads=[ones_f, G])
                    k.op('act', lambda e: e.activation(GA[:, c, 0:32], pbb[:], AF.Exp), reads=[pbb], writes=[GA])
                    k.op('dve', lambda e: e.tensor_tensor(G[:, 0:8], G[:, 0:8], pbb[:, 0:8], ALU.subtract), reads=[pbb, G], writes=[G])
                    k.op('dve', lambda e: e.tensor_tensor(G[:, 16:24], G[:, 16:24], pbb[:, 8:16], ALU.subtract), reads=[pbb, G], writes=[G])
                    k.op('act', lambda e: e.activation(GA[:, c, 32:40], G[:, 0:8], AF.Exp), reads=[G], writes=[GA])
                    k.op('act', lambda e: e.activation(GA[:, c, 40:48], G[:, 16:24], AF.Exp), reads=[G], writes=[GA])
                for hp in range(4):
                    for h in range(2):
                        r0 = hp * 128 + h * 64
                        for q4 in range(4):
                            t0 = q4 * 1088
                            k.dma('pool', qh[:, h, t0:t0 + 1088], FM[b][r0:r0 + 64, t0:t0 + 1088], reads=[FM[b]], pw=[qh])
                            k.dma('pool', kh[:, h, t0:t0 + 1088], FM[b][512 + r0:512 + r0 + 64, t0:t0 + 1088], reads=[FM[b]], pw=[kh])
                    for q4 in range(4):
                        t0 = q4 * 1088
                        k.dma('pool', vT[:, t0:t0 + 1088], FM[b][2560 + hp * 128:2560 + (hp + 1) * 128, t0:t0 + 1088], reads=[FM[b]], pw=[vT])
                    k.dma('sp', moT[:], FM[b][3072 + hp * 128:3072 + (hp + 1) * 128, :], reads=[FM[b]], writes=[moT])
                    for d in range(2):
                        order = list(range(NCH)) if d == 0 else [3, 2, 1, 0] + list(range(NCH - 1, 3, -1))
                        k.op('pool', lambda e: e.memset(Cst[:], 0.0), writes=[Cst])
                        k.op('pool', lambda e: e.memset(Cbf[:], 0.0), writes=[Cbf])
                        for c in order:
                            cs = slice(c * 64, (c + 1) * 64)
                            hh = 2 * hp
                            a_ap = GA[:, c, d * 8 + hh:d * 8 + hh + 2]
                            e_ap = lambda h: GA[:, c, 16 + d * 8 + hh + h:16 + d * 8 + hh + h + 1]
                            c_ap = GA[:, c, 32 + d * 8 + hh:32 + d * 8 + hh + 2]
                            for h in range(2):
                                k.op('pe', lambda e: e.transpose(pk[:, h, :], kh[:, h, cs], identb[0:64, 0:64]), reads=[kh, identb], writes=[pk], inc=(h == 1))
                            k.op('pe', lambda e: e.transpose(pv[:], vT[:, cs], identb[:]), reads=[vT, identb], writes=[pv])
                            for h in range(2):
                                k.op('pe', lambda e: e.matmul(pp[:, h, :], kh[:, h, cs], qh[:, h, cs], start=True, stop=True), reads=[kh, qh], writes=[pp], inc=(h == 1))
                            k.op('act', lambda e: e.activation(Ktm[:], pk[:], AF.Identity), reads=[pk], writes=[Ktm])
                            k.op('dve', lambda e: e.tensor_tensor(Vaug[:, :, 0:64], pv[:].rearrange("p (h e) -> p h e", h=2),
                                                                  c_ap.unsqueeze(2).to_broadcast([64, 2, 64]), ALU.mult), reads=[pv, GA], writes=[Vaug])
                            k.op('dve', lambda e: e.tensor_copy(Vaug[:, :, 64:65], c_ap.unsqueeze(2)), reads=[GA], writes=[Vaug])
                            k.op('dve', lambda e: e.tensor_tensor(PTm[:], pp[:], cm[:, d:d + 1, :].to_broadcast([64, 2, 64]), ALU.mult), reads=[pp, cm], writes=[PTm])
                            for h in range(2):
                                k.op('pe', lambda e: e.matmul(po[:, h, 0:65], PTm[:, h, :], Vaug[:, h, 0:65], start=True, stop=False), reads=[PTm, Vaug], writes=[po], inc=False)
                                k.op('pe', lambda e: e.matmul(po[:, h, 0:65], qh[:, h, cs], Cbf[:, h, 0:65], start=False, stop=True), reads=[qh, Cbf], inc=(h == 1))
                            for h in range(2):
                                k.op('pe', lambda e: e.matmul(pc[:, h, 0:65], Ktm[:, h, :], Vaug[:, h, 0:65], start=True, stop=True), reads=[Ktm, Vaug], writes=[pc], inc=(h == 1))
                            k.op('dve', lambda e: e.tensor_tensor(dn[:], po[:, :, 64], a_ap, ALU.mult), reads=[po, GA], writes=[dn])
                            k.op('act', lambda e: e.activation(dn[:], dn[:], AF.Abs), reads=[dn], writes=[dn])
                            k.op('dve', lambda e: e.tensor_scalar(dn[:], dn[:], 1.0, None, ALU.max), reads=[dn], writes=[dn])
                            k.op('dve', lambda e: e.reciprocal(dn[:], dn[:]), reads=[dn], writes=[dn])
                            k.op('dve', lambda e: e.tensor_tensor(ff[:], dn[:], a_ap, ALU.mult), reads=[dn, GA], writes=[ff])
                            if d == 0:
                                for h in range(2):
                                    k.op('dve', lambda e: e.tensor_scalar(Hf[:, c, h, :], po[:, h, 0:64], ff[:, h:h + 1], None, ALU.mult), reads=[po, ff], writes=[Hf])
                            else:
                                for h in range(2):
                                    k.op('dve', lambda e: e.scalar_tensor_tensor(hs[:, h, :], po[:, h, 0:64], ff[:, h:h + 1], Hf[:, c, h, :], ALU.mult, ALU.add),
                                         reads=[po, ff, Hf], writes=[hs])
                            for h in range(2):
                                k.op('dve', lambda e: e.tensor_scalar(Cst[:, h, 0:65], Cst[:, h, 0:65], e_ap(h), None, ALU.mult), reads=[GA, Cst], writes=[Cst])
                                k.op('dve', lambda e: e.scalar_tensor_tensor(Cst[:, h, 0:65], pc[:, h, 0:65], e_ap(h), Cst[:, h, 0:65], ALU.mult, ALU.add),
                                     reads=[pc, GA, Cst], writes=[Cst])
                            k.op('act', lambda e: e.activation(Cbf[:, :, 0:65], Cst[:, :, 0:65], AF.Identity), reads=[Cst], writes=[Cbf])
                            if d == 1 and c >= 4:
                                for h in range(2):
                                    k.op('dve', lambda e: e.bn_stats(st2[:, h, :], hs[:, h, :]), reads=[hs], writes=[st2])
                                for h in range(2):
                                    k.op('dve', lambda e: e.bn_aggr(mv2[:, h, :], st2[:, h, :]), reads=[st2], writes=[mv2])
                                k.op('act', lambda e: e.activation(rs2[:], mv2[:, :, 1], AF.Sqrt, bias=LN_EPS), reads=[mv2], writes=[rs2])
                                k.op('dve', lambda e: e.reciprocal(rs2[:], rs2[:]), reads=[rs2], writes=[rs2])
                                for h in range(2):
                                    k.op('dve', lambda e: e.tensor_scalar(hs[:, h, :], hs[:, h, :], mv2[:, h, 0:1], rs2[:, h:h + 1], ALU.subtract, ALU.mult),
                                         reads=[hs, mv2, rs2], writes=[hs])
                                k.op('pe', lambda e: e.transpose(pmo[:], moT[:, cs], ident[:]), reads=[moT, ident], writes=[pmo])
                                k.op('dve', lambda e: e.tensor_tensor(om[:], hs[:].rearrange("p h e -> p (h e)"), nw[:, hp * 128:(hp + 1) * 128], ALU.mult),
                                     reads=[hs, nw], writes=[om])
                                k.op('dve', lambda e: e.tensor_tensor(om[:], om[:], pmo[:], ALU.mult), reads=[om, pmo], writes=[om])
                                k.dma('sp', MIX[b][(c - 4) * 64:(c - 3) * 64, hp * 128:(hp + 1) * 128], om[:], reads=[om], pw=[MIX[b]])
        k.barrier()


        with ExitStack() as es:
          if 3 in phases:
            k.es = es
            NBK = 1088
            NBC = 17
            rp = k.sb("rp", [64, 8, 8], F32)
            k.dma('sp', rp[:, :, 0:7], rp_in[:, :, :], writes=[rp])
            k.op('dve', lambda e: e.tensor_scalar(rp[:, :, 7], rp[:, :, 4], -1.0, 1.0, ALU.mult, ALU.add), reads=[rp], writes=[rp])
            smask = k.sb("smask", [64, NBK], F32)
            k.dma('sp', smask[:], smask_in[:, :], writes=[smask])
            rmask = k.sb("rmask", [128, 2, 128], F32)
            k.dma('sp', rmask[:], rmask_in[:, :, :], writes=[rmask])
            nmask = k.sb("nmask", [64, 2, 64], F32)
            k.dma('sp', nmask[:], nmask_in[:, :, :], writes=[nmask])
            gnw = k.sb("gnw", [64, 2, 512], F32)
            k.dma('sp', gnw[:, 0, :], r_norm_w[0:1, :].partition_broadcast(64), pw=[gnw])
            k.dma('sp', gnw[:, 1, :], r_norm_b[0:1, :].partition_broadcast(64), pw=[gnw])
            wBb = k.sb("wBb", [64, 2, 512], BF16)
            aBb = k.sb("aBb", [64, 512], BF16)
            gBb = k.sb("gBb", [128, 512], BF16)
            k.dma('pool', wBb[:, 0, :], r_wB[0, :, :], pw=[wBb])
            k.dma('pool', wBb[:, 1, :], r_wB[1, :, :], pw=[wBb])
            k.dma('pool', aBb[:], r_aB[:, :], writes=[aBb])
            k.dma('pool', gBb[:], r_gB[:, :], writes=[gBb])
            k.op('pool', lambda e: e.memset(ones_f[:], 1.0), writes=[ones_f])
            onesb = k.sb("onesb", [64, 2], BF16)
            k.op('pool', lambda e: e.memset(onesb[:], 1.0), writes=[onesb])
            lgb = k.sb("lgb", [128, T], BF16)
            lab = k.sb("lab", [64, NBK], BF16)
            lwb_ = k.sb("lwb_", [64, NBK], BF16)
            rr = k.sb("rr", [64, NBK], F32)
            rk = k.sb("rk", [64, NBK], F32)
            aa = k.sb("aa", [64, NBK], F32)
            t1 = k.sb("t1", [64, NBK], F32)
            t2 = k.sb("t2", [64, NBK], F32)
            khat = k.sb("khat", [64, NBK], F32)
            kmod = k.sb("kmod", [64, NBK], F32)
            beta = k.sb("beta", [64, NBK], F32)
            lgw = k.sb("lgw", [64, NBK], F32)
            lam = k.sb("lam", [64, NBK], F32)
            ee = k.sb("ee", [64, NBK], F32)
            VTb = k.sb("VTb", [64, 2, 64 + NBK], BF16)
            PRb = k.sb("PRb", [64, 2, NBK], BF16)
            AR = k.sb("AR", [64, 2, NBC, 128], BF16)
            ZT = k.sb("ZT", [64, 2, NBC, 128], BF16)
            GL = k.sb("GL", [64, 2, NBC], F32)
            Yf = k.sb("Yf", [64, NCH, 2, 64], F32)
            Mm = k.sb("Mm", [128, 2, 128], BF16)
            XL = k.sb("XL", [128, 2, 64], BF16)
            SW = k.sb("SW", [128, 2, 64], BF16)
            W = k.sb("W", [128, 2, 64], BF16)
            N0 = k.sb("N0", [64, 2, 64], BF16)
            NG = [k.sb("NG%d" % j, [64, 2, 2, 64], BF16) for j in range(6)]
            Xc = [k.sb("Xc%d" % j, [64, 2, 64], BF16) for j in range(2)]
            Ztm = k.sb("Ztm", [128, 2, 64], BF16)
            ST = k.sb("ST", [64, 2, 64], F32)
            ys = k.sb("ys", [64, 2, 64], F32)
            bon = k.sb("bon", [64, 2], F32)
            om3 = k.sb("om3", [64, 128], F32)
            pA = k.ps("p3a", [128, 512])
            pM = k.ps("p3m", [128, 2, 128])
            pN = k.ps("p3n", [64, 2, 2, 64])
            pX = k.ps("p3x", [64, 2, 64])
            pY = k.ps("p3y", [64, 2, 64])
            pV = k.ps("p3v", [128, 2, 64], BF16)
            pZ = k.ps("p3z", [128, 2, 64], BF16)
            pG = k.ps("p3g", [64, 132])
            k.op('pool', lambda e: e.memset(VTb[:], 0.0), writes=[VTb])

            def prep(b, hp, blk, d):
                t0 = blk * NBK
                k.dma('pool', lab[:], FM[b][3744:3808, t0:t0 + NBK], reads=[FM[b]], writes=[lab])
                lo = 3616 + 64 * d
                k.dma('pool', lwb_[:], FM[b][lo:lo + 64, t0:t0 + NBK], reads=[FM[b]], writes=[lwb_])
                for h in range(2):
                    hh = hp * 2 + h
                    r0 = hp * 128 + h * 64
                    k.dma('sp', rr[:], FM[b][1024 + r0:1024 + r0 + 64, t0:t0 + NBK], reads=[FM[b]], writes=[rr])
                    k.dma('sp', rk[:], FM[b][1536 + r0:1536 + r0 + 64, t0:t0 + NBK], reads=[FM[b]], writes=[rk])
                    k.dma('pool', VTb[:, h, 64:64 + NBK], FM[b][2048 + r0:2048 + r0 + 64, t0:t0 + NBK], reads=[FM[b]], pw=[VTb])
                    for (n0, n) in ((0, 512), (512, 512), (1024, 64)):
                        k.op('pe', lambda e: e.matmul(pA[0:64, 0:n], aBb[:, hh * 64:(hh + 1) * 64], lab[:, n0:n0 + n], start=True, stop=True),
                             reads=[aBb, lab], writes=[pA])
                        k.op('act', lambda e: e.activation(aa[:, n0:n0 + n], pA[0:64, 0:n], AF.Sigmoid, bias=rp[:, hh, 2:3]), reads=[pA, rp], writes=[aa])
                        k.op('pe', lambda e: e.matmul(pA[0:64, 0:n], wBb[:, d, hh * 64:(hh + 1) * 64], lwb_[:, n0:n0 + n], start=True, stop=True),
                             reads=[wBb, lwb_], writes=[pA])
                        k.op('act', lambda e: e.activation(lgw[:, n0:n0 + n], pA[0:64, 0:n], AF.Sigmoid, bias=rp[:, hh, d:d + 1]), reads=[pA, rp], writes=[lgw])
                    k.op('dve', lambda e: e.tensor_scalar(lgw[:], lgw[:], -DS, None, ALU.mult), reads=[lgw], writes=[lgw])
                    k.op('dve', lambda e: e.tensor_scalar(t1[:], rk[:], rp[:, hh, 3:4], None, ALU.mult), reads=[rk, rp], writes=[t1])
                    k.op('dve', lambda e: e.tensor_tensor(t2[:], t1[:], t1[:], ALU.mult), reads=[t1], writes=[t2])
                    for (n0, n) in ((0, 512), (512, 512), (1024, 64)):
                        k.op('pe', lambda e: e.matmul(pA[0:64, 0:n], ones_f[0:64, 0:64], t2[:, n0:n0 + n], start=True, stop=True),
                             reads=[ones_f, t2], writes=[pA])
                        k.op('act', lambda e: e.activation(khat[:, n0:n0 + n], pA[0:64, 0:n], AF.Sqrt), reads=[pA], writes=[khat])
                    k.op('dve', lambda e: e.tensor_scalar(khat[:], khat[:], 1e-12, None, ALU.max), reads=[khat], writes=[khat])
                    k.op('dve', lambda e: e.reciprocal(khat[:], khat[:]), reads=[khat], writes=[khat])
                    k.op('dve', lambda e: e.tensor_tensor(khat[:], khat[:], t1[:], ALU.mult), reads=[khat, t1], writes=[khat])
                    k.op('dve', lambda e: e.tensor_scalar(t1[:], aa[:], rp[:, hh, 4:5], rp[:, hh, 7:8], ALU.mult, ALU.add), reads=[aa, rp], writes=[t1])
                    k.op('dve', lambda e: e.tensor_tensor(kmod[:], rk[:], t1[:], ALU.mult), reads=[rk, t1], writes=[kmod])
                    k.op('dve', lambda e: e.tensor_tensor(beta[:], khat[:], aa[:], ALU.mult), reads=[khat, aa], writes=[beta])
                    k.op('dve', lambda e: e.scalar_tensor_tensor(PRb[:, h, :], rr[:], rp[:, hh, 6:7], kmod[:], ALU.mult, ALU.mult), reads=[rr, rp, kmod], writes=[PRb])
                    k.op('dve', lambda e: e.tensor_tensor_scan(lam[:], smask[:], lgw[:], 0.0, ALU.mult, ALU.add), reads=[smask, lgw], writes=[lam])
                    v3 = lambda t_: t_[:].rearrange("p (c t) -> p c t", t=64)
                    if d == 1:
                        k.op('dve', lambda e: e.tensor_tensor(t1[:], lgw[:], lam[:], ALU.subtract), reads=[lgw, lam], writes=[t1])
                        k.op('dve', lambda e: e.tensor_tensor(v3(t2), v3(t1), v3(lam)[:, :, 63:64].to_broadcast([64, NBC, 64]), ALU.add), reads=[t1, lam], writes=[t2])
                        k.op('dve', lambda e: e.tensor_copy(lam[:], t2[:]), reads=[t2], writes=[lam])
                    k.op('dve', lambda e: e.tensor_tensor(t1[:], lam[:], lgw[:], ALU.subtract), reads=[lam, lgw], writes=[t1])
                    k.op('act', lambda e: e.activation(ee[:], t1[:], AF.Exp), reads=[t1], writes=[ee])
                    k.op('dve', lambda e: e.scalar_tensor_tensor(AR[:, h, :, 0:64], v3(khat), -1.0, v3(ee), ALU.mult, ALU.mult), reads=[khat, ee], writes=[AR])
                    k.op('act', lambda e: e.activation(ee[:], lam[:], AF.Exp), reads=[lam], writes=[ee])
                    k.op('dve', lambda e: e.tensor_tensor(AR[:, h, :, 64:128], v3(rr), v3(ee), ALU.mult), reads=[rr, ee], writes=[AR])
                    gcol = 63 if d == 0 else 0
                    k.op('dve', lambda e: e.tensor_copy(GL[:, h, :], v3(ee)[:, :, gcol]), reads=[ee], writes=[GL])
                    k.op('act', lambda e: e.activation(ee[:], lam[:], AF.Exp, scale=-1.0), reads=[lam], writes=[ee])
                    k.op('dve', lambda e: e.tensor_tensor(ZT[:, h, :, 0:64], v3(beta), v3(ee), ALU.mult), reads=[beta, ee], writes=[ZT])
                    k.op('dve', lambda e: e.tensor_tensor(ZT[:, h, :, 64:128], v3(kmod), v3(ee), ALU.mult), reads=[kmod, ee], writes=[ZT])

            def step(b, hp, c, lc, d):
                ts = slice(lc * 64, (lc + 1) * 64)
                for h in range(2):
                    k.op('pe', lambda e: e.transpose(pV[:, h, :], VTb[:, h, lc * 64:lc * 64 + 128], identb[0:64, 0:64]), reads=[VTb, identb], writes=[pV], inc=(h == 1))
                for h in range(2):
                    k.op('pe', lambda e: e.transpose(pZ[:, h, :], ZT[:, h, lc, :], identb[0:64, 0:64]), reads=[ZT, identb], writes=[pZ], inc=(h == 1))
                for h in range(2):
                    k.op('pe', lambda e: e.matmul(pM[:, h, :], ZT[:, h, lc, :], AR[:, h, lc, :], start=True, stop=True), reads=[ZT, AR], writes=[pM], inc=(h == 1))
                for h in range(2):
                    k.op('pe', lambda e: e.matmul(pN[:, h, 0, :], AR[:, h, lc, 0:64], ZT[:, h, lc, 0:64], start=True, stop=True), reads=[ZT, AR], writes=[pN], inc=(h == 1))
                k.op('act', lambda e: e.activation(SW[64:128, :, :], pV[64:128, :, :], AF.Identity), reads=[pV], writes=[SW])
                k.op('act', lambda e: e.activation(W[64:128, :, :], pV[64:128, :, :], AF.Identity), reads=[pV], writes=[W])
                k.op('act', lambda e: e.activation(Ztm[:], pZ[:], AF.Identity), reads=[pZ], writes=[Ztm])
                k.op('dve', lambda e: e.tensor_tensor(Mm[:], pM[:], rmask[:, d:d + 1, :].to_broadcast([128, 2, 128]), ALU.mult), reads=[pM, rmask], writes=[Mm])
                k.op('dve', lambda e: e.tensor_tensor(NG[0][:, :, 0, :], pN[:, :, 0, :], nmask[:, d:d + 1, :].to_broadcast([64, 2, 64]), ALU.mult), reads=[pN, nmask], writes=[NG[0]])
                k.op('dve', lambda e: e.tensor_copy(NG[0][:, :, 1, :], Mm[0:64, :, 0:64]), reads=[Mm], writes=[NG[0]])
                k.op('dve', lambda e: e.tensor_copy(XL[64:128, :, :], Mm[64:128, :, 0:64]), reads=[Mm], writes=[XL])
                k.op('act', lambda e: e.activation(XL[0:64, :, :], AR[:, :, lc, 0:64], AF.Identity), reads=[AR], writes=[XL])
                for j in range(1, 6):
                    for h in range(2):
                        k.op('pe', lambda e: e.matmul(pN[:, h, 0, :], NG[j - 1][:, h, 1, :], NG[j - 1][:, h, 0, :], start=True, stop=True), reads=[NG[j - 1]], writes=[pN], inc=False)
                        k.op('pe', lambda e: e.matmul(pN[:, h, 1, :], NG[j - 1][:, h, 0, :], NG[j - 1][:, h, 1, :], start=True, stop=True), reads=[NG[j - 1]], inc=(h == 1))
                    k.op('act' if j % 2 else 'dve', (lambda e: e.activation(NG[j][:], pN[:], AF.Identity)) if j % 2 else (lambda e: e.tensor_copy(NG[j][:], pN[:])),
                         reads=[pN], writes=[NG[j]])
                for h in range(2):
                    k.op('pe', lambda e: e.matmul(pX[:, h, :], XL[:, h, :], SW[:, h, :], start=True, stop=True), reads=[XL, SW], writes=[pX], inc=(h == 1))
                k.op('dve', lambda e: e.tensor_copy(Xc[0][:], pX[:]), reads=[pX], writes=[Xc[0]])
                for j in range(6):
                    xi, xo = Xc[j % 2], Xc[(j + 1) % 2]
                    for h in range(2):
                        k.op('pe', lambda e: e.matmul(pX[:, h, :], NG[j][:, h, 1, :], xi[:, h, :], start=True, stop=True), reads=[NG[j], xi], writes=[pX], inc=(h == 1))
                    dst = xo if j < 5 else None
                    if j < 5:
                        k.op('dve', lambda e: e.tensor_tensor(xo[:], pX[:], xi[:], ALU.add), reads=[pX, xi], writes=[xo])
                    else:
                        k.op('dve', lambda e: e.tensor_tensor(W[0:64, :, :], pX[:], xi[:], ALU.add), reads=[pX, xi], writes=[W])
                for h in range(2):
                    k.op('pe', lambda e: e.matmul(pY[:, h, :], AR[:, h, lc, 64:128], SW[0:64, h, :], start=True, stop=False), reads=[AR, SW], writes=[pY], inc=False)
                    k.op('pe', lambda e: e.matmul(pY[:, h, :], Mm[:, h, 64:128], W[:, h, :], start=False, stop=True), reads=[Mm, W], inc=(h == 1))
                if d == 0:
                    k.op('act', lambda e: e.activation(Yf[:, c, :, :], pY[:], AF.Identity), reads=[pY], writes=[Yf])
                else:
                    k.op('dve', lambda e: e.tensor_tensor(ys[:], pY[:], Yf[:, c, :, :], ALU.add), reads=[pY, Yf], writes=[ys])
                for h in range(2):
                    k.op('pe', lambda e: e.matmul(pX[:, h, :], Ztm[:, h, :], W[:, h, :], start=True, stop=True), reads=[Ztm, W], writes=[pX], inc=(h == 1))
                for h in range(2):
                    k.op('dve', lambda e: e.tensor_scalar(ST[:, h, :], ST[:, h, :], GL[:, h, lc:lc + 1], None, ALU.mult), reads=[ST, GL], writes=[ST])
                    k.op('dve', lambda e: e.scalar_tensor_tensor(ST[:, h, :], pX[:, h, :], GL[:, h, lc:lc + 1], ST[:, h, :], ALU.mult, ALU.add), reads=[pX, GL, ST], writes=[ST])
                k.op('act', lambda e: e.activation(SW[0:64, :, :], ST[:], AF.Identity), reads=[ST], writes=[SW])
                if d == 1 and c >= 4:
                    for h in range(2):
                        k.op('dve', lambda e: e.bn_stats(st2[:, h, :], ys[:, h, :]), reads=[ys], writes=[st2])
                    for h in range(2):
                        k.op('dve', lambda e: e.bn_aggr(mv2[:, h, :], st2[:, h, :]), reads=[st2], writes=[mv2])
                    k.op('act', lambda e: e.activation(rs2[:], mv2[:, :, 1], AF.Sqrt, bias=GN_EPS), reads=[mv2], writes=[rs2])
                    k.op('dve', lambda e: e.reciprocal(rs2[:], rs2[:]), reads=[rs2], writes=[rs2])
                    for h in range(2):
                        k.op('dve', lambda e: e.tensor_scalar(ys[:, h, :], ys[:, h, :], mv2[:, h, 0:1], rs2[:, h:h + 1], ALU.subtract, ALU.mult), reads=[ys, mv2, rs2], writes=[ys])
                    ysf = ys[:].rearrange("p h e -> p (h e)")
                    k.op('dve', lambda e: e.tensor_tensor(om3[:], ysf, gnw[:, 0, hp * 128:(hp + 1) * 128], ALU.mult), reads=[ys, gnw], writes=[om3])
                    k.op('dve', lambda e: e.tensor_tensor(om3[:], om3[:], gnw[:, 1, hp * 128:(hp + 1) * 128], ALU.add), reads=[om3, gnw], writes=[om3])
                    for h in range(2):
                        k.op('pe', lambda e: e.matmul(pG[:, 128 + 2 * h:130 + 2 * h], PRb[:, h, ts], onesb[:, 0:2], start=True, stop=True), reads=[PRb, onesb], writes=[pG], inc=False)
                    k.op('pe', lambda e: e.matmul(pG[:, 0:128], lgb[:, c * 64:(c + 1) * 64], gBb[:, hp * 128:(hp + 1) * 128], start=True, stop=True), reads=[lgb, gBb])
                    for h in range(2):
                        k.op('pe', lambda e: e.transpose(pZ[0:64, h, :], VTb[:, h, 64 + lc * 64:128 + lc * 64], identb[0:64, 0:64]), reads=[VTb, identb], writes=[pZ], inc=(h == 1))
                    k.op('dve', lambda e: e.tensor_copy(bon[:], pG[:, 128:132].rearrange("p (h two) -> p h two", two=2)[:, :, 0]), reads=[pG], writes=[bon])
                    for h in range(2):
                        k.op('dve', lambda e: e.scalar_tensor_tensor(om3[:, h * 64:(h + 1) * 64], pZ[0:64, h, :], bon[:, h:h + 1], om3[:, h * 64:(h + 1) * 64], ALU.mult, ALU.add),
                             reads=[pZ, bon, om3], writes=[om3])
                    k.op('dve', lambda e: e.tensor_tensor(om3[:], om3[:], pG[:, 0:128], ALU.mult), reads=[om3, pG], writes=[om3])
                    k.dma('sp', MIX[b][(c - 4) * 64:(c - 3) * 64, 512 + hp * 128:512 + (hp + 1) * 128], om3[:], reads=[om3], pw=[MIX[b]])

            st2 = k.sb("st2r", [64, 2, 6], F32)
            mv2 = k.sb("mv2r", [64, 2, 2], F32)
            rs2 = k.sb("rs2r", [64, 2], F32)
            for b in range(NB):
                for q4 in range(4):
                    k.dma('pool', lgb[:, q4 * NBK:(q4 + 1) * NBK], FM[b][3808:3936, q4 * NBK:(q4 + 1) * NBK], reads=[FM[b]], pw=[lgb])
                for hp in range(int(os.environ.get('P3H', 4))):
                    for d in range(2):
                        order = list(range(NCH)) if d == 0 else [3, 2, 1, 0] + list(range(NCH - 1, 3, -1))
                        k.op('pool', lambda e: e.memset(ST[:], 0.0), writes=[ST])
                        k.op('pool', lambda e: e.memset(SW[0:64, :, :], 0.0), writes=[SW])
                        cur = -1
                        for c in order:
                            if c // NBC != cur:
                                cur = c // NBC
                                prep(b, hp, cur, d)
                            step(b, hp, c, c % NBC, d)
        k.barrier()


        with ExitStack() as es:
          if 4 in phases:
           try:
            P4S = int(os.environ.get('P4S', 9))
            k.es = es
            NT = NB * SEQ // 128
            NBLK = NB * SEQ * 2 // 128 + 32
            LG = k.sb("LG", [128, NT, 36], F32)
            OH1 = k.sb("OH1", [128, NT, 32], F32)
            OH2 = k.sb("OH2", [128, NT, 32], F32)
            W1 = k.sb("W1", [128, NT], F32)
            W2 = k.sb("W2", [128, NT], F32)
            DST = k.sb("DST", [128, NT, 2], I32)
            WIDX = k.sb("WIDX", [128, NBLK, 12], I32)
            g2b = k.sb("g2b", [128, NB, D], F32)
            lnp = k.sb("lnp", [128, 4, D], F32)
            for j, src in enumerate((ln1_g, ln1_b, ln2_g, ln2_b)):
                k.dma('sp', lnp[:, j, :], src[0:1, :].partition_broadcast(128), pw=[lnp])
            for b in range(NB):
                k.dma('sp', g2b[:, b, :], MODD[b:b + 1, 5 * D:6 * D].partition_broadcast(128), reads=[MODD], pw=[g2b])
            with ExitStack() as es4:
                k.es = es4
                wob = k.sb("wob", [128, 8, D], BF16)
                for kc in range(8):
                    k.dma('pool', wob[:, kc, :], w_out[kc * 128:(kc + 1) * 128, :], pw=[wob])
                rt = k.sb("rt", [128, 8, 36], F32)
                k.dma('sp', rt[:], rt_in[:, :].rearrange("(kc p) n -> p kc n", p=128), writes=[rt])
                rtbb = k.sb("rtbb", [128, 36], F32)
                k.dma('sp', rtbb[:], rtb_in[0:1, :].partition_broadcast(128), writes=[rtbb])
                mb4 = k.sb("mb4", [128, 3, D], F32)
                mxb = [k.sb("mxb%d" % i, [128, D], BF16) for i in range(2)]
                mT = k.sb("mT", [128, 8, 128], BF16)
                x4 = [k.sb("x4%d" % i, [128, D], F32) for i in range(2)]
                t4 = k.sb("t4", [128, D], F32)
                y4 = k.sb("y4", [128, D], F32)
                h4 = k.sb("h4", [128, D], F32)
                h4b = k.sb("h4b", [128, D], BF16)
                h4T = k.sb("h4T", [128, 8, 128], F32)
                st4 = k.sb("st4", [128, 2, 6], F32)
                mv4 = k.sb("mv4", [128, 2], F32)
                rs4 = k.sb("rs4", [128, 1], F32)
                ptm = k.ps("p4t", [128, 8, 128], BF16)
                po4 = [k.ps("p4o%d" % i, [128, 512]) for i in range(2)]
                pth = k.ps("p4th", [128, 8, 128])
                plg = k.ps("p4lg", [128, 36])

                def ln_stats(src):
                    for hf in range(2):
                        k.op('dve', lambda e: e.bn_stats(st4[:, hf, :], src[:, hf * 512:(hf + 1) * 512]), reads=[src], writes=[st4])
                    k.op('dve', lambda e: e.bn_aggr(mv4[:], st4[:].rearrange("p a b -> p (a b)")), reads=[st4], writes=[mv4])
                    k.op('act', lambda e: e.activation(rs4[:], mv4[:, 1:2], AF.Sqrt, bias=LN_EPS), reads=[mv4], writes=[rs4])
                    k.op('dve', lambda e: e.reciprocal(rs4[:], rs4[:]), reads=[rs4], writes=[rs4])

                for b in range(NB):
                    for j, c0 in enumerate((2 * D, 4 * D, 3 * D)):
                        k.dma('sp', mb4[:, j, :], MODD[b:b + 1, c0:c0 + D].partition_broadcast(128), reads=[MODD], pw=[mb4])
                    k.op('dve', lambda e: e.tensor_scalar(mb4[:, 1, :], mb4[:, 1, :], 1.0, None, ALU.add), reads=[mb4], writes=[mb4])
                    for i in range(SEQ // 128):
                        gi = b * (SEQ // 128) + i
                        xb_, mb_ = x4[i % 2], mxb[i % 2]
                        k.dma('pool', mb_[:], MIX[b][i * 128:(i + 1) * 128, :], reads=[MIX[b]], writes=[mb_])
                        k.dma('sp', xb_[:], x_in[b, i * 128:(i + 1) * 128, :], writes=[xb_])
                        for kc in range(8):
                            k.op('pe', lambda e: e.transpose(ptm[:, kc, :], mb_[:, kc * 128:(kc + 1) * 128], identb[:]), reads=[mb_, identb], writes=[ptm], inc=(kc == 7))
                        k.op('act', lambda e: e.activation(mT[:], ptm[:], AF.Identity), reads=[ptm], writes=[mT])
                        for n in range(2):
                            for kc in range(8):
                                k.op('pe', lambda e: e.matmul(po4[n][:], mT[:, kc, :], wob[:, kc, n * 512:(n + 1) * 512], start=(kc == 0), stop=(kc == 7)),
                                     reads=[mT, wob], writes=[po4[n]] if kc == 0 else [], inc=(kc == 7))
                            k.op('dve', lambda e: e.tensor_tensor(t4[:, n * 512:(n + 1) * 512], po4[n][:], mb4[:, 0, n * 512:(n + 1) * 512], ALU.mult),
                                 reads=[po4[n], mb4], writes=[t4])
                        k.op('dve', lambda e: e.scalar_tensor_tensor(y4[:], xb_[:], ALPHA, t4[:], ALU.mult, ALU.add), reads=[xb_, t4], writes=[y4])
                        ln_stats(y4)
                        k.op('dve', lambda e: e.tensor_scalar(y4[:], y4[:], mv4[:, 0:1], rs4[:, 0:1], ALU.subtract, ALU.mult), reads=[y4, mv4, rs4], writes=[y4])
                        k.op('dve', lambda e: e.tensor_tensor(y4[:], y4[:], lnp[:, 0, :], ALU.mult), reads=[y4, lnp], writes=[y4])
                        k.op('dve', lambda e: e.tensor_tensor(y4[:], y4[:], lnp[:, 1, :], ALU.add), reads=[y4, lnp], writes=[y4])
                        k.dma('sp', X1[gi * 128:(gi + 1) * 128, :], y4[:], reads=[y4], pw=[X1])
                        ln_stats(y4)
                        k.op('dve', lambda e: e.tensor_scalar(h4[:], y4[:], mv4[:, 0:1], rs4[:, 0:1], ALU.subtract, ALU.mult), reads=[y4, mv4, rs4], writes=[h4])
                        k.op('dve', lambda e: e.tensor_tensor(h4[:], h4[:], mb4[:, 1, :], ALU.mult), reads=[h4, mb4], writes=[h4])
                        k.op('dve', lambda e: e.tensor_tensor(h4[:], h4[:], mb4[:, 2, :], ALU.add), reads=[h4, mb4], writes=[h4])
                        k.op('act', lambda e: e.activation(h4b[:], h4[:], AF.Identity), reads=[h4], writes=[h4b])
                        k.dma('sp', H2[gi * 128:(gi + 1) * 128, :], h4b[:], reads=[h4b], pw=[H2])
                        for kc in range(8):
                            k.op('pe', lambda e: e.transpose(pth[:, kc, :], h4[:, kc * 128:(kc + 1) * 128], ident[:]), reads=[h4, ident], writes=[pth], inc=(kc == 7))
                        k.op('act', lambda e: e.activation(h4T[:], pth[:], AF.Identity), reads=[pth], writes=[h4T])
                        for kc in range(8):
                            k.op('pe', lambda e: e.matmul(plg[:], h4T[:, kc, :], rt[:, kc, :], start=(kc == 0), stop=(kc == 7)),
                                 reads=[h4T, rt], writes=[plg] if kc == 0 else [], inc=(kc == 7))
                        k.op('dve', lambda e: e.tensor_tensor(LG[:, gi, :], plg[:], rtbb[:], ALU.add), reads=[plg, rtbb], writes=[LG])
            k.barrier()
            with ExitStack() as es5:
                k.es = es5
                if P4S < 1:
                    raise _Stop()
                gmx = k.sb("gmx", [128, NT], F32)
                goh = k.sb("goh", [128, NT, 4], F32)
                tg = k.sb("tg", [128, NT, 4], F32)
                ptop = k.sb("ptop", [128, NT], F32)
                lem = k.sb("lem", [128, NT, 32], F32)
                v1 = k.sb("v1", [128, NT], F32)
                v2 = k.sb("v2", [128, NT], F32)
                lgv = LG[:, :, 0:4]
                lev = LG[:, :, 4:36]
                k.op('dve', lambda e: e.tensor_reduce(gmx[:], lgv, AX.X, ALU.max), reads=[LG], writes=[gmx])
                k.op('dve', lambda e: e.tensor_tensor(goh[:], lgv, gmx[:].unsqueeze(2).to_broadcast([128, NT, 4]), ALU.is_equal), reads=[LG, gmx], writes=[goh])
                k.op('dve', lambda e: e.tensor_tensor(tg[:], lgv, gmx[:].unsqueeze(2).to_broadcast([128, NT, 4]), ALU.subtract), reads=[LG, gmx], writes=[tg])
                k.op('act', lambda e: e.activation(tg[:], tg[:], AF.Exp), reads=[tg], writes=[tg])
                k.op('dve', lambda e: e.tensor_reduce(ptop[:], tg[:], AX.X, ALU.add), reads=[tg], writes=[ptop])
                k.op('dve', lambda e: e.reciprocal(ptop[:], ptop[:]), reads=[ptop], writes=[ptop])
                k.op('dve', lambda e: e.tensor_scalar(goh[:], goh[:], -1.0, 1e30, ALU.add, ALU.mult), reads=[goh], writes=[goh])
                for g in range(4):
                    k.op('dve', lambda e: e.tensor_tensor(lem[:, :, g * 8:(g + 1) * 8], LG[:, :, 4 + g * 8:12 + g * 8],
                                                          goh[:, :, g:g + 1].to_broadcast([128, NT, 8]), ALU.add), reads=[LG, goh], writes=[lem])
                k.op('dve', lambda e: e.tensor_reduce(v1[:], lem[:], AX.X, ALU.max), reads=[lem], writes=[v1])
                k.op('dve', lambda e: e.tensor_tensor(OH1[:], lem[:], v1[:].unsqueeze(2).to_broadcast([128, NT, 32]), ALU.is_equal), reads=[lem, v1], writes=[OH1])
                k.op('dve', lambda e: e.scalar_tensor_tensor(lem[:], OH1[:], -1e30, lem[:], ALU.mult, ALU.add), reads=[OH1, lem], writes=[lem])
                k.op('dve', lambda e: e.tensor_reduce(v2[:], lem[:], AX.X, ALU.max), reads=[lem], writes=[v2])
                k.op('dve', lambda e: e.tensor_tensor(OH2[:], lem[:], v2[:].unsqueeze(2).to_broadcast([128, NT, 32]), ALU.is_equal), reads=[lem, v2], writes=[OH2])
                k.op('dve', lambda e: e.tensor_tensor(v2[:], v2[:], v1[:], ALU.subtract), reads=[v1, v2], writes=[v2])
                k.op('act', lambda e: e.activation(v2[:], v2[:], AF.Exp), reads=[v2], writes=[v2])
                k.op('dve', lambda e: e.tensor_scalar(v2[:], v2[:], 1.0, None, ALU.add), reads=[v2], writes=[v2])
                k.op('dve', lambda e: e.reciprocal(v2[:], v2[:]), reads=[v2], writes=[v2])
                k.op('dve', lambda e: e.tensor_tensor(W1[:], v2[:], ptop[:], ALU.mult), reads=[v2, ptop], writes=[W1])
                k.op('dve', lambda e: e.tensor_tensor(W2[:], ptop[:], W1[:], ALU.subtract), reads=[W1, ptop], writes=[W2])
            k.barrier()
            with ExitStack() as es6:
                k.es = es6
                if P4S < 2:
                    raise _Stop()
                OHb = k.sb("OHb", [128, NT, 32], BF16)
                triS = k.sb("triS", [128, 128], BF16)
                onb = k.sb("onb", [128, 128], BF16)
                thr = k.sb("thr", [128, 128], F32)
                blki = k.sb("blki", [128, NBLK], F32)
                kcp = k.sb("kcp", [128, 12], F32)
                cnt = k.sb("cnt", [128, 32], F32)
                big = k.sb("big", [128, 32, 128], F32)
                nbk = k.sb("nbk", [128, 32], F32)
                pend = k.sb("pend", [128, 32], F32)
                pst = k.sb("pst", [128, 32], F32)
                run = k.sb("run", [128, 32], F32)
                RK = k.sb("RK", [128, NT, 32], F32)
                dsf = k.sb("dsf", [128, NT, 2], F32)
                bexp = k.sb("bexp", [128, NBLK], F32)
                bigb = k.sb("bigb", [128, NBLK, 32], F32)
                widxf = k.sb("widxf", [128, NBLK, 12], F32)
                tokid = k.sb("tokid", [128, NT, 16], I32)
                zt = k.sb("zt", [128, 16], I32)
                pcn = k.ps("p5c", [128, 32])
                prk = k.ps("p5r", [128, 32])
                ptt = k.ps("p5t", [128, 32])
                stg = k.sb("stg", [128, 128], F32)
                k.dma('sp', stg[:], tris_in[:, :], writes=[stg])
                k.op('dve', lambda e: e.tensor_copy(triS[:], stg[:]), reads=[stg], writes=[triS])
                k.op('pool', lambda e: e.memset(onb[:], 1.0), writes=[onb])
                k.dma('sp', thr[:], thr_in[:, :], writes=[thr])
                k.dma('sp', blki[:], blki_in[:, 0:NBLK], writes=[blki])
                k.dma('sp', kcp[:], kcp_in[:, :], writes=[kcp])
                k.dma('sp', tokid[:], tokid_in[:, 0:NT, :], writes=[tokid])
                k.op('pool', lambda e: e.memset(zt[:], 0), writes=[zt])
                for bk in range(NBLK):
                    k.dma('sp', TOKB[bk * 128:(bk + 1) * 128, :], zt[:], reads=[zt], pw=[TOKB])
                k.op('dve', lambda e: e.tensor_tensor(OHb[:], OH1[:], OH2[:], ALU.add), reads=[OH1, OH2], writes=[OHb])
                for i in range(NT):
                    k.op('pe', lambda e: e.matmul(pcn[:], onb[:], OHb[:, i, :], start=(i == 0), stop=(i == NT - 1)), reads=[onb, OHb], writes=[pcn] if i == 0 else [], inc=(i == NT - 1))
                k.op('dve', lambda e: e.tensor_copy(cnt[:], pcn[:]), reads=[pcn], writes=[cnt])
                k.op('dve', lambda e: e.tensor_tensor(big[:], cnt[:].unsqueeze(2).to_broadcast([128, 32, 128]), thr[:].unsqueeze(1).to_broadcast([128, 32, 128]), ALU.is_gt),
                     reads=[cnt, thr], writes=[big])
                k.op('dve', lambda e: e.tensor_reduce(nbk[:], big[:], AX.X, ALU.add), reads=[big], writes=[nbk])
                k.op('pool', lambda e: e.memset(run[:], 1.0), writes=[run])
                k.op('dve', lambda e: e.tensor_tensor_scan(pend[:], run[:], nbk[:], 0.0, ALU.mult, ALU.add), reads=[run, nbk], writes=[pend])
                k.op('dve', lambda e: e.tensor_tensor(pst[:], pend[:], nbk[:], ALU.subtract), reads=[pend, nbk], writes=[pst])
                k.op('dve', lambda e: e.tensor_scalar(pst[:], pst[:], 128.0, None, ALU.mult), reads=[pst], writes=[pst])
                k.op('pool', lambda e: e.memset(run[:], 0.0), reads=[run], writes=[run])
                for i in range(NT):
                    k.op('pe', lambda e: e.matmul(prk[:], triS[:], OHb[:, i, :], start=True, stop=True), reads=[triS, OHb], writes=[prk])
                    k.op('pe', lambda e: e.matmul(ptt[:], onb[:], OHb[:, i, :], start=True, stop=True), reads=[onb, OHb], writes=[ptt])
                    k.op('dve', lambda e: e.tensor_tensor(RK[:, i, :], prk[:], run[:], ALU.add), reads=[prk, run], writes=[RK])
                    k.op('dve', lambda e: e.tensor_tensor(run[:], run[:], ptt[:], ALU.add), reads=[run, ptt], writes=[run])
                k.op('dve', lambda e: e.tensor_tensor(RK[:], RK[:], pst[:].unsqueeze(1).to_broadcast([128, NT, 32]), ALU.add), reads=[RK, pst], writes=[RK])
                for j, OH in enumerate((OH1, OH2)):
                    k.op('dve', lambda e: e.tensor_tensor(OH[:], OH[:], RK[:], ALU.mult), reads=[OH, RK], writes=[OH])
                    k.op('dve', lambda e: e.tensor_reduce(dsf[:, :, j], OH[:], AX.X, ALU.add), reads=[OH], writes=[dsf])
                k.op('dve', lambda e: e.tensor_copy(DST[:], dsf[:]), reads=[dsf], writes=[DST])
                k.op('dve', lambda e: e.tensor_tensor(bigb[:], pend[:].unsqueeze(1).to_broadcast([128, NBLK, 32]), blki[:].unsqueeze(2).to_broadcast([128, NBLK, 32]), ALU.is_le),
                     reads=[pend, blki], writes=[bigb])
                k.op('dve', lambda e: e.tensor_reduce(bexp[:], bigb[:], AX.X, ALU.add), reads=[bigb], writes=[bexp])
                k.op('dve', lambda e: e.tensor_scalar(bexp[:], bexp[:], 31.0, None, ALU.min), reads=[bexp], writes=[bexp])
                k.op('dve', lambda e: e.tensor_scalar(widxf[:, :, 0:8], bexp[:].unsqueeze(2).to_broadcast([128, NBLK, 8]), 1024.0, None, ALU.mult), reads=[bexp], writes=[widxf])
                k.op('dve', lambda e: e.tensor_scalar(widxf[:, :, 8:12], bexp[:].unsqueeze(2).to_broadcast([128, NBLK, 4]), 512.0, None, ALU.mult), reads=[bexp], writes=[widxf])
                k.op('dve', lambda e: e.tensor_tensor(widxf[:], widxf[:], kcp[:].unsqueeze(1).to_broadcast([128, NBLK, 12]), ALU.add), reads=[widxf, kcp], writes=[widxf])
                k.op('dve', lambda e: e.tensor_copy(WIDX[:], widxf[:]), reads=[widxf], writes=[WIDX])
                for i in range(NT):
                    for j in range(2):
                        k.dma('pool', TOKB[:, :], tokid[:, i, :], reads=[tokid, DST], pw=[TOKB],
                              indirect=(bass.IndirectOffsetOnAxis(ap=DST[:, i, j:j + 1], axis=0), None))
            k.barrier()
            with ExitStack() as es7:
                k.es = es7
                if P4S < 3:
                    raise _Stop()
                tki = [k.sb("tki%d" % i, [128, 16], I32) for i in range(2)]
                xg = [k.sb("xg%d" % i, [128, D], BF16) for i in range(2)]
                xgT = k.sb("xgT", [128, 8, 128], BF16)
                wg = [k.sb("wg%d" % i, [128, 8, 512], BF16) for i in range(2)]
                wu = [k.sb("wu%d" % i, [128, 8, 512], BF16) for i in range(2)]
                wd = [k.sb("wd%d" % i, [128, 4, D], BF16) for i in range(2)]
                gs = k.sb("gs", [128, 512], F32)
                hb = k.sb("hb", [128, 512], BF16)
                hbT = k.sb("hbT", [128, 4, 128], BF16)
                yb = [k.sb("yb%d" % i, [128, D], F32) for i in range(2)]
                pxt = k.ps("p6x", [128, 8, 128], BF16)
                pgt = k.ps("p6g", [128, 512])
                put = k.ps("p6u", [128, 512])
                pht = k.ps("p6h", [128, 4, 128], BF16)
                pyt = [k.ps("p6y%d" % i, [128, 512]) for i in range(2)]
                for bk in range(NBLK):
                    q = bk % 2
                    k.dma('sp', tki[q][:], TOKB[bk * 128:(bk + 1) * 128, :], reads=[TOKB], writes=[tki[q]])
                    k.dma('pool', xg[q][:], H2[:, :], reads=[H2, tki[q]], writes=[xg[q]],
                          indirect=(None, bass.IndirectOffsetOnAxis(ap=tki[q][:, 0:1], axis=0)))
                    for kc in range(8):
                        k.dma('pool', wg[q][:, kc, :], ex_gate[:, :], reads=[WIDX], pw=[wg[q]],
                              indirect=(None, bass.IndirectOffsetOnAxis(ap=WIDX[:, bk, kc:kc + 1], axis=0)))
                        k.dma('pool', wu[q][:, kc, :], ex_up[:, :], reads=[WIDX], pw=[wu[q]],
                              indirect=(None, bass.IndirectOffsetOnAxis(ap=WIDX[:, bk, kc:kc + 1], axis=0)))
                    for fc in range(4):
                        k.dma('pool', wd[q][:, fc, :], ex_down[:, :], reads=[WIDX], pw=[wd[q]],
                              indirect=(None, bass.IndirectOffsetOnAxis(ap=WIDX[:, bk, 8 + fc:9 + fc], axis=0)))
                    for kc in range(8):
                        k.op('pe', lambda e: e.transpose(pxt[:, kc, :], xg[q][:, kc * 128:(kc + 1) * 128], identb[:]), reads=[xg[q], identb], writes=[pxt], inc=(kc == 7))
                    k.op('act', lambda e: e.activation(xgT[:], pxt[:], AF.Identity), reads=[pxt], writes=[xgT])
                    for kc in range(8):
                        k.op('pe', lambda e: e.matmul(pgt[:], xgT[:, kc, :], wg[q][:, kc, :], start=(kc == 0), stop=(kc == 7)), reads=[xgT, wg[q]], writes=[pgt] if kc == 0 else [], inc=(kc == 7))
                    for kc in range(8):
                        k.op('pe', lambda e: e.matmul(put[:], xgT[:, kc, :], wu[q][:, kc, :], start=(kc == 0), stop=(kc == 7)), reads=[xgT, wu[q]], writes=[put] if kc == 0 else [], inc=(kc == 7))
                    k.op('act', lambda e: e.activation(gs[:], pgt[:], AF.Silu), reads=[pgt], writes=[gs])
                    k.op('dve', lambda e: e.tensor_tensor(hb[:], gs[:], put[:], ALU.mult), reads=[gs, put], writes=[hb])
                    for fc in range(4):
                        k.op('pe', lambda e: e.transpose(pht[:, fc, :], hb[:, fc * 128:(fc + 1) * 128], identb[:]), reads=[hb, identb], writes=[pht], inc=(fc == 3))
                    k.op('dve', lambda e: e.tensor_copy(hbT[:], pht[:]), reads=[pht], writes=[hbT])
                    for n in range(2):
                        for fc in range(4):
                            k.op('pe', lambda e: e.matmul(pyt[n][:], hbT[:, fc, :], wd[q][:, fc, n * 512:(n + 1) * 512], start=(fc == 0), stop=(fc == 3)),
                                 reads=[hbT, wd[q]], writes=[pyt[n]] if fc == 0 else [], inc=(fc == 3))
                        k.op('act' if n == 0 else 'dve', (lambda e: e.activation(yb[q][:, 0:512], pyt[0][:], AF.Identity)) if n == 0 else (lambda e: e.tensor_copy(yb[q][:, 512:1024], pyt[1][:])),
                             reads=[pyt[n]], writes=[yb[q]] if n == 0 else [])
                    yb[q].w['dve'] = k.ecnt['dve']
                    k.dma('sp', YB[bk * 128:(bk + 1) * 128, :], yb[q][:], reads=[yb[q]], pw=[YB])
            k.barrier()
            with ExitStack() as es8:
                k.es = es8
                if P4S < 4:
                    raise _Stop()
                x6 = [k.sb("x6%d" % i, [128, D], F32) for i in range(2)]
                y0 = [k.sb("y0%d" % i, [128, D], F32) for i in range(2)]
                y1 = [k.sb("y1%d" % i, [128, D], F32) for i in range(2)]
                o6 = [k.sb("o6%d" % i, [128, D], F32) for i in range(2)]
                st6 = k.sb("st6", [128, 2, 6], F32)
                mv6 = k.sb("mv6", [128, 2], F32)
                rs6 = k.sb("rs6", [128, 1], F32)
                for gi in range(NT):
                    b, i = gi // (SEQ // 128), gi % (SEQ // 128)
                    q = gi % 2
                    k.dma('sp', x6[q][:], X1[gi * 128:(gi + 1) * 128, :], reads=[X1], writes=[x6[q]])
                    k.dma('pool', y0[q][:], YB[:, :], reads=[YB, DST], writes=[y0[q]],
                          indirect=(None, bass.IndirectOffsetOnAxis(ap=DST[:, gi, 0:1], axis=0)))
                    k.dma('pool', y1[q][:], YB[:, :], reads=[YB, DST], writes=[y1[q]],
                          indirect=(None, bass.IndirectOffsetOnAxis(ap=DST[:, gi, 1:2], axis=0)))
                    o = o6[q]
                    k.op('dve', lambda e: e.tensor_scalar(y0[q][:], y0[q][:], W1[:, gi:gi + 1], None, ALU.mult), reads=[y0[q], W1], writes=[y0[q]])
                    k.op('dve', lambda e: e.scalar_tensor_tensor(y0[q][:], y1[q][:], W2[:, gi:gi + 1], y0[q][:], ALU.mult, ALU.add), reads=[y1[q], W2, y0[q]], writes=[y0[q]])
                    k.op('dve', lambda e: e.tensor_tensor(y0[q][:], y0[q][:], g2b[:, b, :], ALU.mult), reads=[y0[q], g2b], writes=[y0[q]])
                    k.op('dve', lambda e: e.scalar_tensor_tensor(o[:], x6[q][:], ALPHA, y0[q][:], ALU.mult, ALU.add), reads=[x6[q], y0[q]], writes=[o])
                    for hf in range(2):
                        k.op('dve', lambda e: e.bn_stats(st6[:, hf, :], o[:, hf * 512:(hf + 1) * 512]), reads=[o], writes=[st6])
                    k.op('dve', lambda e: e.bn_aggr(mv6[:], st6[:].rearrange("p a b -> p (a b)")), reads=[st6], writes=[mv6])
                    k.op('act', lambda e: e.activation(rs6[:], mv6[:, 1:2], AF.Sqrt, bias=LN_EPS), reads=[mv6], writes=[rs6])
                    k.op('dve', lambda e: e.reciprocal(rs6[:], rs6[:]), reads=[rs6], writes=[rs6])
                    k.op('dve', lambda e: e.tensor_scalar(o[:], o[:], mv6[:, 0:1], rs6[:, 0:1], ALU.subtract, ALU.mult), reads=[o, mv6, rs6], writes=[o])
                    k.op('dve', lambda e: e.tensor_tensor(o[:], o[:], lnp[:, 2, :], ALU.mult), reads=[o, lnp], writes=[o])
                    k.op('dve', lambda e: e.tensor_tensor(o[:], o[:], lnp[:, 3, :], ALU.add), reads=[o, lnp], writes=[o])
                    k.dma('sp', out_d[b, i * 128:(i + 1) * 128, :], o[:], reads=[o])
           except _Stop:
            pass
        k.barrier()

        k.barrier()
    return nc


def host_inputs(inputs, batches, NB):
    f = lambda a: np.ascontiguousarray(a, dtype=np.float32)
    bs = list(batches)
    m = {}
    m["x"] = f(inputs["x"][bs])
    m["ctx"] = f(inputs["ctx"][bs])
    cc = np.zeros((3, D), np.float32)
    for i, b in enumerate(bs):
        cc[i] = inputs["c"][b]
    cc[2] = inputs["c_ctx"]
    m["cc"] = cc
    m["w_ada"] = f(inputs["w_ada"][0])
    m["b_ada"] = f(inputs["b_ada"][0][None, :])
    m["w_in"] = f(inputs["w_in"][0])
    m["conv_w"] = f(inputs["conv_w"][0].reshape(9, 2560))
    bi, bf = inputs["m_bias_i"][0], inputs["m_bias_f"][0]
    m["m_bias"] = f(np.concatenate([bi[0], bf[0], bi[1], bf[1]])[:, None])
    m["ident"] = np.eye(128, dtype=np.float32)
    gm = np.zeros((32, 2), np.float32)
    gm[0:8, 0] = 1; gm[16:24, 0] = 1; gm[8:16, 1] = -1; gm[24:32, 1] = -1
    m["gmask"] = gm
    ii = np.arange(64)
    m["cmask"] = np.stack([(ii[:, None] <= ii[None, :]), (ii[:, None] >= ii[None, :])], axis=1).astype(np.float32)
    m["m_norm_w"] = f(inputs["m_norm_w"][0][None, :])
    hk = lambda v: np.asarray(v, np.float32).reshape(8, 64).T
    m["rp"] = f(np.stack([hk(inputs["r_w0"][0][0]), hk(inputs["r_w0"][0][1]), hk(inputs["r_a0"][0]), hk(inputs["r_kk"][0]),
                          hk(inputs["r_ka"][0]), hk(inputs["r_ka"][0]), hk(inputs["r_bonus"][0].reshape(-1))], axis=2))
    sm = np.ones((64, 1088), np.float32); sm[:, ::64] = 0
    m["smask"] = sm
    jj = np.arange(128) % 64
    tt = np.arange(128)
    rm = np.zeros((128, 2, 128), np.float32)
    for dd in range(2):
        for col in range(128):
            tq = col % 64
            if col < 64:
                rm[:, dd, col] = (jj < tq) if dd == 0 else (jj > tq)
            else:
                rm[:, dd, col] = (jj <= tq) if dd == 0 else (jj >= tq)
    m["rmask"] = rm
    m["nmask"] = np.stack([(ii[None, :] < ii[:, None]), (ii[None, :] > ii[:, None])], axis=1).astype(np.float32)
    m["r_norm_w"] = f(inputs["r_norm_w"][0][None, :])
    m["r_norm_b"] = f(inputs["r_norm_b"][0][None, :])
    m["r_wB"] = f(inputs["r_wB"][0])
    m["r_aB"] = f(inputs["r_aB"][0])
    m["r_gB"] = f(inputs["r_gB"][0])
    m["w_out"] = f(inputs["w_out"][0])
    for nm in ("ln1_g", "ln1_b", "ln2_g", "ln2_b"):
        m[nm] = f(inputs[nm][0][None, :])
    m["rt"] = f(np.concatenate([inputs["rt_g"][0], inputs["rt_e"][0]], axis=1))
    m["rtb"] = f(np.concatenate([inputs["rt_g_b"][0], inputs["rt_e_b"][0]])[None, :])
    m["ex_gate"] = f(inputs["ex_gate"][0].reshape(32 * D, 512))
    m["ex_up"] = f(inputs["ex_up"][0].reshape(32 * D, 512))
    m["ex_down"] = f(inputs["ex_down"][0].reshape(32 * 512, D))
    pp = np.arange(128)
    m["tris"] = (pp[:, None] < pp[None, :]).astype(np.float32)
    m["thr"] = np.broadcast_to((128.0 * pp)[None, :], (128, 128)).astype(np.float32).copy()
    m["blki"] = np.broadcast_to(np.arange(160, dtype=np.float32)[None, :], (128, 160)).copy()
    m["kcp"] = (np.concatenate([np.arange(8), np.arange(4)])[None, :] * 128 + pp[:, None]).astype(np.float32)
    m["tokid"] = np.broadcast_to((np.arange(64)[None, :] * 128 + pp[:, None])[:, :, None], (128, 64, 16)).astype(np.int32).copy()
    return m


_NC_CACHE = {}


def kernel(**inputs):
    inputs = {k_: np.asarray(v) for k_, v in inputs.items()}
    NB = 2
    n_cores = 8
    if NB not in _NC_CACHE:
        _NC_CACHE[NB] = build(NB=NB)
    nc = _NC_CACHE[NB]
    in_maps = [host_inputs(inputs, [NB * c + j for j in range(NB)], NB) for c in range(n_cores)]
    res = run_bass_kernel_spmd(nc, in_maps, core_ids=list(range(n_cores)))
    out = np.concatenate([np.asarray(r["out"]) for r in res.results], axis=0)
    return np.ascontiguousarray(out, dtype=np.float32)
```

```python
import math, os
from contextlib import ExitStack
import numpy as np
import concourse.bass as bass
import concourse.mybir as mybir
from concourse.bass_utils import run_bass_kernel_spmd

F32 = mybir.dt.float32
BF16 = mybir.dt.bfloat16
I32 = mybir.dt.int32
AF = mybir.ActivationFunctionType
ALU = mybir.AluOpType
AX = mybir.AxisListType

D = 1024
SEQ = 4096
CTX = 256
T = SEQ + CTX
NCH = T // 64
INC = 3936
DS = math.exp(-0.5)
ALPHA = 2.0 ** 0.25
LN_EPS = 1e-6
GN_EPS = 64e-5
SEC = dict(mq=0, mk=512, rr=1024, rk=1536, rv=2048, mv=2560, mo=3072, gates=3584,
           lwf=3616, lwb=3680, la=3744, lg=3808)
NDS = 40


class _Stop(Exception):
    pass


class Buf:
    def __init__(self, t):
        self.t = t
        self.w = {}
        self.r = {}

    def __getitem__(self, k):
        return self.t[k]


def _merge(d, s):
    for k, v in s.items():
        if d.get(k, 0) < v:
            d[k] = v


class KB:
    def __init__(self, nc):
        self.nc = nc
        self.engs = {'pe': nc.tensor, 'dve': nc.vector, 'act': nc.scalar, 'pool': nc.gpsimd, 'sp': nc.sync}
        self.esem = {e: nc.alloc_semaphore('es_' + e) for e in self.engs}
        self.ecnt = {e: 0 for e in self.engs}
        self.pending = {e: False for e in self.engs}
        self.seen = {e: {} for e in self.engs}
        self.dsem = [nc.alloc_semaphore('ds%d' % i) for i in range(NDS)]
        self.dcnt = [0] * NDS
        self.dnext = 0
        self.es = None
        self.uid = 0

    def semh(self, key):
        return self.esem[key] if isinstance(key, str) else self.dsem[key[1]]

    def _wait(self, eng, need):
        for key, cnt in need.items():
            if self.seen[eng].get(key, 0) >= cnt:
                continue
            if key == eng and eng in ('pe',):
                continue
            self.engs[eng].wait_ge(self.semh(key), cnt)
            self.seen[eng][key] = cnt

    def op(self, eng, fn, reads=(), writes=(), inc=True, pw_=()):
        need = {}
        for b in pw_:
            _merge(need, b.r)
        for b in reads:
            _merge(need, b.w)
        for b in writes:
            _merge(need, b.w)
            _merge(need, b.r)
        self._wait(eng, need)
        ins = fn(self.engs[eng])
        cnt = self.ecnt[eng] + 1
        if inc:
            ins.then_inc(self.esem[eng], 1)
            self.ecnt[eng] = cnt
        for b in reads:
            b.r[eng] = cnt
        for b in writes:
            b.w = {eng: cnt}
            b.r = {}
        for b in pw_:
            b.w[eng] = cnt
        return ins

    def dma(self, q, out, in_, reads=(), writes=(), pw=(), indirect=None, **kw):
        i = self.dnext
        self.dnext = (i + 1) % NDS
        need = {}
        if self.dcnt[i]:
            need[('d', i)] = self.dcnt[i]
        for b in reads:
            _merge(need, b.w)
        for b in writes:
            _merge(need, b.w)
            _merge(need, b.r)
        for b in pw:
            _merge(need, b.r)
        self._wait(q, need)
        if indirect is None:
            ins = self.engs[q].dma_start(out=out, in_=in_, **kw)
        else:
            ins = self.engs[q].indirect_dma_start(out, indirect[0], in_, indirect[1], **kw)
        self.dcnt[i] += 16
        ins.then_inc(self.dsem[i], 16)
        key = ('d', i)
        cnt = self.dcnt[i]
        for b in reads:
            b.r[key] = cnt
        for b in writes:
            b.w = {key: cnt}
            b.r = {}
        for b in pw:
            b.w[key] = cnt
        return ins

    def barrier(self):
        need = {e: c for e, c in self.ecnt.items() if c}
        for i in range(NDS):
            if self.dcnt[i]:
                need[('d', i)] = self.dcnt[i]
        for e in self.engs:
            self._wait(e, need)

    def sb(self, name, shape, dt):
        self.uid += 1
        return Buf(self.es.enter_context(self.nc.sbuf_tensor("s%d_%s" % (self.uid, name), list(shape), dt)))

    def ps(self, name, shape, dt=F32):
        self.uid += 1
        return Buf(self.es.enter_context(self.nc.psum_tensor("p%d_%s" % (self.uid, name), list(shape), dt)))


def build(NB=2, debug=None, phases=(0, 1, 2, 3, 4, 5, 6)):
    nc = bass.Bass("TRN2", target_bir_lowering=False)
    k = KB(nc)

    def din(name, shape):
        return nc.dram_tensor(name, list(shape), F32, kind="ExternalInput").ap()

    x_in = din("x", [NB, SEQ, D])
    ctx_in = din("ctx", [NB, CTX, D])
    cc_in = din("cc", [3, D])
    w_ada = din("w_ada", [D, 6 * D])
    b_ada = din("b_ada", [1, 6 * D])
    w_in = din("w_in", [D, INC])
    conv_w = din("conv_w", [9, 2560])
    m_bias = din("m_bias", [32, 1])
    ident_in = din("ident", [128, 128])
    gmask_in = din("gmask", [32, 2])
    cmask_in = din("cmask", [64, 2, 64])
    m_norm_w = din("m_norm_w", [1, 512])
    rp_in = din("rp", [64, 8, 7])
    smask_in = din("smask", [64, 1088])
    rmask_in = din("rmask", [128, 2, 128])
    nmask_in = din("nmask", [64, 2, 64])
    r_norm_w = din("r_norm_w", [1, 512])
    r_norm_b = din("r_norm_b", [1, 512])
    r_wB = din("r_wB", [2, 64, 512])
    r_aB = din("r_aB", [64, 512])
    r_gB = din("r_gB", [128, 512])
    w_out = din("w_out", [D, D])
    ln1_g = din("ln1_g", [1, D]); ln1_b = din("ln1_b", [1, D]); ln2_g = din("ln2_g", [1, D]); ln2_b = din("ln2_b", [1, D])
    rt_in = din("rt", [D, 36]); rtb_in = din("rtb", [1, 36])
    ex_gate = din("ex_gate", [32 * D, 512]); ex_up = din("ex_up", [32 * D, 512]); ex_down = din("ex_down", [32 * 512, D])
    tris_in = din("tris", [128, 128]); thr_in = din("thr", [128, 128]); blki_in = din("blki", [128, 160]); kcp_in = din("kcp", [128, 12])
    tokid_in = nc.dram_tensor("tokid", [128, 64, 16], I32, kind="ExternalInput").ap()
    out_d = nc.dram_tensor("out", [NB, SEQ, D], F32, kind="ExternalOutput").ap()

    def dscr(name, shape, dt=F32):
        kind = "ExternalOutput" if (debug and name in debug) else "Internal"
        return Buf(nc.dram_tensor(name, list(shape), dt, kind=kind).ap())

    MODD = dscr("MODD", [3, 6 * D])
    FM = [dscr("FM%d" % b, [INC, T]) for b in range(NB)]
    MIX = [dscr("MIX%d" % b, [SEQ, D]) for b in range(NB)]
    BS = 512
    NBLK_ = NB * SEQ * 2 // BS + 32
    X1 = dscr("X1", [NB * SEQ, D])
    H2 = dscr("H2", [NB * SEQ, D], BF16)
    TOKB = dscr("TOKB", [NBLK_ * BS, 16], I32)
    YB = dscr("YB", [NBLK_ * BS, D])

    with ExitStack() as es0:
        k.es = es0
        ident = k.sb("ident", [128, 128], F32)
        identb = k.sb("identb", [128, 128], BF16)
        ones_f = k.sb("ones_f", [128, 128], F32)
        modT = k.sb("modT", [128, 48, 3], F32)
        k.dma('sp', ident[:], ident_in[:, :], writes=[ident])
        k.op('dve', lambda e: e.tensor_copy(identb[:], ident[:]), reads=[ident], writes=[identb])

        with ExitStack() as es:
            k.es = es
            cc = k.sb("cc", [3, D], F32)
            scT = k.sb("scT", [128, 8, 3], F32)
            bada = k.sb("bada", [3, 6 * D], F32)
            mods = k.sb("mods", [3, 6 * D], F32)
            wa = [k.sb("wa%d" % i, [128, 8, 512], F32) for i in range(2)]
            pst = k.ps("p0t", [128, 8, 3])
            psm = [k.ps("p0m%d" % i, [3, 512]) for i in range(2)]
            pmt = k.ps("p0mt", [128, 48, 3])
            k.dma('sp', cc[:], cc_in[:, :], writes=[cc])
            k.dma('sp', bada[:], b_ada[0:1, :].partition_broadcast(3), writes=[bada])
            k.op('act', lambda e: e.activation(cc[:], cc[:], AF.Silu), reads=[cc], writes=[cc])
            for kc in range(8):
                k.op('pe', lambda e: e.transpose(pst[:, kc, :], cc[:, kc * 128:(kc + 1) * 128], ident[0:3, 0:3]),
                     reads=[cc, ident], writes=[pst], inc=(kc == 7))
            k.op('dve', lambda e: e.tensor_copy(scT[:], pst[:]), reads=[pst], writes=[scT])
            for n in range(12):
                wb = wa[n % 2]
                k.dma('sp', wb[:], w_ada[:, n * 512:(n + 1) * 512].rearrange("(kc p) n -> p kc n", p=128), writes=[wb])
                pm = psm[n % 2]
                for kc in range(8):
                    k.op('pe', lambda e: e.matmul(pm[:], scT[:, kc, :], wb[:, kc, :], start=(kc == 0), stop=(kc == 7)),
                         reads=[scT, wb], writes=[pm] if kc == 0 else [], inc=(kc == 7))
                k.op('dve', lambda e: e.tensor_tensor(mods[:, n * 512:(n + 1) * 512], pm[:], bada[:, n * 512:(n + 1) * 512], ALU.add),
                     reads=[pm, bada], writes=[mods])
            k.dma('sp', MODD[:, :], mods[:], reads=[mods], writes=[MODD])
            for j in range(48):
                k.op('pe', lambda e: e.transpose(pmt[:, j, :], mods[:, j * 128:(j + 1) * 128], ident[0:3, 0:3]),
                     reads=[mods, ident], writes=[pmt], inc=(j == 47))
            k.op('dve', lambda e: e.tensor_copy(modT[:], pmt[:]), reads=[pmt], writes=[modT])
            for j0 in (8, 32):
                k.op('dve', lambda e: e.tensor_scalar(modT[:, j0:j0 + 8, :], modT[:, j0:j0 + 8, :], 1.0, None, ALU.add),
                     reads=[modT], writes=[modT])
        k.barrier()

        with ExitStack() as es:
          if 1 in phases:
              k.es = es
              wbf = k.sb("wbf", [128, 8, INC], BF16)
              hT = k.sb("hT", [128, 8, T], BF16)
              xt = [k.sb("xt%d" % i, [128, D], F32) for i in range(2)]
              xn = [k.sb("xn%d" % i, [128, D], BF16) for i in range(2)]
              st = k.sb("st", [128, 2, 6], F32)
              mv = k.sb("mv", [128, 2], F32)
              rstd = k.sb("rstd", [128, 1], F32)
              pT = [k.sb("pT%d" % i, [128, T], F32) for i in range(2)]
              acc = k.sb("acc", [128, T], F32)
              cw = k.sb("cw", [128, 20, 9], F32)
              mb = k.sb("mb", [32, 4], F32)
              ptr = [k.ps("p1t%d" % i, [128, 8, 128], BF16) for i in range(2)]
              pmm = [k.ps("p1m%d" % i, [128, 512]) for i in range(3)]
              pcw = k.ps("p1cw", [128, 20, 9])
              for kc in range(8):
                  for hf in range(2):
                      k.dma('pool', wbf[:, kc, hf * 1968:(hf + 1) * 1968],
                            w_in[kc * 128:(kc + 1) * 128, hf * 1968:(hf + 1) * 1968], pw=[wbf])
              crow = k.sb("crow", [9, 2560], F32)
              k.dma('sp', crow[:], conv_w[:, :], writes=[crow])
              for c in range(20):
                  k.op('pe', lambda e: e.transpose(pcw[:, c, :], crow[:, c * 128:(c + 1) * 128], ident[0:9, 0:9]),
                       reads=[crow, ident], writes=[pcw], inc=(c == 19))
              k.op('dve', lambda e: e.tensor_copy(cw[:], pcw[:]), reads=[pcw], writes=[cw])
              k.dma('sp', mb[:, 0:1], m_bias[:, :], writes=[mb])
              k.op('dve', lambda e: e.tensor_scalar(mb[:, 1:2], mb[:, 0:1], -1.0, None, ALU.mult), reads=[mb], writes=[mb])
              k.dma('sp', mb[:, 2:4], gmask_in[:, :], pw=[mb])
              for b in range(NB):
                  for i in range(int(os.environ.get('P1A', T // 128))):
                      xb, xnb, pt = xt[i % 2], xn[i % 2], ptr[i % 2]
                      src = ctx_in[b, i * 128:(i + 1) * 128, :] if i < 2 else x_in[b, (i - 2) * 128:(i - 1) * 128, :]
                      r = 2 if i < 2 else b
                      k.dma('sp', xb[:], src, writes=[xb])
                      S1 = int(os.environ.get('P1S', 9))
                      for hf in range(2):
                          k.op('dve', lambda e: e.bn_stats(st[:, hf, :], xb[:, hf * 512:(hf + 1) * 512]), reads=[xb], writes=[st])
                      if S1 >= 2: k.op('dve', lambda e: e.bn_aggr(mv[:], st[:].rearrange("p a b -> p (a b)")), reads=[st], writes=[mv])
                      if S1 >= 3: k.op('act', lambda e: e.activation(rstd[:], mv[:, 1:2], AF.Sqrt, bias=LN_EPS), reads=[mv], writes=[rstd])
                      if S1 >= 4: k.op('dve', lambda e: e.reciprocal(rstd[:], rstd[:]), reads=[rstd], writes=[rstd])
                      if S1 >= 5: k.op('dve', lambda e: e.tensor_scalar(xnb[:], xb[:], mv[:, 0:1], rstd[:, 0:1], ALU.subtract, ALU.mult),
                           reads=[xb, mv, rstd], writes=[xnb])
                      for kc in range(8 if S1 >= 6 else 0):
                          k.op('pe', lambda e: e.transpose(pt[:, kc, :], xnb[:, kc * 128:(kc + 1) * 128], identb[:]),
                               reads=[xnb, identb], writes=[pt] if kc == 0 else [], inc=(kc == 7))
                      for kc in range(8 if S1 >= 7 else 0):
                          if i % 2 == 0:
                              k.op('act', lambda e: e.activation(hT[:, kc, i * 128:(i + 1) * 128], pt[:, kc, :], AF.Identity,
                                                                 bias=modT[:, kc, r:r + 1], scale=modT[:, 8 + kc, r:r + 1]),
                                   reads=[pt, modT], writes=[] if (i or kc) else [hT])
                          else:
                              k.op('dve', lambda e: e.tensor_scalar(hT[:, kc, i * 128:(i + 1) * 128], pt[:, kc, :],
                                                                    modT[:, 8 + kc, r:r + 1], modT[:, kc, r:r + 1], ALU.mult, ALU.add),
                                   reads=[pt, modT], writes=[])
                      hT.w['act'] = k.ecnt['act']
                      hT.w['dve'] = k.ecnt['dve']
                  chunks = [(c * 128, 128) for c in range(28)] + [(3584, 32), (3616, 64), (3680, 64), (3744, 64), (3808, 128)]
                  for ci, (c0, M) in enumerate(chunks[:int(os.environ.get('P1C', 99))]):
                      pb = pT[ci % 2]
                      for g in range(9):
                          t0 = g * 512
                          n = min(512, T - t0)
                          pm = pmm[(ci * 9 + g) % 3]
                          for kc in range(8):
                              k.op('pe', lambda e: e.matmul(pm[0:M, 0:n], wbf[:, kc, c0:c0 + M], hT[:, kc, t0:t0 + n],
                                                            start=(kc == 0), stop=(kc == 7)),
                                   reads=[wbf, hT], writes=[pm] if kc == 0 else [], inc=(kc == 7))
                          k.op('act', lambda e: e.activation(pb[0:M, t0:t0 + n], pm[0:M, 0:n], AF.Identity),
                               reads=[pm], writes=[pb] if g == 0 else [])
                          pb.w['act'] = k.ecnt['act']
                      src = pb
                      if c0 < 2560:
                          c = c0 // 128
                          k.op('act', lambda e: e.activation(acc[:, :], pb[:, :], AF.Identity, scale=cw[:, c, 4:5]),
                               reads=[pb, cw], writes=[acc])
                          k.op('dve', lambda e: e.scalar_tensor_tensor(acc[:, 1:CTX], pb[:, 0:CTX - 1], cw[:, c, 3:4], acc[:, 1:CTX], ALU.mult, ALU.add),
                               reads=[pb, cw, acc], writes=[acc])
                          k.op('dve', lambda e: e.scalar_tensor_tensor(acc[:, 0:CTX - 1], pb[:, 1:CTX], cw[:, c, 5:6], acc[:, 0:CTX - 1], ALU.mult, ALU.add),
                               reads=[pb, cw, acc], writes=[acc])
                          a3 = acc[:, CTX:T].rearrange("p (r c) -> p r c", c=64)
                          p3 = pb[:, CTX:T].rearrange("p (r c) -> p r c", c=64)
                          for ky in range(3):
                              for kx in range(3):
                                  if ky == 1 and kx == 1:
                                      continue
                                  dy, dx = ky - 1, kx - 1
                                  oy0, oy1 = max(0, -dy), 64 - max(0, dy)
                                  ox0, ox1 = max(0, -dx), 64 - max(0, dx)
                                  k.op('dve', lambda e: e.scalar_tensor_tensor(
                                      a3[:, oy0:oy1, ox0:ox1], p3[:, oy0 + dy:oy1 + dy, ox0 + dx:ox1 + dx],
                                      cw[:, c, ky * 3 + kx:ky * 3 + kx + 1], a3[:, oy0:oy1, ox0:ox1], ALU.mult, ALU.add),
                                      reads=[pb, cw, acc], writes=[acc])
                          src = acc
                      sec = [s for s, v in SEC.items() if v <= c0][-1]
                      if sec in ('mq', 'mk'):
                          k.op('act', lambda e: e.activation(acc[:, :], src[:, :], AF.Silu), reads=[src], writes=[acc])
                          if sec == 'mk':
                              k.op('dve', lambda e: e.tensor_scalar(acc[:, :], acc[:, :], 0.125, None, ALU.mult), reads=[acc], writes=[acc])
                          src = acc
                      elif sec in ('mo', 'lg'):
                          k.op('act', lambda e: e.activation(acc[0:M, :], src[0:M, :], AF.Sigmoid), reads=[src], writes=[acc])
                          src = acc
                      elif sec in ('lwf', 'lwb'):
                          k.op('act', lambda e: e.activation(acc[0:M, :], src[0:M, :], AF.Tanh), reads=[src], writes=[acc])
                          src = acc
                      elif sec == 'gates':
                          tmp = pT[1 - ci % 2]
                          k.op('act', lambda e: e.activation(tmp[0:32, :], pb[0:32, :], AF.Exp, bias=mb[:, 1:2], scale=-1.0),
                               reads=[pb, mb], writes=[tmp])
                          k.op('act', lambda e: e.activation(tmp[0:32, :], tmp[0:32, :], AF.Ln, bias=1.0), reads=[tmp], writes=[tmp])
                          k.op('dve', lambda e: e.tensor_scalar(tmp[0:32, :], tmp[0:32, :], mb[:, 3:4], None, ALU.mult), reads=[tmp, mb], writes=[tmp])
                          k.op('dve', lambda e: e.tensor_scalar(acc[0:32, :], pb[0:32, :], mb[:, 0:1], mb[:, 2:3], ALU.add, ALU.mult),
                               reads=[pb, mb], writes=[acc])
                          k.op('dve', lambda e: e.tensor_tensor(acc[0:32, :], acc[0:32, :], tmp[0:32, :], ALU.add), reads=[acc, tmp], writes=[acc])
                          src = acc
                      k.dma('sp', FM[b][c0:c0 + M, :], src[0:M, :], reads=[src], pw=[FM[b]])
        k.barrier()


        with ExitStack() as es:
          if 2 in phases:
            k.es = es
            cm = k.sb("cm", [64, 2, 64], F32)
            k.dma('sp', cm[:], cmask_in[:, :, :], writes=[cm])
            nw = k.sb("nw", [64, 512], F32)
            k.dma('sp', nw[:], m_norm_w[0:1, :].partition_broadcast(64), writes=[nw])
            k.op('pool', lambda e: e.memset(ones_f[:], 1.0), writes=[ones_f])
            GA = k.sb("GA", [64, NCH, 48], F32)
            gT = k.sb("gT", [32, T], F32)
            G = k.sb("G", [64, 32], F32)
            qh = k.sb("qh", [64, 2, T], BF16)
            kh = k.sb("kh", [64, 2, T], BF16)
            vT = k.sb("vT", [128, T], BF16)
            moT = k.sb("moT", [128, T], F32)
            Hf = k.sb("Hf", [64, NCH, 2, 64], F32)
            Ktm = k.sb("Ktm", [64, 2, 64], BF16)
            Vaug = k.sb("Vaug", [64, 2, 66], BF16)
            PTm = k.sb("PTm", [64, 2, 64], BF16)
            Cst = k.sb("Cst", [64, 2, 66], F32)
            Cbf = k.sb("Cbf", [64, 2, 66], BF16)
            dn = k.sb("dn", [64, 2], F32)
            ff = k.sb("ff", [64, 2], F32)
            hs = k.sb("hs", [64, 2, 64], F32)
            st2 = k.sb("st2", [64, 2, 6], F32)
            mv2 = k.sb("mv2", [64, 2, 2], F32)
            rs2 = k.sb("rs2", [64, 2], F32)
            om = k.sb("om", [64, 128], F32)
            pg = k.ps("p2g", [64, 32])
            pbb = k.ps("p2b", [64, 32])
            pk = k.ps("p2k", [64, 2, 64], BF16)
            pv = k.ps("p2v", [64, 128], BF16)
            pp = k.ps("p2p", [64, 2, 64])
            po = k.ps("p2o", [64, 2, 66])
            pc = k.ps("p2c", [64, 2, 66])
            pmo = k.ps("p2mo", [64, 128])
            for b in range(NB):
                k.dma('sp', gT[:], FM[b][3584:3616, :], reads=[FM[b]], writes=[gT])
                for c in range(NCH):
                    k.op('pe', lambda e: e.transpose(pg[:], gT[:, c * 64:(c + 1) * 64], ident[0:32, 0:32]), reads=[gT, ident], writes=[pg])
                    k.op('dve', lambda e: e.tensor_copy(G[:], pg[:]), reads=[pg], writes=[G])
                    k.op('pe', lambda e: e.matmul(pbb[:, 0:8], cm[:, 0, :], G[:, 8:16], start=True, stop=True), reads=[cm, G], writes=[pbb], inc=False)
                    k.op('pe', lambda e: e.matmul(pbb[:, 8:16], cm[:, 1, :], G[:, 24:32], start=True, stop=True), reads=[cm, G], inc=False)
                    k.op('pe', lambda e: e.matmul(pbb[:, 16:24], ones_f[0:64, 0:64], G[:, 8:16], start=True, stop=True), reads=[ones_f, G], inc=False)
                    k.op('pe', lambda e: e.matmul(pbb[:, 24:32], ones_f[0:64, 0:64], G[:, 24:32], start=True, stop=True), reads=[ones_f, G])
                    k.op('act', lambda e: e.activation(GA[:, c, 0:32], pbb[:], AF.Exp), reads=[pbb], writes=[GA])
                    k.op('dve', lambda e: e.tensor_tensor(G[:, 0:8], G[:, 0:8], pbb[:, 0:8], ALU.subtract), reads=[pbb, G], writes=[G])
                    k.op('dve', lambda e: e.tensor_tensor(G[:, 16:24], G[:, 16:24], pbb[:, 8:16], ALU.subtract), reads=[pbb, G], writes=[G])
                    k.op('act', lambda e: e.activation(GA[:, c, 32:40], G[:, 0:8], AF.Exp), reads=[G], writes=[GA])
                    k.op('act', lambda e: e.activation(GA[:, c, 40:48], G[:, 16:24], AF.Exp), reads=[G], writes=[GA])
                for hp in range(4):
                    for h in range(2):
                        r0 = hp * 128 + h * 64
                        for q4 in range(4):
                            t0 = q4 * 1088
                            k.dma('pool', qh[:, h, t0:t0 + 1088], FM[b][r0:r0 + 64, t0:t0 + 1088], reads=[FM[b]], pw=[qh])
                            k.dma('pool', kh[:, h, t0:t0 + 1088], FM[b][512 + r0:512 + r0 + 64, t0:t0 + 1088], reads=[FM[b]], pw=[kh])
                    for q4 in range(4):
                        t0 = q4 * 1088
                        k.dma('pool', vT[:, t0:t0 + 1088], FM[b][2560 + hp * 128:2560 + (hp + 1) * 128, t0:t0 + 1088], reads=[FM[b]], pw=[vT])
                    k.dma('sp', moT[:], FM[b][3072 + hp * 128:3072 + (hp + 1) * 128, :], reads=[FM[b]], writes=[moT])
                    for d in range(2):
                        order = list(range(NCH)) if d == 0 else [3, 2, 1, 0] + list(range(NCH - 1, 3, -1))
                        k.op('pool', lambda e: e.memset(Cst[:], 0.0), writes=[Cst])
                        k.op('pool', lambda e: e.memset(Cbf[:], 0.0), writes=[Cbf])
                        for c in order:
                            cs = slice(c * 64, (c + 1) * 64)
                            hh = 2 * hp
                            a_ap = GA[:, c, d * 8 + hh:d * 8 + hh + 2]
                            e_ap = lambda h: GA[:, c, 16 + d * 8 + hh + h:16 + d * 8 + hh + h + 1]
                            c_ap = GA[:, c, 32 + d * 8 + hh:32 + d * 8 + hh + 2]
                            for h in range(2):
                                k.op('pe', lambda e: e.transpose(pk[:, h, :], kh[:, h, cs], identb[0:64, 0:64]), reads=[kh, identb], writes=[pk], inc=(h == 1))
                            k.op('pe', lambda e: e.transpose(pv[:], vT[:, cs], identb[:]), reads=[vT, identb], writes=[pv])
                            for h in range(2):
                                k.op('pe', lambda e: e.matmul(pp[:, h, :], kh[:, h, cs], qh[:, h, cs], start=True, stop=True), reads=[kh, qh], writes=[pp], inc=(h == 1))
                            k.op('act', lambda e: e.activation(Ktm[:], pk[:], AF.Identity), reads=[pk], writes=[Ktm])
                            k.op('dve', lambda e: e.tensor_tensor(Vaug[:, :, 0:64], pv[:].rearrange("p (h e) -> p h e", h=2),
                                                                  c_ap.unsqueeze(2).to_broadcast([64, 2, 64]), ALU.mult), reads=[pv, GA], writes=[Vaug])
                            k.op('dve', lambda e: e.tensor_copy(Vaug[:, :, 64:65], c_ap.unsqueeze(2)), reads=[GA], writes=[Vaug])
                            k.op('dve', lambda e: e.tensor_tensor(PTm[:], pp[:], cm[:, d:d + 1, :].to_broadcast([64, 2, 64]), ALU.mult), reads=[pp, cm], writes=[PTm])
                            for h in range(2):
                                k.op('pe', lambda e: e.matmul(po[:, h, 0:65], PTm[:, h, :], Vaug[:, h, 0:65], start=True, stop=False), reads=[PTm, Vaug], writes=[po], inc=False)
                                k.op('pe', lambda e: e.matmul(po[:, h, 0:65], qh[:, h, cs], Cbf[:, h, 0:65], start=False, stop=True), reads=[qh, Cbf], inc=(h == 1))
                            for h in range(2):
                                k.op('pe', lambda e: e.matmul(pc[:, h, 0:65], Ktm[:, h, :], Vaug[:, h, 0:65], start=True, stop=True), reads=[Ktm, Vaug], writes=[pc], inc=(h == 1))
                            k.op('dve', lambda e: e.tensor_tensor(dn[:], po[:, :, 64], a_ap, ALU.mult), reads=[po, GA], writes=[dn])
                            k.op('act', lambda e: e.activation(dn[:], dn[:], AF.Abs), reads=[dn], writes=[dn])
                            k.op('dve', lambda e: e.tensor_scalar(dn[:], dn[:], 1.0, None, ALU.max), reads=[dn], writes=[dn])
                            k.op('dve', lambda e: e.reciprocal(dn[:], dn[:]), reads=[dn], writes=[dn])
                            k.op('dve', lambda e: e.tensor_tensor(ff[:], dn[:], a_ap, ALU.mult), reads=[dn, GA], writes=[ff])
                            if d == 0:
                                for h in range(2):
                                    k.op('dve', lambda e: e.tensor_scalar(Hf[:, c, h, :], po[:, h, 0:64], ff[:, h:h + 1], None, ALU.mult), reads=[po, ff], writes=[Hf])
                            else:
                                for h in range(2):
                                    k.op('dve', lambda e: e.scalar_tensor_tensor(hs[:, h, :], po[:, h, 0:64], ff[:, h:h + 1], Hf[:, c, h, :], ALU.mult, ALU.add),
                                         reads=[po, ff, Hf], writes=[hs])
                            for h in range(2):
                                k.op('dve', lambda e: e.tensor_scalar(Cst[:, h, 0:65], Cst[:, h, 0:65], e_ap(h), None, ALU.mult), reads=[GA, Cst], writes=[Cst])
                                k.op('dve', lambda e: e.scalar_tensor_tensor(Cst[:, h, 0:65], pc[:, h, 0:65], e_ap(h), Cst[:, h, 0:65], ALU.mult, ALU.add),
                                     reads=[pc, GA, Cst], writes=[Cst])
                            k.op('act', lambda e: e.activation(Cbf[:, :, 0:65], Cst[:, :, 0:65], AF.Identity), reads=[Cst], writes=[Cbf])
                            if d == 1 and c >= 4:
                                for h in range(2):
                                    k.op('dve', lambda e: e.bn_stats(st2[:, h, :], hs[:, h, :]), reads=[hs], writes=[st2])
                                for h in range(2):
                                    k.op('dve', lambda e: e.bn_aggr(mv2[:, h, :], st2[:, h, :]), reads=[st2], writes=[mv2])
                                k.op('act', lambda e: e.activation(rs2[:], mv2[:, :, 1], AF.Sqrt, bias=LN_EPS), reads=[mv2], writes=[rs2])
                                k.op('dve', lambda e: e.reciprocal(rs2[:], rs2[:]), reads=[rs2], writes=[rs2])
                                for h in range(2):
                                    k.op('dve', lambda e: e.tensor_scalar(hs[:, h, :], hs[:, h, :], mv2[:, h, 0:1], rs2[:, h:h + 1], ALU.subtract, ALU.mult),
                                         reads=[hs, mv2, rs2], writes=[hs])
                                k.op('pe', lambda e: e.transpose(pmo[:], moT[:, cs], ident[:]), reads=[moT, ident], writes=[pmo])
                                k.op('dve', lambda e: e.tensor_tensor(om[:], hs[:].rearrange("p h e -> p (h e)"), nw[:, hp * 128:(hp + 1) * 128], ALU.mult),
                                     reads=[hs, nw], writes=[om])
                                k.op('dve', lambda e: e.tensor_tensor(om[:], om[:], pmo[:], ALU.mult), reads=[om, pmo], writes=[om])
                                k.dma('sp', MIX[b][(c - 4) * 64:(c - 3) * 64, hp * 128:(hp + 1) * 128], om[:], reads=[om], pw=[MIX[b]])
        k.barrier()


        with ExitStack() as es:
          if 3 in phases:
            k.es = es
            NBK = 512
            NBC = 8
            rp = k.sb("rp", [64, 8, 8], F32)
            k.dma('sp', rp[:, :, 0:7], rp_in[:, :, :], writes=[rp])
            k.op('dve', lambda e: e.tensor_scalar(rp[:, :, 7], rp[:, :, 4], -1.0, 1.0, ALU.mult, ALU.add), reads=[rp], writes=[rp])
            smask = k.sb("smask", [64, NBK], F32)
            k.dma('sp', smask[:], smask_in[:, 0:NBK], writes=[smask])
            rmask = k.sb("rmask", [128, 2, 128], F32)
            k.dma('sp', rmask[:], rmask_in[:, :, :], writes=[rmask])
            nmask = k.sb("nmask", [64, 2, 64], F32)
            k.dma('sp', nmask[:], nmask_in[:, :, :], writes=[nmask])
            gnw = k.sb("gnw", [64, 2, 512], F32)
            k.dma('sp', gnw[:, 0, :], r_norm_w[0:1, :].partition_broadcast(64), pw=[gnw])
            k.dma('sp', gnw[:, 1, :], r_norm_b[0:1, :].partition_broadcast(64), pw=[gnw])
            wBb = k.sb("wBb", [64, 2, 512], BF16)
            aBb = k.sb("aBb", [64, 512], BF16)
            gBb = k.sb("gBb", [128, 512], BF16)
            k.dma('pool', wBb[:, 0, :], r_wB[0, :, :], pw=[wBb])
            k.dma('pool', wBb[:, 1, :], r_wB[1, :, :], pw=[wBb])
            k.dma('pool', aBb[:], r_aB[:, :], writes=[aBb])
            k.dma('pool', gBb[:], r_gB[:, :], writes=[gBb])
            k.op('pool', lambda e: e.memset(ones_f[:], 1.0), writes=[ones_f])
            onesb = k.sb("onesb", [64, 2], BF16)
            k.op('pool', lambda e: e.memset(onesb[:], 1.0), writes=[onesb])
            lgb = k.sb("lgb", [128, T], BF16)
            lab = k.sb("lab", [64, NBK], BF16)
            lwb_ = k.sb("lwb_", [64, NBK], BF16)
            rr = k.sb("rr", [64, NBK], F32)
            rk = k.sb("rk", [64, NBK], F32)
            aa = k.sb("aa", [64, NBK], F32)
            t1 = k.sb("t1", [64, NBK], F32)
            t2 = k.sb("t2", [64, NBK], F32)
            khat = k.sb("khat", [64, NBK], F32)
            kmod = k.sb("kmod", [64, NBK], F32)
            beta = k.sb("beta", [64, NBK], F32)
            lgw = k.sb("lgw", [64, NBK], F32)
            lam = k.sb("lam", [64, NBK], F32)
            ee = k.sb("ee", [64, NBK], F32)
            class _S:
                pass
            SS = []
            for si in range(2):
                S = _S()
                S.i = si
                S.VTb = k.sb("VTb", [64, 2, 64 + NBK], BF16)
                S.PRb = k.sb("PRb", [64, 2, NBK], BF16)
                S.AR = k.sb("AR", [64, 2, NBC, 128], BF16)
                S.ZT = k.sb("ZT", [64, 2, NBC, 128], BF16)
                S.GL = k.sb("GL", [64, 2, NBC], F32)
                S.MmA = k.sb("MmA", [128, 2, NBC, 128], BF16)
                S.XLA = k.sb("XLA", [128, 2, NBC, 64], BF16)
                S.SWA = k.sb("SWA", [128, 2, NBC + 1, 64], BF16)
                S.WA = k.sb("WA", [128, 2, NBC, 64], BF16)
                S.ZtA = k.sb("ZtA", [128, 2, NBC, 64], BF16)
                S.TTA = k.sb("TTA", [64, 2, NBC, 64], BF16)
                S.NGa = [k.sb("NGa%d" % j, [64, 2, 8, 64], BF16) for j in range(2)]
                S.TTg = [k.sb("TTg%d" % j, [64, 8, 64], BF16) for j in range(2)]
                S.Xb = k.sb("Xb", [64, 2, 64], BF16)
                S.ST = k.sb("ST", [64, 2, 64], F32)
                S.tS = k.sb("tS", [64, 2, 64], F32)
                S.ys = k.sb("ys", [64, 2, 64], F32)
                S.bon = k.sb("bon", [64, 2], F32)
                S.om3 = k.sb("om3", [64, 128], F32)
                S.st2 = k.sb("st2r", [64, 2, 6], F32)
                S.mv2 = k.sb("mv2r", [64, 2, 2], F32)
                S.rs2 = k.sb("rs2r", [64, 2], F32)
                S.pXU = k.ps("p3x", [64, 2, 2, 64])
                S.pYS = k.ps("p3y", [64, 512])
                SS.append(S)
            Yf = k.sb("Yf", [64, NCH, 2, 64], F32)
            identg = k.sb("identg", [64, 8, 64], F32)
            pMg = k.ps("p3m", [128, 8, 128])
            pSg = k.ps("p3s", [128, 2, 512])
            pA = pMg
            for S in SS:
                k.op('pool', lambda e: e.memset(S.VTb[:], 0.0), writes=[S.VTb])
                k.op('pool', lambda e: e.memset(S.SWA[:], 0.0), writes=[S.SWA])
            for m in range(8):
                k.op('dve', lambda e: e.tensor_copy(identg[:, m, :], ident[0:64, 0:64]), reads=[ident], writes=[identg])

            def prep(S, b, hp, tok0, ntok, d):
                VTb, PRb, AR, ZT, GL = S.VTb, S.PRb, S.AR, S.ZT, S.GL
                nch = ntok // 64
                groups = [(n0, min(512, ntok - n0)) for n0 in range(0, ntok, 512)]
                k.dma('pool', lab[:, 0:ntok], FM[b][3744:3808, tok0:tok0 + ntok], reads=[FM[b]], writes=[lab])
                lo = 3616 + 64 * d
                k.dma('pool', lwb_[:, 0:ntok], FM[b][lo:lo + 64, tok0:tok0 + ntok], reads=[FM[b]], writes=[lwb_])
                for h in range(2):
                    hh = hp * 2 + h
                    r0 = hp * 128 + h * 64
                    NS = slice(0, ntok)
                    k.dma('sp', rr[:, NS], FM[b][1024 + r0:1024 + r0 + 64, tok0:tok0 + ntok], reads=[FM[b]], writes=[rr])
                    k.dma('sp', rk[:, NS], FM[b][1536 + r0:1536 + r0 + 64, tok0:tok0 + ntok], reads=[FM[b]], writes=[rk])
                    k.dma('pool', VTb[:, h, 64:64 + ntok], FM[b][2048 + r0:2048 + r0 + 64, tok0:tok0 + ntok], reads=[FM[b]], pw=[VTb])
                    for (n0, n) in groups:
                        k.op('pe', lambda e: e.matmul(pA[0:64, 0, 0:n] if False else pA[0:64, 0:4, :].rearrange("p a b -> p (a b)")[:, 0:n], aBb[:, hh * 64:(hh + 1) * 64], lab[:, n0:n0 + n], start=True, stop=True),
                             reads=[aBb, lab], writes=[pA])
                        k.op('act', lambda e: e.activation(aa[:, n0:n0 + n], pA[0:64, 0:4, :].rearrange("p a b -> p (a b)")[:, 0:n], AF.Sigmoid, bias=rp[:, hh, 2:3]), reads=[pA, rp], writes=[aa])
                        k.op('pe', lambda e: e.matmul(pA[0:64, 4:8, :].rearrange("p a b -> p (a b)")[:, 0:n], wBb[:, d, hh * 64:(hh + 1) * 64], lwb_[:, n0:n0 + n], start=True, stop=True),
                             reads=[wBb, lwb_], writes=[pA])
                        k.op('act', lambda e: e.activation(lgw[:, n0:n0 + n], pA[0:64, 4:8, :].rearrange("p a b -> p (a b)")[:, 0:n], AF.Sigmoid, bias=rp[:, hh, d:d + 1]), reads=[pA, rp], writes=[lgw])
                    k.op('dve', lambda e: e.tensor_scalar(lgw[:, NS], lgw[:, NS], -DS, None, ALU.mult), reads=[lgw], writes=[lgw])
                    k.op('dve', lambda e: e.tensor_scalar(t1[:, NS], rk[:, NS], rp[:, hh, 3:4], None, ALU.mult), reads=[rk, rp], writes=[t1])
                    k.op('dve', lambda e: e.tensor_tensor(t2[:, NS], t1[:, NS], t1[:, NS], ALU.mult), reads=[t1], writes=[t2])
                    for (n0, n) in groups:
                        k.op('pe', lambda e: e.matmul(pA[0:64, 0:4, :].rearrange("p a b -> p (a b)")[:, 0:n], ones_f[0:64, 0:64], t2[:, n0:n0 + n], start=True, stop=True),
                             reads=[ones_f, t2], writes=[pA])
                        k.op('act', lambda e: e.activation(khat[:, n0:n0 + n], pA[0:64, 0:4, :].rearrange("p a b -> p (a b)")[:, 0:n], AF.Sqrt), reads=[pA], writes=[khat])
                    k.op('dve', lambda e: e.tensor_scalar(khat[:, NS], khat[:, NS], 1e-12, None, ALU.max), reads=[khat], writes=[khat])
                    k.op('dve', lambda e: e.reciprocal(khat[:, NS], khat[:, NS]), reads=[khat], writes=[khat])
                    k.op('dve', lambda e: e.tensor_tensor(khat[:, NS], khat[:, NS], t1[:, NS], ALU.mult), reads=[khat, t1], writes=[khat])
                    k.op('dve', lambda e: e.tensor_scalar(t1[:, NS], aa[:, NS], rp[:, hh, 4:5], rp[:, hh, 7:8], ALU.mult, ALU.add), reads=[aa, rp], writes=[t1])
                    k.op('dve', lambda e: e.tensor_tensor(kmod[:, NS], rk[:, NS], t1[:, NS], ALU.mult), reads=[rk, t1], writes=[kmod])
                    k.op('dve', lambda e: e.tensor_tensor(beta[:, NS], khat[:, NS], aa[:, NS], ALU.mult), reads=[khat, aa], writes=[beta])
                    k.op('dve', lambda e: e.scalar_tensor_tensor(PRb[:, h, NS], rr[:, NS], rp[:, hh, 6:7], kmod[:, NS], ALU.mult, ALU.mult), reads=[rr, rp, kmod], writes=[PRb])
                    k.op('dve', lambda e: e.tensor_tensor_scan(lam[:, NS], smask[:, NS], lgw[:, NS], 0.0, ALU.mult, ALU.add), reads=[smask, lgw], writes=[lam])
                    v3 = lambda t_: t_[:, NS].rearrange("p (c t) -> p c t", t=64)
                    if d == 1:
                        k.op('dve', lambda e: e.tensor_tensor(t1[:, NS], lgw[:, NS], lam[:, NS], ALU.subtract), reads=[lgw, lam], writes=[t1])
                        k.op('dve', lambda e: e.tensor_tensor(v3(t2), v3(t1), v3(lam)[:, :, 63:64].to_broadcast([64, nch, 64]), ALU.add), reads=[t1, lam], writes=[t2])
                        k.op('dve', lambda e: e.tensor_copy(lam[:, NS], t2[:, NS]), reads=[t2], writes=[lam])
                    k.op('dve', lambda e: e.tensor_tensor(t1[:, NS], lam[:, NS], lgw[:, NS], ALU.subtract), reads=[lam, lgw], writes=[t1])
                    k.op('act', lambda e: e.activation(ee[:, NS], t1[:, NS], AF.Exp), reads=[t1], writes=[ee])
                    k.op('dve', lambda e: e.scalar_tensor_tensor(AR[:, h, 0:nch, 0:64], v3(khat), -1.0, v3(ee), ALU.mult, ALU.mult), reads=[khat, ee], writes=[AR])
                    k.op('act', lambda e: e.activation(ee[:, NS], lam[:, NS], AF.Exp), reads=[lam], writes=[ee])
                    k.op('dve', lambda e: e.tensor_tensor(AR[:, h, 0:nch, 64:128], v3(rr), v3(ee), ALU.mult), reads=[rr, ee], writes=[AR])
                    gcol = 63 if d == 0 else 0
                    k.op('dve', lambda e: e.tensor_copy(GL[:, h, 0:nch], v3(ee)[:, :, gcol]), reads=[ee], writes=[GL])
                    k.op('act', lambda e: e.activation(ee[:, NS], lam[:, NS], AF.Exp, scale=-1.0), reads=[lam], writes=[ee])
                    k.op('dve', lambda e: e.tensor_tensor(ZT[:, h, 0:nch, 0:64], v3(beta), v3(ee), ALU.mult), reads=[beta, ee], writes=[ZT])
                    k.op('dve', lambda e: e.tensor_tensor(ZT[:, h, 0:nch, 64:128], v3(kmod), v3(ee), ALU.mult), reads=[kmod, ee], writes=[ZT])

            pSf = lambda: pSg[:].rearrange("p a b -> p (a b)")

            def precompute(S, l0, d):
                VTb, AR, ZT, MmA, XLA, SWA, WA, ZtA, TTA, NGa, TTg = S.VTb, S.AR, S.ZT, S.MmA, S.XLA, S.SWA, S.WA, S.ZtA, S.TTA, S.NGa, S.TTg
                G4 = slice(l0, l0 + 4)
                pTb = pSg[:, 0, :].bitcast(BF16)
                for h in range(2):
                    for j in range(4):
                        m = h * 4 + j
                        k.op('pe', lambda e: e.transpose(pTb[:, m * 64:(m + 1) * 64], ZT[:, h, l0 + j, :], identb[0:64, 0:64]), reads=[ZT, identb], writes=[pSg], inc=False)
                for h in range(2):
                    for j in range(4):
                        m = 8 + h * 4 + j
                        k.op('pe', lambda e: e.transpose(pTb[:, m * 64:(m + 1) * 64], VTb[:, h, (l0 + j) * 64:(l0 + j) * 64 + 128], identb[0:64, 0:64]), reads=[VTb, identb], inc=(h == 1 and j == 3))
                zsrc = pTb[:, 0:512].rearrange("p (h j e) -> p h j e", h=2, j=4)
                vsrc = pTb[64:128, 512:1024].rearrange("p (h j e) -> p h j e", h=2, j=4)
                k.op('act', lambda e: e.activation(ZtA[:, :, G4, :], zsrc, AF.Identity), reads=[pSg], writes=[ZtA])
                k.op('act', lambda e: e.activation(SWA[64:128, :, G4, :], vsrc, AF.Identity), reads=[pSg], writes=[SWA])
                k.op('act', lambda e: e.activation(WA[64:128, :, G4, :], vsrc, AF.Identity), reads=[pSg], writes=[WA])
                yield
                for h in range(2):
                    for j in range(4):
                        m = h * 4 + j
                        k.op('pe', lambda e: e.matmul(pMg[:, m, :], ZT[:, h, l0 + j, :], AR[:, h, l0 + j, :], start=True, stop=True), reads=[ZT, AR], writes=[pMg], inc=(m == 7))
                for h in range(2):
                    for j in range(4):
                        m = h * 4 + j
                        k.op('pe', lambda e: e.matmul(pSg[0:64, 1, m * 64:(m + 1) * 64], AR[:, h, l0 + j, 0:64], ZT[:, h, l0 + j, 0:64], start=True, stop=True), reads=[ZT, AR], writes=[pSg], inc=(m == 7))
                for h in range(2):
                    k.op('dve', lambda e: e.tensor_tensor(MmA[:, h, G4, :], pMg[:, h * 4:(h + 1) * 4, :], rmask[:, d:d + 1, :].to_broadcast([128, 4, 128]), ALU.mult),
                         reads=[pMg, rmask], writes=[MmA])
                N0, N1 = NGa[0], NGa[1]
                k.op('dve', lambda e: e.tensor_tensor(N0[:, 0, :, :], pSg[0:64, 1, :].rearrange("p (m e) -> p m e", e=64), nmask[:, d:d + 1, :].to_broadcast([64, 8, 64]), ALU.mult),
                     reads=[pSg, nmask], writes=[N0])
                k.op('act', lambda e: e.activation(N0[:, 1, :, :].rearrange("p (h j) e -> p h j e", h=2), MmA[0:64, :, G4, 0:64], AF.Identity), reads=[MmA], writes=[N0])
                k.op('act', lambda e: e.activation(XLA[64:128, :, G4, :], MmA[64:128, :, G4, 0:64], AF.Identity), reads=[MmA], writes=[XLA])
                k.op('act', lambda e: e.activation(XLA[0:64, :, G4, :], AR[:, :, G4, 0:64], AF.Identity), reads=[AR], writes=[XLA])
                yield
                k.op('dve', lambda e: e.tensor_tensor(TTg[0][:], N0[:, 1, :, :], identg[:], ALU.add), reads=[N0, identg], writes=[TTg[0]])
                cur = 0
                for lv in range(1, 6):
                    yield
                    src, dst = NGa[cur], NGa[1 - cur]
                    for m in range(8):
                        k.op('pe', lambda e: e.matmul(pSg[0:64, 0, m * 64:(m + 1) * 64], src[:, 1, m, :], src[:, 0, m, :], start=True, stop=True), reads=[src], writes=[pSg], inc=False)
                    for m in range(8):
                        k.op('pe', lambda e: e.matmul(pSg[0:64, 1, m * 64:(m + 1) * 64], src[:, 0, m, :], src[:, 1, m, :], start=True, stop=True), reads=[src], inc=(m == 7))
                    k.op('act', lambda e: e.activation(dst[:, 0, :, :].rearrange("p m e -> p (m e)"), pSg[0:64, 0, :], AF.Identity), reads=[pSg], writes=[dst])
                    k.op('dve', lambda e: e.tensor_copy(dst[:, 1, :, :].rearrange("p m e -> p (m e)"), pSg[0:64, 1, :]), reads=[pSg], writes=[dst])
                    cur = 1 - cur
                    ti, to = TTg[(lv - 1) % 2], TTg[lv % 2]
                    for m in range(8):
                        k.op('pe', lambda e: e.matmul(pMg[0:64, m, 0:64], dst[:, 0, m, :], ti[:, m, :], start=True, stop=True), reads=[dst, ti], writes=[pMg], inc=(m == 7))
                    if lv < 5:
                        k.op('dve', lambda e: e.tensor_tensor(to[:], pMg[0:64, :, 0:64], ti[:], ALU.add), reads=[pMg, ti], writes=[to])
                    else:
                        k.op('dve', lambda e: e.tensor_tensor(TTA[:, :, G4, :], pMg[0:64, :, 0:64].rearrange("p (h j) e -> p h j e", h=2), ti[:].rearrange("p (h j) e -> p h j e", h=2), ALU.add),
                             reads=[pMg, ti], writes=[TTA])

            def step(S, b, hp, c, lc, lnext, d, done):
                VTb, PRb, AR, GL, MmA, XLA, SWA, WA, ZtA, TTA = S.VTb, S.PRb, S.AR, S.GL, S.MmA, S.XLA, S.SWA, S.WA, S.ZtA, S.TTA
                Xb, ST, tS, ys, bon, om3, st2, mv2, rs2, pXU, pYS = S.Xb, S.ST, S.tS, S.ys, S.bon, S.om3, S.st2, S.mv2, S.rs2, S.pXU, S.pYS
                pX = pXU[:, 0, :, :]
                pU = pXU[:, 1, :, :]
                pY = pYS[:, 0:128].rearrange("p (h e) -> p h e", h=2)
                pS_ = pYS[:, 128:256].rearrange("p (h e) -> p h e", h=2)
                for h in range(2):
                    k.op('pe', lambda e: e.matmul(pXU[:, 0, h, :], XLA[:, h, lc, :], SWA[:, h, lc, :], start=True, stop=True), reads=[XLA, SWA], writes=[pXU], inc=(h == 1))
                k.op('act', lambda e: e.activation(Xb[:], pX, AF.Identity), reads=[pXU], writes=[Xb])
                yield
                for h in range(2):
                    k.op('pe', lambda e: e.matmul(pXU[:, 1, h, :], TTA[:, h, lc, :], Xb[:, h, :], start=True, stop=True), reads=[TTA, Xb], writes=[pXU], inc=(h == 1))
                k.op('dve', lambda e: e.tensor_copy(WA[0:64, :, lc, :], pU), reads=[pXU], writes=[WA])
                yield
                second = (c in done)
                needy = (c >= 4)
                wfirst = [pYS]
                if needy:
                    for h in range(2):
                        k.op('pe', lambda e: e.matmul(pYS[:, h * 64:(h + 1) * 64], AR[:, h, lc, 64:128], SWA[0:64, h, lc, :], start=True, stop=False), reads=[AR, SWA], writes=wfirst, inc=False)
                        wfirst = []
                        k.op('pe', lambda e: e.matmul(pYS[:, h * 64:(h + 1) * 64], MmA[:, h, lc, 64:128], WA[:, h, lc, :], start=False, stop=True), reads=[MmA, WA], inc=False)
                fin = (needy and second)
                if fin:
                    ts = slice(lc * 64, (lc + 1) * 64)
                    for h in range(2):
                        k.op('pe', lambda e: e.matmul(pYS[:, 384 + 2 * h:386 + 2 * h], PRb[:, h, ts], onesb[:, 0:2], start=True, stop=True), reads=[PRb, onesb], inc=False)
                    k.op('pe', lambda e: e.matmul(pYS[:, 256:384], lgb[:, c * 64:(c + 1) * 64], gBb[:, hp * 128:(hp + 1) * 128], start=True, stop=True), reads=[lgb, gBb], inc=False)
                    pvt = pYS[:, 448:512].bitcast(BF16)
                    for h in range(2):
                        k.op('pe', lambda e: e.transpose(pvt[:, h * 64:(h + 1) * 64], VTb[:, h, 64 + lc * 64:128 + lc * 64], identb[0:64, 0:64]), reads=[VTb, identb], inc=False)
                for h in range(2):
                    k.op('pe', lambda e: e.matmul(pYS[:, 128 + h * 64:128 + (h + 1) * 64], ZtA[:, h, lc, :], WA[:, h, lc, :], start=True, stop=True), reads=[ZtA, WA], writes=wfirst, inc=(h == 1))
                    wfirst = []
                k.op('dve', lambda e: e.tensor_tensor(tS[:], pS_, ST[:], ALU.add), reads=[pYS, ST], writes=[tS])
                k.op('dve', lambda e: e.tensor_tensor(ST[:], tS[:], GL[:, :, lc:lc + 1].to_broadcast([64, 2, 64]), ALU.mult), reads=[tS, GL], writes=[ST])
                k.op('act', lambda e: e.activation(SWA[0:64, :, lnext, :], ST[:], AF.Identity), reads=[ST], writes=[SWA])
                if needy and not second:
                    k.op('dve', lambda e: e.tensor_copy(Yf[:, c, :, :], pY), reads=[pYS], pw_=[Yf])
                    done[c] = S.i
                elif needy:
                    k.op('dve', lambda e: e.tensor_tensor(ys[:], pY, Yf[:, c, :, :], ALU.add), reads=[pYS, Yf], writes=[ys])
                if fin:
                    for h in range(2):
                        k.op('dve', lambda e: e.bn_stats(st2[:, h, :], ys[:, h, :]), reads=[ys], writes=[st2])
                    for h in range(2):
                        k.op('dve', lambda e: e.bn_aggr(mv2[:, h, :], st2[:, h, :]), reads=[st2], writes=[mv2])
                    k.op('act', lambda e: e.activation(rs2[:], mv2[:, :, 1], AF.Sqrt, bias=GN_EPS), reads=[mv2], writes=[rs2])
                    k.op('dve', lambda e: e.reciprocal(rs2[:], rs2[:]), reads=[rs2], writes=[rs2])
                    for h in range(2):
                        k.op('dve', lambda e: e.tensor_scalar(ys[:, h, :], ys[:, h, :], mv2[:, h, 0:1], rs2[:, h:h + 1], ALU.subtract, ALU.mult), reads=[ys, mv2, rs2], writes=[ys])
                    ysf = ys[:].rearrange("p h e -> p (h e)")
                    k.op('dve', lambda e: e.tensor_tensor(om3[:], ysf, gnw[:, 0, hp * 128:(hp + 1) * 128], ALU.mult), reads=[ys, gnw], writes=[om3])
                    k.op('dve', lambda e: e.tensor_tensor(om3[:], om3[:], gnw[:, 1, hp * 128:(hp + 1) * 128], ALU.add), reads=[om3, gnw], writes=[om3])
                    k.op('dve', lambda e: e.tensor_copy(bon[:], pYS[:, 384:388].rearrange("p (h two) -> p h two", two=2)[:, :, 0]), reads=[pYS], writes=[bon])
                    for h in range(2):
                        k.op('dve', lambda e: e.scalar_tensor_tensor(om3[:, h * 64:(h + 1) * 64], pvt[:, h * 64:(h + 1) * 64], bon[:, h:h + 1], om3[:, h * 64:(h + 1) * 64], ALU.mult, ALU.add),
                             reads=[pYS, bon, om3], writes=[om3])
                    k.op('dve', lambda e: e.tensor_tensor(om3[:], om3[:], pYS[:, 256:384], ALU.mult), reads=[om3, pYS], writes=[om3])
                    k.dma('sp', MIX[b][(c - 4) * 64:(c - 3) * 64, 512 + hp * 128:512 + (hp + 1) * 128], om3[:], reads=[om3], pw=[MIX[b]])

            blocks = [(0, 256)] + [(256 + i * 512, 512) for i in range(8)]

            def stream(S, b, hp, d, done):
                k.op('pool', lambda e: e.memset(S.ST[:], 0.0), writes=[S.ST])
                border = list(range(9)) if d == 0 else [0] + list(range(8, 0, -1))
                first = True
                for bi in border:
                    tok0, ntok = blocks[bi]
                    nch = ntok // 64
                    prep(S, b, hp, tok0, ntok, d)
                    lcs = list(range(nch)) if d == 0 else list(range(nch - 1, -1, -1))
                    if first:
                        k.op('pool', lambda e: e.memset(S.SWA[0:64, :, lcs[0], :], 0.0), writes=[S.SWA])
                        first = False
                    else:
                        k.op('act', lambda e: e.activation(S.SWA[0:64, :, lcs[0], :], S.ST[:], AF.Identity), reads=[S.ST], writes=[S.SWA])
                    yield
                    for g0 in range(0, nch, 4):
                        yield from precompute(S, g0, d)
                        yield
                    for ii, lc in enumerate(lcs):
                        lnext = lcs[ii + 1] if ii + 1 < len(lcs) else NBC
                        yield from step(S, b, hp, tok0 // 64 + lc, lc, lnext, d, done)
                        yield

            for b in range(NB):
                for q4 in range(4):
                    k.dma('pool', lgb[:, q4 * 1088:(q4 + 1) * 1088], FM[b][3808:3936, q4 * 1088:(q4 + 1) * 1088], reads=[FM[b]], pw=[lgb])
                for hp in range(int(os.environ.get('P3H', 4))):
                    done = {}
                    gens = [stream(SS[0], b, hp, 0, done), stream(SS[1], b, hp, 1, done)]
                    alive = [True, True]
                    while any(alive):
                        for gi in range(2):
                            if alive[gi]:
                                try:
                                    next(gens[gi])
                                except StopIteration:
                                    alive[gi] = False
        k.barrier()

        with ExitStack() as es:
          if 4 in phases:
           try:
            P4S = int(os.environ.get('P4S', 9))
            k.es = es
            NT = NB * SEQ // 128
            NBLK = NBLK_
            SUB = BS // 128
            LG = k.sb("LG", [128, NT, 36], F32)
            OH1 = k.sb("OH1", [128, NT, 32], F32)
            OH2 = k.sb("OH2", [128, NT, 32], F32)
            W1 = k.sb("W1", [128, NT], F32)
            W2 = k.sb("W2", [128, NT], F32)
            DST = k.sb("DST", [128, NT, 2], I32)
            WIDX = k.sb("WIDX", [128, NBLK, 12], I32)
            g2b = k.sb("g2b", [128, NB, D], F32)
            lnp = k.sb("lnp", [128, 4, D], F32)
            for j, src in enumerate((ln1_g, ln1_b, ln2_g, ln2_b)):
                k.dma('sp', lnp[:, j, :], src[0:1, :].partition_broadcast(128), pw=[lnp])
            for b in range(NB):
                k.dma('sp', g2b[:, b, :], MODD[b:b + 1, 5 * D:6 * D].partition_broadcast(128), reads=[MODD], pw=[g2b])
            with ExitStack() as es4:
                k.es = es4
                wob = k.sb("wob", [128, 8, D], BF16)
                for kc in range(8):
                    k.dma('pool', wob[:, kc, :], w_out[kc * 128:(kc + 1) * 128, :], pw=[wob])
                rt = k.sb("rt", [128, 8, 36], F32)
                k.dma('sp', rt[:], rt_in[:, :].rearrange("(kc p) n -> p kc n", p=128), writes=[rt])
                rtbb = k.sb("rtbb", [128, 36], F32)
                k.dma('sp', rtbb[:], rtb_in[0:1, :].partition_broadcast(128), writes=[rtbb])
                mb4 = k.sb("mb4", [128, 3, D], F32)
                mxb = [k.sb("mxb%d" % i, [128, D], BF16) for i in range(2)]
                mT = k.sb("mT", [128, 8, 128], BF16)
                x4 = [k.sb("x4%d" % i, [128, D], F32) for i in range(2)]
                t4 = k.sb("t4", [128, D], F32)
                y4 = k.sb("y4", [128, D], F32)
                h4 = k.sb("h4", [128, D], F32)
                h4b = k.sb("h4b", [128, D], BF16)
                h4T = k.sb("h4T", [128, 8, 128], F32)
                st4 = k.sb("st4", [128, 2, 6], F32)
                mv4 = k.sb("mv4", [128, 2], F32)
                rs4 = k.sb("rs4", [128, 1], F32)
                ptm = k.ps("p4t", [128, 8, 128], BF16)
                po4 = [k.ps("p4o%d" % i, [128, 512]) for i in range(2)]
                pth = k.ps("p4th", [128, 8, 128])
                plg = k.ps("p4lg", [128, 36])

                def ln_stats(src):
                    for hf in range(2):
                        k.op('dve', lambda e: e.bn_stats(st4[:, hf, :], src[:, hf * 512:(hf + 1) * 512]), reads=[src], writes=[st4])
                    k.op('dve', lambda e: e.bn_aggr(mv4[:], st4[:].rearrange("p a b -> p (a b)")), reads=[st4], writes=[mv4])
                    k.op('act', lambda e: e.activation(rs4[:], mv4[:, 1:2], AF.Sqrt, bias=LN_EPS), reads=[mv4], writes=[rs4])
                    k.op('dve', lambda e: e.reciprocal(rs4[:], rs4[:]), reads=[rs4], writes=[rs4])

                for b in range(NB):
                    for j, c0 in enumerate((2 * D, 4 * D, 3 * D)):
                        k.dma('sp', mb4[:, j, :], MODD[b:b + 1, c0:c0 + D].partition_broadcast(128), reads=[MODD], pw=[mb4])
                    k.op('dve', lambda e: e.tensor_scalar(mb4[:, 1, :], mb4[:, 1, :], 1.0, None, ALU.add), reads=[mb4], writes=[mb4])
                    for i in range(SEQ // 128):
                        gi = b * (SEQ // 128) + i
                        xb_, mb_ = x4[i % 2], mxb[i % 2]
                        k.dma('pool', mb_[:], MIX[b][i * 128:(i + 1) * 128, :], reads=[MIX[b]], writes=[mb_])
                        k.dma('sp', xb_[:], x_in[b, i * 128:(i + 1) * 128, :], writes=[xb_])
                        for kc in range(8):
                            k.op('pe', lambda e: e.transpose(ptm[:, kc, :], mb_[:, kc * 128:(kc + 1) * 128], identb[:]), reads=[mb_, identb], writes=[ptm], inc=(kc == 7))
                        k.op('act', lambda e: e.activation(mT[:], ptm[:], AF.Identity), reads=[ptm], writes=[mT])
                        for n in range(2):
                            for kc in range(8):
                                k.op('pe', lambda e: e.matmul(po4[n][:], mT[:, kc, :], wob[:, kc, n * 512:(n + 1) * 512], start=(kc == 0), stop=(kc == 7)),
                                     reads=[mT, wob], writes=[po4[n]] if kc == 0 else [], inc=(kc == 7))
                            k.op('dve', lambda e: e.tensor_tensor(t4[:, n * 512:(n + 1) * 512], po4[n][:], mb4[:, 0, n * 512:(n + 1) * 512], ALU.mult),
                                 reads=[po4[n], mb4], writes=[t4])
                        k.op('dve', lambda e: e.scalar_tensor_tensor(y4[:], xb_[:], ALPHA, t4[:], ALU.mult, ALU.add), reads=[xb_, t4], writes=[y4])
                        ln_stats(y4)
                        k.op('dve', lambda e: e.tensor_scalar(y4[:], y4[:], mv4[:, 0:1], rs4[:, 0:1], ALU.subtract, ALU.mult), reads=[y4, mv4, rs4], writes=[y4])
                        k.op('dve', lambda e: e.tensor_tensor(y4[:], y4[:], lnp[:, 0, :], ALU.mult), reads=[y4, lnp], writes=[y4])
                        k.op('dve', lambda e: e.tensor_tensor(y4[:], y4[:], lnp[:, 1, :], ALU.add), reads=[y4, lnp], writes=[y4])
                        k.dma('sp', X1[gi * 128:(gi + 1) * 128, :], y4[:], reads=[y4], pw=[X1])
                        ln_stats(y4)
                        k.op('dve', lambda e: e.tensor_scalar(h4[:], y4[:], mv4[:, 0:1], rs4[:, 0:1], ALU.subtract, ALU.mult), reads=[y4, mv4, rs4], writes=[h4])
                        k.op('dve', lambda e: e.tensor_tensor(h4[:], h4[:], mb4[:, 1, :], ALU.mult), reads=[h4, mb4], writes=[h4])
                        k.op('dve', lambda e: e.tensor_tensor(h4[:], h4[:], mb4[:, 2, :], ALU.add), reads=[h4, mb4], writes=[h4])
                        k.op('act', lambda e: e.activation(h4b[:], h4[:], AF.Identity), reads=[h4], writes=[h4b])
                        k.dma('sp', H2[gi * 128:(gi + 1) * 128, :], h4b[:], reads=[h4b], pw=[H2])
                        for kc in range(8):
                            k.op('pe', lambda e: e.transpose(pth[:, kc, :], h4[:, kc * 128:(kc + 1) * 128], ident[:]), reads=[h4, ident], writes=[pth], inc=(kc == 7))
                        k.op('act', lambda e: e.activation(h4T[:], pth[:], AF.Identity), reads=[pth], writes=[h4T])
                        for kc in range(8):
                            k.op('pe', lambda e: e.matmul(plg[:], h4T[:, kc, :], rt[:, kc, :], start=(kc == 0), stop=(kc == 7)),
                                 reads=[h4T, rt], writes=[plg] if kc == 0 else [], inc=(kc == 7))
                        k.op('dve', lambda e: e.tensor_tensor(LG[:, gi, :], plg[:], rtbb[:], ALU.add), reads=[plg, rtbb], writes=[LG])
            k.barrier()
            with ExitStack() as es5:
                k.es = es5
                if P4S < 1:
                    raise _Stop()
                gmx = k.sb("gmx", [128, NT], F32)
                goh = k.sb("goh", [128, NT, 4], F32)
                tg = k.sb("tg", [128, NT, 4], F32)
                ptop = k.sb("ptop", [128, NT], F32)
                lem = k.sb("lem", [128, NT, 32], F32)
                v1 = k.sb("v1", [128, NT], F32)
                v2 = k.sb("v2", [128, NT], F32)
                lgv = LG[:, :, 0:4]
                lev = LG[:, :, 4:36]
                k.op('dve', lambda e: e.tensor_reduce(gmx[:], lgv, AX.X, ALU.max), reads=[LG], writes=[gmx])
                k.op('dve', lambda e: e.tensor_tensor(goh[:], lgv, gmx[:].unsqueeze(2).to_broadcast([128, NT, 4]), ALU.is_equal), reads=[LG, gmx], writes=[goh])
                k.op('dve', lambda e: e.tensor_tensor(tg[:], lgv, gmx[:].unsqueeze(2).to_broadcast([128, NT, 4]), ALU.subtract), reads=[LG, gmx], writes=[tg])
                k.op('act', lambda e: e.activation(tg[:], tg[:], AF.Exp), reads=[tg], writes=[tg])
                k.op('dve', lambda e: e.tensor_reduce(ptop[:], tg[:], AX.X, ALU.add), reads=[tg], writes=[ptop])
                k.op('dve', lambda e: e.reciprocal(ptop[:], ptop[:]), reads=[ptop], writes=[ptop])
                k.op('dve', lambda e: e.tensor_scalar(goh[:], goh[:], -1.0, 1e30, ALU.add, ALU.mult), reads=[goh], writes=[goh])
                for g in range(4):
                    k.op('dve', lambda e: e.tensor_tensor(lem[:, :, g * 8:(g + 1) * 8], LG[:, :, 4 + g * 8:12 + g * 8],
                                                          goh[:, :, g:g + 1].to_broadcast([128, NT, 8]), ALU.add), reads=[LG, goh], writes=[lem])
                k.op('dve', lambda e: e.tensor_reduce(v1[:], lem[:], AX.X, ALU.max), reads=[lem], writes=[v1])
                k.op('dve', lambda e: e.tensor_tensor(OH1[:], lem[:], v1[:].unsqueeze(2).to_broadcast([128, NT, 32]), ALU.is_equal), reads=[lem, v1], writes=[OH1])
                k.op('dve', lambda e: e.scalar_tensor_tensor(lem[:], OH1[:], -1e30, lem[:], ALU.mult, ALU.add), reads=[OH1, lem], writes=[lem])
                k.op('dve', lambda e: e.tensor_reduce(v2[:], lem[:], AX.X, ALU.max), reads=[lem], writes=[v2])
                k.op('dve', lambda e: e.tensor_tensor(OH2[:], lem[:], v2[:].unsqueeze(2).to_broadcast([128, NT, 32]), ALU.is_equal), reads=[lem, v2], writes=[OH2])
                k.op('dve', lambda e: e.tensor_tensor(v2[:], v2[:], v1[:], ALU.subtract), reads=[v1, v2], writes=[v2])
                k.op('act', lambda e: e.activation(v2[:], v2[:], AF.Exp), reads=[v2], writes=[v2])
                k.op('dve', lambda e: e.tensor_scalar(v2[:], v2[:], 1.0, None, ALU.add), reads=[v2], writes=[v2])
                k.op('dve', lambda e: e.reciprocal(v2[:], v2[:]), reads=[v2], writes=[v2])
                k.op('dve', lambda e: e.tensor_tensor(W1[:], v2[:], ptop[:], ALU.mult), reads=[v2, ptop], writes=[W1])
                k.op('dve', lambda e: e.tensor_tensor(W2[:], ptop[:], W1[:], ALU.subtract), reads=[W1, ptop], writes=[W2])
            k.barrier()
            with ExitStack() as es6:
                k.es = es6
                if P4S < 2:
                    raise _Stop()
                OHb = k.sb("OHb", [128, NT, 32], BF16)
                triS = k.sb("triS", [128, 128], BF16)
                onb = k.sb("onb", [128, 128], BF16)
                thr = k.sb("thr", [128, 128], F32)
                blki = k.sb("blki", [128, NBLK], F32)
                kcp = k.sb("kcp", [128, 12], F32)
                cnt = k.sb("cnt", [128, 32], F32)
                big = k.sb("big", [128, 32, 128], F32)
                nbk = k.sb("nbk", [128, 32], F32)
                pend = k.sb("pend", [128, 32], F32)
                pst = k.sb("pst", [128, 32], F32)
                run = k.sb("run", [128, 32], F32)
                RK = k.sb("RK", [128, NT, 32], F32)
                dsf = k.sb("dsf", [128, NT, 2], F32)
                bexp = k.sb("bexp", [128, NBLK], F32)
                bigb = k.sb("bigb", [128, NBLK, 32], F32)
                widxf = k.sb("widxf", [128, NBLK, 12], F32)
                tokid = k.sb("tokid", [128, NT, 16], I32)
                zt = k.sb("zt", [128, 16], I32)
                pcn = k.ps("p5c", [128, 32])
                prk = k.ps("p5r", [128, 32])
                ptt = k.ps("p5t", [128, 32])
                stg = k.sb("stg", [128, 128], F32)
                k.dma('sp', stg[:], tris_in[:, :], writes=[stg])
                k.op('dve', lambda e: e.tensor_copy(triS[:], stg[:]), reads=[stg], writes=[triS])
                k.op('pool', lambda e: e.memset(onb[:], 1.0), writes=[onb])
                k.dma('sp', thr[:], thr_in[:, :], writes=[thr])
                k.dma('sp', blki[:], blki_in[:, 0:NBLK], writes=[blki])
                k.dma('sp', kcp[:], kcp_in[:, :], writes=[kcp])
                k.dma('sp', tokid[:], tokid_in[:, 0:NT, :], writes=[tokid])
                k.op('pool', lambda e: e.memset(zt[:], 0), writes=[zt])
                k.dma('sp', TOKB[:, :].rearrange("(b p) c -> p b c", p=128), zt[:].unsqueeze(1).to_broadcast([128, NBLK * SUB, 16]), reads=[zt], writes=[TOKB])
                k.op('dve', lambda e: e.tensor_tensor(OHb[:], OH1[:], OH2[:], ALU.add), reads=[OH1, OH2], writes=[OHb])
                for i in range(NT):
                    k.op('pe', lambda e: e.matmul(pcn[:], onb[:], OHb[:, i, :], start=(i == 0), stop=(i == NT - 1)), reads=[onb, OHb], writes=[pcn] if i == 0 else [], inc=(i == NT - 1))
                k.op('dve', lambda e: e.tensor_copy(cnt[:], pcn[:]), reads=[pcn], writes=[cnt])
                k.op('dve', lambda e: e.tensor_tensor(big[:], cnt[:].unsqueeze(2).to_broadcast([128, 32, 128]), thr[:].unsqueeze(1).to_broadcast([128, 32, 128]), ALU.is_gt),
                     reads=[cnt, thr], writes=[big])
                k.op('dve', lambda e: e.tensor_reduce(nbk[:], big[:], AX.X, ALU.add), reads=[big], writes=[nbk])
                k.op('pool', lambda e: e.memset(run[:], 1.0), writes=[run])
                k.op('dve', lambda e: e.tensor_tensor_scan(pend[:], run[:], nbk[:], 0.0, ALU.mult, ALU.add), reads=[run, nbk], writes=[pend])
                k.op('dve', lambda e: e.tensor_tensor(pst[:], pend[:], nbk[:], ALU.subtract), reads=[pend, nbk], writes=[pst])
                k.op('dve', lambda e: e.tensor_scalar(pst[:], pst[:], float(BS), None, ALU.mult), reads=[pst], writes=[pst])
                k.op('pool', lambda e: e.memset(run[:], 0.0), reads=[run], writes=[run])
                for i in range(NT):
                    k.op('pe', lambda e: e.matmul(prk[:], triS[:], OHb[:, i, :], start=True, stop=True), reads=[triS, OHb], writes=[prk])
                    k.op('pe', lambda e: e.matmul(ptt[:], onb[:], OHb[:, i, :], start=True, stop=True), reads=[onb, OHb], writes=[ptt])
                    k.op('dve', lambda e: e.tensor_tensor(RK[:, i, :], prk[:], run[:], ALU.add), reads=[prk, run], writes=[RK])
                    k.op('dve', lambda e: e.tensor_tensor(run[:], run[:], ptt[:], ALU.add), reads=[run, ptt], writes=[run])
                k.op('dve', lambda e: e.tensor_tensor(RK[:], RK[:], pst[:].unsqueeze(1).to_broadcast([128, NT, 32]), ALU.add), reads=[RK, pst], writes=[RK])
                for j, OH in enumerate((OH1, OH2)):
                    k.op('dve', lambda e: e.tensor_tensor(OH[:], OH[:], RK[:], ALU.mult), reads=[OH, RK], writes=[OH])
                    k.op('dve', lambda e: e.tensor_reduce(dsf[:, :, j], OH[:], AX.X, ALU.add), reads=[OH], writes=[dsf])
                k.op('dve', lambda e: e.tensor_copy(DST[:], dsf[:]), reads=[dsf], writes=[DST])
                k.op('dve', lambda e: e.tensor_tensor(bigb[:], pend[:].unsqueeze(1).to_broadcast([128, NBLK, 32]), blki[:].unsqueeze(2).to_broadcast([128, NBLK, 32]), ALU.is_le),
                     reads=[pend, blki], writes=[bigb])
                k.op('dve', lambda e: e.tensor_reduce(bexp[:], bigb[:], AX.X, ALU.add), reads=[bigb], writes=[bexp])
                k.op('dve', lambda e: e.tensor_scalar(bexp[:], bexp[:], 31.0, None, ALU.min), reads=[bexp], writes=[bexp])
                k.op('dve', lambda e: e.tensor_scalar(widxf[:, :, 0:8], bexp[:].unsqueeze(2).to_broadcast([128, NBLK, 8]), 1024.0, None, ALU.mult), reads=[bexp], writes=[widxf])
                k.op('dve', lambda e: e.tensor_scalar(widxf[:, :, 8:12], bexp[:].unsqueeze(2).to_broadcast([128, NBLK, 4]), 512.0, None, ALU.mult), reads=[bexp], writes=[widxf])
                k.op('dve', lambda e: e.tensor_tensor(widxf[:], widxf[:], kcp[:].unsqueeze(1).to_broadcast([128, NBLK, 12]), ALU.add), reads=[widxf, kcp], writes=[widxf])
                k.op('dve', lambda e: e.tensor_copy(WIDX[:], widxf[:]), reads=[widxf], writes=[WIDX])
                for i in range(NT):
                    for j in range(2):
                        k.dma('pool', TOKB[:, :], tokid[:, i, :], reads=[tokid, DST], pw=[TOKB],
                              indirect=(bass.IndirectOffsetOnAxis(ap=DST[:, i, j:j + 1], axis=0), None))
            k.barrier()
            with ExitStack() as es7:
                k.es = es7
                if P4S < 3:
                    raise _Stop()
                tki = [k.sb("tki%d" % i, [128, 16], I32) for i in range(2)]
                xg = [k.sb("xg%d" % i, [128, D], BF16) for i in range(2)]
                xgT = k.sb("xgT", [128, 8, 128], BF16)
                wg = [k.sb("wg%d" % i, [128, 8, 512], BF16) for i in range(2)]
                wu = [k.sb("wu%d" % i, [128, 8, 512], BF16) for i in range(2)]
                wd = [k.sb("wd%d" % i, [128, 4, D], BF16) for i in range(2)]
                gs = k.sb("gs", [128, 512], F32)
                hb = k.sb("hb", [128, 512], BF16)
                hbT = k.sb("hbT", [128, 4, 128], BF16)
                yb = [k.sb("yb%d" % i, [128, D], F32) for i in range(2)]
                pxt = k.ps("p6x", [128, 8, 128], BF16)
                pgt = k.ps("p6g", [128, 512])
                put = k.ps("p6u", [128, 512])
                pht = k.ps("p6h", [128, 4, 128], BF16)
                pyt = [k.ps("p6y%d" % i, [128, 512]) for i in range(2)]
                for bk in range(NBLK):
                    q = bk % 2
                    for kc in range(8):
                        k.dma('pool', wg[q][:, kc, :], ex_gate[:, :], reads=[WIDX], pw=[wg[q]],
                              indirect=(None, bass.IndirectOffsetOnAxis(ap=WIDX[:, bk, kc:kc + 1], axis=0)))
                        k.dma('pool', wu[q][:, kc, :], ex_up[:, :], reads=[WIDX], pw=[wu[q]],
                              indirect=(None, bass.IndirectOffsetOnAxis(ap=WIDX[:, bk, kc:kc + 1], axis=0)))
                    for fc in range(4):
                        k.dma('pool', wd[q][:, fc, :], ex_down[:, :], reads=[WIDX], pw=[wd[q]],
                              indirect=(None, bass.IndirectOffsetOnAxis(ap=WIDX[:, bk, 8 + fc:9 + fc], axis=0)))
                    for sub in range(SUB):
                        sq = (bk * SUB + sub) % 2
                        r0 = (bk * SUB + sub) * 128
                        k.dma('sp', tki[sq][:], TOKB[r0:r0 + 128, :], reads=[TOKB], writes=[tki[sq]])
                        k.dma('pool', xg[sq][:], H2[:, :], reads=[H2, tki[sq]], writes=[xg[sq]],
                              indirect=(None, bass.IndirectOffsetOnAxis(ap=tki[sq][:, 0:1], axis=0)))
                        for kc in range(8):
                            k.op('pe', lambda e: e.transpose(pxt[:, kc, :], xg[sq][:, kc * 128:(kc + 1) * 128], identb[:]), reads=[xg[sq], identb], writes=[pxt], inc=(kc == 7))
                        k.op('act', lambda e: e.activation(xgT[:], pxt[:], AF.Identity), reads=[pxt], writes=[xgT])
                        for kc in range(8):
                            k.op('pe', lambda e: e.matmul(pgt[:], xgT[:, kc, :], wg[q][:, kc, :], start=(kc == 0), stop=(kc == 7)), reads=[xgT, wg[q]], writes=[pgt] if kc == 0 else [], inc=(kc == 7))
                        for kc in range(8):
                            k.op('pe', lambda e: e.matmul(put[:], xgT[:, kc, :], wu[q][:, kc, :], start=(kc == 0), stop=(kc == 7)), reads=[xgT, wu[q]], writes=[put] if kc == 0 else [], inc=(kc == 7))
                        k.op('act', lambda e: e.activation(gs[:], pgt[:], AF.Silu), reads=[pgt], writes=[gs])
                        k.op('dve', lambda e: e.tensor_tensor(hb[:], gs[:], put[:], ALU.mult), reads=[gs, put], writes=[hb])
                        for fc in range(4):
                            k.op('pe', lambda e: e.transpose(pht[:, fc, :], hb[:, fc * 128:(fc + 1) * 128], identb[:]), reads=[hb, identb], writes=[pht], inc=(fc == 3))
                        k.op('dve', lambda e: e.tensor_copy(hbT[:], pht[:]), reads=[pht], writes=[hbT])
                        ybq = yb[sq]
                        for n in range(2):
                            for fc in range(4):
                                k.op('pe', lambda e: e.matmul(pyt[n][:], hbT[:, fc, :], wd[q][:, fc, n * 512:(n + 1) * 512], start=(fc == 0), stop=(fc == 3)),
                                     reads=[hbT, wd[q]], writes=[pyt[n]] if fc == 0 else [], inc=(fc == 3))
                            k.op('act', lambda e: e.activation(ybq[:, n * 512:(n + 1) * 512], pyt[n][:], AF.Identity), reads=[pyt[n]], writes=[ybq])
                        k.dma('sp', YB[r0:r0 + 128, :], ybq[:], reads=[ybq], pw=[YB])
            k.barrier()
            with ExitStack() as es8:
                k.es = es8
                if P4S < 4:
                    raise _Stop()
                x6 = [k.sb("x6%d" % i, [128, D], F32) for i in range(2)]
                y0 = [k.sb("y0%d" % i, [128, D], F32) for i in range(2)]
                y1 = [k.sb("y1%d" % i, [128, D], F32) for i in range(2)]
                o6 = [k.sb("o6%d" % i, [128, D], F32) for i in range(2)]
                st6 = k.sb("st6", [128, 2, 6], F32)
                mv6 = k.sb("mv6", [128, 2], F32)
                rs6 = k.sb("rs6", [128, 1], F32)
                for gi in range(NT):
                    b, i = gi // (SEQ // 128), gi % (SEQ // 128)
                    q = gi % 2
                    k.dma('sp', x6[q][:], X1[gi * 128:(gi + 1) * 128, :], reads=[X1], writes=[x6[q]])
                    k.dma('pool', y0[q][:], YB[:, :], reads=[YB, DST], writes=[y0[q]],
                          indirect=(None, bass.IndirectOffsetOnAxis(ap=DST[:, gi, 0:1], axis=0)))
                    k.dma('pool', y1[q][:], YB[:, :], reads=[YB, DST], writes=[y1[q]],
                          indirect=(None, bass.IndirectOffsetOnAxis(ap=DST[:, gi, 1:2], axis=0)))
                    o = o6[q]
                    k.op('dve', lambda e: e.tensor_scalar(y0[q][:], y0[q][:], W1[:, gi:gi + 1], None, ALU.mult), reads=[y0[q], W1], writes=[y0[q]])
                    k.op('dve', lambda e: e.scalar_tensor_tensor(y0[q][:], y1[q][:], W2[:, gi:gi + 1], y0[q][:], ALU.mult, ALU.add), reads=[y1[q], W2, y0[q]], writes=[y0[q]])
                    k.op('dve', lambda e: e.tensor_tensor(y0[q][:], y0[q][:], g2b[:, b, :], ALU.mult), reads=[y0[q], g2b], writes=[y0[q]])
                    k.op('dve', lambda e: e.scalar_tensor_tensor(o[:], x6[q][:], ALPHA, y0[q][:], ALU.mult, ALU.add), reads=[x6[q], y0[q]], writes=[o])
                    for hf in range(2):
                        k.op('dve', lambda e: e.bn_stats(st6[:, hf, :], o[:, hf * 512:(hf + 1) * 512]), reads=[o], writes=[st6])
                    k.op('dve', lambda e: e.bn_aggr(mv6[:], st6[:].rearrange("p a b -> p (a b)")), reads=[st6], writes=[mv6])
                    k.op('act', lambda e: e.activation(rs6[:], mv6[:, 1:2], AF.Sqrt, bias=LN_EPS), reads=[mv6], writes=[rs6])
                    k.op('dve', lambda e: e.reciprocal(rs6[:], rs6[:]), reads=[rs6], writes=[rs6])
                    k.op('dve', lambda e: e.tensor_scalar(o[:], o[:], mv6[:, 0:1], rs6[:, 0:1], ALU.subtract, ALU.mult), reads=[o, mv6, rs6], writes=[o])
                    k.op('dve', lambda e: e.tensor_tensor(o[:], o[:], lnp[:, 2, :], ALU.mult), reads=[o, lnp], writes=[o])
                    k.op('dve', lambda e: e.tensor_tensor(o[:], o[:], lnp[:, 3, :], ALU.add), reads=[o, lnp], writes=[o])
                    k.dma('sp', out_d[b, i * 128:(i + 1) * 128, :], o[:], reads=[o])
           except _Stop:
            pass
        k.barrier()

        k.barrier()
    return nc


def host_inputs(inputs, batches, NB):
    f = lambda a: np.ascontiguousarray(a, dtype=np.float32)
    bs = list(batches)
    m = {}
    m["x"] = f(inputs["x"][bs])
    m["ctx"] = f(inputs["ctx"][bs])
    cc = np.zeros((3, D), np.float32)
    for i, b in enumerate(bs):
        cc[i] = inputs["c"][b]
    cc[2] = inputs["c_ctx"]
    m["cc"] = cc
    m["w_ada"] = f(inputs["w_ada"][0])
    m["b_ada"] = f(inputs["b_ada"][0][None, :])
    m["w_in"] = f(inputs["w_in"][0])
    m["conv_w"] = f(inputs["conv_w"][0].reshape(9, 2560))
    bi, bf = inputs["m_bias_i"][0], inputs["m_bias_f"][0]
    m["m_bias"] = f(np.concatenate([bi[0], bf[0], bi[1], bf[1]])[:, None])
    m["ident"] = np.eye(128, dtype=np.float32)
    gm = np.zeros((32, 2), np.float32)
    gm[0:8, 0] = 1; gm[16:24, 0] = 1; gm[8:16, 1] = -1; gm[24:32, 1] = -1
    m["gmask"] = gm
    ii = np.arange(64)
    m["cmask"] = np.stack([(ii[:, None] <= ii[None, :]), (ii[:, None] >= ii[None, :])], axis=1).astype(np.float32)
    m["m_norm_w"] = f(inputs["m_norm_w"][0][None, :])
    hk = lambda v: np.asarray(v, np.float32).reshape(8, 64).T
    m["rp"] = f(np.stack([hk(inputs["r_w0"][0][0]), hk(inputs["r_w0"][0][1]), hk(inputs["r_a0"][0]), hk(inputs["r_kk"][0]),
                          hk(inputs["r_ka"][0]), hk(inputs["r_ka"][0]), hk(inputs["r_bonus"][0].reshape(-1))], axis=2))
    sm = np.ones((64, 1088), np.float32); sm[:, ::64] = 0
    m["smask"] = sm
    jj = np.arange(128) % 64
    tt = np.arange(128)
    rm = np.zeros((128, 2, 128), np.float32)
    for dd in range(2):
        for col in range(128):
            tq = col % 64
            if col < 64:
                rm[:, dd, col] = (jj < tq) if dd == 0 else (jj > tq)
            else:
                rm[:, dd, col] = (jj <= tq) if dd == 0 else (jj >= tq)
    m["rmask"] = rm
    m["nmask"] = np.stack([(ii[None, :] < ii[:, None]), (ii[None, :] > ii[:, None])], axis=1).astype(np.float32)
    m["r_norm_w"] = f(inputs["r_norm_w"][0][None, :])
    m["r_norm_b"] = f(inputs["r_norm_b"][0][None, :])
    m["r_wB"] = f(inputs["r_wB"][0])
    m["r_aB"] = f(inputs["r_aB"][0])
    m["r_gB"] = f(inputs["r_gB"][0])
    m["w_out"] = f(inputs["w_out"][0])
    for nm in ("ln1_g", "ln1_b", "ln2_g", "ln2_b"):
        m[nm] = f(inputs[nm][0][None, :])
    m["rt"] = f(np.concatenate([inputs["rt_g"][0], inputs["rt_e"][0]], axis=1))
    m["rtb"] = f(np.concatenate([inputs["rt_g_b"][0], inputs["rt_e_b"][0]])[None, :])
    m["ex_gate"] = f(inputs["ex_gate"][0].reshape(32 * D, 512))
    m["ex_up"] = f(inputs["ex_up"][0].reshape(32 * D, 512))
    m["ex_down"] = f(inputs["ex_down"][0].reshape(32 * 512, D))
    pp = np.arange(128)
    m["tris"] = (pp[:, None] < pp[None, :]).astype(np.float32)
    m["thr"] = np.broadcast_to((512.0 * pp)[None, :], (128, 128)).astype(np.float32).copy()
    m["blki"] = np.broadcast_to(np.arange(160, dtype=np.float32)[None, :], (128, 160)).copy()
    m["kcp"] = (np.concatenate([np.arange(8), np.arange(4)])[None, :] * 128 + pp[:, None]).astype(np.float32)
    m["tokid"] = np.broadcast_to((np.arange(64)[None, :] * 128 + pp[:, None])[:, :, None], (128, 64, 16)).astype(np.int32).copy()
    return m


_NC_CACHE = {}


def kernel(**inputs):
    inputs = {k_: np.asarray(v) for k_, v in inputs.items()}
    NB = 2
    n_cores = 8
    if NB not in _NC_CACHE:
        _NC_CACHE[NB] = build(NB=NB)
    nc = _NC_CACHE[NB]
    in_maps = [host_inputs(inputs, [NB * c + j for j in range(NB)], NB) for c in range(n_cores)]
    res = run_bass_kernel_spmd(nc, in_maps, core_ids=list(range(n_cores)))
    out = np.concatenate([np.asarray(r["out"]) for r in res.results], axis=0)
    return np.ascontiguousarray(out, dtype=np.float32)
```

```python
import math, os
from contextlib import ExitStack
import numpy as np
import concourse.bass as bass
import concourse.mybir as mybir
from concourse.bass_utils import run_bass_kernel_spmd

F32 = mybir.dt.float32
BF16 = mybir.dt.bfloat16
I32 = mybir.dt.int32
AF = mybir.ActivationFunctionType
ALU = mybir.AluOpType
AX = mybir.AxisListType

D = 1024
SEQ = 4096
CTX = 256
T = SEQ + CTX
NCH = T // 64
INC = 3936
DS = math.exp(-0.5)
ALPHA = 2.0 ** 0.25
LN_EPS = 1e-6
GN_EPS = 64e-5
SEC = dict(mq=0, mk=512, rr=1024, rk=1536, rv=2048, mv=2560, mo=3072, gates=3584,
           lwf=3616, lwb=3680, la=3744, lg=3808)
NDS = 40


class _Stop(Exception):
    pass


class Buf:
    def __init__(self, t):
        self.t = t
        self.w = {}
        self.r = {}

    def __getitem__(self, k):
        return self.t[k]


def _merge(d, s):
    for k, v in s.items():
        if d.get(k, 0) < v:
            d[k] = v


class KB:
    def __init__(self, nc):
        self.nc = nc
        self.engs = {'pe': nc.tensor, 'dve': nc.vector, 'act': nc.scalar, 'pool': nc.gpsimd, 'sp': nc.sync}
        self.esem = {e: nc.alloc_semaphore('es_' + e) for e in self.engs}
        self.ecnt = {e: 0 for e in self.engs}
        self.pending = {e: False for e in self.engs}
        self.seen = {e: {} for e in self.engs}
        self.dsem = [nc.alloc_semaphore('ds%d' % i) for i in range(NDS)]
        self.dcnt = [0] * NDS
        self.dnext = 0
        self.es = None
        self.uid = 0

    def semh(self, key):
        return self.esem[key] if isinstance(key, str) else self.dsem[key[1]]

    def _wait(self, eng, need):
        for key, cnt in need.items():
            if self.seen[eng].get(key, 0) >= cnt:
                continue
            if key == eng and eng in ('pe',):
                continue
            self.engs[eng].wait_ge(self.semh(key), cnt)
            self.seen[eng][key] = cnt

    def op(self, eng, fn, reads=(), writes=(), inc=True, pw_=()):
        need = {}
        for b in pw_:
            _merge(need, b.r)
        for b in reads:
            _merge(need, b.w)
            if getattr(b, 'excl', False):
                _merge(need, {kk: vv for kk, vv in b.r.items() if kk != eng})
        for b in writes:
            _merge(need, b.w)
            _merge(need, b.r)
        self._wait(eng, need)
        ins = fn(self.engs[eng])
        cnt = self.ecnt[eng] + 1
        if inc:
            ins.then_inc(self.esem[eng], 1)
            self.ecnt[eng] = cnt
        for b in reads:
            b.r[eng] = cnt
        for b in writes:
            b.w = {eng: cnt}
            b.r = {}
        for b in pw_:
            b.w[eng] = cnt
        return ins

    def dma(self, q, out, in_, reads=(), writes=(), pw=(), indirect=None, **kw):
        i = self.dnext
        self.dnext = (i + 1) % NDS
        need = {}
        if self.dcnt[i]:
            need[('d', i)] = self.dcnt[i]
        for b in reads:
            _merge(need, b.w)
        for b in writes:
            _merge(need, b.w)
            _merge(need, b.r)
        for b in pw:
            _merge(need, b.r)
        self._wait(q, need)
        if indirect is None:
            ins = self.engs[q].dma_start(out=out, in_=in_, **kw)
        else:
            ins = self.engs[q].indirect_dma_start(out, indirect[0], in_, indirect[1], **kw)
        self.dcnt[i] += 16
        ins.then_inc(self.dsem[i], 16)
        key = ('d', i)
        cnt = self.dcnt[i]
        for b in reads:
            b.r[key] = cnt
        for b in writes:
            b.w = {key: cnt}
            b.r = {}
        for b in pw:
            b.w[key] = cnt
        return ins

    def barrier(self):
        need = {e: c for e, c in self.ecnt.items() if c}
        for i in range(NDS):
            if self.dcnt[i]:
                need[('d', i)] = self.dcnt[i]
        for e in self.engs:
            self._wait(e, need)

    def sb(self, name, shape, dt):
        self.uid += 1
        return Buf(self.es.enter_context(self.nc.sbuf_tensor("s%d_%s" % (self.uid, name), list(shape), dt)))

    def ps(self, name, shape, dt=F32):
        self.uid += 1
        return Buf(self.es.enter_context(self.nc.psum_tensor("p%d_%s" % (self.uid, name), list(shape), dt)))


def build(NB=2, debug=None, phases=(0, 1, 2, 3, 4, 5, 6)):
    nc = bass.Bass("TRN2", target_bir_lowering=False)
    k = KB(nc)

    def din(name, shape):
        return nc.dram_tensor(name, list(shape), F32, kind="ExternalInput").ap()

    x_in = din("x", [NB, SEQ, D])
    ctx_in = din("ctx", [NB, CTX, D])
    cc_in = din("cc", [3, D])
    w_ada = din("w_ada", [D, 6 * D])
    b_ada = din("b_ada", [1, 6 * D])
    w_in = din("w_in", [D, INC])
    conv_w = din("conv_w", [9, 2560])
    m_bias = din("m_bias", [32, 1])
    ident_in = din("ident", [128, 128])
    gmask_in = din("gmask", [32, 2])
    cmask_in = din("cmask", [64, 2, 64])
    m_norm_w = din("m_norm_w", [1, 512])
    rp_in = din("rp", [128, 4, 7])
    smask_in = din("smask", [128, 1088])
    onesbd_in = din("onesbd", [128, 128])
    rmask_in = din("rmask", [128, 2, 128])
    nmask_in = din("nmask", [64, 2, 64])
    r_norm_w = din("r_norm_w", [1, 512])
    r_norm_b = din("r_norm_b", [1, 512])
    r_wB = din("r_wB", [2, 64, 512])
    r_aB = din("r_aB", [64, 512])
    r_gB = din("r_gB", [128, 512])
    w_out = din("w_out", [D, D])
    ln1_g = din("ln1_g", [1, D]); ln1_b = din("ln1_b", [1, D]); ln2_g = din("ln2_g", [1, D]); ln2_b = din("ln2_b", [1, D])
    rt_in = din("rt", [D, 36]); rtb_in = din("rtb", [1, 36])
    ex_gate = din("ex_gate", [8192, 2048]); ex_up = din("ex_up", [8192, 2048]); ex_down = din("ex_down", [8192, 2048])
    tris_in = din("tris", [128, 128]); thr_in = din("thr", [128, 128]); blki_in = din("blki", [128, 160]); kcp_in = din("kcp", [128, 12])
    tokid_in = nc.dram_tensor("tokid", [128, 64, 16], I32, kind="ExternalInput").ap()
    out_d = nc.dram_tensor("out", [NB, SEQ, D], F32, kind="ExternalOutput").ap()

    def dscr(name, shape, dt=F32):
        kind = "ExternalOutput" if (debug and name in debug) else "Internal"
        return Buf(nc.dram_tensor(name, list(shape), dt, kind=kind).ap())

    MODD = dscr("MODD", [3, 6 * D])
    FM = [dscr("FM%d" % b, [INC, T]) for b in range(NB)]
    MIX = [dscr("MIX%d" % b, [SEQ, D]) for b in range(NB)]
    BS = 512
    NBLK_ = NB * SEQ * 2 // BS + 32
    X1 = dscr("X1", [NB * SEQ, D])
    H2 = dscr("H2", [NB * SEQ, D], BF16)
    TOKB = dscr("TOKB", [NBLK_ * BS, 16], I32)
    YB = dscr("YB", [NBLK_ * BS, D])

    with ExitStack() as es0:
        k.es = es0
        ident = k.sb("ident", [128, 128], F32)
        identb = k.sb("identb", [128, 128], BF16)
        ones_f = k.sb("ones_f", [128, 128], F32)
        modT = k.sb("modT", [128, 48, 3], F32)
        k.dma('sp', ident[:], ident_in[:, :], writes=[ident])
        k.op('dve', lambda e: e.tensor_copy(identb[:], ident[:]), reads=[ident], writes=[identb])

        with ExitStack() as es:
            k.es = es
            cc = k.sb("cc", [3, D], F32)
            scT = k.sb("scT", [128, 8, 3], F32)
            bada = k.sb("bada", [3, 6 * D], F32)
            mods = k.sb("mods", [3, 6 * D], F32)
            wa = [k.sb("wa%d" % i, [128, 8, 512], F32) for i in range(2)]
            pst = k.ps("p0t", [128, 8, 3])
            psm = [k.ps("p0m%d" % i, [3, 512]) for i in range(2)]
            pmt = k.ps("p0mt", [128, 48, 3])
            k.dma('sp', cc[:], cc_in[:, :], writes=[cc])
            k.dma('sp', bada[:], b_ada[0:1, :].partition_broadcast(3), writes=[bada])
            k.op('act', lambda e: e.activation(cc[:], cc[:], AF.Silu), reads=[cc], writes=[cc])
            for kc in range(8):
                k.op('pe', lambda e: e.transpose(pst[:, kc, :], cc[:, kc * 128:(kc + 1) * 128], ident[0:3, 0:3]),
                     reads=[cc, ident], writes=[pst], inc=(kc == 7))
            k.op('dve', lambda e: e.tensor_copy(scT[:], pst[:]), reads=[pst], writes=[scT])
            for n in range(12):
                wb = wa[n % 2]
                k.dma('sp', wb[:], w_ada[:, n * 512:(n + 1) * 512].rearrange("(kc p) n -> p kc n", p=128), writes=[wb])
                pm = psm[n % 2]
                for kc in range(8):
                    k.op('pe', lambda e: e.matmul(pm[:], scT[:, kc, :], wb[:, kc, :], start=(kc == 0), stop=(kc == 7)),
                         reads=[scT, wb], writes=[pm] if kc == 0 else [], inc=(kc == 7))
                k.op('dve', lambda e: e.tensor_tensor(mods[:, n * 512:(n + 1) * 512], pm[:], bada[:, n * 512:(n + 1) * 512], ALU.add),
                     reads=[pm, bada], writes=[mods])
            k.dma('sp', MODD[:, :], mods[:], reads=[mods], writes=[MODD])
            for j in range(48):
                k.op('pe', lambda e: e.transpose(pmt[:, j, :], mods[:, j * 128:(j + 1) * 128], ident[0:3, 0:3]),
                     reads=[mods, ident], writes=[pmt], inc=(j == 47))
            k.op('dve', lambda e: e.tensor_copy(modT[:], pmt[:]), reads=[pmt], writes=[modT])
            for j0 in (8, 32):
                k.op('dve', lambda e: e.tensor_scalar(modT[:, j0:j0 + 8, :], modT[:, j0:j0 + 8, :], 1.0, None, ALU.add),
                     reads=[modT], writes=[modT])
        k.barrier()

        with ExitStack() as es:
          if 1 in phases:
              k.es = es
              wbf = k.sb("wbf", [128, 8, INC], BF16)
              hT = k.sb("hT", [128, 8, T], BF16)
              xt = [k.sb("xt%d" % i, [128, D], F32) for i in range(2)]
              xn = [k.sb("xn%d" % i, [128, D], BF16) for i in range(2)]
              st = k.sb("st", [128, 2, 6], F32)
              mv = k.sb("mv", [128, 2], F32)
              rstd = k.sb("rstd", [128, 1], F32)
              pT = [k.sb("pT%d" % i, [128, T], F32) for i in range(2)]
              acc = k.sb("acc", [128, T], F32)
              cw = k.sb("cw", [128, 20, 9], F32)
              mb = k.sb("mb", [32, 4], F32)
              ptr = [k.ps("p1t%d" % i, [128, 8, 128], BF16) for i in range(2)]
              pmm = [k.ps("p1m%d" % i, [128, 512]) for i in range(3)]
              pcw = k.ps("p1cw", [128, 20, 9])
              for kc in range(8):
                  for hf in range(2):
                      k.dma('pool', wbf[:, kc, hf * 1968:(hf + 1) * 1968],
                            w_in[kc * 128:(kc + 1) * 128, hf * 1968:(hf + 1) * 1968], pw=[wbf])
              crow = k.sb("crow", [9, 2560], F32)
              k.dma('sp', crow[:], conv_w[:, :], writes=[crow])
              for c in range(20):
                  k.op('pe', lambda e: e.transpose(pcw[:, c, :], crow[:, c * 128:(c + 1) * 128], ident[0:9, 0:9]),
                       reads=[crow, ident], writes=[pcw], inc=(c == 19))
              k.op('dve', lambda e: e.tensor_copy(cw[:], pcw[:]), reads=[pcw], writes=[cw])
              k.dma('sp', mb[:, 0:1], m_bias[:, :], writes=[mb])
              k.op('dve', lambda e: e.tensor_scalar(mb[:, 1:2], mb[:, 0:1], -1.0, None, ALU.mult), reads=[mb], writes=[mb])
              k.dma('sp', mb[:, 2:4], gmask_in[:, :], pw=[mb])
              for b in range(NB):
                  for i in range(int(os.environ.get('P1A', T // 128))):
                      xb, xnb, pt = xt[i % 2], xn[i % 2], ptr[i % 2]
                      src = ctx_in[b, i * 128:(i + 1) * 128, :] if i < 2 else x_in[b, (i - 2) * 128:(i - 1) * 128, :]
                      r = 2 if i < 2 else b
                      k.dma('sp', xb[:], src, writes=[xb])
                      S1 = int(os.environ.get('P1S', 9))
                      for hf in range(2):
                          k.op('dve', lambda e: e.bn_stats(st[:, hf, :], xb[:, hf * 512:(hf + 1) * 512]), reads=[xb], writes=[st])
                      if S1 >= 2: k.op('dve', lambda e: e.bn_aggr(mv[:], st[:].rearrange("p a b -> p (a b)")), reads=[st], writes=[mv])
                      if S1 >= 3: k.op('act', lambda e: e.activation(rstd[:], mv[:, 1:2], AF.Sqrt, bias=LN_EPS), reads=[mv], writes=[rstd])
                      if S1 >= 4: k.op('dve', lambda e: e.reciprocal(rstd[:], rstd[:]), reads=[rstd], writes=[rstd])
                      if S1 >= 5: k.op('dve', lambda e: e.tensor_scalar(xnb[:], xb[:], mv[:, 0:1], rstd[:, 0:1], ALU.subtract, ALU.mult),
                           reads=[xb, mv, rstd], writes=[xnb])
                      for kc in range(8 if S1 >= 6 else 0):
                          k.op('pe', lambda e: e.transpose(pt[:, kc, :], xnb[:, kc * 128:(kc + 1) * 128], identb[:]),
                               reads=[xnb, identb], writes=[pt] if kc == 0 else [], inc=(kc == 7))
                      for kc in range(8 if S1 >= 7 else 0):
                          if i % 2 == 0:
                              k.op('act', lambda e: e.activation(hT[:, kc, i * 128:(i + 1) * 128], pt[:, kc, :], AF.Identity,
                                                                 bias=modT[:, kc, r:r + 1], scale=modT[:, 8 + kc, r:r + 1]),
                                   reads=[pt, modT], writes=[] if (i or kc) else [hT])
                          else:
                              k.op('dve', lambda e: e.tensor_scalar(hT[:, kc, i * 128:(i + 1) * 128], pt[:, kc, :],
                                                                    modT[:, 8 + kc, r:r + 1], modT[:, kc, r:r + 1], ALU.mult, ALU.add),
                                   reads=[pt, modT], writes=[])
                      hT.w['act'] = k.ecnt['act']
                      hT.w['dve'] = k.ecnt['dve']
                  chunks = [(c * 128, 128) for c in range(28)] + [(3584, 32), (3616, 64), (3680, 64), (3744, 64), (3808, 128)]
                  for ci, (c0, M) in enumerate(chunks[:int(os.environ.get('P1C', 99))]):
                      pb = pT[ci % 2]
                      for g in range(9):
                          t0 = g * 512
                          n = min(512, T - t0)
                          pm = pmm[(ci * 9 + g) % 3]
                          for kc in range(8):
                              k.op('pe', lambda e: e.matmul(pm[0:M, 0:n], wbf[:, kc, c0:c0 + M], hT[:, kc, t0:t0 + n],
                                                            start=(kc == 0), stop=(kc == 7)),
                                   reads=[wbf, hT], writes=[pm] if kc == 0 else [], inc=(kc == 7))
                          k.op('act', lambda e: e.activation(pb[0:M, t0:t0 + n], pm[0:M, 0:n], AF.Identity),
                               reads=[pm], writes=[pb] if g == 0 else [])
                          pb.w['act'] = k.ecnt['act']
                      src = pb
                      if c0 < 2560:
                          c = c0 // 128
                          k.op('act', lambda e: e.activation(acc[:, :], pb[:, :], AF.Identity, scale=cw[:, c, 4:5]),
                               reads=[pb, cw], writes=[acc])
                          k.op('dve', lambda e: e.scalar_tensor_tensor(acc[:, 1:CTX], pb[:, 0:CTX - 1], cw[:, c, 3:4], acc[:, 1:CTX], ALU.mult, ALU.add),
                               reads=[pb, cw, acc], writes=[acc])
                          k.op('dve', lambda e: e.scalar_tensor_tensor(acc[:, 0:CTX - 1], pb[:, 1:CTX], cw[:, c, 5:6], acc[:, 0:CTX - 1], ALU.mult, ALU.add),
                               reads=[pb, cw, acc], writes=[acc])
                          a3 = acc[:, CTX:T].rearrange("p (r c) -> p r c", c=64)
                          p3 = pb[:, CTX:T].rearrange("p (r c) -> p r c", c=64)
                          for ky in range(3):
                              for kx in range(3):
                                  if ky == 1 and kx == 1:
                                      continue
                                  dy, dx = ky - 1, kx - 1
                                  oy0, oy1 = max(0, -dy), 64 - max(0, dy)
                                  ox0, ox1 = max(0, -dx), 64 - max(0, dx)
                                  k.op('dve', lambda e: e.scalar_tensor_tensor(
                                      a3[:, oy0:oy1, ox0:ox1], p3[:, oy0 + dy:oy1 + dy, ox0 + dx:ox1 + dx],
                                      cw[:, c, ky * 3 + kx:ky * 3 + kx + 1], a3[:, oy0:oy1, ox0:ox1], ALU.mult, ALU.add),
                                      reads=[pb, cw, acc], writes=[acc])
                          src = acc
                      sec = [s for s, v in SEC.items() if v <= c0][-1]
                      if sec in ('mq', 'mk'):
                          k.op('act', lambda e: e.activation(acc[:, :], src[:, :], AF.Silu), reads=[src], writes=[acc])
                          if sec == 'mk':
                              k.op('dve', lambda e: e.tensor_scalar(acc[:, :], acc[:, :], 0.125, None, ALU.mult), reads=[acc], writes=[acc])
                          src = acc
                      elif sec in ('mo', 'lg'):
                          k.op('act', lambda e: e.activation(acc[0:M, :], src[0:M, :], AF.Sigmoid), reads=[src], writes=[acc])
                          src = acc
                      elif sec in ('lwf', 'lwb'):
                          k.op('act', lambda e: e.activation(acc[0:M, :], src[0:M, :], AF.Tanh), reads=[src], writes=[acc])
                          src = acc
                      elif sec == 'gates':
                          tmp = pT[1 - ci % 2]
                          k.op('act', lambda e: e.activation(tmp[0:32, :], pb[0:32, :], AF.Exp, bias=mb[:, 1:2], scale=-1.0),
                               reads=[pb, mb], writes=[tmp])
                          k.op('act', lambda e: e.activation(tmp[0:32, :], tmp[0:32, :], AF.Ln, bias=1.0), reads=[tmp], writes=[tmp])
                          k.op('dve', lambda e: e.tensor_scalar(tmp[0:32, :], tmp[0:32, :], mb[:, 3:4], None, ALU.mult), reads=[tmp, mb], writes=[tmp])
                          k.op('dve', lambda e: e.tensor_scalar(acc[0:32, :], pb[0:32, :], mb[:, 0:1], mb[:, 2:3], ALU.add, ALU.mult),
                               reads=[pb, mb], writes=[acc])
                          k.op('dve', lambda e: e.tensor_tensor(acc[0:32, :], acc[0:32, :], tmp[0:32, :], ALU.add), reads=[acc, tmp], writes=[acc])
                          src = acc
                      k.dma('sp', FM[b][c0:c0 + M, :], src[0:M, :], reads=[src], pw=[FM[b]])
        k.barrier()


        with ExitStack() as es:
          if 2 in phases:
            k.es = es
            cm = k.sb("cm", [64, 2, 64], F32)
            k.dma('sp', cm[:], cmask_in[:, :, :], writes=[cm])
            nw = k.sb("nw", [64, 512], F32)
            k.dma('sp', nw[:], m_norm_w[0:1, :].partition_broadcast(64), writes=[nw])
            k.op('pool', lambda e: e.memset(ones_f[:], 1.0), writes=[ones_f])
            GA = k.sb("GA", [64, NCH, 48], F32)
            gT = k.sb("gT", [32, T], F32)
            G = k.sb("G", [64, 32], F32)
            qh = k.sb("qh", [64, 2, T], BF16)
            kh = k.sb("kh", [64, 2, T], BF16)
            vT = k.sb("vT", [128, T], BF16)
            moT = k.sb("moT", [128, T], F32)
            Hf = k.sb("Hf", [64, NCH, 2, 64], F32)
            class _M:
                pass
            MS = []
            for si in range(2):
                M = _M()
                M.i = si
                M.Ktm = k.sb("Ktm", [64, 2, 64], BF16)
                M.Vaug = k.sb("Vaug", [64, 2, 66], BF16)
                M.PTm = k.sb("PTm", [64, 2, 64], BF16)
                M.Cst = k.sb("Cst", [64, 2, 66], F32)
                M.Cbf = k.sb("Cbf", [64, 2, 66], BF16)
                M.dn = k.sb("dn", [64, 2], F32)
                M.ff = k.sb("ff", [64, 2], F32)
                M.hs = k.sb("hs", [64, 2, 64], F32)
                M.st2 = k.sb("st2", [64, 2, 6], F32)
                M.mv2 = k.sb("mv2", [64, 2, 2], F32)
                M.rs2 = k.sb("rs2", [64, 2], F32)
                M.om = k.sb("om", [64, 128], F32)
                M.bA = k.ps("p2A", [64, 512])
                M.bB = k.ps("p2B", [64, 512])
                M.bA.excl = True
                M.bB.excl = True
                MS.append(M)
            pg = k.ps("p2g", [64, 32])
            pbb = k.ps("p2b", [64, 32])
            for b in range(NB):
                k.dma('sp', gT[:], FM[b][3584:3616, :], reads=[FM[b]], writes=[gT])
                for c in range(NCH):
                    k.op('pe', lambda e: e.transpose(pg[:], gT[:, c * 64:(c + 1) * 64], ident[0:32, 0:32]), reads=[gT, ident], writes=[pg])
                    k.op('dve', lambda e: e.tensor_copy(G[:], pg[:]), reads=[pg], writes=[G])
                    k.op('pe', lambda e: e.matmul(pbb[:, 0:8], cm[:, 0, :], G[:, 8:16], start=True, stop=True), reads=[cm, G], writes=[pbb], inc=False)
                    k.op('pe', lambda e: e.matmul(pbb[:, 8:16], cm[:, 1, :], G[:, 24:32], start=True, stop=True), reads=[cm, G], inc=False)
                    k.op('pe', lambda e: e.matmul(pbb[:, 16:24], ones_f[0:64, 0:64], G[:, 8:16], start=True, stop=True), reads=[ones_f, G], inc=False)
                    k.op('pe', lambda e: e.matmul(pbb[:, 24:32], ones_f[0:64, 0:64], G[:, 24:32], start=True, stop=True), reads=[ones_f, G])
                    k.op('act', lambda e: e.activation(GA[:, c, 0:32], pbb[:], AF.Exp), reads=[pbb], writes=[GA])
                    k.op('dve', lambda e: e.tensor_tensor(G[:, 0:8], G[:, 0:8], pbb[:, 0:8], ALU.subtract), reads=[pbb, G], writes=[G])
                    k.op('dve', lambda e: e.tensor_tensor(G[:, 16:24], G[:, 16:24], pbb[:, 8:16], ALU.subtract), reads=[pbb, G], writes=[G])
                    k.op('act', lambda e: e.activation(GA[:, c, 32:40], G[:, 0:8], AF.Exp), reads=[G], writes=[GA])
                    k.op('act', lambda e: e.activation(GA[:, c, 40:48], G[:, 16:24], AF.Exp), reads=[G], writes=[GA])
                for hp in range(4):
                    for h in range(2):
                        r0 = hp * 128 + h * 64
                        for q4 in range(4):
                            t0 = q4 * 1088
                            k.dma('pool', qh[:, h, t0:t0 + 1088], FM[b][r0:r0 + 64, t0:t0 + 1088], reads=[FM[b]], pw=[qh])
                            k.dma('pool', kh[:, h, t0:t0 + 1088], FM[b][512 + r0:512 + r0 + 64, t0:t0 + 1088], reads=[FM[b]], pw=[kh])
                    for q4 in range(4):
                        t0 = q4 * 1088
                        k.dma('pool', vT[:, t0:t0 + 1088], FM[b][2560 + hp * 128:2560 + (hp + 1) * 128, t0:t0 + 1088], reads=[FM[b]], pw=[vT])
                    k.dma('sp', moT[:], FM[b][3072 + hp * 128:3072 + (hp + 1) * 128, :], reads=[FM[b]], writes=[moT])
                    def mstream(M, d, done):
                        Ktm, Vaug, PTm, Cst, Cbf, dn, ff, hs, st2, mv2, rs2, om = M.Ktm, M.Vaug, M.PTm, M.Cst, M.Cbf, M.dn, M.ff, M.hs, M.st2, M.mv2, M.rs2, M.om
                        bA, bB = M.bA, M.bB
                        pk = bA[:, 0:64].bitcast(BF16).rearrange("p (h e) -> p h e", h=2)
                        pv = bA[:, 64:128].bitcast(BF16)
                        pp = bA[:, 128:256].rearrange("p (h e) -> p h e", h=2)
                        po = bB[:, 0:132].rearrange("p (h e) -> p h e", h=2)
                        pc = bB[:, 132:264].rearrange("p (h e) -> p h e", h=2)
                        pmo = bB[:, 264:392]
                        order = list(range(NCH)) if d == 0 else [3, 2, 1, 0] + list(range(NCH - 1, 3, -1))
                        k.op('pool', lambda e: e.memset(Cst[:], 0.0), writes=[Cst])
                        k.op('pool', lambda e: e.memset(Cbf[:], 0.0), writes=[Cbf])
                        for c in order:
                            cs = slice(c * 64, (c + 1) * 64)
                            hh = 2 * hp
                            a_ap = GA[:, c, d * 8 + hh:d * 8 + hh + 2]
                            e_ap = GA[:, c, 16 + d * 8 + hh:16 + d * 8 + hh + 2]
                            c_ap = GA[:, c, 32 + d * 8 + hh:32 + d * 8 + hh + 2]
                            needy = c >= 4
                            second = c in done
                            fin = needy and second
                            for h in range(2):
                                k.op('pe', lambda e: e.transpose(pk[:, h, :], kh[:, h, cs], identb[0:64, 0:64]), reads=[kh, identb], writes=[bA] if h == 0 else [], inc=False)
                            k.op('pe', lambda e: e.transpose(pv, vT[:, cs], identb[:]), reads=[vT, identb], inc=False)
                            for h in range(2):
                                k.op('pe', lambda e: e.matmul(pp[:, h, :], kh[:, h, cs], qh[:, h, cs], start=True, stop=True), reads=[kh, qh], inc=(h == 1))
                            k.op('dve', lambda e: e.tensor_copy(Ktm[:], pk), reads=[bA], writes=[Ktm])
                            k.op('dve', lambda e: e.tensor_tensor(Vaug[:, :, 0:64], pv.rearrange("p (h e) -> p h e", h=2),
                                                                  c_ap.unsqueeze(2).to_broadcast([64, 2, 64]), ALU.mult), reads=[bA, GA], writes=[Vaug])
                            k.op('act', lambda e: e.activation(Vaug[:, :, 64:65], c_ap.unsqueeze(2), AF.Identity), reads=[GA, Vaug], writes=[Vaug])
                            k.op('dve', lambda e: e.tensor_tensor(PTm[:], pp, cm[:, d:d + 1, :].to_broadcast([64, 2, 64]), ALU.mult), reads=[bA, cm], writes=[PTm])
                            yield
                            for h in range(2):
                                k.op('pe', lambda e: e.matmul(po[:, h, 0:65], PTm[:, h, :], Vaug[:, h, 0:65], start=True, stop=False), reads=[PTm, Vaug], writes=[bB] if h == 0 else [], inc=False)
                                k.op('pe', lambda e: e.matmul(po[:, h, 0:65], qh[:, h, cs], Cbf[:, h, 0:65], start=False, stop=True), reads=[qh, Cbf], inc=False)
                            if fin:
                                k.op('pe', lambda e: e.transpose(pmo, moT[:, cs], ident[:]), reads=[moT, ident], inc=False)
                            for h in range(2):
                                k.op('pe', lambda e: e.matmul(pc[:, h, 0:65], Ktm[:, h, :], Vaug[:, h, 0:65], start=True, stop=True), reads=[Ktm, Vaug], inc=(h == 1))
                            yield
                            k.op('dve', lambda e: e.tensor_tensor(Cst[:, :, 0:65], Cst[:, :, 0:65], pc[:, :, 0:65], ALU.add), reads=[bB, Cst], writes=[Cst])
                            k.op('dve', lambda e: e.tensor_tensor(Cst[:, :, 0:65], Cst[:, :, 0:65], e_ap.unsqueeze(2).to_broadcast([64, 2, 65]), ALU.mult), reads=[GA, Cst], writes=[Cst])
                            k.op('act', lambda e: e.activation(Cbf[:, :, 0:65], Cst[:, :, 0:65], AF.Identity), reads=[Cst], writes=[Cbf])
                            if needy:
                                k.op('dve', lambda e: e.tensor_tensor(dn[:], po[:, :, 64], a_ap, ALU.mult), reads=[bB, GA], writes=[dn])
                                k.op('dve', lambda e: e.tensor_scalar(ff[:], dn[:], -1.0, None, ALU.mult), reads=[dn], writes=[ff])
                                k.op('dve', lambda e: e.tensor_tensor(dn[:], dn[:], ff[:], ALU.max), reads=[dn, ff], writes=[dn])
                                k.op('dve', lambda e: e.tensor_scalar(dn[:], dn[:], 1.0, None, ALU.max), reads=[dn], writes=[dn])
                                k.op('dve', lambda e: e.reciprocal(dn[:], dn[:]), reads=[dn], writes=[dn])
                                k.op('dve', lambda e: e.tensor_tensor(ff[:], dn[:], a_ap, ALU.mult), reads=[dn, GA], writes=[ff])
                                if not second:
                                    k.op('dve', lambda e: e.tensor_tensor(Hf[:, c, :, :], po[:, :, 0:64], ff[:].unsqueeze(2).to_broadcast([64, 2, 64]), ALU.mult), reads=[bB, ff], pw_=[Hf])
                                    done[c] = M.i
                                else:
                                    k.op('dve', lambda e: e.tensor_tensor(hs[:], po[:, :, 0:64], ff[:].unsqueeze(2).to_broadcast([64, 2, 64]), ALU.mult), reads=[bB, ff], writes=[hs])
                                    k.op('dve', lambda e: e.tensor_tensor(hs[:], hs[:], Hf[:, c, :, :], ALU.add), reads=[hs, Hf], writes=[hs])
                            if fin:
                                for h in range(2):
                                    k.op('dve', lambda e: e.bn_stats(st2[:, h, :], hs[:, h, :]), reads=[hs], writes=[st2])
                                for h in range(2):
                                    k.op('dve', lambda e: e.bn_aggr(mv2[:, h, :], st2[:, h, :]), reads=[st2], writes=[mv2])
                                k.op('act', lambda e: e.activation(rs2[:], mv2[:, :, 1], AF.Sqrt, bias=LN_EPS), reads=[mv2], writes=[rs2])
                                k.op('dve', lambda e: e.reciprocal(rs2[:], rs2[:]), reads=[rs2], writes=[rs2])
                                for h in range(2):
                                    k.op('dve', lambda e: e.tensor_scalar(hs[:, h, :], hs[:, h, :], mv2[:, h, 0:1], rs2[:, h:h + 1], ALU.subtract, ALU.mult),
                                         reads=[hs, mv2, rs2], writes=[hs])
                                k.op('dve', lambda e: e.tensor_tensor(om[:], hs[:].rearrange("p h e -> p (h e)"), nw[:, hp * 128:(hp + 1) * 128], ALU.mult),
                                     reads=[hs, nw], writes=[om])
                                k.op('dve', lambda e: e.tensor_tensor(om[:], om[:], pmo, ALU.mult), reads=[om, bB], writes=[om])
                                k.dma('sp', MIX[b][(c - 4) * 64:(c - 3) * 64, hp * 128:(hp + 1) * 128], om[:], reads=[om], pw=[MIX[b]])
                            yield

                    done = {}
                    gens = [mstream(MS[0], 0, done), mstream(MS[1], 1, done)]
                    alive = [True, True]
                    while any(alive):
                        for gi in range(2):
                            if alive[gi]:
                                try:
                                    next(gens[gi])
                                except StopIteration:
                                    alive[gi] = False
        k.barrier()

        with ExitStack() as es:
          if 3 in phases:
            k.es = es
            NBK = 512
            NBC = 8
            rp1 = k.sb("rp1", [128, 4, 8], F32)
            k.dma('sp', rp1[:, :, 0:7], rp_in[:, :, :], writes=[rp1])
            k.op('dve', lambda e: e.tensor_scalar(rp1[:, :, 7], rp1[:, :, 4], -1.0, 1.0, ALU.mult, ALU.add), reads=[rp1], writes=[rp1])
            smask = k.sb("smask", [128, NBK], F32)
            k.dma('sp', smask[:], smask_in[:, 0:NBK], writes=[smask])
            onesbd = k.sb("onesbd", [128, 128], F32)
            k.dma('sp', onesbd[:], onesbd_in[:, :], writes=[onesbd])
            ARs = k.sb("ARs", [128, NBC, 128], BF16)
            ZTs = k.sb("ZTs", [128, NBC, 128], BF16)
            PRs = k.sb("PRs", [128, NBK], BF16)
            GLs = k.sb("GLs", [128, NBC], F32)
            rmask = k.sb("rmask", [128, 2, 128], F32)
            k.dma('sp', rmask[:], rmask_in[:, :, :], writes=[rmask])
            nmask = k.sb("nmask", [64, 2, 64], F32)
            k.dma('sp', nmask[:], nmask_in[:, :, :], writes=[nmask])
            gnw = k.sb("gnw", [64, 2, 512], F32)
            k.dma('sp', gnw[:, 0, :], r_norm_w[0:1, :].partition_broadcast(64), pw=[gnw])
            k.dma('sp', gnw[:, 1, :], r_norm_b[0:1, :].partition_broadcast(64), pw=[gnw])
            wBb = k.sb("wBb", [64, 2, 512], BF16)
            aBb = k.sb("aBb", [64, 512], BF16)
            gBb = k.sb("gBb", [128, 512], BF16)
            k.dma('pool', wBb[:, 0, :], r_wB[0, :, :], pw=[wBb])
            k.dma('pool', wBb[:, 1, :], r_wB[1, :, :], pw=[wBb])
            k.dma('pool', aBb[:], r_aB[:, :], writes=[aBb])
            k.dma('pool', gBb[:], r_gB[:, :], writes=[gBb])
            k.op('pool', lambda e: e.memset(ones_f[:], 1.0), writes=[ones_f])
            onesb = k.sb("onesb", [64, 2], BF16)
            k.op('pool', lambda e: e.memset(onesb[:], 1.0), writes=[onesb])
            lgb = k.sb("lgb", [128, T], BF16)
            lab = k.sb("lab", [64, NBK], BF16)
            lwb_ = k.sb("lwb_", [64, NBK], BF16)
            rr = k.sb("rr", [128, NBK], F32)
            rk = k.sb("rk", [128, NBK], F32)
            aa = k.sb("aa", [128, NBK], F32)
            t1 = k.sb("t1", [128, NBK], F32)
            t2 = k.sb("t2", [128, NBK], F32)
            khat = k.sb("khat", [128, NBK], F32)
            kmod = k.sb("kmod", [128, NBK], F32)
            beta = k.sb("beta", [128, NBK], F32)
            lgw = k.sb("lgw", [128, NBK], F32)
            lam = k.sb("lam", [128, NBK], F32)
            ee = k.sb("ee", [128, NBK], F32)
            class _S:
                pass
            SS = []
            for si in range(2):
                S = _S()
                S.i = si
                S.VTb = k.sb("VTb", [64, 2, 64 + NBK], BF16)
                S.PRb = k.sb("PRb", [64, 2, NBK], BF16)
                S.AR = k.sb("AR", [64, 2, NBC, 128], BF16)
                S.ZT = k.sb("ZT", [64, 2, NBC, 128], BF16)
                S.GL = k.sb("GL", [64, 2, NBC], F32)
                S.MmA = k.sb("MmA", [128, 2, NBC, 128], BF16)
                S.XLA = k.sb("XLA", [128, 2, NBC, 64], BF16)
                S.SWA = k.sb("SWA", [128, 2, NBC + 1, 64], BF16)
                S.WA = k.sb("WA", [128, 2, NBC, 64], BF16)
                S.ZtA = k.sb("ZtA", [128, 2, NBC, 64], BF16)
                S.TTA = k.sb("TTA", [64, 2, NBC, 64], BF16)
                S.NGa = [k.sb("NGa%d" % j, [64, 2, 8, 64], BF16) for j in range(2)]
                S.TTg = [k.sb("TTg%d" % j, [64, 8, 64], BF16) for j in range(2)]
                S.Xb = k.sb("Xb", [64, 2, 64], BF16)
                S.ST = k.sb("ST", [64, 2, 64], F32)
                S.tS = k.sb("tS", [64, 2, 64], F32)
                S.ys = k.sb("ys", [64, 2, 64], F32)
                S.bon = k.sb("bon", [64, 2], F32)
                S.om3 = k.sb("om3", [64, 128], F32)
                S.st2 = k.sb("st2r", [64, 2, 6], F32)
                S.mv2 = k.sb("mv2r", [64, 2, 2], F32)
                S.rs2 = k.sb("rs2r", [64, 2], F32)
                S.pXU = k.ps("p3x", [64, 2, 2, 64])
                S.pYS = k.ps("p3y", [64, 512])
                SS.append(S)
            Yf = k.sb("Yf", [64, NCH, 2, 64], F32)
            identg = k.sb("identg", [64, 8, 64], F32)
            pMg = k.ps("p3m", [128, 8, 128])
            pSg = k.ps("p3s", [128, 2, 512])
            pA = pMg
            for S in SS:
                k.op('pool', lambda e: e.memset(S.VTb[:], 0.0), writes=[S.VTb])
                k.op('pool', lambda e: e.memset(S.SWA[:], 0.0), writes=[S.SWA])
            for m in range(8):
                k.op('dve', lambda e: e.tensor_copy(identg[:, m, :], ident[0:64, 0:64]), reads=[ident], writes=[identg])

            def prep(S, b, hp, tok0, ntok, d):
                VTb, PRb, AR, ZT, GL = S.VTb, S.PRb, S.AR, S.ZT, S.GL
                nch = ntok // 64
                n = ntok
                NS = slice(0, ntok)
                r0 = hp * 128
                k.dma('pool', lab[:, NS], FM[b][3744:3808, tok0:tok0 + ntok], reads=[FM[b]], writes=[lab])
                lo = 3616 + 64 * d
                k.dma('pool', lwb_[:, NS], FM[b][lo:lo + 64, tok0:tok0 + ntok], reads=[FM[b]], writes=[lwb_])
                k.dma('sp', rr[:, NS], FM[b][1024 + r0:1024 + r0 + 128, tok0:tok0 + ntok], reads=[FM[b]], writes=[rr])
                k.dma('sp', rk[:, NS], FM[b][1536 + r0:1536 + r0 + 128, tok0:tok0 + ntok], reads=[FM[b]], writes=[rk])
                for h in range(2):
                    k.dma('pool', VTb[:, h, 64:64 + ntok], FM[b][2048 + r0 + h * 64:2048 + r0 + (h + 1) * 64, tok0:tok0 + ntok], reads=[FM[b]], pw=[VTb])
                pA0 = pMg[:, 0:4, :].rearrange("p a b -> p (a b)")[:, 0:n]
                pA1 = pMg[:, 4:8, :].rearrange("p a b -> p (a b)")[:, 0:n]
                k.op('pe', lambda e: e.matmul(pA0, aBb[:, hp * 128:(hp + 1) * 128], lab[:, NS], start=True, stop=True), reads=[aBb, lab], writes=[pMg], inc=False)
                k.op('pe', lambda e: e.matmul(pA1, wBb[:, d, hp * 128:(hp + 1) * 128], lwb_[:, NS], start=True, stop=True), reads=[wBb, lwb_])
                k.op('act', lambda e: e.activation(aa[:, NS], pA0, AF.Sigmoid, bias=rp1[:, hp, 2:3]), reads=[pMg, rp1], writes=[aa])
                k.op('act', lambda e: e.activation(lgw[:, NS], pA1, AF.Sigmoid, bias=rp1[:, hp, d:d + 1]), reads=[pMg, rp1], writes=[lgw])
                k.op('dve', lambda e: e.tensor_scalar(lgw[:, NS], lgw[:, NS], -DS, None, ALU.mult), reads=[lgw], writes=[lgw])
                k.op('dve', lambda e: e.tensor_scalar(t1[:, NS], rk[:, NS], rp1[:, hp, 3:4], None, ALU.mult), reads=[rk, rp1], writes=[t1])
                k.op('dve', lambda e: e.tensor_tensor(t2[:, NS], t1[:, NS], t1[:, NS], ALU.mult), reads=[t1], writes=[t2])
                k.op('pe', lambda e: e.matmul(pA0, onesbd[:], t2[:, NS], start=True, stop=True), reads=[onesbd, t2], writes=[pMg])
                k.op('act', lambda e: e.activation(khat[:, NS], pA0, AF.Sqrt), reads=[pMg], writes=[khat])
                k.op('dve', lambda e: e.tensor_scalar(khat[:, NS], khat[:, NS], 1e-12, None, ALU.max), reads=[khat], writes=[khat])
                k.op('dve', lambda e: e.reciprocal(khat[:, NS], khat[:, NS]), reads=[khat], writes=[khat])
                k.op('dve', lambda e: e.tensor_tensor(khat[:, NS], khat[:, NS], t1[:, NS], ALU.mult), reads=[khat, t1], writes=[khat])
                k.op('dve', lambda e: e.tensor_scalar(t1[:, NS], aa[:, NS], rp1[:, hp, 4:5], rp1[:, hp, 7:8], ALU.mult, ALU.add), reads=[aa, rp1], writes=[t1])
                k.op('dve', lambda e: e.tensor_tensor(kmod[:, NS], rk[:, NS], t1[:, NS], ALU.mult), reads=[rk, t1], writes=[kmod])
                k.op('dve', lambda e: e.tensor_tensor(beta[:, NS], khat[:, NS], aa[:, NS], ALU.mult), reads=[khat, aa], writes=[beta])
                k.op('dve', lambda e: e.scalar_tensor_tensor(PRs[:, NS], rr[:, NS], rp1[:, hp, 6:7], kmod[:, NS], ALU.mult, ALU.mult), reads=[rr, rp1, kmod], writes=[PRs])
                k.op('dve', lambda e: e.tensor_tensor_scan(lam[:, NS], smask[:, NS], lgw[:, NS], 0.0, ALU.mult, ALU.add), reads=[smask, lgw], writes=[lam])
                v3 = lambda t_: t_[:, NS].rearrange("p (c t) -> p c t", t=64)
                if d == 1:
                    k.op('dve', lambda e: e.tensor_tensor(t1[:, NS], lgw[:, NS], lam[:, NS], ALU.subtract), reads=[lgw, lam], writes=[t1])
                    k.op('dve', lambda e: e.tensor_tensor(v3(t2), v3(t1), v3(lam)[:, :, 63:64].to_broadcast([128, nch, 64]), ALU.add), reads=[t1, lam], writes=[t2])
                    k.op('act', lambda e: e.activation(lam[:, NS], t2[:, NS], AF.Identity), reads=[t2], writes=[lam])
                k.op('dve', lambda e: e.tensor_tensor(t1[:, NS], lam[:, NS], lgw[:, NS], ALU.subtract), reads=[lam, lgw], writes=[t1])
                k.op('act', lambda e: e.activation(ee[:, NS], t1[:, NS], AF.Exp), reads=[t1], writes=[ee])
                k.op('dve', lambda e: e.scalar_tensor_tensor(ARs[:, 0:nch, 0:64], v3(khat), -1.0, v3(ee), ALU.mult, ALU.mult), reads=[khat, ee], writes=[ARs])
                k.op('act', lambda e: e.activation(ee[:, NS], lam[:, NS], AF.Exp), reads=[lam], writes=[ee])
                k.op('dve', lambda e: e.tensor_tensor(ARs[:, 0:nch, 64:128], v3(rr), v3(ee), ALU.mult), reads=[rr, ee], writes=[ARs])
                gcol = 63 if d == 0 else 0
                k.op('act', lambda e: e.activation(GLs[:, 0:nch], v3(ee)[:, :, gcol], AF.Identity), reads=[ee], writes=[GLs])
                k.op('act', lambda e: e.activation(t1[:, NS], lam[:, NS], AF.Exp, scale=-1.0), reads=[lam], writes=[t1])
                k.op('dve', lambda e: e.tensor_tensor(ZTs[:, 0:nch, 0:64], v3(beta), v3(t1), ALU.mult), reads=[beta, t1], writes=[ZTs])
                k.op('dve', lambda e: e.tensor_tensor(ZTs[:, 0:nch, 64:128], v3(kmod), v3(t1), ALU.mult), reads=[kmod, t1], writes=[ZTs])
                k.op('act', lambda e: e.activation(AR[:, 0, 0:nch, :], ARs[0:64, 0:nch, :], AF.Identity), reads=[ARs], writes=[AR])
                k.op('act', lambda e: e.activation(ZT[:, 0, 0:nch, :], ZTs[0:64, 0:nch, :], AF.Identity), reads=[ZTs], writes=[ZT])
                k.op('act', lambda e: e.activation(PRb[:, 0, NS], PRs[0:64, NS], AF.Identity), reads=[PRs], writes=[PRb])
                k.op('act', lambda e: e.activation(GL[:, 0, 0:nch], GLs[0:64, 0:nch], AF.Identity), reads=[GLs], writes=[GL])
                k.dma('sp', AR[:, 1, 0:nch, :], ARs[64:128, 0:nch, :], reads=[ARs], pw=[AR])
                k.dma('sp', ZT[:, 1, 0:nch, :], ZTs[64:128, 0:nch, :], reads=[ZTs], pw=[ZT])
                k.dma('sp', PRb[:, 1, NS], PRs[64:128, NS], reads=[PRs], pw=[PRb])
                k.dma('sp', GL[:, 1, 0:nch], GLs[64:128, 0:nch], reads=[GLs], pw=[GL])

            pSf = lambda: pSg[:].rearrange("p a b -> p (a b)")

            def precompute(S, l0, d):
                VTb, AR, ZT, MmA, XLA, SWA, WA, ZtA, TTA, NGa, TTg = S.VTb, S.AR, S.ZT, S.MmA, S.XLA, S.SWA, S.WA, S.ZtA, S.TTA, S.NGa, S.TTg
                G4 = slice(l0, l0 + 4)
                pTb = pSg[:, 0, :].bitcast(BF16)
                for h in range(2):
                    for j in range(4):
                        m = h * 4 + j
                        k.op('pe', lambda e: e.transpose(pTb[:, m * 64:(m + 1) * 64], ZT[:, h, l0 + j, :], identb[0:64, 0:64]), reads=[ZT, identb], writes=[pSg], inc=False)
                for h in range(2):
                    for j in range(4):
                        m = 8 + h * 4 + j
                        k.op('pe', lambda e: e.transpose(pTb[:, m * 64:(m + 1) * 64], VTb[:, h, (l0 + j) * 64:(l0 + j) * 64 + 128], identb[0:64, 0:64]), reads=[VTb, identb], inc=(h == 1 and j == 3))
                zsrc = pTb[:, 0:512].rearrange("p (h j e) -> p h j e", h=2, j=4)
                vsrc = pTb[64:128, 512:1024].rearrange("p (h j e) -> p h j e", h=2, j=4)
                k.op('act', lambda e: e.activation(ZtA[:, :, G4, :], zsrc, AF.Identity), reads=[pSg], writes=[ZtA])
                k.op('act', lambda e: e.activation(SWA[64:128, :, G4, :], vsrc, AF.Identity), reads=[pSg], writes=[SWA])
                k.op('act', lambda e: e.activation(WA[64:128, :, G4, :], vsrc, AF.Identity), reads=[pSg], writes=[WA])
                yield
                for h in range(2):
                    for j in range(4):
                        m = h * 4 + j
                        k.op('pe', lambda e: e.matmul(pMg[:, m, :], ZT[:, h, l0 + j, :], AR[:, h, l0 + j, :], start=True, stop=True), reads=[ZT, AR], writes=[pMg], inc=(m == 7))
                for h in range(2):
                    for j in range(4):
                        m = h * 4 + j
                        k.op('pe', lambda e: e.matmul(pSg[0:64, 1, m * 64:(m + 1) * 64], AR[:, h, l0 + j, 0:64], ZT[:, h, l0 + j, 0:64], start=True, stop=True), reads=[ZT, AR], writes=[pSg], inc=(m == 7))
                for h in range(2):
                    k.op('dve', lambda e: e.tensor_tensor(MmA[:, h, G4, :], pMg[:, h * 4:(h + 1) * 4, :], rmask[:, d:d + 1, :].to_broadcast([128, 4, 128]), ALU.mult),
                         reads=[pMg, rmask], writes=[MmA])
                N0, N1 = NGa[0], NGa[1]
                k.op('dve', lambda e: e.tensor_tensor(N0[:, 0, :, :], pSg[0:64, 1, :].rearrange("p (m e) -> p m e", e=64), nmask[:, d:d + 1, :].to_broadcast([64, 8, 64]), ALU.mult),
                     reads=[pSg, nmask], writes=[N0])
                k.op('act', lambda e: e.activation(N0[:, 1, :, :].rearrange("p (h j) e -> p h j e", h=2), MmA[0:64, :, G4, 0:64], AF.Identity), reads=[MmA], writes=[N0])
                k.op('act', lambda e: e.activation(XLA[64:128, :, G4, :], MmA[64:128, :, G4, 0:64], AF.Identity), reads=[MmA], writes=[XLA])
                k.op('act', lambda e: e.activation(XLA[0:64, :, G4, :], AR[:, :, G4, 0:64], AF.Identity), reads=[AR], writes=[XLA])
                yield
                k.op('dve', lambda e: e.tensor_tensor(TTg[0][:], N0[:, 1, :, :], identg[:], ALU.add), reads=[N0, identg], writes=[TTg[0]])
                cur = 0
                for lv in range(1, 6):
                    yield
                    src, dst = NGa[cur], NGa[1 - cur]
                    for m in range(8):
                        k.op('pe', lambda e: e.matmul(pSg[0:64, 0, m * 64:(m + 1) * 64], src[:, 1, m, :], src[:, 0, m, :], start=True, stop=True), reads=[src], writes=[pSg], inc=False)
                    for m in range(8):
                        k.op('pe', lambda e: e.matmul(pSg[0:64, 1, m * 64:(m + 1) * 64], src[:, 0, m, :], src[:, 1, m, :], start=True, stop=True), reads=[src], inc=(m == 7))
                    k.op('act', lambda e: e.activation(dst[:, 0, :, :].rearrange("p m e -> p (m e)"), pSg[0:64, 0, :], AF.Identity), reads=[pSg], writes=[dst])
                    k.op('dve', lambda e: e.tensor_copy(dst[:, 1, :, :].rearrange("p m e -> p (m e)"), pSg[0:64, 1, :]), reads=[pSg], writes=[dst])
                    cur = 1 - cur
                    ti, to = TTg[(lv - 1) % 2], TTg[lv % 2]
                    for m in range(8):
                        k.op('pe', lambda e: e.matmul(pMg[0:64, m, 0:64], dst[:, 0, m, :], ti[:, m, :], start=True, stop=True), reads=[dst, ti], writes=[pMg], inc=(m == 7))
                    if lv < 5:
                        k.op('dve', lambda e: e.tensor_tensor(to[:], pMg[0:64, :, 0:64], ti[:], ALU.add), reads=[pMg, ti], writes=[to])
                    else:
                        k.op('dve', lambda e: e.tensor_tensor(TTA[:, :, G4, :], pMg[0:64, :, 0:64].rearrange("p (h j) e -> p h j e", h=2), ti[:].rearrange("p (h j) e -> p h j e", h=2), ALU.add),
                             reads=[pMg, ti], writes=[TTA])

            def step(S, b, hp, c, lc, lnext, d, done):
                VTb, PRb, AR, GL, MmA, XLA, SWA, WA, ZtA, TTA = S.VTb, S.PRb, S.AR, S.GL, S.MmA, S.XLA, S.SWA, S.WA, S.ZtA, S.TTA
                Xb, ST, tS, ys, bon, om3, st2, mv2, rs2, pXU, pYS = S.Xb, S.ST, S.tS, S.ys, S.bon, S.om3, S.st2, S.mv2, S.rs2, S.pXU, S.pYS
                pX = pXU[:, 0, :, :]
                pU = pXU[:, 1, :, :]
                pY = pYS[:, 0:128].rearrange("p (h e) -> p h e", h=2)
                pS_ = pYS[:, 128:256].rearrange("p (h e) -> p h e", h=2)
                for h in range(2):
                    k.op('pe', lambda e: e.matmul(pXU[:, 0, h, :], XLA[:, h, lc, :], SWA[:, h, lc, :], start=True, stop=True), reads=[XLA, SWA], writes=[pXU], inc=(h == 1))
                k.op('act', lambda e: e.activation(Xb[:], pX, AF.Identity), reads=[pXU], writes=[Xb])
                yield
                for h in range(2):
                    k.op('pe', lambda e: e.matmul(pXU[:, 1, h, :], TTA[:, h, lc, :], Xb[:, h, :], start=True, stop=True), reads=[TTA, Xb], writes=[pXU], inc=(h == 1))
                k.op('dve', lambda e: e.tensor_copy(WA[0:64, :, lc, :], pU), reads=[pXU], writes=[WA])
                yield
                second = (c in done)
                needy = (c >= 4)
                wfirst = [pYS]
                if needy:
                    for h in range(2):
                        k.op('pe', lambda e: e.matmul(pYS[:, h * 64:(h + 1) * 64], AR[:, h, lc, 64:128], SWA[0:64, h, lc, :], start=True, stop=False), reads=[AR, SWA], writes=wfirst, inc=False)
                        wfirst = []
                        k.op('pe', lambda e: e.matmul(pYS[:, h * 64:(h + 1) * 64], MmA[:, h, lc, 64:128], WA[:, h, lc, :], start=False, stop=True), reads=[MmA, WA], inc=False)
                fin = (needy and second)
                if fin:
                    ts = slice(lc * 64, (lc + 1) * 64)
                    for h in range(2):
                        k.op('pe', lambda e: e.matmul(pYS[:, 384 + 2 * h:386 + 2 * h], PRb[:, h, ts], onesb[:, 0:2], start=True, stop=True), reads=[PRb, onesb], inc=False)
                    k.op('pe', lambda e: e.matmul(pYS[:, 256:384], lgb[:, c * 64:(c + 1) * 64], gBb[:, hp * 128:(hp + 1) * 128], start=True, stop=True), reads=[lgb, gBb], inc=False)
                    pvt = pYS[:, 448:512].bitcast(BF16)
                    for h in range(2):
                        k.op('pe', lambda e: e.transpose(pvt[:, h * 64:(h + 1) * 64], VTb[:, h, 64 + lc * 64:128 + lc * 64], identb[0:64, 0:64]), reads=[VTb, identb], inc=False)
                for h in range(2):
                    k.op('pe', lambda e: e.matmul(pYS[:, 128 + h * 64:128 + (h + 1) * 64], ZtA[:, h, lc, :], WA[:, h, lc, :], start=True, stop=True), reads=[ZtA, WA], writes=wfirst, inc=(h == 1))
                    wfirst = []
                k.op('dve', lambda e: e.tensor_tensor(tS[:], pS_, ST[:], ALU.add), reads=[pYS, ST], writes=[tS])
                k.op('dve', lambda e: e.tensor_tensor(ST[:], tS[:], GL[:, :, lc:lc + 1].to_broadcast([64, 2, 64]), ALU.mult), reads=[tS, GL], writes=[ST])
                k.op('act', lambda e: e.activation(SWA[0:64, :, lnext, :], ST[:], AF.Identity), reads=[ST], writes=[SWA])
                if needy and not second:
                    k.op('dve', lambda e: e.tensor_copy(Yf[:, c, :, :], pY), reads=[pYS], pw_=[Yf])
                    done[c] = S.i
                elif needy:
                    k.op('dve', lambda e: e.tensor_tensor(ys[:], pY, Yf[:, c, :, :], ALU.add), reads=[pYS, Yf], writes=[ys])
                if fin:
                    for h in range(2):
                        k.op('dve', lambda e: e.bn_stats(st2[:, h, :], ys[:, h, :]), reads=[ys], writes=[st2])
                    for h in range(2):
                        k.op('dve', lambda e: e.bn_aggr(mv2[:, h, :], st2[:, h, :]), reads=[st2], writes=[mv2])
                    k.op('act', lambda e: e.activation(rs2[:], mv2[:, :, 1], AF.Sqrt, bias=GN_EPS), reads=[mv2], writes=[rs2])
                    k.op('dve', lambda e: e.reciprocal(rs2[:], rs2[:]), reads=[rs2], writes=[rs2])
                    for h in range(2):
                        k.op('dve', lambda e: e.tensor_scalar(ys[:, h, :], ys[:, h, :], mv2[:, h, 0:1], rs2[:, h:h + 1], ALU.subtract, ALU.mult), reads=[ys, mv2, rs2], writes=[ys])
                    ysf = ys[:].rearrange("p h e -> p (h e)")
                    k.op('dve', lambda e: e.tensor_tensor(om3[:], ysf, gnw[:, 0, hp * 128:(hp + 1) * 128], ALU.mult), reads=[ys, gnw], writes=[om3])
                    k.op('dve', lambda e: e.tensor_tensor(om3[:], om3[:], gnw[:, 1, hp * 128:(hp + 1) * 128], ALU.add), reads=[om3, gnw], writes=[om3])
                    k.op('dve', lambda e: e.tensor_copy(bon[:], pYS[:, 384:388].rearrange("p (h two) -> p h two", two=2)[:, :, 0]), reads=[pYS], writes=[bon])
                    for h in range(2):
                        k.op('dve', lambda e: e.scalar_tensor_tensor(om3[:, h * 64:(h + 1) * 64], pvt[:, h * 64:(h + 1) * 64], bon[:, h:h + 1], om3[:, h * 64:(h + 1) * 64], ALU.mult, ALU.add),
                             reads=[pYS, bon, om3], writes=[om3])
                    k.op('dve', lambda e: e.tensor_tensor(om3[:], om3[:], pYS[:, 256:384], ALU.mult), reads=[om3, pYS], writes=[om3])
                    k.dma('sp', MIX[b][(c - 4) * 64:(c - 3) * 64, 512 + hp * 128:512 + (hp + 1) * 128], om3[:], reads=[om3], pw=[MIX[b]])

            blocks = [(0, 256)] + [(256 + i * 512, 512) for i in range(8)]

            def stream(S, b, hp, d, done):
                k.op('pool', lambda e: e.memset(S.ST[:], 0.0), writes=[S.ST])
                border = list(range(9)) if d == 0 else [0] + list(range(8, 0, -1))
                first = True
                for bi in border:
                    tok0, ntok = blocks[bi]
                    nch = ntok // 64
                    prep(S, b, hp, tok0, ntok, d)
                    lcs = list(range(nch)) if d == 0 else list(range(nch - 1, -1, -1))
                    if first:
                        k.op('pool', lambda e: e.memset(S.SWA[0:64, :, lcs[0], :], 0.0), writes=[S.SWA])
                        first = False
                    else:
                        k.op('act', lambda e: e.activation(S.SWA[0:64, :, lcs[0], :], S.ST[:], AF.Identity), reads=[S.ST], writes=[S.SWA])
                    yield
                    for g0 in range(0, nch, 4):
                        yield from precompute(S, g0, d)
                        yield
                    for ii, lc in enumerate(lcs):
                        lnext = lcs[ii + 1] if ii + 1 < len(lcs) else NBC
                        yield from step(S, b, hp, tok0 // 64 + lc, lc, lnext, d, done)
                        yield

            for b in range(NB):
                for q4 in range(4):
                    k.dma('pool', lgb[:, q4 * 1088:(q4 + 1) * 1088], FM[b][3808:3936, q4 * 1088:(q4 + 1) * 1088], reads=[FM[b]], pw=[lgb])
                for hp in range(int(os.environ.get('P3H', 4))):
                    done = {}
                    gens = [stream(SS[0], b, hp, 0, done), stream(SS[1], b, hp, 1, done)]
                    alive = [True, True]
                    while any(alive):
                        for gi in range(2):
                            if alive[gi]:
                                try:
                                    next(gens[gi])
                                except StopIteration:
                                    alive[gi] = False
        k.barrier()

        with ExitStack() as es:
          if 4 in phases:
           try:
            P4S = int(os.environ.get('P4S', 9))
            k.es = es
            NT = NB * SEQ // 128
            NBLK = NBLK_
            SUB = BS // 128
            LG = k.sb("LG", [128, NT, 36], F32)
            OH1 = k.sb("OH1", [128, NT, 32], F32)
            OH2 = k.sb("OH2", [128, NT, 32], F32)
            W1 = k.sb("W1", [128, NT], F32)
            W2 = k.sb("W2", [128, NT], F32)
            DST = k.sb("DST", [128, NT, 2], I32)
            WIDX = k.sb("WIDX", [128, NBLK, 12], I32)
            g2b = k.sb("g2b", [128, NB, D], F32)
            lnp = k.sb("lnp", [128, 4, D], F32)
            for j, src in enumerate((ln1_g, ln1_b, ln2_g, ln2_b)):
                k.dma('sp', lnp[:, j, :], src[0:1, :].partition_broadcast(128), pw=[lnp])
            for b in range(NB):
                k.dma('sp', g2b[:, b, :], MODD[b:b + 1, 5 * D:6 * D].partition_broadcast(128), reads=[MODD], pw=[g2b])
            with ExitStack() as es4:
                k.es = es4
                wob = k.sb("wob", [128, 8, D], BF16)
                for kc in range(8):
                    k.dma('pool', wob[:, kc, :], w_out[kc * 128:(kc + 1) * 128, :], pw=[wob])
                rt = k.sb("rt", [128, 8, 36], F32)
                k.dma('sp', rt[:], rt_in[:, :].rearrange("(kc p) n -> p kc n", p=128), writes=[rt])
                rtbb = k.sb("rtbb", [128, 36], F32)
                k.dma('sp', rtbb[:], rtb_in[0:1, :].partition_broadcast(128), writes=[rtbb])
                mb4 = k.sb("mb4", [128, 3, D], F32)
                mxb = [k.sb("mxb%d" % i, [128, D], BF16) for i in range(2)]
                mT = k.sb("mT", [128, 8, 128], BF16)
                x4 = [k.sb("x4%d" % i, [128, D], F32) for i in range(2)]
                t4 = k.sb("t4", [128, D], F32)
                y4 = k.sb("y4", [128, D], F32)
                h4 = k.sb("h4", [128, D], F32)
                h4b = k.sb("h4b", [128, D], BF16)
                h4T = k.sb("h4T", [128, 8, 128], F32)
                st4 = k.sb("st4", [128, 2, 6], F32)
                mv4 = k.sb("mv4", [128, 2], F32)
                rs4 = k.sb("rs4", [128, 1], F32)
                ptm = k.ps("p4t", [128, 8, 128], BF16)
                po4 = [k.ps("p4o%d" % i, [128, 512]) for i in range(2)]
                pth = k.ps("p4th", [128, 8, 128])
                plg = k.ps("p4lg", [128, 36])

                def ln_stats(src):
                    for hf in range(2):
                        k.op('dve', lambda e: e.bn_stats(st4[:, hf, :], src[:, hf * 512:(hf + 1) * 512]), reads=[src], writes=[st4])
                    k.op('dve', lambda e: e.bn_aggr(mv4[:], st4[:].rearrange("p a b -> p (a b)")), reads=[st4], writes=[mv4])
                    k.op('act', lambda e: e.activation(rs4[:], mv4[:, 1:2], AF.Sqrt, bias=LN_EPS), reads=[mv4], writes=[rs4])
                    k.op('dve', lambda e: e.reciprocal(rs4[:], rs4[:]), reads=[rs4], writes=[rs4])

                for b in range(NB):
                    for j, c0 in enumerate((2 * D, 4 * D, 3 * D)):
                        k.dma('sp', mb4[:, j, :], MODD[b:b + 1, c0:c0 + D].partition_broadcast(128), reads=[MODD], pw=[mb4])
                    k.op('dve', lambda e: e.tensor_scalar(mb4[:, 1, :], mb4[:, 1, :], 1.0, None, ALU.add), reads=[mb4], writes=[mb4])
                    for i in range(SEQ // 128):
                        gi = b * (SEQ // 128) + i
                        xb_, mb_ = x4[i % 2], mxb[i % 2]
                        k.dma('pool', mb_[:], MIX[b][i * 128:(i + 1) * 128, :], reads=[MIX[b]], writes=[mb_])
                        k.dma('sp', xb_[:], x_in[b, i * 128:(i + 1) * 128, :], writes=[xb_])
                        for kc in range(8):
                            k.op('pe', lambda e: e.transpose(ptm[:, kc, :], mb_[:, kc * 128:(kc + 1) * 128], identb[:]), reads=[mb_, identb], writes=[ptm], inc=(kc == 7))
                        k.op('act', lambda e: e.activation(mT[:], ptm[:], AF.Identity), reads=[ptm], writes=[mT])
                        for n in range(2):
                            for kc in range(8):
                                k.op('pe', lambda e: e.matmul(po4[n][:], mT[:, kc, :], wob[:, kc, n * 512:(n + 1) * 512], start=(kc == 0), stop=(kc == 7)),
                                     reads=[mT, wob], writes=[po4[n]] if kc == 0 else [], inc=(kc == 7))
                            k.op('dve', lambda e: e.tensor_tensor(t4[:, n * 512:(n + 1) * 512], po4[n][:], mb4[:, 0, n * 512:(n + 1) * 512], ALU.mult),
                                 reads=[po4[n], mb4], writes=[t4])
                        k.op('dve', lambda e: e.scalar_tensor_tensor(y4[:], xb_[:], ALPHA, t4[:], ALU.mult, ALU.add), reads=[xb_, t4], writes=[y4])
                        ln_stats(y4)
                        k.op('dve', lambda e: e.tensor_scalar(y4[:], y4[:], mv4[:, 0:1], rs4[:, 0:1], ALU.subtract, ALU.mult), reads=[y4, mv4, rs4], writes=[y4])
                        k.op('dve', lambda e: e.tensor_tensor(y4[:], y4[:], lnp[:, 0, :], ALU.mult), reads=[y4, lnp], writes=[y4])
                        k.op('dve', lambda e: e.tensor_tensor(y4[:], y4[:], lnp[:, 1, :], ALU.add), reads=[y4, lnp], writes=[y4])
                        k.dma('sp', X1[gi * 128:(gi + 1) * 128, :], y4[:], reads=[y4], pw=[X1])
                        ln_stats(y4)
                        k.op('dve', lambda e: e.tensor_scalar(h4[:], y4[:], mv4[:, 0:1], rs4[:, 0:1], ALU.subtract, ALU.mult), reads=[y4, mv4, rs4], writes=[h4])
                        k.op('dve', lambda e: e.tensor_tensor(h4[:], h4[:], mb4[:, 1, :], ALU.mult), reads=[h4, mb4], writes=[h4])
                        k.op('dve', lambda e: e.tensor_tensor(h4[:], h4[:], mb4[:, 2, :], ALU.add), reads=[h4, mb4], writes=[h4])
                        k.op('act', lambda e: e.activation(h4b[:], h4[:], AF.Identity), reads=[h4], writes=[h4b])
                        k.dma('sp', H2[gi * 128:(gi + 1) * 128, :], h4b[:], reads=[h4b], pw=[H2])
                        for kc in range(8):
                            k.op('pe', lambda e: e.transpose(pth[:, kc, :], h4[:, kc * 128:(kc + 1) * 128], ident[:]), reads=[h4, ident], writes=[pth], inc=(kc == 7))
                        k.op('act', lambda e: e.activation(h4T[:], pth[:], AF.Identity), reads=[pth], writes=[h4T])
                        for kc in range(8):
                            k.op('pe', lambda e: e.matmul(plg[:], h4T[:, kc, :], rt[:, kc, :], start=(kc == 0), stop=(kc == 7)),
                                 reads=[h4T, rt], writes=[plg] if kc == 0 else [], inc=(kc == 7))
                        k.op('dve', lambda e: e.tensor_tensor(LG[:, gi, :], plg[:], rtbb[:], ALU.add), reads=[plg, rtbb], writes=[LG])
            k.barrier()
            with ExitStack() as es5:
                k.es = es5
                if P4S < 1:
                    raise _Stop()
                gmx = k.sb("gmx", [128, NT], F32)
                goh = k.sb("goh", [128, NT, 4], F32)
                tg = k.sb("tg", [128, NT, 4], F32)
                ptop = k.sb("ptop", [128, NT], F32)
                lem = k.sb("lem", [128, NT, 32], F32)
                v1 = k.sb("v1", [128, NT], F32)
                v2 = k.sb("v2", [128, NT], F32)
                lgv = LG[:, :, 0:4]
                lev = LG[:, :, 4:36]
                k.op('dve', lambda e: e.tensor_reduce(gmx[:], lgv, AX.X, ALU.max), reads=[LG], writes=[gmx])
                k.op('dve', lambda e: e.tensor_tensor(goh[:], lgv, gmx[:].unsqueeze(2).to_broadcast([128, NT, 4]), ALU.is_equal), reads=[LG, gmx], writes=[goh])
                k.op('dve', lambda e: e.tensor_tensor(tg[:], lgv, gmx[:].unsqueeze(2).to_broadcast([128, NT, 4]), ALU.subtract), reads=[LG, gmx], writes=[tg])
                k.op('act', lambda e: e.activation(tg[:], tg[:], AF.Exp), reads=[tg], writes=[tg])
                k.op('dve', lambda e: e.tensor_reduce(ptop[:], tg[:], AX.X, ALU.add), reads=[tg], writes=[ptop])
                k.op('dve', lambda e: e.reciprocal(ptop[:], ptop[:]), reads=[ptop], writes=[ptop])
                k.op('dve', lambda e: e.tensor_scalar(goh[:], goh[:], -1.0, 1e30, ALU.add, ALU.mult), reads=[goh], writes=[goh])
                for g in range(4):
                    k.op('dve', lambda e: e.tensor_tensor(lem[:, :, g * 8:(g + 1) * 8], LG[:, :, 4 + g * 8:12 + g * 8],
                                                          goh[:, :, g:g + 1].to_broadcast([128, NT, 8]), ALU.add), reads=[LG, goh], writes=[lem])
                k.op('dve', lambda e: e.tensor_reduce(v1[:], lem[:], AX.X, ALU.max), reads=[lem], writes=[v1])
                k.op('dve', lambda e: e.tensor_tensor(OH1[:], lem[:], v1[:].unsqueeze(2).to_broadcast([128, NT, 32]), ALU.is_equal), reads=[lem, v1], writes=[OH1])
                k.op('dve', lambda e: e.scalar_tensor_tensor(lem[:], OH1[:], -1e30, lem[:], ALU.mult, ALU.add), reads=[OH1, lem], writes=[lem])
                k.op('dve', lambda e: e.tensor_reduce(v2[:], lem[:], AX.X, ALU.max), reads=[lem], writes=[v2])
                k.op('dve', lambda e: e.tensor_tensor(OH2[:], lem[:], v2[:].unsqueeze(2).to_broadcast([128, NT, 32]), ALU.is_equal), reads=[lem, v2], writes=[OH2])
                k.op('dve', lambda e: e.tensor_tensor(v2[:], v2[:], v1[:], ALU.subtract), reads=[v1, v2], writes=[v2])
                k.op('act', lambda e: e.activation(v2[:], v2[:], AF.Exp), reads=[v2], writes=[v2])
                k.op('dve', lambda e: e.tensor_scalar(v2[:], v2[:], 1.0, None, ALU.add), reads=[v2], writes=[v2])
                k.op('dve', lambda e: e.reciprocal(v2[:], v2[:]), reads=[v2], writes=[v2])
                k.op('dve', lambda e: e.tensor_tensor(W1[:], v2[:], ptop[:], ALU.mult), reads=[v2, ptop], writes=[W1])
                k.op('dve', lambda e: e.tensor_tensor(W2[:], ptop[:], W1[:], ALU.subtract), reads=[W1, ptop], writes=[W2])
            k.barrier()
            with ExitStack() as es6:
                k.es = es6
                if P4S < 2:
                    raise _Stop()
                OHb = k.sb("OHb", [128, NT, 32], BF16)
                triS = k.sb("triS", [128, 128], BF16)
                onb = k.sb("onb", [128, 128], BF16)
                thr = k.sb("thr", [128, 128], F32)
                blki = k.sb("blki", [128, NBLK], F32)
                kcp = k.sb("kcp", [128, 12], F32)
                cnt = k.sb("cnt", [128, 32], F32)
                big = k.sb("big", [128, 32, 128], F32)
                nbk = k.sb("nbk", [128, 32], F32)
                pend = k.sb("pend", [128, 32], F32)
                pst = k.sb("pst", [128, 32], F32)
                run = k.sb("run", [128, 32], F32)
                RK = k.sb("RK", [128, NT, 32], F32)
                dsf = k.sb("dsf", [128, NT, 2], F32)
                bexp = k.sb("bexp", [128, NBLK], F32)
                bigb = k.sb("bigb", [128, NBLK, 32], F32)
                widxf = k.sb("widxf", [128, NBLK, 12], F32)
                tokid = k.sb("tokid", [128, NT, 16], I32)
                zt = k.sb("zt", [128, 16], I32)
                pcn = k.ps("p5c", [128, 32])
                prk = k.ps("p5r", [128, 32])
                ptt = k.ps("p5t", [128, 32])
                stg = k.sb("stg", [128, 128], F32)
                k.dma('sp', stg[:], tris_in[:, :], writes=[stg])
                k.op('dve', lambda e: e.tensor_copy(triS[:], stg[:]), reads=[stg], writes=[triS])
                k.op('pool', lambda e: e.memset(onb[:], 1.0), writes=[onb])
                k.dma('sp', thr[:], thr_in[:, :], writes=[thr])
                k.dma('sp', blki[:], blki_in[:, 0:NBLK], writes=[blki])
                k.dma('sp', kcp[:], kcp_in[:, :], writes=[kcp])
                k.dma('sp', tokid[:], tokid_in[:, 0:NT, :], writes=[tokid])
                k.op('pool', lambda e: e.memset(zt[:], 0), writes=[zt])
                k.dma('sp', TOKB[:, :].rearrange("(b p) c -> p b c", p=128), zt[:].unsqueeze(1).to_broadcast([128, NBLK * SUB, 16]), reads=[zt], writes=[TOKB])
                k.op('dve', lambda e: e.tensor_tensor(OHb[:], OH1[:], OH2[:], ALU.add), reads=[OH1, OH2], writes=[OHb])
                for i in range(NT):
                    k.op('pe', lambda e: e.matmul(pcn[:], onb[:], OHb[:, i, :], start=(i == 0), stop=(i == NT - 1)), reads=[onb, OHb], writes=[pcn] if i == 0 else [], inc=(i == NT - 1))
                k.op('dve', lambda e: e.tensor_copy(cnt[:], pcn[:]), reads=[pcn], writes=[cnt])
                k.op('dve', lambda e: e.tensor_tensor(big[:], cnt[:].unsqueeze(2).to_broadcast([128, 32, 128]), thr[:].unsqueeze(1).to_broadcast([128, 32, 128]), ALU.is_gt),
                     reads=[cnt, thr], writes=[big])
                k.op('dve', lambda e: e.tensor_reduce(nbk[:], big[:], AX.X, ALU.add), reads=[big], writes=[nbk])
                k.op('pool', lambda e: e.memset(run[:], 1.0), writes=[run])
                k.op('dve', lambda e: e.tensor_tensor_scan(pend[:], run[:], nbk[:], 0.0, ALU.mult, ALU.add), reads=[run, nbk], writes=[pend])
                k.op('dve', lambda e: e.tensor_tensor(pst[:], pend[:], nbk[:], ALU.subtract), reads=[pend, nbk], writes=[pst])
                k.op('dve', lambda e: e.tensor_scalar(pst[:], pst[:], float(BS), None, ALU.mult), reads=[pst], writes=[pst])
                k.op('pool', lambda e: e.memset(run[:], 0.0), reads=[run], writes=[run])
                for i in range(NT):
                    k.op('pe', lambda e: e.matmul(prk[:], triS[:], OHb[:, i, :], start=True, stop=True), reads=[triS, OHb], writes=[prk])
                    k.op('pe', lambda e: e.matmul(ptt[:], onb[:], OHb[:, i, :], start=True, stop=True), reads=[onb, OHb], writes=[ptt])
                    k.op('dve', lambda e: e.tensor_tensor(RK[:, i, :], prk[:], run[:], ALU.add), reads=[prk, run], writes=[RK])
                    k.op('dve', lambda e: e.tensor_tensor(run[:], run[:], ptt[:], ALU.add), reads=[run, ptt], writes=[run])
                k.op('dve', lambda e: e.tensor_tensor(RK[:], RK[:], pst[:].unsqueeze(1).to_broadcast([128, NT, 32]), ALU.add), reads=[RK, pst], writes=[RK])
                for j, OH in enumerate((OH1, OH2)):
                    k.op('dve', lambda e: e.tensor_tensor(OH[:], OH[:], RK[:], ALU.mult), reads=[OH, RK], writes=[OH])
                    k.op('dve', lambda e: e.tensor_reduce(dsf[:, :, j], OH[:], AX.X, ALU.add), reads=[OH], writes=[dsf])
                k.op('dve', lambda e: e.tensor_copy(DST[:], dsf[:]), reads=[dsf], writes=[DST])
                k.op('dve', lambda e: e.tensor_tensor(bigb[:], pend[:].unsqueeze(1).to_broadcast([128, NBLK, 32]), blki[:].unsqueeze(2).to_broadcast([128, NBLK, 32]), ALU.is_le),
                     reads=[pend, blki], writes=[bigb])
                k.op('dve', lambda e: e.tensor_reduce(bexp[:], bigb[:], AX.X, ALU.add), reads=[bigb], writes=[bexp])
                k.op('dve', lambda e: e.tensor_scalar(bexp[:], bexp[:], 31.0, None, ALU.min), reads=[bexp], writes=[bexp])
                k.op('dve', lambda e: e.tensor_scalar(widxf[:, :, 0:8], bexp[:].unsqueeze(2).to_broadcast([128, NBLK, 8]), 256.0, None, ALU.mult), reads=[bexp], writes=[widxf])
                k.op('dve', lambda e: e.tensor_scalar(widxf[:, :, 8:12], bexp[:].unsqueeze(2).to_broadcast([128, NBLK, 4]), 256.0, None, ALU.mult), reads=[bexp], writes=[widxf])
                k.op('dve', lambda e: e.tensor_tensor(widxf[:], widxf[:], kcp[:].unsqueeze(1).to_broadcast([128, NBLK, 12]), ALU.add), reads=[widxf, kcp], writes=[widxf])
                k.op('dve', lambda e: e.tensor_copy(WIDX[:], widxf[:]), reads=[widxf], writes=[WIDX])
                for i in range(NT):
                    for j in range(2):
                        k.dma('pool', TOKB[:, :], tokid[:, i, :], reads=[tokid, DST], pw=[TOKB],
                              indirect=(bass.IndirectOffsetOnAxis(ap=DST[:, i, j:j + 1], axis=0), None))
            k.barrier()
            with ExitStack() as es7:
                k.es = es7
                if P4S < 3:
                    raise _Stop()
                wg = [k.sb("wg%d" % i, [128, 8, 512], BF16) for i in range(3)]
                wu = [k.sb("wu%d" % i, [128, 8, 512], BF16) for i in range(3)]
                wd = [k.sb("wd%d" % i, [128, 4, D], BF16) for i in range(3)]
                class _E:
                    pass
                ES = []
                for si in range(2):
                    E = _E()
                    E.tki = k.sb("tki", [128, 16], I32)
                    E.xg = k.sb("xg", [128, D], BF16)
                    E.xgT = k.sb("xgT", [128, 8, 128], BF16)
                    E.gs = k.sb("gs", [128, 512], F32)
                    E.hb = k.sb("hb", [128, 512], BF16)
                    E.hbT = k.sb("hbT", [128, 4, 128], BF16)
                    E.yb = k.sb("yb", [128, D], F32)
                    E.bG = k.ps("p6g", [128, 512])
                    E.bU = k.ps("p6u", [128, 512])
                    E.bY = k.ps("p6y", [128, 512])
                    ES.append(E)

                def load_w(bk):
                    q = bk % 3
                    for hf in range(2):
                        ix = bass.IndirectOffsetOnAxis(ap=WIDX[:, bk, hf:hf + 1], axis=0)
                        k.dma('pool', wg[q][:, hf * 4:(hf + 1) * 4, :].rearrange("p a b -> p (a b)"), ex_gate[:, :], reads=[WIDX], pw=[wg[q]], indirect=(None, ix))
                        k.dma('pool', wu[q][:, hf * 4:(hf + 1) * 4, :].rearrange("p a b -> p (a b)"), ex_up[:, :], reads=[WIDX], pw=[wu[q]], indirect=(None, ix))
                        k.dma('pool', wd[q][:, hf * 2:(hf + 1) * 2, :].rearrange("p a b -> p (a b)"), ex_down[:, :], reads=[WIDX], pw=[wd[q]], indirect=(None, ix))

                def subtile(E, bk, sub):
                    q = bk % 3
                    r0 = (bk * SUB + sub) * 128
                    k.dma('sp', E.tki[:], TOKB[r0:r0 + 128, :], reads=[TOKB], writes=[E.tki])
                    k.dma('pool', E.xg[:], H2[:, :], reads=[H2, E.tki], writes=[E.xg],
                          indirect=(None, bass.IndirectOffsetOnAxis(ap=E.tki[:, 0:1], axis=0)))
                    yield
                    pxt = E.bU[:].bitcast(BF16)
                    xv = E.xg[:].rearrange("p (j kc) -> p kc j", kc=8)
                    for kc in range(8):
                        k.op('pe', lambda e: e.transpose(pxt[:, kc * 128:(kc + 1) * 128], xv[:, kc, :], identb[:]), reads=[E.xg, identb], writes=[E.bU] if kc == 0 else [], inc=(kc == 7))
                    k.op('act', lambda e: e.activation(E.xgT[:].rearrange("p a b -> p (a b)"), pxt, AF.Identity), reads=[E.bU], writes=[E.xgT])
                    yield
                    for kc in range(8):
                        k.op('pe', lambda e: e.matmul(E.bG[:], E.xgT[:, kc, :], wg[q][:, kc, :], start=(kc == 0), stop=(kc == 7)), reads=[E.xgT, wg[q]], writes=[E.bG] if kc == 0 else [], inc=(kc == 7))
                    for kc in range(8):
                        k.op('pe', lambda e: e.matmul(E.bU[:], E.xgT[:, kc, :], wu[q][:, kc, :], start=(kc == 0), stop=(kc == 7)), reads=[E.xgT, wu[q]], writes=[E.bU] if kc == 0 else [], inc=(kc == 7))
                    yield
                    k.op('act', lambda e: e.activation(E.gs[:], E.bG[:], AF.Silu), reads=[E.bG], writes=[E.gs])
                    k.op('dve', lambda e: e.tensor_tensor(E.hb[:], E.gs[:], E.bU[:], ALU.mult), reads=[E.gs, E.bU], writes=[E.hb])
                    yield
                    pht = E.bG[:].bitcast(BF16)
                    hv = E.hb[:].rearrange("p (j fc) -> p fc j", fc=4)
                    for fc in range(4):
                        k.op('pe', lambda e: e.transpose(pht[:, fc * 128:(fc + 1) * 128], hv[:, fc, :], identb[:]), reads=[E.hb, identb], writes=[E.bG] if fc == 0 else [], inc=(fc == 3))
                    k.op('dve', lambda e: e.tensor_copy(E.hbT[:].rearrange("p a b -> p (a b)"), pht[:, 0:512]), reads=[E.bG], writes=[E.hbT])
                    yield
                    for n in range(2):
                        for fc in range(4):
                            k.op('pe', lambda e: e.matmul(E.bY[:], E.hbT[:, fc, :], wd[q][:, fc, n * 512:(n + 1) * 512], start=(fc == 0), stop=(fc == 3)),
                                 reads=[E.hbT, wd[q]], writes=[E.bY] if fc == 0 else [], inc=(fc == 3))
                        k.op('act', lambda e: e.activation(E.yb[:, n * 512:(n + 1) * 512], E.bY[:], AF.Identity), reads=[E.bY], writes=[E.yb])
                        yield
                    k.dma('sp', YB[r0:r0 + 128, :], E.yb[:], reads=[E.yb], pw=[YB])

                def estream(si):
                    for gsub in range(si, NBLK * SUB, 2):
                        bk, sub = gsub // SUB, gsub % SUB
                        if sub == 0 or (sub == 1 and si == 1):
                            for bb in (bk, bk + 1):
                                if bb < NBLK and bb not in loaded:
                                    loaded.add(bb)
                                    load_w(bb)
                        yield from subtile(ES[si], bk, sub)
                        yield

                loaded = set()
                gens = [estream(0), estream(1)]
                alive = [True, True]
                while any(alive):
                    for gi in range(2):
                        if alive[gi]:
                            try:
                                next(gens[gi])
                            except StopIteration:
                                alive[gi] = False
            k.barrier()
            with ExitStack() as es8:
                k.es = es8
                if P4S < 4:
                    raise _Stop()
                x6 = [k.sb("x6%d" % i, [128, D], F32) for i in range(2)]
                y0 = [k.sb("y0%d" % i, [128, D], F32) for i in range(2)]
                y1 = [k.sb("y1%d" % i, [128, D], F32) for i in range(2)]
                o6 = [k.sb("o6%d" % i, [128, D], F32) for i in range(2)]
                st6 = k.sb("st6", [128, 2, 6], F32)
                mv6 = k.sb("mv6", [128, 2], F32)
                rs6 = k.sb("rs6", [128, 1], F32)
                for gi in range(NT):
                    b, i = gi // (SEQ // 128), gi % (SEQ // 128)
                    q = gi % 2
                    k.dma('sp', x6[q][:], X1[gi * 128:(gi + 1) * 128, :], reads=[X1], writes=[x6[q]])
                    k.dma('pool', y0[q][:], YB[:, :], reads=[YB, DST], writes=[y0[q]],
                          indirect=(None, bass.IndirectOffsetOnAxis(ap=DST[:, gi, 0:1], axis=0)))
                    k.dma('pool', y1[q][:], YB[:, :], reads=[YB, DST], writes=[y1[q]],
                          indirect=(None, bass.IndirectOffsetOnAxis(ap=DST[:, gi, 1:2], axis=0)))
                    o = o6[q]
                    k.op('dve', lambda e: e.tensor_scalar(y0[q][:], y0[q][:], W1[:, gi:gi + 1], None, ALU.mult), reads=[y0[q], W1], writes=[y0[q]])
                    k.op('dve', lambda e: e.scalar_tensor_tensor(y0[q][:], y1[q][:], W2[:, gi:gi + 1], y0[q][:], ALU.mult, ALU.add), reads=[y1[q], W2, y0[q]], writes=[y0[q]])
                    k.op('dve', lambda e: e.tensor_tensor(y0[q][:], y0[q][:], g2b[:, b, :], ALU.mult), reads=[y0[q], g2b], writes=[y0[q]])
                    k.op('dve', lambda e: e.scalar_tensor_tensor(o[:], x6[q][:], ALPHA, y0[q][:], ALU.mult, ALU.add), reads=[x6[q], y0[q]], writes=[o])
                    for hf in range(2):
                        k.op('dve', lambda e: e.bn_stats(st6[:, hf, :], o[:, hf * 512:(hf + 1) * 512]), reads=[o], writes=[st6])
                    k.op('dve', lambda e: e.bn_aggr(mv6[:], st6[:].rearrange("p a b -> p (a b)")), reads=[st6], writes=[mv6])
                    k.op('act', lambda e: e.activation(rs6[:], mv6[:, 1:2], AF.Sqrt, bias=LN_EPS), reads=[mv6], writes=[rs6])
                    k.op('dve', lambda e: e.reciprocal(rs6[:], rs6[:]), reads=[rs6], writes=[rs6])
                    k.op('dve', lambda e: e.tensor_scalar(o[:], o[:], mv6[:, 0:1], rs6[:, 0:1], ALU.subtract, ALU.mult), reads=[o, mv6, rs6], writes=[o])
                    k.op('dve', lambda e: e.tensor_tensor(o[:], o[:], lnp[:, 2, :], ALU.mult), reads=[o, lnp], writes=[o])
                    k.op('dve', lambda e: e.tensor_tensor(o[:], o[:], lnp[:, 3, :], ALU.add), reads=[o, lnp], writes=[o])
                    k.dma('sp', out_d[b, i * 128:(i + 1) * 128, :], o[:], reads=[o])
           except _Stop:
            pass
        k.barrier()

        k.barrier()
    return nc


def host_inputs(inputs, batches, NB):
    f = lambda a: np.ascontiguousarray(a, dtype=np.float32)
    bs = list(batches)
    m = {}
    m["x"] = f(inputs["x"][bs])
    m["ctx"] = f(inputs["ctx"][bs])
    cc = np.zeros((3, D), np.float32)
    for i, b in enumerate(bs):
        cc[i] = inputs["c"][b]
    cc[2] = inputs["c_ctx"]
    m["cc"] = cc
    m["w_ada"] = f(inputs["w_ada"][0])
    m["b_ada"] = f(inputs["b_ada"][0][None, :])
    m["w_in"] = f(inputs["w_in"][0])
    m["conv_w"] = f(inputs["conv_w"][0].reshape(9, 2560))
    bi, bf = inputs["m_bias_i"][0], inputs["m_bias_f"][0]
    m["m_bias"] = f(np.concatenate([bi[0], bf[0], bi[1], bf[1]])[:, None])
    m["ident"] = np.eye(128, dtype=np.float32)
    gm = np.zeros((32, 2), np.float32)
    gm[0:8, 0] = 1; gm[16:24, 0] = 1; gm[8:16, 1] = -1; gm[24:32, 1] = -1
    m["gmask"] = gm
    ii = np.arange(64)
    m["cmask"] = np.stack([(ii[:, None] <= ii[None, :]), (ii[:, None] >= ii[None, :])], axis=1).astype(np.float32)
    m["m_norm_w"] = f(inputs["m_norm_w"][0][None, :])
    hk = lambda v: np.asarray(v, np.float32).reshape(4, 128).T
    m["rp"] = f(np.stack([hk(inputs["r_w0"][0][0]), hk(inputs["r_w0"][0][1]), hk(inputs["r_a0"][0]), hk(inputs["r_kk"][0]),
                          hk(inputs["r_ka"][0]), hk(inputs["r_ka"][0]), hk(inputs["r_bonus"][0].reshape(-1))], axis=2))
    sm = np.ones((128, 1088), np.float32); sm[:, ::64] = 0
    obd = np.zeros((128, 128), np.float32); obd[:64, :64] = 1; obd[64:, 64:] = 1
    m["onesbd"] = obd
    m["smask"] = sm
    jj = np.arange(128) % 64
    tt = np.arange(128)
    rm = np.zeros((128, 2, 128), np.float32)
    for dd in range(2):
        for col in range(128):
            tq = col % 64
            if col < 64:
                rm[:, dd, col] = (jj < tq) if dd == 0 else (jj > tq)
            else:
                rm[:, dd, col] = (jj <= tq) if dd == 0 else (jj >= tq)
    m["rmask"] = rm
    m["nmask"] = np.stack([(ii[None, :] < ii[:, None]), (ii[None, :] > ii[:, None])], axis=1).astype(np.float32)
    m["r_norm_w"] = f(inputs["r_norm_w"][0][None, :])
    m["r_norm_b"] = f(inputs["r_norm_b"][0][None, :])
    m["r_wB"] = f(inputs["r_wB"][0])
    m["r_aB"] = f(inputs["r_aB"][0])
    m["r_gB"] = f(inputs["r_gB"][0])
    m["w_out"] = f(inputs["w_out"][0])
    for nm in ("ln1_g", "ln1_b", "ln2_g", "ln2_b"):
        m[nm] = f(inputs[nm][0][None, :])
    m["rt"] = f(np.concatenate([inputs["rt_g"][0], inputs["rt_e"][0]], axis=1))
    m["rtb"] = f(np.concatenate([inputs["rt_g_b"][0], inputs["rt_e_b"][0]])[None, :])
    m["ex_gate"] = f(inputs["ex_gate"][0].reshape(8192, 2048))
    m["ex_up"] = f(inputs["ex_up"][0].reshape(8192, 2048))
    m["ex_down"] = f(inputs["ex_down"][0].reshape(8192, 2048))
    pp = np.arange(128)
    m["tris"] = (pp[:, None] < pp[None, :]).astype(np.float32)
    m["thr"] = np.broadcast_to((512.0 * pp)[None, :], (128, 128)).astype(np.float32).copy()
    m["blki"] = np.broadcast_to(np.arange(160, dtype=np.float32)[None, :], (128, 160)).copy()
    m["kcp"] = (2 * pp[:, None] + (np.arange(12) % 2)[None, :]).astype(np.float32)
    m["tokid"] = np.broadcast_to((np.arange(64)[None, :] * 128 + pp[:, None])[:, :, None], (128, 64, 16)).astype(np.int32).copy()
    return m


_NC_CACHE = {}


def kernel(**inputs):
    inputs = {k_: np.asarray(v) for k_, v in inputs.items()}
    NB = 2
    n_cores = 8
    if NB not in _NC_CACHE:
        _NC_CACHE[NB] = build(NB=NB)
    nc = _NC_CACHE[NB]
    in_maps = [host_inputs(inputs, [NB * c + j for j in range(NB)], NB) for c in range(n_cores)]
    res = run_bass_kernel_spmd(nc, in_maps, core_ids=list(range(n_cores)))
    out = np.concatenate([np.asarray(r["out"]) for r in res.results], axis=0)
    return np.ascontiguousarray(out, dtype=np.float32)
```

```python
import math, os
from contextlib import ExitStack
import numpy as np
import concourse.bass as bass
import concourse.mybir as mybir
from concourse.bass_utils import run_bass_kernel_spmd

F32 = mybir.dt.float32
BF16 = mybir.dt.bfloat16
I32 = mybir.dt.int32
AF = mybir.ActivationFunctionType
ALU = mybir.AluOpType
AX = mybir.AxisListType

D = 1024
SEQ = 4096
CTX = 256
T = SEQ + CTX
NCH = T // 64
INC = 3936
DS = math.exp(-0.5)
ALPHA = 2.0 ** 0.25
LN_EPS = 1e-6
GN_EPS = 64e-5
SEC = dict(mq=0, mk=512, rr=1024, rk=1536, rv=2048, mv=2560, mo=3072, gates=3584,
           lwf=3616, lwb=3680, la=3744, lg=3808)
NDS = 40


class _Stop(Exception):
    pass


class Buf:
    def __init__(self, t):
        self.t = t
        self.w = {}
        self.r = {}

    def __getitem__(self, k):
        return self.t[k]


def _merge(d, s):
    for k, v in s.items():
        if d.get(k, 0) < v:
            d[k] = v


class KB:
    def __init__(self, nc):
        self.nc = nc
        self.engs = {'pe': nc.tensor, 'dve': nc.vector, 'act': nc.scalar, 'pool': nc.gpsimd, 'sp': nc.sync}
        self.esem = {e: nc.alloc_semaphore('es_' + e) for e in self.engs}
        self.ecnt = {e: 0 for e in self.engs}
        self.pending = {e: False for e in self.engs}
        self.seen = {e: {} for e in self.engs}
        self.dsem = [nc.alloc_semaphore('ds%d' % i) for i in range(NDS)]
        self.dcnt = [0] * NDS
        self.dnext = 0
        self.es = None
        self.uid = 0

    def semh(self, key):
        return self.esem[key] if isinstance(key, str) else self.dsem[key[1]]

    def _wait(self, eng, need):
        for key, cnt in need.items():
            if self.seen[eng].get(key, 0) >= cnt:
                continue
            if key == eng and eng in ('pe',):
                continue
            self.engs[eng].wait_ge(self.semh(key), cnt)
            self.seen[eng][key] = cnt

    def op(self, eng, fn, reads=(), writes=(), inc=True, pw_=()):
        need = {}
        for b in pw_:
            _merge(need, b.r)
        for b in reads:
            _merge(need, b.w)
            if getattr(b, 'excl', False):
                _merge(need, {kk: vv for kk, vv in b.r.items() if kk != eng})
        for b in writes:
            _merge(need, b.w)
            _merge(need, b.r)
        self._wait(eng, need)
        ins = fn(self.engs[eng])
        cnt = self.ecnt[eng] + 1
        if inc:
            ins.then_inc(self.esem[eng], 1)
            self.ecnt[eng] = cnt
        for b in reads:
            b.r[eng] = cnt
        for b in writes:
            b.w = {eng: cnt}
            b.r = {}
        for b in pw_:
            b.w[eng] = cnt
        return ins

    def dma(self, q, out, in_, reads=(), writes=(), pw=(), indirect=None, **kw):
        i = self.dnext
        self.dnext = (i + 1) % NDS
        need = {}
        if self.dcnt[i]:
            need[('d', i)] = self.dcnt[i]
        for b in reads:
            _merge(need, b.w)
        for b in writes:
            _merge(need, b.w)
            _merge(need, b.r)
        for b in pw:
            _merge(need, b.r)
        self._wait(q, need)
        if indirect is None:
            ins = self.engs[q].dma_start(out=out, in_=in_, **kw)
        else:
            ins = self.engs[q].indirect_dma_start(out, indirect[0], in_, indirect[1], **kw)
        self.dcnt[i] += 16
        ins.then_inc(self.dsem[i], 16)
        key = ('d', i)
        cnt = self.dcnt[i]
        for b in reads:
            b.r[key] = cnt
        for b in writes:
            b.w = {key: cnt}
            b.r = {}
        for b in pw:
            b.w[key] = cnt
        return ins

    def barrier(self):
        need = {e: c for e, c in self.ecnt.items() if c}
        for i in range(NDS):
            if self.dcnt[i]:
                need[('d', i)] = self.dcnt[i]
        for e in self.engs:
            self._wait(e, need)

    def sb(self, name, shape, dt):
        self.uid += 1
        return Buf(self.es.enter_context(self.nc.sbuf_tensor("s%d_%s" % (self.uid, name), list(shape), dt)))

    def ps(self, name, shape, dt=F32):
        self.uid += 1
        return Buf(self.es.enter_context(self.nc.psum_tensor("p%d_%s" % (self.uid, name), list(shape), dt)))


def build(NB=2, debug=None, phases=(0, 1, 2, 3, 4, 5, 6)):
    nc = bass.Bass("TRN2", target_bir_lowering=False)
    k = KB(nc)

    def din(name, shape):
        return nc.dram_tensor(name, list(shape), F32, kind="ExternalInput").ap()

    x_in = din("x", [NB, SEQ, D])
    ctx_in = din("ctx", [NB, CTX, D])
    cc_in = din("cc", [3, D])
    w_ada = din("w_ada", [D, 6 * D])
    b_ada = din("b_ada", [1, 6 * D])
    w_in = din("w_in", [D, INC])
    conv_w = din("conv_w", [9, 2560])
    m_bias = din("m_bias", [32, 1])
    ident_in = din("ident", [128, 128])
    gmask_in = din("gmask", [32, 2])
    cmask_in = din("cmask", [64, 2, 64])
    m_norm_w = din("m_norm_w", [1, 512])
    rp_in = din("rp", [128, 4, 7])
    smask_in = din("smask", [128, 1088])
    onesbd_in = din("onesbd", [128, 128])
    rmask_in = din("rmask", [128, 2, 128])
    nmask_in = din("nmask", [64, 2, 64])
    r_norm_w = din("r_norm_w", [1, 512])
    r_norm_b = din("r_norm_b", [1, 512])
    r_wB = din("r_wB", [2, 64, 512])
    r_aB = din("r_aB", [64, 512])
    r_gB = din("r_gB", [128, 512])
    w_out = din("w_out", [D, D])
    ln1_g = din("ln1_g", [1, D]); ln1_b = din("ln1_b", [1, D]); ln2_g = din("ln2_g", [1, D]); ln2_b = din("ln2_b", [1, D])
    rt_in = din("rt", [D, 36]); rtb_in = din("rtb", [1, 36])
    ex_gate = din("ex_gate", [8192, 2048]); ex_up = din("ex_up", [8192, 2048]); ex_down = din("ex_down", [8192, 2048])
    tris_in = din("tris", [128, 128]); thr_in = din("thr", [128, 128]); blki_in = din("blki", [128, 160]); kcp_in = din("kcp", [128, 12])
    tokid_in = nc.dram_tensor("tokid", [128, 64, 16], I32, kind="ExternalInput").ap()
    out_d = nc.dram_tensor("out", [NB, SEQ, D], F32, kind="ExternalOutput").ap()

    def dscr(name, shape, dt=F32):
        kind = "ExternalOutput" if (debug and name in debug) else "Internal"
        return Buf(nc.dram_tensor(name, list(shape), dt, kind=kind).ap())

    MODD = dscr("MODD", [3, 6 * D])
    FM = [dscr("FM%d" % b, [INC, T]) for b in range(NB)]
    MIX = [dscr("MIX%d" % b, [SEQ, D]) for b in range(NB)]
    BS = 512
    NBLK_ = NB * SEQ * 2 // BS + 32
    X1 = dscr("X1", [NB * SEQ, D])
    H2 = dscr("H2", [NB * SEQ, D], BF16)
    TOKB = dscr("TOKB", [NBLK_ * BS, 16], I32)
    YB = dscr("YB", [NBLK_ * BS, D])

    with ExitStack() as es0:
        k.es = es0
        ident = k.sb("ident", [128, 128], F32)
        identb = k.sb("identb", [128, 128], BF16)
        ones_f = k.sb("ones_f", [128, 128], F32)
        modT = k.sb("modT", [128, 48, 3], F32)
        k.dma('sp', ident[:], ident_in[:, :], writes=[ident])
        k.op('dve', lambda e: e.tensor_copy(identb[:], ident[:]), reads=[ident], writes=[identb])

        with ExitStack() as es:
            k.es = es
            cc = k.sb("cc", [3, D], F32)
            scT = k.sb("scT", [128, 8, 3], F32)
            bada = k.sb("bada", [3, 6 * D], F32)
            mods = k.sb("mods", [3, 6 * D], F32)
            wa = [k.sb("wa%d" % i, [128, 8, 512], F32) for i in range(2)]
            pst = k.ps("p0t", [128, 8, 3])
            psm = [k.ps("p0m%d" % i, [3, 512]) for i in range(2)]
            pmt = k.ps("p0mt", [128, 48, 3])
            k.dma('sp', cc[:], cc_in[:, :], writes=[cc])
            k.dma('sp', bada[:], b_ada[0:1, :].partition_broadcast(3), writes=[bada])
            k.op('act', lambda e: e.activation(cc[:], cc[:], AF.Silu), reads=[cc], writes=[cc])
            for kc in range(8):
                k.op('pe', lambda e: e.transpose(pst[:, kc, :], cc[:, kc * 128:(kc + 1) * 128], ident[0:3, 0:3]),
                     reads=[cc, ident], writes=[pst], inc=(kc == 7))
            k.op('dve', lambda e: e.tensor_copy(scT[:], pst[:]), reads=[pst], writes=[scT])
            for n in range(12):
                wb = wa[n % 2]
                k.dma('sp', wb[:], w_ada[:, n * 512:(n + 1) * 512].rearrange("(kc p) n -> p kc n", p=128), writes=[wb])
                pm = psm[n % 2]
                for kc in range(8):
                    k.op('pe', lambda e: e.matmul(pm[:], scT[:, kc, :], wb[:, kc, :], start=(kc == 0), stop=(kc == 7)),
                         reads=[scT, wb], writes=[pm] if kc == 0 else [], inc=(kc == 7))
                k.op('dve', lambda e: e.tensor_tensor(mods[:, n * 512:(n + 1) * 512], pm[:], bada[:, n * 512:(n + 1) * 512], ALU.add),
                     reads=[pm, bada], writes=[mods])
            k.dma('sp', MODD[:, :], mods[:], reads=[mods], writes=[MODD])
            for j in range(48):
                k.op('pe', lambda e: e.transpose(pmt[:, j, :], mods[:, j * 128:(j + 1) * 128], ident[0:3, 0:3]),
                     reads=[mods, ident], writes=[pmt], inc=(j == 47))
            k.op('dve', lambda e: e.tensor_copy(modT[:], pmt[:]), reads=[pmt], writes=[modT])
            for j0 in (8, 32):
                k.op('dve', lambda e: e.tensor_scalar(modT[:, j0:j0 + 8, :], modT[:, j0:j0 + 8, :], 1.0, None, ALU.add),
                     reads=[modT], writes=[modT])
        k.barrier()

        with ExitStack() as es:
          if 1 in phases:
              k.es = es
              wbf = k.sb("wbf", [128, 8, INC], BF16)
              hT = k.sb("hT", [128, 8, T], BF16)
              xt = [k.sb("xt%d" % i, [128, D], F32) for i in range(2)]
              xn = [k.sb("xn%d" % i, [128, D], BF16) for i in range(2)]
              st = k.sb("st", [128, 2, 6], F32)
              mv = k.sb("mv", [128, 2], F32)
              rstd = k.sb("rstd", [128, 1], F32)
              pT = [k.sb("pT%d" % i, [128, T], F32) for i in range(2)]
              acc = k.sb("acc", [128, T], F32)
              cw = k.sb("cw", [128, 20, 9], F32)
              mb = k.sb("mb", [32, 4], F32)
              ptr = [k.ps("p1t%d" % i, [128, 8, 128], BF16) for i in range(2)]
              pmm = [k.ps("p1m%d" % i, [128, 512]) for i in range(3)]
              pcw = k.ps("p1cw", [128, 20, 9])
              for kc in range(8):
                  for hf in range(2):
                      k.dma('pool', wbf[:, kc, hf * 1968:(hf + 1) * 1968],
                            w_in[kc * 128:(kc + 1) * 128, hf * 1968:(hf + 1) * 1968], pw=[wbf])
              crow = k.sb("crow", [9, 2560], F32)
              k.dma('sp', crow[:], conv_w[:, :], writes=[crow])
              for c in range(20):
                  k.op('pe', lambda e: e.transpose(pcw[:, c, :], crow[:, c * 128:(c + 1) * 128], ident[0:9, 0:9]),
                       reads=[crow, ident], writes=[pcw], inc=(c == 19))
              k.op('dve', lambda e: e.tensor_copy(cw[:], pcw[:]), reads=[pcw], writes=[cw])
              k.dma('sp', mb[:, 0:1], m_bias[:, :], writes=[mb])
              k.op('dve', lambda e: e.tensor_scalar(mb[:, 1:2], mb[:, 0:1], -1.0, None, ALU.mult), reads=[mb], writes=[mb])
              k.dma('sp', mb[:, 2:4], gmask_in[:, :], pw=[mb])
              for b in range(NB):
                  for i in range(int(os.environ.get('P1A', T // 128))):
                      xb, xnb, pt = xt[i % 2], xn[i % 2], ptr[i % 2]
                      src = ctx_in[b, i * 128:(i + 1) * 128, :] if i < 2 else x_in[b, (i - 2) * 128:(i - 1) * 128, :]
                      r = 2 if i < 2 else b
                      k.dma('sp', xb[:], src, writes=[xb])
                      S1 = int(os.environ.get('P1S', 9))
                      for hf in range(2):
                          k.op('dve', lambda e: e.bn_stats(st[:, hf, :], xb[:, hf * 512:(hf + 1) * 512]), reads=[xb], writes=[st])
                      if S1 >= 2: k.op('dve', lambda e: e.bn_aggr(mv[:], st[:].rearrange("p a b -> p (a b)")), reads=[st], writes=[mv])
                      if S1 >= 3: k.op('act', lambda e: e.activation(rstd[:], mv[:, 1:2], AF.Sqrt, bias=LN_EPS), reads=[mv], writes=[rstd])
                      if S1 >= 4: k.op('dve', lambda e: e.reciprocal(rstd[:], rstd[:]), reads=[rstd], writes=[rstd])
                      if S1 >= 5: k.op('dve', lambda e: e.tensor_scalar(xnb[:], xb[:], mv[:, 0:1], rstd[:, 0:1], ALU.subtract, ALU.mult),
                           reads=[xb, mv, rstd], writes=[xnb])
                      for kc in range(8 if S1 >= 6 else 0):
                          k.op('pe', lambda e: e.transpose(pt[:, kc, :], xnb[:, kc * 128:(kc + 1) * 128], identb[:]),
                               reads=[xnb, identb], writes=[pt] if kc == 0 else [], inc=(kc == 7))
                      for kc in range(8 if S1 >= 7 else 0):
                          if i % 2 == 0:
                              k.op('act', lambda e: e.activation(hT[:, kc, i * 128:(i + 1) * 128], pt[:, kc, :], AF.Identity,
                                                                 bias=modT[:, kc, r:r + 1], scale=modT[:, 8 + kc, r:r + 1]),
                                   reads=[pt, modT], writes=[] if (i or kc) else [hT])
                          else:
                              k.op('dve', lambda e: e.tensor_scalar(hT[:, kc, i * 128:(i + 1) * 128], pt[:, kc, :],
                                                                    modT[:, 8 + kc, r:r + 1], modT[:, kc, r:r + 1], ALU.mult, ALU.add),
                                   reads=[pt, modT], writes=[])
                      hT.w['act'] = k.ecnt['act']
                      hT.w['dve'] = k.ecnt['dve']
                  chunks = [(c * 128, 128) for c in range(28)] + [(3584, 32), (3616, 64), (3680, 64), (3744, 64), (3808, 128)]
                  for ci, (c0, M) in enumerate(chunks[:int(os.environ.get('P1C', 99))]):
                      pb = pT[ci % 2]
                      for g in range(9):
                          t0 = g * 512
                          n = min(512, T - t0)
                          pm = pmm[(ci * 9 + g) % 3]
                          for kc in range(8):
                              k.op('pe', lambda e: e.matmul(pm[0:M, 0:n], wbf[:, kc, c0:c0 + M], hT[:, kc, t0:t0 + n],
                                                            start=(kc == 0), stop=(kc == 7)),
                                   reads=[wbf, hT], writes=[pm] if kc == 0 else [], inc=(kc == 7))
                          k.op('act', lambda e: e.activation(pb[0:M, t0:t0 + n], pm[0:M, 0:n], AF.Identity),
                               reads=[pm], writes=[pb] if g == 0 else [])
                          pb.w['act'] = k.ecnt['act']
                      src = pb
                      if c0 < 2560:
                          c = c0 // 128
                          k.op('act', lambda e: e.activation(acc[:, :], pb[:, :], AF.Identity, scale=cw[:, c, 4:5]),
                               reads=[pb, cw], writes=[acc])
                          k.op('dve', lambda e: e.scalar_tensor_tensor(acc[:, 1:CTX], pb[:, 0:CTX - 1], cw[:, c, 3:4], acc[:, 1:CTX], ALU.mult, ALU.add),
                               reads=[pb, cw, acc], writes=[acc])
                          k.op('dve', lambda e: e.scalar_tensor_tensor(acc[:, 0:CTX - 1], pb[:, 1:CTX], cw[:, c, 5:6], acc[:, 0:CTX - 1], ALU.mult, ALU.add),
                               reads=[pb, cw, acc], writes=[acc])
                          a3 = acc[:, CTX:T].rearrange("p (r c) -> p r c", c=64)
                          p3 = pb[:, CTX:T].rearrange("p (r c) -> p r c", c=64)
                          for ky in range(3):
                              for kx in range(3):
                                  if ky == 1 and kx == 1:
                                      continue
                                  dy, dx = ky - 1, kx - 1
                                  oy0, oy1 = max(0, -dy), 64 - max(0, dy)
                                  ox0, ox1 = max(0, -dx), 64 - max(0, dx)
                                  k.op('dve', lambda e: e.scalar_tensor_tensor(
                                      a3[:, oy0:oy1, ox0:ox1], p3[:, oy0 + dy:oy1 + dy, ox0 + dx:ox1 + dx],
                                      cw[:, c, ky * 3 + kx:ky * 3 + kx + 1], a3[:, oy0:oy1, ox0:ox1], ALU.mult, ALU.add),
                                      reads=[pb, cw, acc], writes=[acc])
                          src = acc
                      sec = [s for s, v in SEC.items() if v <= c0][-1]
                      if sec in ('mq', 'mk'):
                          k.op('act', lambda e: e.activation(acc[:, :], src[:, :], AF.Silu), reads=[src], writes=[acc])
                          if sec == 'mk':
                              k.op('dve', lambda e: e.tensor_scalar(acc[:, :], acc[:, :], 0.125, None, ALU.mult), reads=[acc], writes=[acc])
                          src = acc
                      elif sec in ('mo', 'lg'):
                          k.op('act', lambda e: e.activation(acc[0:M, :], src[0:M, :], AF.Sigmoid), reads=[src], writes=[acc])
                          src = acc
                      elif sec in ('lwf', 'lwb'):
                          k.op('act', lambda e: e.activation(acc[0:M, :], src[0:M, :], AF.Tanh), reads=[src], writes=[acc])
                          src = acc
                      elif sec == 'gates':
                          tmp = pT[1 - ci % 2]
                          k.op('act', lambda e: e.activation(tmp[0:32, :], pb[0:32, :], AF.Exp, bias=mb[:, 1:2], scale=-1.0),
                               reads=[pb, mb], writes=[tmp])
                          k.op('act', lambda e: e.activation(tmp[0:32, :], tmp[0:32, :], AF.Ln, bias=1.0), reads=[tmp], writes=[tmp])
                          k.op('dve', lambda e: e.tensor_scalar(tmp[0:32, :], tmp[0:32, :], mb[:, 3:4], None, ALU.mult), reads=[tmp, mb], writes=[tmp])
                          k.op('dve', lambda e: e.tensor_scalar(acc[0:32, :], pb[0:32, :], mb[:, 0:1], mb[:, 2:3], ALU.add, ALU.mult),
                               reads=[pb, mb], writes=[acc])
                          k.op('dve', lambda e: e.tensor_tensor(acc[0:32, :], acc[0:32, :], tmp[0:32, :], ALU.add), reads=[acc, tmp], writes=[acc])
                          src = acc
                      k.dma('sp', FM[b][c0:c0 + M, :], src[0:M, :], reads=[src], pw=[FM[b]])
        k.barrier()


        with ExitStack() as es:
          if 2 in phases:
            k.es = es
            cm = k.sb("cm", [64, 2, 64], F32)
            k.dma('sp', cm[:], cmask_in[:, :, :], writes=[cm])
            nw = k.sb("nw", [64, 512], F32)
            k.dma('sp', nw[:], m_norm_w[0:1, :].partition_broadcast(64), writes=[nw])
            k.op('pool', lambda e: e.memset(ones_f[:], 1.0), writes=[ones_f])
            GA = k.sb("GA", [64, NCH, 48], F32)
            gT = k.sb("gT", [32, T], F32)
            G = k.sb("G", [64, 32], F32)
            qh = k.sb("qh", [64, 2, T], BF16)
            kh = k.sb("kh", [64, 2, T], BF16)
            vT = k.sb("vT", [128, T], BF16)
            moT = k.sb("moT", [128, T], F32)
            Hf = k.sb("Hf", [64, NCH, 2, 64], F32)
            class _M:
                pass
            MS = []
            for si in range(2):
                M = _M()
                M.i = si
                M.Ktm = k.sb("Ktm", [64, 2, 64], BF16)
                M.Vaug = k.sb("Vaug", [64, 2, 66], BF16)
                M.PTm = k.sb("PTm", [64, 2, 64], BF16)
                M.Cst = k.sb("Cst", [64, 2, 66], F32)
                M.Cbf = k.sb("Cbf", [64, 2, 66], BF16)
                M.dn = k.sb("dn", [64, 2], F32)
                M.ff = k.sb("ff", [64, 2], F32)
                M.hs = k.sb("hs", [64, 2, 64], F32)
                M.st2 = k.sb("st2", [64, 2, 6], F32)
                M.mv2 = k.sb("mv2", [64, 2, 2], F32)
                M.rs2 = k.sb("rs2", [64, 2], F32)
                M.om = k.sb("om", [64, 128], F32)
                M.bA = k.ps("p2A", [64, 512])
                M.bB = k.ps("p2B", [64, 512])
                M.bA.excl = True
                M.bB.excl = True
                MS.append(M)
            pg = k.ps("p2g", [64, 32])
            pbb = k.ps("p2b", [64, 32])
            for b in range(NB):
                k.dma('sp', gT[:], FM[b][3584:3616, :], reads=[FM[b]], writes=[gT])
                for c in range(NCH):
                    k.op('pe', lambda e: e.transpose(pg[:], gT[:, c * 64:(c + 1) * 64], ident[0:32, 0:32]), reads=[gT, ident], writes=[pg])
                    k.op('dve', lambda e: e.tensor_copy(G[:], pg[:]), reads=[pg], writes=[G])
                    k.op('pe', lambda e: e.matmul(pbb[:, 0:8], cm[:, 0, :], G[:, 8:16], start=True, stop=True), reads=[cm, G], writes=[pbb], inc=False)
                    k.op('pe', lambda e: e.matmul(pbb[:, 8:16], cm[:, 1, :], G[:, 24:32], start=True, stop=True), reads=[cm, G], inc=False)
                    k.op('pe', lambda e: e.matmul(pbb[:, 16:24], ones_f[0:64, 0:64], G[:, 8:16], start=True, stop=True), reads=[ones_f, G], inc=False)
                    k.op('pe', lambda e: e.matmul(pbb[:, 24:32], ones_f[0:64, 0:64], G[:, 24:32], start=True, stop=True), reads=[ones_f, G])
                    k.op('act', lambda e: e.activation(GA[:, c, 0:32], pbb[:], AF.Exp), reads=[pbb], writes=[GA])
                    k.op('dve', lambda e: e.tensor_tensor(G[:, 0:8], G[:, 0:8], pbb[:, 0:8], ALU.subtract), reads=[pbb, G], writes=[G])
                    k.op('dve', lambda e: e.tensor_tensor(G[:, 16:24], G[:, 16:24], pbb[:, 8:16], ALU.subtract), reads=[pbb, G], writes=[G])
                    k.op('act', lambda e: e.activation(GA[:, c, 32:40], G[:, 0:8], AF.Exp), reads=[G], writes=[GA])
                    k.op('act', lambda e: e.activation(GA[:, c, 40:48], G[:, 16:24], AF.Exp), reads=[G], writes=[GA])
                for hp in range(4):
                    for h in range(2):
                        r0 = hp * 128 + h * 64
                        for q4 in range(4):
                            t0 = q4 * 1088
                            k.dma('pool', qh[:, h, t0:t0 + 1088], FM[b][r0:r0 + 64, t0:t0 + 1088], reads=[FM[b]], pw=[qh])
                            k.dma('pool', kh[:, h, t0:t0 + 1088], FM[b][512 + r0:512 + r0 + 64, t0:t0 + 1088], reads=[FM[b]], pw=[kh])
                    for q4 in range(4):
                        t0 = q4 * 1088
                        k.dma('pool', vT[:, t0:t0 + 1088], FM[b][2560 + hp * 128:2560 + (hp + 1) * 128, t0:t0 + 1088], reads=[FM[b]], pw=[vT])
                    k.dma('sp', moT[:], FM[b][3072 + hp * 128:3072 + (hp + 1) * 128, :], reads=[FM[b]], writes=[moT])
                    def mstream(M, d, done):
                        Ktm, Vaug, PTm, Cst, Cbf, dn, ff, hs, st2, mv2, rs2, om = M.Ktm, M.Vaug, M.PTm, M.Cst, M.Cbf, M.dn, M.ff, M.hs, M.st2, M.mv2, M.rs2, M.om
                        bA, bB = M.bA, M.bB
                        pk = bA[:, 0:64].bitcast(BF16).rearrange("p (h e) -> p h e", h=2)
                        pv = bA[:, 64:128].bitcast(BF16)
                        pp = bA[:, 128:256].rearrange("p (h e) -> p h e", h=2)
                        po = bB[:, 0:132].rearrange("p (h e) -> p h e", h=2)
                        pc = bB[:, 132:264].rearrange("p (h e) -> p h e", h=2)
                        pmo = bB[:, 264:392]
                        order = list(range(NCH)) if d == 0 else [3, 2, 1, 0] + list(range(NCH - 1, 3, -1))
                        k.op('pool', lambda e: e.memset(Cst[:], 0.0), writes=[Cst])
                        k.op('pool', lambda e: e.memset(Cbf[:], 0.0), writes=[Cbf])
                        for c in order:
                            cs = slice(c * 64, (c + 1) * 64)
                            hh = 2 * hp
                            a_ap = GA[:, c, d * 8 + hh:d * 8 + hh + 2]
                            e_ap = GA[:, c, 16 + d * 8 + hh:16 + d * 8 + hh + 2]
                            c_ap = GA[:, c, 32 + d * 8 + hh:32 + d * 8 + hh + 2]
                            needy = c >= 4
                            second = c in done
                            fin = needy and second
                            for h in range(2):
                                k.op('pe', lambda e: e.transpose(pk[:, h, :], kh[:, h, cs], identb[0:64, 0:64]), reads=[kh, identb], writes=[bA] if h == 0 else [], inc=False)
                            k.op('pe', lambda e: e.transpose(pv, vT[:, cs], identb[:]), reads=[vT, identb], inc=False)
                            for h in range(2):
                                k.op('pe', lambda e: e.matmul(pp[:, h, :], kh[:, h, cs], qh[:, h, cs], start=True, stop=True), reads=[kh, qh], inc=(h == 1))
                            k.op('dve', lambda e: e.tensor_copy(Ktm[:], pk), reads=[bA], writes=[Ktm])
                            k.op('dve', lambda e: e.tensor_tensor(Vaug[:, :, 0:64], pv.rearrange("p (h e) -> p h e", h=2),
                                                                  c_ap.unsqueeze(2).to_broadcast([64, 2, 64]), ALU.mult), reads=[bA, GA], writes=[Vaug])
                            k.op('act', lambda e: e.activation(Vaug[:, :, 64:65], c_ap.unsqueeze(2), AF.Identity), reads=[GA, Vaug], writes=[Vaug])
                            k.op('dve', lambda e: e.tensor_tensor(PTm[:], pp, cm[:, d:d + 1, :].to_broadcast([64, 2, 64]), ALU.mult), reads=[bA, cm], writes=[PTm])
                            yield
                            for h in range(2):
                                k.op('pe', lambda e: e.matmul(po[:, h, 0:65], PTm[:, h, :], Vaug[:, h, 0:65], start=True, stop=False), reads=[PTm, Vaug], writes=[bB] if h == 0 else [], inc=False)
                                k.op('pe', lambda e: e.matmul(po[:, h, 0:65], qh[:, h, cs], Cbf[:, h, 0:65], start=False, stop=True), reads=[qh, Cbf], inc=False)
                            if fin:
                                k.op('pe', lambda e: e.transpose(pmo, moT[:, cs], ident[:]), reads=[moT, ident], inc=False)
                            for h in range(2):
                                k.op('pe', lambda e: e.matmul(pc[:, h, 0:65], Ktm[:, h, :], Vaug[:, h, 0:65], start=True, stop=True), reads=[Ktm, Vaug], inc=(h == 1))
                            yield
                            k.op('dve', lambda e: e.tensor_tensor(Cst[:, :, 0:65], Cst[:, :, 0:65], pc[:, :, 0:65], ALU.add), reads=[bB, Cst], writes=[Cst])
                            k.op('dve', lambda e: e.tensor_tensor(Cst[:, :, 0:65], Cst[:, :, 0:65], e_ap.unsqueeze(2).to_broadcast([64, 2, 65]), ALU.mult), reads=[GA, Cst], writes=[Cst])
                            k.op('act', lambda e: e.activation(Cbf[:, :, 0:65], Cst[:, :, 0:65], AF.Identity), reads=[Cst], writes=[Cbf])
                            if needy:
                                k.op('dve', lambda e: e.tensor_tensor(dn[:], po[:, :, 64], a_ap, ALU.mult), reads=[bB, GA], writes=[dn])
                                k.op('dve', lambda e: e.tensor_scalar(ff[:], dn[:], -1.0, None, ALU.mult), reads=[dn], writes=[ff])
                                k.op('dve', lambda e: e.tensor_tensor(dn[:], dn[:], ff[:], ALU.max), reads=[dn, ff], writes=[dn])
                                k.op('dve', lambda e: e.tensor_scalar(dn[:], dn[:], 1.0, None, ALU.max), reads=[dn], writes=[dn])
                                k.op('dve', lambda e: e.reciprocal(dn[:], dn[:]), reads=[dn], writes=[dn])
                                k.op('dve', lambda e: e.tensor_tensor(ff[:], dn[:], a_ap, ALU.mult), reads=[dn, GA], writes=[ff])
                                if not second:
                                    k.op('dve', lambda e: e.tensor_tensor(Hf[:, c, :, :], po[:, :, 0:64], ff[:].unsqueeze(2).to_broadcast([64, 2, 64]), ALU.mult), reads=[bB, ff], pw_=[Hf])
                                    done[c] = M.i
                                else:
                                    k.op('dve', lambda e: e.tensor_tensor(hs[:], po[:, :, 0:64], ff[:].unsqueeze(2).to_broadcast([64, 2, 64]), ALU.mult), reads=[bB, ff], writes=[hs])
                                    k.op('dve', lambda e: e.tensor_tensor(hs[:], hs[:], Hf[:, c, :, :], ALU.add), reads=[hs, Hf], writes=[hs])
                            if fin:
                                for h in range(2):
                                    k.op('dve', lambda e: e.bn_stats(st2[:, h, :], hs[:, h, :]), reads=[hs], writes=[st2])
                                for h in range(2):
                                    k.op('dve', lambda e: e.bn_aggr(mv2[:, h, :], st2[:, h, :]), reads=[st2], writes=[mv2])
                                k.op('act', lambda e: e.activation(rs2[:], mv2[:, :, 1], AF.Sqrt, bias=LN_EPS), reads=[mv2], writes=[rs2])
                                k.op('dve', lambda e: e.reciprocal(rs2[:], rs2[:]), reads=[rs2], writes=[rs2])
                                for h in range(2):
                                    k.op('dve', lambda e: e.tensor_scalar(hs[:, h, :], hs[:, h, :], mv2[:, h, 0:1], rs2[:, h:h + 1], ALU.subtract, ALU.mult),
                                         reads=[hs, mv2, rs2], writes=[hs])
                                k.op('dve', lambda e: e.tensor_tensor(om[:], hs[:].rearrange("p h e -> p (h e)"), nw[:, hp * 128:(hp + 1) * 128], ALU.mult),
                                     reads=[hs, nw], writes=[om])
                                k.op('dve', lambda e: e.tensor_tensor(om[:], om[:], pmo, ALU.mult), reads=[om, bB], writes=[om])
                                k.dma('sp', MIX[b][(c - 4) * 64:(c - 3) * 64, hp * 128:(hp + 1) * 128], om[:], reads=[om], pw=[MIX[b]])
                            yield

                    done = {}
                    gens = [mstream(MS[0], 0, done), mstream(MS[1], 1, done)]
                    alive = [True, True]
                    while any(alive):
                        for gi in range(2):
                            if alive[gi]:
                                try:
                                    next(gens[gi])
                                except StopIteration:
                                    alive[gi] = False
        k.barrier()

        with ExitStack() as es:
          if 3 in phases:
            k.es = es
            NBK = 512
            NBC = 8
            rp1 = k.sb("rp1", [128, 4, 8], F32)
            k.dma('sp', rp1[:, :, 0:7], rp_in[:, :, :], writes=[rp1])
            k.op('dve', lambda e: e.tensor_scalar(rp1[:, :, 7], rp1[:, :, 4], -1.0, 1.0, ALU.mult, ALU.add), reads=[rp1], writes=[rp1])
            smask = k.sb("smask", [128, NBK], F32)
            k.dma('sp', smask[:], smask_in[:, 0:NBK], writes=[smask])
            onesbd = k.sb("onesbd", [128, 128], F32)
            k.dma('sp', onesbd[:], onesbd_in[:, :], writes=[onesbd])
            ARs = k.sb("ARs", [128, NBC, 128], BF16)
            ZTs = k.sb("ZTs", [128, NBC, 128], BF16)
            PRs = k.sb("PRs", [128, NBK], BF16)
            GLs = k.sb("GLs", [128, NBC], F32)
            rmask = k.sb("rmask", [128, 2, 128], F32)
            k.dma('sp', rmask[:], rmask_in[:, :, :], writes=[rmask])
            nmask = k.sb("nmask", [64, 2, 64], F32)
            k.dma('sp', nmask[:], nmask_in[:, :, :], writes=[nmask])
            gnw = k.sb("gnw", [64, 2, 512], F32)
            k.dma('sp', gnw[:, 0, :], r_norm_w[0:1, :].partition_broadcast(64), pw=[gnw])
            k.dma('sp', gnw[:, 1, :], r_norm_b[0:1, :].partition_broadcast(64), pw=[gnw])
            wBb = k.sb("wBb", [64, 2, 512], BF16)
            aBb = k.sb("aBb", [64, 512], BF16)
            gBb = k.sb("gBb", [128, 512], BF16)
            k.dma('pool', wBb[:, 0, :], r_wB[0, :, :], pw=[wBb])
            k.dma('pool', wBb[:, 1, :], r_wB[1, :, :], pw=[wBb])
            k.dma('pool', aBb[:], r_aB[:, :], writes=[aBb])
            k.dma('pool', gBb[:], r_gB[:, :], writes=[gBb])
            k.op('pool', lambda e: e.memset(ones_f[:], 1.0), writes=[ones_f])
            onesb = k.sb("onesb", [64, 2], BF16)
            k.op('pool', lambda e: e.memset(onesb[:], 1.0), writes=[onesb])
            lgb = k.sb("lgb", [128, T], BF16)
            lab = k.sb("lab", [64, NBK], BF16)
            lwb_ = k.sb("lwb_", [64, NBK], BF16)
            rr = k.sb("rr", [128, NBK], F32)
            rk = k.sb("rk", [128, NBK], F32)
            aa = k.sb("aa", [128, NBK], F32)
            t1 = k.sb("t1", [128, NBK], F32)
            t2 = k.sb("t2", [128, NBK], F32)
            khat = k.sb("khat", [128, NBK], F32)
            kmod = k.sb("kmod", [128, NBK], F32)
            beta = k.sb("beta", [128, NBK], F32)
            lgw = k.sb("lgw", [128, NBK], F32)
            lam = k.sb("lam", [128, NBK], F32)
            ee = k.sb("ee", [128, NBK], F32)
            class _S:
                pass
            SS = []
            for si in range(2):
                S = _S()
                S.i = si
                S.VTb = k.sb("VTb", [64, 2, 64 + NBK], BF16)
                S.PRb = k.sb("PRb", [64, 2, NBK], BF16)
                S.AR = k.sb("AR", [64, 2, NBC, 128], BF16)
                S.ZT = k.sb("ZT", [64, 2, NBC, 128], BF16)
                S.GL = k.sb("GL", [64, 2, NBC], F32)
                S.MmA = k.sb("MmA", [128, 2, NBC, 128], BF16)
                S.XLA = k.sb("XLA", [128, 2, NBC, 64], BF16)
                S.SWA = k.sb("SWA", [128, 2, NBC + 1, 64], BF16)
                S.WA = k.sb("WA", [128, 2, NBC, 64], BF16)
                S.ZtA = k.sb("ZtA", [128, 2, NBC, 64], BF16)
                S.TTA = k.sb("TTA", [64, 2, NBC, 64], BF16)
                S.NGa = [k.sb("NGa%d" % j, [64, 2, 8, 64], BF16) for j in range(2)]
                S.TTg = [k.sb("TTg%d" % j, [64, 8, 64], BF16) for j in range(2)]
                S.Xb = k.sb("Xb", [64, 2, 64], BF16)
                S.ST = k.sb("ST", [64, 2, 64], F32)
                S.tS = k.sb("tS", [64, 2, 64], F32)
                S.ys = k.sb("ys", [64, 2, 64], F32)
                S.bon = k.sb("bon", [64, 2], F32)
                S.om3 = k.sb("om3", [64, 128], F32)
                S.st2 = k.sb("st2r", [64, 2, 6], F32)
                S.mv2 = k.sb("mv2r", [64, 2, 2], F32)
                S.rs2 = k.sb("rs2r", [64, 2], F32)
                S.pXU = k.ps("p3x", [64, 2, 2, 64])
                S.pYS = k.ps("p3y", [64, 512])
                SS.append(S)
            Yf = k.sb("Yf", [64, NCH, 2, 64], F32)
            identg = k.sb("identg", [64, 8, 64], F32)
            pMg = k.ps("p3m", [128, 8, 128])
            pSg = k.ps("p3s", [128, 2, 512])
            pA = pMg
            for S in SS:
                k.op('pool', lambda e: e.memset(S.VTb[:], 0.0), writes=[S.VTb])
                k.op('pool', lambda e: e.memset(S.SWA[:], 0.0), writes=[S.SWA])
            for m in range(8):
                k.op('dve', lambda e: e.tensor_copy(identg[:, m, :], ident[0:64, 0:64]), reads=[ident], writes=[identg])

            def prep(S, b, hp, tok0, ntok, d):
                VTb, PRb, AR, ZT, GL = S.VTb, S.PRb, S.AR, S.ZT, S.GL
                nch = ntok // 64
                n = ntok
                NS = slice(0, ntok)
                r0 = hp * 128
                k.dma('pool', lab[:, NS], FM[b][3744:3808, tok0:tok0 + ntok], reads=[FM[b]], writes=[lab])
                lo = 3616 + 64 * d
                k.dma('pool', lwb_[:, NS], FM[b][lo:lo + 64, tok0:tok0 + ntok], reads=[FM[b]], writes=[lwb_])
                k.dma('sp', rr[:, NS], FM[b][1024 + r0:1024 + r0 + 128, tok0:tok0 + ntok], reads=[FM[b]], writes=[rr])
                k.dma('sp', rk[:, NS], FM[b][1536 + r0:1536 + r0 + 128, tok0:tok0 + ntok], reads=[FM[b]], writes=[rk])
                for h in range(2):
                    k.dma('pool', VTb[:, h, 64:64 + ntok], FM[b][2048 + r0 + h * 64:2048 + r0 + (h + 1) * 64, tok0:tok0 + ntok], reads=[FM[b]], pw=[VTb])
                pA0 = pMg[:, 0:4, :].rearrange("p a b -> p (a b)")[:, 0:n]
                pA1 = pMg[:, 4:8, :].rearrange("p a b -> p (a b)")[:, 0:n]
                k.op('pe', lambda e: e.matmul(pA0, aBb[:, hp * 128:(hp + 1) * 128], lab[:, NS], start=True, stop=True), reads=[aBb, lab], writes=[pMg], inc=False)
                k.op('pe', lambda e: e.matmul(pA1, wBb[:, d, hp * 128:(hp + 1) * 128], lwb_[:, NS], start=True, stop=True), reads=[wBb, lwb_])
                k.op('act', lambda e: e.activation(aa[:, NS], pA0, AF.Sigmoid, bias=rp1[:, hp, 2:3]), reads=[pMg, rp1], writes=[aa])
                k.op('act', lambda e: e.activation(lgw[:, NS], pA1, AF.Sigmoid, bias=rp1[:, hp, d:d + 1]), reads=[pMg, rp1], writes=[lgw])
                k.op('dve', lambda e: e.tensor_scalar(lgw[:, NS], lgw[:, NS], -DS, None, ALU.mult), reads=[lgw], writes=[lgw])
                k.op('dve', lambda e: e.tensor_scalar(t1[:, NS], rk[:, NS], rp1[:, hp, 3:4], None, ALU.mult), reads=[rk, rp1], writes=[t1])
                k.op('dve', lambda e: e.tensor_tensor(t2[:, NS], t1[:, NS], t1[:, NS], ALU.mult), reads=[t1], writes=[t2])
                k.op('pe', lambda e: e.matmul(pA0, onesbd[:], t2[:, NS], start=True, stop=True), reads=[onesbd, t2], writes=[pMg])
                k.op('act', lambda e: e.activation(khat[:, NS], pA0, AF.Sqrt), reads=[pMg], writes=[khat])
                k.op('dve', lambda e: e.tensor_scalar(khat[:, NS], khat[:, NS], 1e-12, None, ALU.max), reads=[khat], writes=[khat])
                k.op('dve', lambda e: e.reciprocal(khat[:, NS], khat[:, NS]), reads=[khat], writes=[khat])
                k.op('dve', lambda e: e.tensor_tensor(khat[:, NS], khat[:, NS], t1[:, NS], ALU.mult), reads=[khat, t1], writes=[khat])
                k.op('dve', lambda e: e.tensor_scalar(t1[:, NS], aa[:, NS], rp1[:, hp, 4:5], rp1[:, hp, 7:8], ALU.mult, ALU.add), reads=[aa, rp1], writes=[t1])
                k.op('dve', lambda e: e.tensor_tensor(kmod[:, NS], rk[:, NS], t1[:, NS], ALU.mult), reads=[rk, t1], writes=[kmod])
                k.op('dve', lambda e: e.tensor_tensor(beta[:, NS], khat[:, NS], aa[:, NS], ALU.mult), reads=[khat, aa], writes=[beta])
                k.op('dve', lambda e: e.scalar_tensor_tensor(PRs[:, NS], rr[:, NS], rp1[:, hp, 6:7], kmod[:, NS], ALU.mult, ALU.mult), reads=[rr, rp1, kmod], writes=[PRs])
                k.op('dve', lambda e: e.tensor_tensor_scan(lam[:, NS], smask[:, NS], lgw[:, NS], 0.0, ALU.mult, ALU.add), reads=[smask, lgw], writes=[lam])
                v3 = lambda t_: t_[:, NS].rearrange("p (c t) -> p c t", t=64)
                if d == 1:
                    k.op('dve', lambda e: e.tensor_tensor(t1[:, NS], lgw[:, NS], lam[:, NS], ALU.subtract), reads=[lgw, lam], writes=[t1])
                    k.op('dve', lambda e: e.tensor_tensor(v3(t2), v3(t1), v3(lam)[:, :, 63:64].to_broadcast([128, nch, 64]), ALU.add), reads=[t1, lam], writes=[t2])
                    k.op('act', lambda e: e.activation(lam[:, NS], t2[:, NS], AF.Identity), reads=[t2], writes=[lam])
                k.op('dve', lambda e: e.tensor_tensor(t1[:, NS], lam[:, NS], lgw[:, NS], ALU.subtract), reads=[lam, lgw], writes=[t1])
                k.op('act', lambda e: e.activation(ee[:, NS], t1[:, NS], AF.Exp), reads=[t1], writes=[ee])
                k.op('dve', lambda e: e.scalar_tensor_tensor(ARs[:, 0:nch, 0:64], v3(khat), -1.0, v3(ee), ALU.mult, ALU.mult), reads=[khat, ee], writes=[ARs])
                k.op('act', lambda e: e.activation(ee[:, NS], lam[:, NS], AF.Exp), reads=[lam], writes=[ee])
                k.op('dve', lambda e: e.tensor_tensor(ARs[:, 0:nch, 64:128], v3(rr), v3(ee), ALU.mult), reads=[rr, ee], writes=[ARs])
                gcol = 63 if d == 0 else 0
                k.op('act', lambda e: e.activation(GLs[:, 0:nch], v3(ee)[:, :, gcol], AF.Identity), reads=[ee], writes=[GLs])
                k.op('act', lambda e: e.activation(t1[:, NS], lam[:, NS], AF.Exp, scale=-1.0), reads=[lam], writes=[t1])
                k.op('dve', lambda e: e.tensor_tensor(ZTs[:, 0:nch, 0:64], v3(beta), v3(t1), ALU.mult), reads=[beta, t1], writes=[ZTs])
                k.op('dve', lambda e: e.tensor_tensor(ZTs[:, 0:nch, 64:128], v3(kmod), v3(t1), ALU.mult), reads=[kmod, t1], writes=[ZTs])
                k.op('act', lambda e: e.activation(AR[:, 0, 0:nch, :], ARs[0:64, 0:nch, :], AF.Identity), reads=[ARs], writes=[AR])
                k.op('act', lambda e: e.activation(ZT[:, 0, 0:nch, :], ZTs[0:64, 0:nch, :], AF.Identity), reads=[ZTs], writes=[ZT])
                k.op('act', lambda e: e.activation(PRb[:, 0, NS], PRs[0:64, NS], AF.Identity), reads=[PRs], writes=[PRb])
                k.op('act', lambda e: e.activation(GL[:, 0, 0:nch], GLs[0:64, 0:nch], AF.Identity), reads=[GLs], writes=[GL])
                k.dma('sp', AR[:, 1, 0:nch, :], ARs[64:128, 0:nch, :], reads=[ARs], pw=[AR])
                k.dma('sp', ZT[:, 1, 0:nch, :], ZTs[64:128, 0:nch, :], reads=[ZTs], pw=[ZT])
                k.dma('sp', PRb[:, 1, NS], PRs[64:128, NS], reads=[PRs], pw=[PRb])
                k.dma('sp', GL[:, 1, 0:nch], GLs[64:128, 0:nch], reads=[GLs], pw=[GL])

            pSf = lambda: pSg[:].rearrange("p a b -> p (a b)")

            def precompute(S, l0, d):
                VTb, AR, ZT, MmA, XLA, SWA, WA, ZtA, TTA, NGa, TTg = S.VTb, S.AR, S.ZT, S.MmA, S.XLA, S.SWA, S.WA, S.ZtA, S.TTA, S.NGa, S.TTg
                G4 = slice(l0, l0 + 4)
                pTb = pSg[:, 0, :].bitcast(BF16)
                for h in range(2):
                    for j in range(4):
                        m = h * 4 + j
                        k.op('pe', lambda e: e.transpose(pTb[:, m * 64:(m + 1) * 64], ZT[:, h, l0 + j, :], identb[0:64, 0:64]), reads=[ZT, identb], writes=[pSg], inc=False)
                for h in range(2):
                    for j in range(4):
                        m = 8 + h * 4 + j
                        k.op('pe', lambda e: e.transpose(pTb[:, m * 64:(m + 1) * 64], VTb[:, h, (l0 + j) * 64:(l0 + j) * 64 + 128], identb[0:64, 0:64]), reads=[VTb, identb], inc=(h == 1 and j == 3))
                zsrc = pTb[:, 0:512].rearrange("p (h j e) -> p h j e", h=2, j=4)
                vsrc = pTb[64:128, 512:1024].rearrange("p (h j e) -> p h j e", h=2, j=4)
                k.op('act', lambda e: e.activation(ZtA[:, :, G4, :], zsrc, AF.Identity), reads=[pSg], writes=[ZtA])
                k.op('act', lambda e: e.activation(SWA[64:128, :, G4, :], vsrc, AF.Identity), reads=[pSg], writes=[SWA])
                k.op('act', lambda e: e.activation(WA[64:128, :, G4, :], vsrc, AF.Identity), reads=[pSg], writes=[WA])
                yield
                for h in range(2):
                    for j in range(4):
                        m = h * 4 + j
                        k.op('pe', lambda e: e.matmul(pMg[:, m, :], ZT[:, h, l0 + j, :], AR[:, h, l0 + j, :], start=True, stop=True), reads=[ZT, AR], writes=[pMg], inc=(m == 7))
                for h in range(2):
                    for j in range(4):
                        m = h * 4 + j
                        k.op('pe', lambda e: e.matmul(pSg[0:64, 1, m * 64:(m + 1) * 64], AR[:, h, l0 + j, 0:64], ZT[:, h, l0 + j, 0:64], start=True, stop=True), reads=[ZT, AR], writes=[pSg], inc=(m == 7))
                for h in range(2):
                    k.op('dve', lambda e: e.tensor_tensor(MmA[:, h, G4, :], pMg[:, h * 4:(h + 1) * 4, :], rmask[:, d:d + 1, :].to_broadcast([128, 4, 128]), ALU.mult),
                         reads=[pMg, rmask], writes=[MmA])
                N0, N1 = NGa[0], NGa[1]
                k.op('dve', lambda e: e.tensor_tensor(N0[:, 0, :, :], pSg[0:64, 1, :].rearrange("p (m e) -> p m e", e=64), nmask[:, d:d + 1, :].to_broadcast([64, 8, 64]), ALU.mult),
                     reads=[pSg, nmask], writes=[N0])
                k.op('act', lambda e: e.activation(N0[:, 1, :, :].rearrange("p (h j) e -> p h j e", h=2), MmA[0:64, :, G4, 0:64], AF.Identity), reads=[MmA], writes=[N0])
                k.op('act', lambda e: e.activation(XLA[64:128, :, G4, :], MmA[64:128, :, G4, 0:64], AF.Identity), reads=[MmA], writes=[XLA])
                k.op('act', lambda e: e.activation(XLA[0:64, :, G4, :], AR[:, :, G4, 0:64], AF.Identity), reads=[AR], writes=[XLA])
                yield
                k.op('dve', lambda e: e.tensor_tensor(TTg[0][:], N0[:, 1, :, :], identg[:], ALU.add), reads=[N0, identg], writes=[TTg[0]])
                cur = 0
                for lv in range(1, 6):
                    yield
                    src, dst = NGa[cur], NGa[1 - cur]
                    for m in range(8):
                        k.op('pe', lambda e: e.matmul(pSg[0:64, 0, m * 64:(m + 1) * 64], src[:, 1, m, :], src[:, 0, m, :], start=True, stop=True), reads=[src], writes=[pSg], inc=False)
                    for m in range(8):
                        k.op('pe', lambda e: e.matmul(pSg[0:64, 1, m * 64:(m + 1) * 64], src[:, 0, m, :], src[:, 1, m, :], start=True, stop=True), reads=[src], inc=(m == 7))
                    k.op('act', lambda e: e.activation(dst[:, 0, :, :].rearrange("p m e -> p (m e)"), pSg[0:64, 0, :], AF.Identity), reads=[pSg], writes=[dst])
                    k.op('dve', lambda e: e.tensor_copy(dst[:, 1, :, :].rearrange("p m e -> p (m e)"), pSg[0:64, 1, :]), reads=[pSg], writes=[dst])
                    cur = 1 - cur
                    ti, to = TTg[(lv - 1) % 2], TTg[lv % 2]
                    for m in range(8):
                        k.op('pe', lambda e: e.matmul(pMg[0:64, m, 0:64], dst[:, 0, m, :], ti[:, m, :], start=True, stop=True), reads=[dst, ti], writes=[pMg], inc=(m == 7))
                    if lv < 5:
                        k.op('dve', lambda e: e.tensor_tensor(to[:], pMg[0:64, :, 0:64], ti[:], ALU.add), reads=[pMg, ti], writes=[to])
                    else:
                        k.op('dve', lambda e: e.tensor_tensor(TTA[:, :, G4, :], pMg[0:64, :, 0:64].rearrange("p (h j) e -> p h j e", h=2), ti[:].rearrange("p (h j) e -> p h j e", h=2), ALU.add),
                             reads=[pMg, ti], writes=[TTA])

            def step(S, b, hp, c, lc, lnext, d, done):
                VTb, PRb, AR, GL, MmA, XLA, SWA, WA, ZtA, TTA = S.VTb, S.PRb, S.AR, S.GL, S.MmA, S.XLA, S.SWA, S.WA, S.ZtA, S.TTA
                Xb, ST, tS, ys, bon, om3, st2, mv2, rs2, pXU, pYS = S.Xb, S.ST, S.tS, S.ys, S.bon, S.om3, S.st2, S.mv2, S.rs2, S.pXU, S.pYS
                pX = pXU[:, 0, :, :]
                pU = pXU[:, 1, :, :]
                pY = pYS[:, 0:128].rearrange("p (h e) -> p h e", h=2)
                pS_ = pYS[:, 128:256].rearrange("p (h e) -> p h e", h=2)
                for h in range(2):
                    k.op('pe', lambda e: e.matmul(pXU[:, 0, h, :], XLA[:, h, lc, :], SWA[:, h, lc, :], start=True, stop=True), reads=[XLA, SWA], writes=[pXU], inc=(h == 1))
                k.op('act', lambda e: e.activation(Xb[:], pX, AF.Identity), reads=[pXU], writes=[Xb])
                yield
                for h in range(2):
                    k.op('pe', lambda e: e.matmul(pXU[:, 1, h, :], TTA[:, h, lc, :], Xb[:, h, :], start=True, stop=True), reads=[TTA, Xb], writes=[pXU], inc=(h == 1))
                k.op('dve', lambda e: e.tensor_copy(WA[0:64, :, lc, :], pU), reads=[pXU], writes=[WA])
                yield
                second = (c in done)
                needy = (c >= 4)
                wfirst = [pYS]
                if needy:
                    for h in range(2):
                        k.op('pe', lambda e: e.matmul(pYS[:, h * 64:(h + 1) * 64], AR[:, h, lc, 64:128], SWA[0:64, h, lc, :], start=True, stop=False), reads=[AR, SWA], writes=wfirst, inc=False)
                        wfirst = []
                        k.op('pe', lambda e: e.matmul(pYS[:, h * 64:(h + 1) * 64], MmA[:, h, lc, 64:128], WA[:, h, lc, :], start=False, stop=True), reads=[MmA, WA], inc=False)
                fin = (needy and second)
                if fin:
                    ts = slice(lc * 64, (lc + 1) * 64)
                    for h in range(2):
                        k.op('pe', lambda e: e.matmul(pYS[:, 384 + 2 * h:386 + 2 * h], PRb[:, h, ts], onesb[:, 0:2], start=True, stop=True), reads=[PRb, onesb], inc=False)
                    k.op('pe', lambda e: e.matmul(pYS[:, 256:384], lgb[:, c * 64:(c + 1) * 64], gBb[:, hp * 128:(hp + 1) * 128], start=True, stop=True), reads=[lgb, gBb], inc=False)
                    pvt = pYS[:, 448:512].bitcast(BF16)
                    for h in range(2):
                        k.op('pe', lambda e: e.transpose(pvt[:, h * 64:(h + 1) * 64], VTb[:, h, 64 + lc * 64:128 + lc * 64], identb[0:64, 0:64]), reads=[VTb, identb], inc=False)
                for h in range(2):
                    k.op('pe', lambda e: e.matmul(pYS[:, 128 + h * 64:128 + (h + 1) * 64], ZtA[:, h, lc, :], WA[:, h, lc, :], start=True, stop=True), reads=[ZtA, WA], writes=wfirst, inc=(h == 1))
                    wfirst = []
                k.op('dve', lambda e: e.tensor_tensor(tS[:], pS_, ST[:], ALU.add), reads=[pYS, ST], writes=[tS])
                k.op('dve', lambda e: e.tensor_tensor(ST[:], tS[:], GL[:, :, lc:lc + 1].to_broadcast([64, 2, 64]), ALU.mult), reads=[tS, GL], writes=[ST])
                k.op('act', lambda e: e.activation(SWA[0:64, :, lnext, :], ST[:], AF.Identity), reads=[ST], writes=[SWA])
                if needy and not second:
                    k.op('dve', lambda e: e.tensor_copy(Yf[:, c, :, :], pY), reads=[pYS], pw_=[Yf])
                    done[c] = S.i
                elif needy:
                    k.op('dve', lambda e: e.tensor_tensor(ys[:], pY, Yf[:, c, :, :], ALU.add), reads=[pYS, Yf], writes=[ys])
                if fin:
                    for h in range(2):
                        k.op('dve', lambda e: e.bn_stats(st2[:, h, :], ys[:, h, :]), reads=[ys], writes=[st2])
                    for h in range(2):
                        k.op('dve', lambda e: e.bn_aggr(mv2[:, h, :], st2[:, h, :]), reads=[st2], writes=[mv2])
                    k.op('act', lambda e: e.activation(rs2[:], mv2[:, :, 1], AF.Sqrt, bias=GN_EPS), reads=[mv2], writes=[rs2])
                    k.op('dve', lambda e: e.reciprocal(rs2[:], rs2[:]), reads=[rs2], writes=[rs2])
                    for h in range(2):
                        k.op('dve', lambda e: e.tensor_scalar(ys[:, h, :], ys[:, h, :], mv2[:, h, 0:1], rs2[:, h:h + 1], ALU.subtract, ALU.mult), reads=[ys, mv2, rs2], writes=[ys])
                    ysf = ys[:].rearrange("p h e -> p (h e)")
                    k.op('dve', lambda e: e.tensor_tensor(om3[:], ysf, gnw[:, 0, hp * 128:(hp + 1) * 128], ALU.mult), reads=[ys, gnw], writes=[om3])
                    k.op('dve', lambda e: e.tensor_tensor(om3[:], om3[:], gnw[:, 1, hp * 128:(hp + 1) * 128], ALU.add), reads=[om3, gnw], writes=[om3])
                    k.op('dve', lambda e: e.tensor_copy(bon[:], pYS[:, 384:388].rearrange("p (h two) -> p h two", two=2)[:, :, 0]), reads=[pYS], writes=[bon])
                    for h in range(2):
                        k.op('dve', lambda e: e.scalar_tensor_tensor(om3[:, h * 64:(h + 1) * 64], pvt[:, h * 64:(h + 1) * 64], bon[:, h:h + 1], om3[:, h * 64:(h + 1) * 64], ALU.mult, ALU.add),
                             reads=[pYS, bon, om3], writes=[om3])
                    k.op('dve', lambda e: e.tensor_tensor(om3[:], om3[:], pYS[:, 256:384], ALU.mult), reads=[om3, pYS], writes=[om3])
                    k.dma('sp', MIX[b][(c - 4) * 64:(c - 3) * 64, 512 + hp * 128:512 + (hp + 1) * 128], om3[:], reads=[om3], pw=[MIX[b]])

            blocks = [(0, 256)] + [(256 + i * 512, 512) for i in range(8)]

            def stream(S, b, hp, d, done):
                k.op('pool', lambda e: e.memset(S.ST[:], 0.0), writes=[S.ST])
                border = list(range(9)) if d == 0 else [0] + list(range(8, 0, -1))
                first = True
                for bi in border:
                    tok0, ntok = blocks[bi]
                    nch = ntok // 64
                    prep(S, b, hp, tok0, ntok, d)
                    lcs = list(range(nch)) if d == 0 else list(range(nch - 1, -1, -1))
                    if first:
                        k.op('pool', lambda e: e.memset(S.SWA[0:64, :, lcs[0], :], 0.0), writes=[S.SWA])
                        first = False
                    else:
                        k.op('act', lambda e: e.activation(S.SWA[0:64, :, lcs[0], :], S.ST[:], AF.Identity), reads=[S.ST], writes=[S.SWA])
                    yield
                    for g0 in range(0, nch, 4):
                        yield from precompute(S, g0, d)
                        yield
                    for ii, lc in enumerate(lcs):
                        lnext = lcs[ii + 1] if ii + 1 < len(lcs) else NBC
                        yield from step(S, b, hp, tok0 // 64 + lc, lc, lnext, d, done)
                        yield

            for b in range(NB):
                for q4 in range(4):
                    k.dma('pool', lgb[:, q4 * 1088:(q4 + 1) * 1088], FM[b][3808:3936, q4 * 1088:(q4 + 1) * 1088], reads=[FM[b]], pw=[lgb])
                for hp in range(int(os.environ.get('P3H', 4))):
                    done = {}
                    gens = [stream(SS[0], b, hp, 0, done), stream(SS[1], b, hp, 1, done)]
                    alive = [True, True]
                    while any(alive):
                        for gi in range(2):
                            if alive[gi]:
                                try:
                                    next(gens[gi])
                                except StopIteration:
                                    alive[gi] = False
        k.barrier()

        with ExitStack() as es:
          if 4 in phases:
           try:
            P4S = int(os.environ.get('P4S', 9))
            k.es = es
            NT = NB * SEQ // 128
            NBLK = NBLK_
            SUB = BS // 128
            LG = k.sb("LG", [128, NT, 36], F32)
            OH1 = k.sb("OH1", [128, NT, 32], F32)
            OH2 = k.sb("OH2", [128, NT, 32], F32)
            W1 = k.sb("W1", [128, NT], F32)
            W2 = k.sb("W2", [128, NT], F32)
            DST = k.sb("DST", [128, NT, 2], I32)
            WIDX = k.sb("WIDX", [128, NBLK, 12], I32)
            g2b = k.sb("g2b", [128, NB, D], F32)
            lnp = k.sb("lnp", [128, 4, D], F32)
            for j, src in enumerate((ln1_g, ln1_b, ln2_g, ln2_b)):
                k.dma('sp', lnp[:, j, :], src[0:1, :].partition_broadcast(128), pw=[lnp])
            for b in range(NB):
                k.dma('sp', g2b[:, b, :], MODD[b:b + 1, 5 * D:6 * D].partition_broadcast(128), reads=[MODD], pw=[g2b])
            with ExitStack() as es4:
                k.es = es4
                wob = k.sb("wob", [128, 8, D], BF16)
                for kc in range(8):
                    k.dma('pool', wob[:, kc, :], w_out[kc * 128:(kc + 1) * 128, :], pw=[wob])
                rt = k.sb("rt", [128, 8, 36], F32)
                k.dma('sp', rt[:], rt_in[:, :].rearrange("(kc p) n -> p kc n", p=128), writes=[rt])
                rtbb = k.sb("rtbb", [128, 36], F32)
                k.dma('sp', rtbb[:], rtb_in[0:1, :].partition_broadcast(128), writes=[rtbb])
                mb4 = k.sb("mb4", [128, 3, D], F32)
                class _A:
                    pass
                AS = []
                for si in range(2):
                    A = _A()
                    A.mxb = k.sb("mxb", [128, D], BF16)
                    A.mT = k.sb("mT", [128, 8, 128], BF16)
                    A.x4 = k.sb("x4", [128, D], F32)
                    A.t4 = k.sb("t4", [128, D], F32)
                    A.y4 = k.sb("y4", [128, D], F32)
                    A.h4 = k.sb("h4", [128, D], F32)
                    A.h4b = k.sb("h4b", [128, D], BF16)
                    A.h4T = k.sb("h4T", [128, 8, 128], F32)
                    A.st4 = k.sb("st4", [128, 2, 6], F32)
                    A.mv4 = k.sb("mv4", [128, 2], F32)
                    A.rs4 = k.sb("rs4", [128, 1], F32)
                    A.nb4 = k.sb("nb4", [128, 1], F32)
                    A.P01 = k.ps("p4o", [128, 2, 512])
                    A.P01.excl = True
                    A.plg = k.ps("p4lg", [128, 36])
                    AS.append(A)

                def ln_stats(A, src):
                    for hf in range(2):
                        k.op('dve', lambda e: e.bn_stats(A.st4[:, hf, :], src[:, hf * 512:(hf + 1) * 512]), reads=[src], writes=[A.st4])
                    k.op('dve', lambda e: e.bn_aggr(A.mv4[:], A.st4[:].rearrange("p a b -> p (a b)")), reads=[A.st4], writes=[A.mv4])
                    k.op('act', lambda e: e.activation(A.rs4[:], A.mv4[:, 1:2], AF.Sqrt, bias=LN_EPS), reads=[A.mv4], writes=[A.rs4])
                    k.op('dve', lambda e: e.reciprocal(A.rs4[:], A.rs4[:]), reads=[A.rs4], writes=[A.rs4])
                    k.op('dve', lambda e: e.scalar_tensor_tensor(A.nb4[:], A.mv4[:, 0:1], -1.0, A.rs4[:], ALU.mult, ALU.mult), reads=[A.mv4, A.rs4], writes=[A.nb4])

                def tile4(A, b, i):
                    gi = b * (SEQ // 128) + i
                    P01 = A.P01
                    k.dma('pool', A.mxb[:], MIX[b][i * 128:(i + 1) * 128, :], reads=[MIX[b]], writes=[A.mxb])
                    k.dma('sp', A.x4[:], x_in[b, i * 128:(i + 1) * 128, :], writes=[A.x4])
                    yield
                    ptm = P01[:, 0, :].bitcast(BF16)
                    for kc in range(8):
                        k.op('pe', lambda e: e.transpose(ptm[:, kc * 128:(kc + 1) * 128], A.mxb[:, kc * 128:(kc + 1) * 128], identb[:]), reads=[A.mxb, identb], writes=[P01] if kc == 0 else [], inc=(kc == 7))
                    k.op('act', lambda e: e.activation(A.mT[:].rearrange("p a b -> p (a b)"), ptm, AF.Identity), reads=[P01], writes=[A.mT])
                    yield
                    for n in range(2):
                        for kc in range(8):
                            k.op('pe', lambda e: e.matmul(P01[:, n, :], A.mT[:, kc, :], wob[:, kc, n * 512:(n + 1) * 512], start=(kc == 0), stop=(kc == 7)),
                                 reads=[A.mT, wob], writes=[P01] if (kc == 0 and n == 0) else [], inc=(kc == 7 and n == 1))
                    k.op('dve', lambda e: e.tensor_tensor(A.t4[:], P01[:].rearrange("p a b -> p (a b)"), mb4[:, 0, :], ALU.mult), reads=[P01, mb4], writes=[A.t4])
                    k.op('dve', lambda e: e.scalar_tensor_tensor(A.y4[:], A.x4[:], ALPHA, A.t4[:], ALU.mult, ALU.add), reads=[A.x4, A.t4], writes=[A.y4])
                    yield
                    ln_stats(A, A.y4)
                    k.op('act', lambda e: e.activation(A.y4[:], A.y4[:], AF.Identity, bias=A.nb4[:, 0:1], scale=A.rs4[:, 0:1]), reads=[A.y4, A.nb4, A.rs4], writes=[A.y4])
                    yield
                    k.op('dve', lambda e: e.tensor_tensor(A.y4[:], A.y4[:], lnp[:, 0, :], ALU.mult), reads=[A.y4, lnp], writes=[A.y4])
                    k.op('dve', lambda e: e.tensor_tensor(A.y4[:], A.y4[:], lnp[:, 1, :], ALU.add), reads=[A.y4, lnp], writes=[A.y4])
                    k.dma('sp', X1[gi * 128:(gi + 1) * 128, :], A.y4[:], reads=[A.y4], pw=[X1])
                    yield
                    ln_stats(A, A.y4)
                    k.op('act', lambda e: e.activation(A.h4[:], A.y4[:], AF.Identity, bias=A.nb4[:, 0:1], scale=A.rs4[:, 0:1]), reads=[A.y4, A.nb4, A.rs4], writes=[A.h4])
                    yield
                    k.op('dve', lambda e: e.tensor_tensor(A.h4[:], A.h4[:], mb4[:, 1, :], ALU.mult), reads=[A.h4, mb4], writes=[A.h4])
                    k.op('dve', lambda e: e.tensor_tensor(A.h4[:], A.h4[:], mb4[:, 2, :], ALU.add), reads=[A.h4, mb4], writes=[A.h4])
                    k.op('act', lambda e: e.activation(A.h4b[:], A.h4[:], AF.Identity), reads=[A.h4], writes=[A.h4b])
                    k.dma('sp', H2[gi * 128:(gi + 1) * 128, :], A.h4b[:], reads=[A.h4b], pw=[H2])
                    yield
                    pth = P01[:].rearrange("p a b -> p (a b)")
                    for kc in range(8):
                        k.op('pe', lambda e: e.transpose(pth[:, kc * 128:(kc + 1) * 128], A.h4[:, kc * 128:(kc + 1) * 128], ident[:]), reads=[A.h4, ident], writes=[P01] if kc == 0 else [], inc=(kc == 7))
                    k.op('act', lambda e: e.activation(A.h4T[:].rearrange("p a b -> p (a b)"), pth, AF.Identity), reads=[P01], writes=[A.h4T])
                    yield
                    for kc in range(8):
                        k.op('pe', lambda e: e.matmul(A.plg[:], A.h4T[:, kc, :], rt[:, kc, :], start=(kc == 0), stop=(kc == 7)),
                             reads=[A.h4T, rt], writes=[A.plg] if kc == 0 else [], inc=(kc == 7))
                    k.op('dve', lambda e: e.tensor_tensor(LG[:, gi, :], A.plg[:], rtbb[:], ALU.add), reads=[A.plg, rtbb], pw_=[LG])

                for b in range(NB):
                    for j, c0 in enumerate((2 * D, 4 * D, 3 * D)):
                        k.dma('sp', mb4[:, j, :], MODD[b:b + 1, c0:c0 + D].partition_broadcast(128), reads=[MODD], writes=[mb4] if j == 0 else [], pw=[] if j == 0 else [mb4])
                    k.op('dve', lambda e: e.tensor_scalar(mb4[:, 1, :], mb4[:, 1, :], 1.0, None, ALU.add), reads=[mb4], writes=[mb4])

                    def astream(si):
                        for i in range(si, SEQ // 128, 2):
                            yield from tile4(AS[si], b, i)
                            yield
                    gens = [astream(0), astream(1)]
                    alive = [True, True]
                    while any(alive):
                        for gq in range(2):
                            if alive[gq]:
                                try:
                                    next(gens[gq])
                                except StopIteration:
                                    alive[gq] = False
            k.barrier()
            with ExitStack() as es5:
                k.es = es5
                if P4S < 1:
                    raise _Stop()
                gmx = k.sb("gmx", [128, NT], F32)
                goh = k.sb("goh", [128, NT, 4], F32)
                tg = k.sb("tg", [128, NT, 4], F32)
                ptop = k.sb("ptop", [128, NT], F32)
                lem = k.sb("lem", [128, NT, 32], F32)
                v1 = k.sb("v1", [128, NT], F32)
                v2 = k.sb("v2", [128, NT], F32)
                lgv = LG[:, :, 0:4]
                lev = LG[:, :, 4:36]
                k.op('dve', lambda e: e.tensor_reduce(gmx[:], lgv, AX.X, ALU.max), reads=[LG], writes=[gmx])
                k.op('dve', lambda e: e.tensor_tensor(goh[:], lgv, gmx[:].unsqueeze(2).to_broadcast([128, NT, 4]), ALU.is_equal), reads=[LG, gmx], writes=[goh])
                k.op('dve', lambda e: e.tensor_tensor(tg[:], lgv, gmx[:].unsqueeze(2).to_broadcast([128, NT, 4]), ALU.subtract), reads=[LG, gmx], writes=[tg])
                k.op('act', lambda e: e.activation(tg[:], tg[:], AF.Exp), reads=[tg], writes=[tg])
                k.op('dve', lambda e: e.tensor_reduce(ptop[:], tg[:], AX.X, ALU.add), reads=[tg], writes=[ptop])
                k.op('dve', lambda e: e.reciprocal(ptop[:], ptop[:]), reads=[ptop], writes=[ptop])
                k.op('dve', lambda e: e.tensor_scalar(goh[:], goh[:], -1.0, 1e30, ALU.add, ALU.mult), reads=[goh], writes=[goh])
                for g in range(4):
                    k.op('dve', lambda e: e.tensor_tensor(lem[:, :, g * 8:(g + 1) * 8], LG[:, :, 4 + g * 8:12 + g * 8],
                                                          goh[:, :, g:g + 1].to_broadcast([128, NT, 8]), ALU.add), reads=[LG, goh], writes=[lem])
                k.op('dve', lambda e: e.tensor_reduce(v1[:], lem[:], AX.X, ALU.max), reads=[lem], writes=[v1])
                k.op('dve', lambda e: e.tensor_tensor(OH1[:], lem[:], v1[:].unsqueeze(2).to_broadcast([128, NT, 32]), ALU.is_equal), reads=[lem, v1], writes=[OH1])
                k.op('dve', lambda e: e.scalar_tensor_tensor(lem[:], OH1[:], -1e30, lem[:], ALU.mult, ALU.add), reads=[OH1, lem], writes=[lem])
                k.op('dve', lambda e: e.tensor_reduce(v2[:], lem[:], AX.X, ALU.max), reads=[lem], writes=[v2])
                k.op('dve', lambda e: e.tensor_tensor(OH2[:], lem[:], v2[:].unsqueeze(2).to_broadcast([128, NT, 32]), ALU.is_equal), reads=[lem, v2], writes=[OH2])
                k.op('dve', lambda e: e.tensor_tensor(v2[:], v2[:], v1[:], ALU.subtract), reads=[v1, v2], writes=[v2])
                k.op('act', lambda e: e.activation(v2[:], v2[:], AF.Exp), reads=[v2], writes=[v2])
                k.op('dve', lambda e: e.tensor_scalar(v2[:], v2[:], 1.0, None, ALU.add), reads=[v2], writes=[v2])
                k.op('dve', lambda e: e.reciprocal(v2[:], v2[:]), reads=[v2], writes=[v2])
                k.op('dve', lambda e: e.tensor_tensor(W1[:], v2[:], ptop[:], ALU.mult), reads=[v2, ptop], writes=[W1])
                k.op('dve', lambda e: e.tensor_tensor(W2[:], ptop[:], W1[:], ALU.subtract), reads=[W1, ptop], writes=[W2])
            k.barrier()
            with ExitStack() as es6:
                k.es = es6
                if P4S < 2:
                    raise _Stop()
                OHb = k.sb("OHb", [128, NT, 32], BF16)
                triS = k.sb("triS", [128, 128], BF16)
                onb = k.sb("onb", [128, 128], BF16)
                thr = k.sb("thr", [128, 128], F32)
                blki = k.sb("blki", [128, NBLK], F32)
                kcp = k.sb("kcp", [128, 12], F32)
                cnt = k.sb("cnt", [128, 32], F32)
                big = k.sb("big", [128, 32, 128], F32)
                nbk = k.sb("nbk", [128, 32], F32)
                pend = k.sb("pend", [128, 32], F32)
                pst = k.sb("pst", [128, 32], F32)
                run = k.sb("run", [128, 32], F32)
                RK = k.sb("RK", [128, NT, 32], F32)
                dsf = k.sb("dsf", [128, NT, 2], F32)
                bexp = k.sb("bexp", [128, NBLK], F32)
                bigb = k.sb("bigb", [128, NBLK, 32], F32)
                widxf = k.sb("widxf", [128, NBLK, 12], F32)
                tokid = k.sb("tokid", [128, NT, 16], I32)
                zt = k.sb("zt", [128, 16], I32)
                pcn = k.ps("p5c", [128, 32])
                prk = k.ps("p5r", [128, 32])
                ptt = k.ps("p5t", [128, 32])
                stg = k.sb("stg", [128, 128], F32)
                k.dma('sp', stg[:], tris_in[:, :], writes=[stg])
                k.op('dve', lambda e: e.tensor_copy(triS[:], stg[:]), reads=[stg], writes=[triS])
                k.op('pool', lambda e: e.memset(onb[:], 1.0), writes=[onb])
                k.dma('sp', thr[:], thr_in[:, :], writes=[thr])
                k.dma('sp', blki[:], blki_in[:, 0:NBLK], writes=[blki])
                k.dma('sp', kcp[:], kcp_in[:, :], writes=[kcp])
                k.dma('sp', tokid[:], tokid_in[:, 0:NT, :], writes=[tokid])
                k.op('pool', lambda e: e.memset(zt[:], 0), writes=[zt])
                k.dma('sp', TOKB[:, :].rearrange("(b p) c -> p b c", p=128), zt[:].unsqueeze(1).to_broadcast([128, NBLK * SUB, 16]), reads=[zt], writes=[TOKB])
                k.op('dve', lambda e: e.tensor_tensor(OHb[:], OH1[:], OH2[:], ALU.add), reads=[OH1, OH2], writes=[OHb])
                for i in range(NT):
                    k.op('pe', lambda e: e.matmul(pcn[:], onb[:], OHb[:, i, :], start=(i == 0), stop=(i == NT - 1)), reads=[onb, OHb], writes=[pcn] if i == 0 else [], inc=(i == NT - 1))
                k.op('dve', lambda e: e.tensor_copy(cnt[:], pcn[:]), reads=[pcn], writes=[cnt])
                k.op('dve', lambda e: e.tensor_tensor(big[:], cnt[:].unsqueeze(2).to_broadcast([128, 32, 128]), thr[:].unsqueeze(1).to_broadcast([128, 32, 128]), ALU.is_gt),
                     reads=[cnt, thr], writes=[big])
                k.op('dve', lambda e: e.tensor_reduce(nbk[:], big[:], AX.X, ALU.add), reads=[big], writes=[nbk])
                k.op('pool', lambda e: e.memset(run[:], 1.0), writes=[run])
                k.op('dve', lambda e: e.tensor_tensor_scan(pend[:], run[:], nbk[:], 0.0, ALU.mult, ALU.add), reads=[run, nbk], writes=[pend])
                k.op('dve', lambda e: e.tensor_tensor(pst[:], pend[:], nbk[:], ALU.subtract), reads=[pend, nbk], writes=[pst])
                k.op('dve', lambda e: e.tensor_scalar(pst[:], pst[:], float(BS), None, ALU.mult), reads=[pst], writes=[pst])
                k.op('pool', lambda e: e.memset(run[:], 0.0), reads=[run], writes=[run])
                for i in range(NT):
                    k.op('pe', lambda e: e.matmul(prk[:], triS[:], OHb[:, i, :], start=True, stop=True), reads=[triS, OHb], writes=[prk])
                    k.op('pe', lambda e: e.matmul(ptt[:], onb[:], OHb[:, i, :], start=True, stop=True), reads=[onb, OHb], writes=[ptt])
                    k.op('dve', lambda e: e.tensor_tensor(RK[:, i, :], prk[:], run[:], ALU.add), reads=[prk, run], writes=[RK])
                    k.op('dve', lambda e: e.tensor_tensor(run[:], run[:], ptt[:], ALU.add), reads=[run, ptt], writes=[run])
                k.op('dve', lambda e: e.tensor_tensor(RK[:], RK[:], pst[:].unsqueeze(1).to_broadcast([128, NT, 32]), ALU.add), reads=[RK, pst], writes=[RK])
                for j, OH in enumerate((OH1, OH2)):
                    k.op('dve', lambda e: e.tensor_tensor(OH[:], OH[:], RK[:], ALU.mult), reads=[OH, RK], writes=[OH])
                    k.op('dve', lambda e: e.tensor_reduce(dsf[:, :, j], OH[:], AX.X, ALU.add), reads=[OH], writes=[dsf])
                k.op('dve', lambda e: e.tensor_copy(DST[:], dsf[:]), reads=[dsf], writes=[DST])
                k.op('dve', lambda e: e.tensor_tensor(bigb[:], pend[:].unsqueeze(1).to_broadcast([128, NBLK, 32]), blki[:].unsqueeze(2).to_broadcast([128, NBLK, 32]), ALU.is_le),
                     reads=[pend, blki], writes=[bigb])
                k.op('dve', lambda e: e.tensor_reduce(bexp[:], bigb[:], AX.X, ALU.add), reads=[bigb], writes=[bexp])
                k.op('dve', lambda e: e.tensor_scalar(bexp[:], bexp[:], 31.0, None, ALU.min), reads=[bexp], writes=[bexp])
                k.op('dve', lambda e: e.tensor_scalar(widxf[:, :, 0:8], bexp[:].unsqueeze(2).to_broadcast([128, NBLK, 8]), 256.0, None, ALU.mult), reads=[bexp], writes=[widxf])
                k.op('dve', lambda e: e.tensor_scalar(widxf[:, :, 8:12], bexp[:].unsqueeze(2).to_broadcast([128, NBLK, 4]), 256.0, None, ALU.mult), reads=[bexp], writes=[widxf])
                k.op('dve', lambda e: e.tensor_tensor(widxf[:], widxf[:], kcp[:].unsqueeze(1).to_broadcast([128, NBLK, 12]), ALU.add), reads=[widxf, kcp], writes=[widxf])
                k.op('dve', lambda e: e.tensor_copy(WIDX[:], widxf[:]), reads=[widxf], writes=[WIDX])
                for i in range(NT):
                    for j in range(2):
                        k.dma('pool', TOKB[:, :], tokid[:, i, :], reads=[tokid, DST], pw=[TOKB],
                              indirect=(bass.IndirectOffsetOnAxis(ap=DST[:, i, j:j + 1], axis=0), None))
            k.barrier()
            with ExitStack() as es7:
                k.es = es7
                if P4S < 3:
                    raise _Stop()
                wg = [k.sb("wg%d" % i, [128, 8, 512], BF16) for i in range(3)]
                wu = [k.sb("wu%d" % i, [128, 8, 512], BF16) for i in range(3)]
                wd = [k.sb("wd%d" % i, [128, 4, D], BF16) for i in range(3)]
                class _E:
                    pass
                ES = []
                for si in range(2):
                    E = _E()
                    E.tki = k.sb("tki", [128, 16], I32)
                    E.xg = k.sb("xg", [128, D], BF16)
                    E.xgT = k.sb("xgT", [128, 8, 128], BF16)
                    E.gs = k.sb("gs", [128, 512], F32)
                    E.hb = k.sb("hb", [128, 512], BF16)
                    E.hbT = k.sb("hbT", [128, 4, 128], BF16)
                    E.yb = k.sb("yb", [128, D], F32)
                    E.bG = k.ps("p6g", [128, 512])
                    E.bU = k.ps("p6u", [128, 512])
                    E.bY = k.ps("p6y", [128, 512])
                    ES.append(E)

                def load_w(bk):
                    q = bk % 3
                    for hf in range(2):
                        ix = bass.IndirectOffsetOnAxis(ap=WIDX[:, bk, hf:hf + 1], axis=0)
                        k.dma('pool', wg[q][:, hf * 4:(hf + 1) * 4, :].rearrange("p a b -> p (a b)"), ex_gate[:, :], reads=[WIDX], pw=[wg[q]], indirect=(None, ix))
                        k.dma('pool', wu[q][:, hf * 4:(hf + 1) * 4, :].rearrange("p a b -> p (a b)"), ex_up[:, :], reads=[WIDX], pw=[wu[q]], indirect=(None, ix))
                        k.dma('pool', wd[q][:, hf * 2:(hf + 1) * 2, :].rearrange("p a b -> p (a b)"), ex_down[:, :], reads=[WIDX], pw=[wd[q]], indirect=(None, ix))

                def subtile(E, bk, sub):
                    q = bk % 3
                    r0 = (bk * SUB + sub) * 128
                    k.dma('sp', E.tki[:], TOKB[r0:r0 + 128, :], reads=[TOKB], writes=[E.tki])
                    k.dma('pool', E.xg[:], H2[:, :], reads=[H2, E.tki], writes=[E.xg],
                          indirect=(None, bass.IndirectOffsetOnAxis(ap=E.tki[:, 0:1], axis=0)))
                    yield
                    pxt = E.bU[:].bitcast(BF16)
                    xv = E.xg[:].rearrange("p (j kc) -> p kc j", kc=8)
                    for kc in range(8):
                        k.op('pe', lambda e: e.transpose(pxt[:, kc * 128:(kc + 1) * 128], xv[:, kc, :], identb[:]), reads=[E.xg, identb], writes=[E.bU] if kc == 0 else [], inc=(kc == 7))
                    k.op('act', lambda e: e.activation(E.xgT[:].rearrange("p a b -> p (a b)"), pxt, AF.Identity), reads=[E.bU], writes=[E.xgT])
                    yield
                    for kc in range(8):
                        k.op('pe', lambda e: e.matmul(E.bG[:], E.xgT[:, kc, :], wg[q][:, kc, :], start=(kc == 0), stop=(kc == 7)), reads=[E.xgT, wg[q]], writes=[E.bG] if kc == 0 else [], inc=(kc == 7))
                    for kc in range(8):
                        k.op('pe', lambda e: e.matmul(E.bU[:], E.xgT[:, kc, :], wu[q][:, kc, :], start=(kc == 0), stop=(kc == 7)), reads=[E.xgT, wu[q]], writes=[E.bU] if kc == 0 else [], inc=(kc == 7))
                    yield
                    k.op('act', lambda e: e.activation(E.gs[:], E.bG[:], AF.Silu), reads=[E.bG], writes=[E.gs])
                    k.op('dve', lambda e: e.tensor_tensor(E.hb[:], E.gs[:], E.bU[:], ALU.mult), reads=[E.gs, E.bU], writes=[E.hb])
                    yield
                    pht = E.bG[:].bitcast(BF16)
                    hv = E.hb[:].rearrange("p (j fc) -> p fc j", fc=4)
                    for fc in range(4):
                        k.op('pe', lambda e: e.transpose(pht[:, fc * 128:(fc + 1) * 128], hv[:, fc, :], identb[:]), reads=[E.hb, identb], writes=[E.bG] if fc == 0 else [], inc=(fc == 3))
                    k.op('dve', lambda e: e.tensor_copy(E.hbT[:].rearrange("p a b -> p (a b)"), pht[:, 0:512]), reads=[E.bG], writes=[E.hbT])
                    yield
                    for n in range(2):
                        for fc in range(4):
                            k.op('pe', lambda e: e.matmul(E.bY[:], E.hbT[:, fc, :], wd[q][:, fc, n * 512:(n + 1) * 512], start=(fc == 0), stop=(fc == 3)),
                                 reads=[E.hbT, wd[q]], writes=[E.bY] if fc == 0 else [], inc=(fc == 3))
                        k.op('act', lambda e: e.activation(E.yb[:, n * 512:(n + 1) * 512], E.bY[:], AF.Identity), reads=[E.bY], writes=[E.yb])
                        yield
                    k.dma('sp', YB[r0:r0 + 128, :], E.yb[:], reads=[E.yb], pw=[YB])

                def estream(si):
                    for gsub in range(si, NBLK * SUB, 2):
                        bk, sub = gsub // SUB, gsub % SUB
                        if sub == 0 or (sub == 1 and si == 1):
                            for bb in (bk, bk + 1):
                                if bb < NBLK and bb not in loaded:
                                    loaded.add(bb)
                                    load_w(bb)
                        yield from subtile(ES[si], bk, sub)
                        yield

                loaded = set()
                gens = [estream(0), estream(1)]
                alive = [True, True]
                while any(alive):
                    for gi in range(2):
                        if alive[gi]:
                            try:
                                next(gens[gi])
                            except StopIteration:
                                alive[gi] = False
            k.barrier()
            with ExitStack() as es8:
                k.es = es8
                if P4S < 4:
                    raise _Stop()
                x6 = [k.sb("x6%d" % i, [128, D], F32) for i in range(2)]
                y0 = [k.sb("y0%d" % i, [128, D], F32) for i in range(2)]
                y1 = [k.sb("y1%d" % i, [128, D], F32) for i in range(2)]
                o6 = [k.sb("o6%d" % i, [128, D], F32) for i in range(2)]
                st6 = k.sb("st6", [128, 2, 6], F32)
                mv6 = k.sb("mv6", [128, 2], F32)
                rs6 = k.sb("rs6", [128, 1], F32)
                for gi in range(NT):
                    b, i = gi // (SEQ // 128), gi % (SEQ // 128)
                    q = gi % 2
                    k.dma('sp', x6[q][:], X1[gi * 128:(gi + 1) * 128, :], reads=[X1], writes=[x6[q]])
                    k.dma('pool', y0[q][:], YB[:, :], reads=[YB, DST], writes=[y0[q]],
                          indirect=(None, bass.IndirectOffsetOnAxis(ap=DST[:, gi, 0:1], axis=0)))
                    k.dma('pool', y1[q][:], YB[:, :], reads=[YB, DST], writes=[y1[q]],
                          indirect=(None, bass.IndirectOffsetOnAxis(ap=DST[:, gi, 1:2], axis=0)))
                    o = o6[q]
                    k.op('dve', lambda e: e.tensor_scalar(y0[q][:], y0[q][:], W1[:, gi:gi + 1], None, ALU.mult), reads=[y0[q], W1], writes=[y0[q]])
                    k.op('dve', lambda e: e.scalar_tensor_tensor(y0[q][:], y1[q][:], W2[:, gi:gi + 1], y0[q][:], ALU.mult, ALU.add), reads=[y1[q], W2, y0[q]], writes=[y0[q]])
                    k.op('dve', lambda e: e.tensor_tensor(y0[q][:], y0[q][:], g2b[:, b, :], ALU.mult), reads=[y0[q], g2b], writes=[y0[q]])
                    k.op('dve', lambda e: e.scalar_tensor_tensor(o[:], x6[q][:], ALPHA, y0[q][:], ALU.mult, ALU.add), reads=[x6[q], y0[q]], writes=[o])
                    for hf in range(2):
                        k.op('dve', lambda e: e.bn_stats(st6[:, hf, :], o[:, hf * 512:(hf + 1) * 512]), reads=[o], writes=[st6])
                    k.op('dve', lambda e: e.bn_aggr(mv6[:], st6[:].rearrange("p a b -> p (a b)")), reads=[st6], writes=[mv6])
                    k.op('act', lambda e: e.activation(rs6[:], mv6[:, 1:2], AF.Sqrt, bias=LN_EPS), reads=[mv6], writes=[rs6])
                    k.op('dve', lambda e: e.reciprocal(rs6[:], rs6[:]), reads=[rs6], writes=[rs6])
                    k.op('dve', lambda e: e.tensor_scalar(o[:], o[:], mv6[:, 0:1], rs6[:, 0:1], ALU.subtract, ALU.mult), reads=[o, mv6, rs6], writes=[o])
                    k.op('dve', lambda e: e.tensor_tensor(o[:], o[:], lnp[:, 2, :], ALU.mult), reads=[o, lnp], writes=[o])
                    k.op('dve', lambda e: e.tensor_tensor(o[:], o[:], lnp[:, 3, :], ALU.add), reads=[o, lnp], writes=[o])
                    k.dma('sp', out_d[b, i * 128:(i + 1) * 128, :], o[:], reads=[o])
           except _Stop:
            pass
        k.barrier()

        k.barrier()
    return nc


def host_inputs(inputs, batches, NB):
    f = lambda a: np.ascontiguousarray(a, dtype=np.float32)
    bs = list(batches)
    m = {}
    m["x"] = f(inputs["x"][bs])
    m["ctx"] = f(inputs["ctx"][bs])
    cc = np.zeros((3, D), np.float32)
    for i, b in enumerate(bs):
        cc[i] = inputs["c"][b]
    cc[2] = inputs["c_ctx"]
    m["cc"] = cc
    m["w_ada"] = f(inputs["w_ada"][0])
    m["b_ada"] = f(inputs["b_ada"][0][None, :])
    m["w_in"] = f(inputs["w_in"][0])
    m["conv_w"] = f(inputs["conv_w"][0].reshape(9, 2560))
    bi, bf = inputs["m_bias_i"][0], inputs["m_bias_f"][0]
    m["m_bias"] = f(np.concatenate([bi[0], bf[0], bi[1], bf[1]])[:, None])
    m["ident"] = np.eye(128, dtype=np.float32)
    gm = np.zeros((32, 2), np.float32)
    gm[0:8, 0] = 1; gm[16:24, 0] = 1; gm[8:16, 1] = -1; gm[24:32, 1] = -1
    m["gmask"] = gm
    ii = np.arange(64)
    m["cmask"] = np.stack([(ii[:, None] <= ii[None, :]), (ii[:, None] >= ii[None, :])], axis=1).astype(np.float32)
    m["m_norm_w"] = f(inputs["m_norm_w"][0][None, :])
    hk = lambda v: np.asarray(v, np.float32).reshape(4, 128).T
    m["rp"] = f(np.stack([hk(inputs["r_w0"][0][0]), hk(inputs["r_w0"][0][1]), hk(inputs["r_a0"][0]), hk(inputs["r_kk"][0]),
                          hk(inputs["r_ka"][0]), hk(inputs["r_ka"][0]), hk(inputs["r_bonus"][0].reshape(-1))], axis=2))
    sm = np.ones((128, 1088), np.float32); sm[:, ::64] = 0
    obd = np.zeros((128, 128), np.float32); obd[:64, :64] = 1; obd[64:, 64:] = 1
    m["onesbd"] = obd
    m["smask"] = sm
    jj = np.arange(128) % 64
    tt = np.arange(128)
    rm = np.zeros((128, 2, 128), np.float32)
    for dd in range(2):
        for col in range(128):
            tq = col % 64
            if col < 64:
                rm[:, dd, col] = (jj < tq) if dd == 0 else (jj > tq)
            else:
                rm[:, dd, col] = (jj <= tq) if dd == 0 else (jj >= tq)
    m["rmask"] = rm
    m["nmask"] = np.stack([(ii[None, :] < ii[:, None]), (ii[None, :] > ii[:, None])], axis=1).astype(np.float32)
    m["r_norm_w"] = f(inputs["r_norm_w"][0][None, :])
    m["r_norm_b"] = f(inputs["r_norm_b"][0][None, :])
    m["r_wB"] = f(inputs["r_wB"][0])
    m["r_aB"] = f(inputs["r_aB"][0])
    m["r_gB"] = f(inputs["r_gB"][0])
    m["w_out"] = f(inputs["w_out"][0])
    for nm in ("ln1_g", "ln1_b", "ln2_g", "ln2_b"):
        m[nm] = f(inputs[nm][0][None, :])
    m["rt"] = f(np.concatenate([inputs["rt_g"][0], inputs["rt_e"][0]], axis=1))
    m["rtb"] = f(np.concatenate([inputs["rt_g_b"][0], inputs["rt_e_b"][0]])[None, :])
    m["ex_gate"] = f(inputs["ex_gate"][0].reshape(8192, 2048))
    m["ex_up"] = f(inputs["ex_up"][0].reshape(8192, 2048))
    m["ex_down"] = f(inputs["ex_down"][0].reshape(8192, 2048))
    pp = np.arange(128)
    m["tris"] = (pp[:, None] < pp[None, :]).astype(np.float32)
    m["thr"] = np.broadcast_to((512.0 * pp)[None, :], (128, 128)).astype(np.float32).copy()
    m["blki"] = np.broadcast_to(np.arange(160, dtype=np.float32)[None, :], (128, 160)).copy()
    m["kcp"] = (2 * pp[:, None] + (np.arange(12) % 2)[None, :]).astype(np.float32)
    m["tokid"] = np.broadcast_to((np.arange(64)[None, :] * 128 + pp[:, None])[:, :, None], (128, 64, 16)).astype(np.int32).copy()
    return m


_NC_CACHE = {}


def kernel(**inputs):
    inputs = {k_: np.asarray(v) for k_, v in inputs.items()}
    NB = 2
    n_cores = 8
    if NB not in _NC_CACHE:
        _NC_CACHE[NB] = build(NB=NB)
    nc = _NC_CACHE[NB]
    in_maps = [host_inputs(inputs, [NB * c + j for j in range(NB)], NB) for c in range(n_cores)]
    res = run_bass_kernel_spmd(nc, in_maps, core_ids=list(range(n_cores)))
    out = np.concatenate([np.asarray(r["out"]) for r in res.results], axis=0)
    return np.ascontiguousarray(out, dtype=np.float32)
```

```python
import math, os
from contextlib import ExitStack
import numpy as np
import concourse.bass as bass
import concourse.mybir as mybir
from concourse.bass_utils import run_bass_kernel_spmd

F32 = mybir.dt.float32
BF16 = mybir.dt.bfloat16
I32 = mybir.dt.int32
AF = mybir.ActivationFunctionType
ALU = mybir.AluOpType
AX = mybir.AxisListType

D = 1024
SEQ = 4096
CTX = 256
T = SEQ + CTX
NCH = T // 64
INC = 3936
DS = math.exp(-0.5)
ALPHA = 2.0 ** 0.25
LN_EPS = 1e-6
GN_EPS = 64e-5
SEC = dict(mq=0, mk=512, rr=1024, rk=1536, rv=2048, mv=2560, mo=3072, gates=3584,
           lwf=3616, lwb=3680, la=3744, lg=3808)
NDS = 40


class _Stop(Exception):
    pass


class Buf:
    def __init__(self, t):
        self.t = t
        self.w = {}
        self.r = {}

    def __getitem__(self, k):
        return self.t[k]


def _merge(d, s):
    for k, v in s.items():
        if d.get(k, 0) < v:
            d[k] = v


class KB:
    def __init__(self, nc):
        self.nc = nc
        self.engs = {'pe': nc.tensor, 'dve': nc.vector, 'act': nc.scalar, 'pool': nc.gpsimd, 'sp': nc.sync}
        self.esem = {e: nc.alloc_semaphore('es_' + e) for e in self.engs}
        self.ecnt = {e: 0 for e in self.engs}
        self.pending = {e: False for e in self.engs}
        self.seen = {e: {} for e in self.engs}
        self.dsem = [nc.alloc_semaphore('ds%d' % i) for i in range(NDS)]
        self.dcnt = [0] * NDS
        self.dnext = 0
        self.es = None
        self.uid = 0

    def semh(self, key):
        return self.esem[key] if isinstance(key, str) else self.dsem[key[1]]

    def _wait(self, eng, need):
        for key, cnt in need.items():
            if self.seen[eng].get(key, 0) >= cnt:
                continue
            if key == eng and eng in ('pe',):
                continue
            self.engs[eng].wait_ge(self.semh(key), cnt)
            self.seen[eng][key] = cnt

    def op(self, eng, fn, reads=(), writes=(), inc=True, pw_=()):
        need = {}
        for b in pw_:
            _merge(need, b.r)
        for b in reads:
            _merge(need, b.w)
            if getattr(b, 'excl', False):
                _merge(need, {kk: vv for kk, vv in b.r.items() if kk != eng})
        for b in writes:
            _merge(need, b.w)
            _merge(need, b.r)
        self._wait(eng, need)
        ins = fn(self.engs[eng])
        cnt = self.ecnt[eng] + 1
        if inc:
            ins.then_inc(self.esem[eng], 1)
            self.ecnt[eng] = cnt
        for b in reads:
            b.r[eng] = cnt
        for b in writes:
            b.w = {eng: cnt}
            b.r = {}
        for b in pw_:
            b.w[eng] = cnt
        return ins

    def dma(self, q, out, in_, reads=(), writes=(), pw=(), indirect=None, **kw):
        i = self.dnext
        self.dnext = (i + 1) % NDS
        need = {}
        if self.dcnt[i]:
            need[('d', i)] = self.dcnt[i]
        for b in reads:
            _merge(need, b.w)
        for b in writes:
            _merge(need, b.w)
            _merge(need, b.r)
        for b in pw:
            _merge(need, b.r)
        self._wait(q, need)
        if indirect is None:
            ins = self.engs[q].dma_start(out=out, in_=in_, **kw)
        else:
            ins = self.engs[q].indirect_dma_start(out, indirect[0], in_, indirect[1], **kw)
        self.dcnt[i] += 16
        ins.then_inc(self.dsem[i], 16)
        key = ('d', i)
        cnt = self.dcnt[i]
        for b in reads:
            b.r[key] = cnt
        for b in writes:
            b.w = {key: cnt}
            b.r = {}
        for b in pw:
            b.w[key] = cnt
        return ins

    def barrier(self):
        need = {e: c for e, c in self.ecnt.items() if c}
        for i in range(NDS):
            if self.dcnt[i]:
                need[('d', i)] = self.dcnt[i]
        for e in self.engs:
            self._wait(e, need)

    def sb(self, name, shape, dt):
        self.uid += 1
        return Buf(self.es.enter_context(self.nc.sbuf_tensor("s%d_%s" % (self.uid, name), list(shape), dt)))

    def ps(self, name, shape, dt=F32):
        self.uid += 1
        return Buf(self.es.enter_context(self.nc.psum_tensor("p%d_%s" % (self.uid, name), list(shape), dt)))


def build(NB=2, debug=None, phases=(0, 1, 2, 3, 4, 5, 6)):
    nc = bass.Bass("TRN2", target_bir_lowering=False)
    k = KB(nc)

    def din(name, shape):
        return nc.dram_tensor(name, list(shape), F32, kind="ExternalInput").ap()

    x_in = din("x", [NB, SEQ, D])
    ctx_in = din("ctx", [NB, CTX, D])
    cc_in = din("cc", [3, D])
    w_ada = din("w_ada", [D, 6 * D])
    b_ada = din("b_ada", [1, 6 * D])
    w_in = din("w_in", [D, INC])
    conv_w = din("conv_w", [9, 2560])
    m_bias = din("m_bias", [32, 1])
    ident_in = din("ident", [128, 128])
    gmask_in = din("gmask", [32, 2])
    cmask_in = din("cmask", [64, 2, 64])
    m_norm_w = din("m_norm_w", [1, 512])
    rp_in = din("rp", [128, 4, 7])
    smask_in = din("smask", [128, 1088])
    onesbd_in = din("onesbd", [128, 128])
    rmask_in = din("rmask", [128, 2, 128])
    nmask_in = din("nmask", [64, 2, 64])
    r_norm_w = din("r_norm_w", [1, 512])
    r_norm_b = din("r_norm_b", [1, 512])
    r_wB = din("r_wB", [2, 64, 512])
    r_aB = din("r_aB", [64, 512])
    r_gB = din("r_gB", [128, 512])
    w_out = din("w_out", [D, D])
    ln1_g = din("ln1_g", [1, D]); ln1_b = din("ln1_b", [1, D]); ln2_g = din("ln2_g", [1, D]); ln2_b = din("ln2_b", [1, D])
    rt_in = din("rt", [D, 36]); rtb_in = din("rtb", [1, 36])
    ex_gate = din("ex_gate", [8192, 2048]); ex_up = din("ex_up", [8192, 2048]); ex_down = din("ex_down", [8192, 2048])
    tris_in = din("tris", [128, 128]); thr_in = din("thr", [128, 128]); blki_in = din("blki", [128, 160]); kcp_in = din("kcp", [128, 12])
    tokid_in = nc.dram_tensor("tokid", [128, 64, 16], I32, kind="ExternalInput").ap()
    out_d = nc.dram_tensor("out", [NB, SEQ, D], F32, kind="ExternalOutput").ap()

    def dscr(name, shape, dt=F32):
        kind = "ExternalOutput" if (debug and name in debug) else "Internal"
        return Buf(nc.dram_tensor(name, list(shape), dt, kind=kind).ap())

    MODD = dscr("MODD", [3, 6 * D])
    FM = [dscr("FM%d" % b, [INC, T]) for b in range(NB)]
    MIX = [dscr("MIX%d" % b, [SEQ, D]) for b in range(NB)]
    BS = 512
    NBLK_ = NB * SEQ * 2 // BS + 32
    X1 = dscr("X1", [NB * SEQ, D])
    H2 = dscr("H2", [NB * SEQ, D], BF16)
    TOKB = dscr("TOKB", [NBLK_ * BS, 16], I32)
    YB = dscr("YB", [NBLK_ * BS, D])

    with ExitStack() as es0:
        k.es = es0
        ident = k.sb("ident", [128, 128], F32)
        identb = k.sb("identb", [128, 128], BF16)
        ones_f = k.sb("ones_f", [128, 128], F32)
        modT = k.sb("modT", [128, 48, 3], F32)
        k.dma('sp', ident[:], ident_in[:, :], writes=[ident])
        k.op('dve', lambda e: e.tensor_copy(identb[:], ident[:]), reads=[ident], writes=[identb])

        with ExitStack() as es:
            k.es = es
            cc = k.sb("cc", [3, D], F32)
            scT = k.sb("scT", [128, 8, 3], F32)
            bada = k.sb("bada", [3, 6 * D], F32)
            mods = k.sb("mods", [3, 6 * D], F32)
            wa = [k.sb("wa%d" % i, [128, 8, 512], F32) for i in range(2)]
            pst = k.ps("p0t", [128, 8, 3])
            psm = [k.ps("p0m%d" % i, [3, 512]) for i in range(2)]
            pmt = k.ps("p0mt", [128, 48, 3])
            k.dma('sp', cc[:], cc_in[:, :], writes=[cc])
            k.dma('sp', bada[:], b_ada[0:1, :].partition_broadcast(3), writes=[bada])
            k.op('act', lambda e: e.activation(cc[:], cc[:], AF.Silu), reads=[cc], writes=[cc])
            for kc in range(8):
                k.op('pe', lambda e: e.transpose(pst[:, kc, :], cc[:, kc * 128:(kc + 1) * 128], ident[0:3, 0:3]),
                     reads=[cc, ident], writes=[pst], inc=(kc == 7))
            k.op('dve', lambda e: e.tensor_copy(scT[:], pst[:]), reads=[pst], writes=[scT])
            for n in range(12):
                wb = wa[n % 2]
                k.dma('sp', wb[:], w_ada[:, n * 512:(n + 1) * 512].rearrange("(kc p) n -> p kc n", p=128), writes=[wb])
                pm = psm[n % 2]
                for kc in range(8):
                    k.op('pe', lambda e: e.matmul(pm[:], scT[:, kc, :], wb[:, kc, :], start=(kc == 0), stop=(kc == 7)),
                         reads=[scT, wb], writes=[pm] if kc == 0 else [], inc=(kc == 7))
                k.op('dve', lambda e: e.tensor_tensor(mods[:, n * 512:(n + 1) * 512], pm[:], bada[:, n * 512:(n + 1) * 512], ALU.add),
                     reads=[pm, bada], writes=[mods])
            k.dma('sp', MODD[:, :], mods[:], reads=[mods], writes=[MODD])
            for j in range(48):
                k.op('pe', lambda e: e.transpose(pmt[:, j, :], mods[:, j * 128:(j + 1) * 128], ident[0:3, 0:3]),
                     reads=[mods, ident], writes=[pmt], inc=(j == 47))
            k.op('dve', lambda e: e.tensor_copy(modT[:], pmt[:]), reads=[pmt], writes=[modT])
            for j0 in (8, 32):
                k.op('dve', lambda e: e.tensor_scalar(modT[:, j0:j0 + 8, :], modT[:, j0:j0 + 8, :], 1.0, None, ALU.add),
                     reads=[modT], writes=[modT])
        k.barrier()

        with ExitStack() as es:
          if 1 in phases:
              k.es = es
              wbf = k.sb("wbf", [128, 8, INC], BF16)
              hT = k.sb("hT", [128, 8, T], BF16)
              xt = [k.sb("xt%d" % i, [128, D], F32) for i in range(2)]
              xn = [k.sb("xn%d" % i, [128, D], BF16) for i in range(2)]
              st = k.sb("st", [128, 2, 6], F32)
              mv = k.sb("mv", [128, 2], F32)
              rstd = k.sb("rstd", [128, 1], F32)
              pT = [k.sb("pT%d" % i, [128, T], F32) for i in range(2)]
              acc = k.sb("acc", [128, T], F32)
              cw = k.sb("cw", [128, 20, 9], F32)
              mb = k.sb("mb", [32, 4], F32)
              ptr = [k.ps("p1t%d" % i, [128, 8, 128], BF16) for i in range(2)]
              pmm = [k.ps("p1m%d" % i, [128, 512]) for i in range(3)]
              pcw = k.ps("p1cw", [128, 20, 9])
              for kc in range(8):
                  for hf in range(2):
                      k.dma('pool', wbf[:, kc, hf * 1968:(hf + 1) * 1968],
                            w_in[kc * 128:(kc + 1) * 128, hf * 1968:(hf + 1) * 1968], pw=[wbf])
              crow = k.sb("crow", [9, 2560], F32)
              k.dma('sp', crow[:], conv_w[:, :], writes=[crow])
              for c in range(20):
                  k.op('pe', lambda e: e.transpose(pcw[:, c, :], crow[:, c * 128:(c + 1) * 128], ident[0:9, 0:9]),
                       reads=[crow, ident], writes=[pcw], inc=(c == 19))
              k.op('dve', lambda e: e.tensor_copy(cw[:], pcw[:]), reads=[pcw], writes=[cw])
              k.dma('sp', mb[:, 0:1], m_bias[:, :], writes=[mb])
              k.op('dve', lambda e: e.tensor_scalar(mb[:, 1:2], mb[:, 0:1], -1.0, None, ALU.mult), reads=[mb], writes=[mb])
              k.dma('sp', mb[:, 2:4], gmask_in[:, :], pw=[mb])
              for b in range(NB):
                  for i in range(int(os.environ.get('P1A', T // 128))):
                      xb, xnb, pt = xt[i % 2], xn[i % 2], ptr[i % 2]
                      src = ctx_in[b, i * 128:(i + 1) * 128, :] if i < 2 else x_in[b, (i - 2) * 128:(i - 1) * 128, :]
                      r = 2 if i < 2 else b
                      k.dma('sp', xb[:], src, writes=[xb])
                      S1 = int(os.environ.get('P1S', 9))
                      for hf in range(2):
                          k.op('dve', lambda e: e.bn_stats(st[:, hf, :], xb[:, hf * 512:(hf + 1) * 512]), reads=[xb], writes=[st])
                      if S1 >= 2: k.op('dve', lambda e: e.bn_aggr(mv[:], st[:].rearrange("p a b -> p (a b)")), reads=[st], writes=[mv])
                      if S1 >= 3: k.op('act', lambda e: e.activation(rstd[:], mv[:, 1:2], AF.Sqrt, bias=LN_EPS), reads=[mv], writes=[rstd])
                      if S1 >= 4: k.op('dve', lambda e: e.reciprocal(rstd[:], rstd[:]), reads=[rstd], writes=[rstd])
                      if S1 >= 5: k.op('dve', lambda e: e.tensor_scalar(xnb[:], xb[:], mv[:, 0:1], rstd[:, 0:1], ALU.subtract, ALU.mult),
                           reads=[xb, mv, rstd], writes=[xnb])
                      for kc in range(8 if S1 >= 6 else 0):
                          k.op('pe', lambda e: e.transpose(pt[:, kc, :], xnb[:, kc * 128:(kc + 1) * 128], identb[:]),
                               reads=[xnb, identb], writes=[pt] if kc == 0 else [], inc=(kc == 7))
                      for kc in range(8 if S1 >= 7 else 0):
                          if i % 2 == 0:
                              k.op('act', lambda e: e.activation(hT[:, kc, i * 128:(i + 1) * 128], pt[:, kc, :], AF.Identity,
                                                                 bias=modT[:, kc, r:r + 1], scale=modT[:, 8 + kc, r:r + 1]),
                                   reads=[pt, modT], writes=[] if (i or kc) else [hT])
                          else:
                              k.op('dve', lambda e: e.tensor_scalar(hT[:, kc, i * 128:(i + 1) * 128], pt[:, kc, :],
                                                                    modT[:, 8 + kc, r:r + 1], modT[:, kc, r:r + 1], ALU.mult, ALU.add),
                                   reads=[pt, modT], writes=[])
                      hT.w['act'] = k.ecnt['act']
                      hT.w['dve'] = k.ecnt['dve']
                  chunks = [(c * 128, 128) for c in range(28)] + [(3584, 32), (3616, 64), (3680, 64), (3744, 64), (3808, 128)]
                  for ci, (c0, M) in enumerate(chunks[:int(os.environ.get('P1C', 99))]):
                      pb = pT[ci % 2]
                      for g in range(9):
                          t0 = g * 512
                          n = min(512, T - t0)
                          pm = pmm[(ci * 9 + g) % 3]
                          for kc in range(8):
                              k.op('pe', lambda e: e.matmul(pm[0:M, 0:n], wbf[:, kc, c0:c0 + M], hT[:, kc, t0:t0 + n],
                                                            start=(kc == 0), stop=(kc == 7)),
                                   reads=[wbf, hT], writes=[pm] if kc == 0 else [], inc=(kc == 7))
                          k.op('act', lambda e: e.activation(pb[0:M, t0:t0 + n], pm[0:M, 0:n], AF.Identity),
                               reads=[pm], writes=[pb] if g == 0 else [])
                          pb.w['act'] = k.ecnt['act']
                      src = pb
                      if c0 < 2560:
                          c = c0 // 128
                          k.op('act', lambda e: e.activation(acc[:, :], pb[:, :], AF.Identity, scale=cw[:, c, 4:5]),
                               reads=[pb, cw], writes=[acc])
                          k.op('dve', lambda e: e.scalar_tensor_tensor(acc[:, 1:CTX], pb[:, 0:CTX - 1], cw[:, c, 3:4], acc[:, 1:CTX], ALU.mult, ALU.add),
                               reads=[pb, cw, acc], writes=[acc])
                          k.op('dve', lambda e: e.scalar_tensor_tensor(acc[:, 0:CTX - 1], pb[:, 1:CTX], cw[:, c, 5:6], acc[:, 0:CTX - 1], ALU.mult, ALU.add),
                               reads=[pb, cw, acc], writes=[acc])
                          a3 = acc[:, CTX:T].rearrange("p (r c) -> p r c", c=64)
                          p3 = pb[:, CTX:T].rearrange("p (r c) -> p r c", c=64)
                          for ky in range(3):
                              for kx in range(3):
                                  if ky == 1 and kx == 1:
                                      continue
                                  dy, dx = ky - 1, kx - 1
                                  oy0, oy1 = max(0, -dy), 64 - max(0, dy)
                                  ox0, ox1 = max(0, -dx), 64 - max(0, dx)
                                  k.op('dve', lambda e: e.scalar_tensor_tensor(
                                      a3[:, oy0:oy1, ox0:ox1], p3[:, oy0 + dy:oy1 + dy, ox0 + dx:ox1 + dx],
                                      cw[:, c, ky * 3 + kx:ky * 3 + kx + 1], a3[:, oy0:oy1, ox0:ox1], ALU.mult, ALU.add),
                                      reads=[pb, cw, acc], writes=[acc])
                          src = acc
                      sec = [s for s, v in SEC.items() if v <= c0][-1]
                      if sec in ('mq', 'mk'):
                          k.op('act', lambda e: e.activation(acc[:, :], src[:, :], AF.Silu), reads=[src], writes=[acc])
                          if sec == 'mk':
                              k.op('dve', lambda e: e.tensor_scalar(acc[:, :], acc[:, :], 0.125, None, ALU.mult), reads=[acc], writes=[acc])
                          src = acc
                      elif sec in ('mo', 'lg'):
                          k.op('act', lambda e: e.activation(acc[0:M, :], src[0:M, :], AF.Sigmoid), reads=[src], writes=[acc])
                          src = acc
                      elif sec in ('lwf', 'lwb'):
                          k.op('act', lambda e: e.activation(acc[0:M, :], src[0:M, :], AF.Tanh), reads=[src], writes=[acc])
                          src = acc
                      elif sec == 'gates':
                          tmp = pT[1 - ci % 2]
                          k.op('act', lambda e: e.activation(tmp[0:32, :], pb[0:32, :], AF.Exp, bias=mb[:, 1:2], scale=-1.0),
                               reads=[pb, mb], writes=[tmp])
                          k.op('act', lambda e: e.activation(tmp[0:32, :], tmp[0:32, :], AF.Ln, bias=1.0), reads=[tmp], writes=[tmp])
                          k.op('dve', lambda e: e.tensor_scalar(tmp[0:32, :], tmp[0:32, :], mb[:, 3:4], None, ALU.mult), reads=[tmp, mb], writes=[tmp])
                          k.op('dve', lambda e: e.tensor_scalar(acc[0:32, :], pb[0:32, :], mb[:, 0:1], mb[:, 2:3], ALU.add, ALU.mult),
                               reads=[pb, mb], writes=[acc])
                          k.op('dve', lambda e: e.tensor_tensor(acc[0:32, :], acc[0:32, :], tmp[0:32, :], ALU.add), reads=[acc, tmp], writes=[acc])
                          src = acc
                      k.dma('sp', FM[b][c0:c0 + M, :], src[0:M, :], reads=[src], pw=[FM[b]])
        k.barrier()


        with ExitStack() as es:
          if 2 in phases:
            k.es = es
            cm = k.sb("cm", [64, 2, 64], F32)
            k.dma('sp', cm[:], cmask_in[:, :, :], writes=[cm])
            nw = k.sb("nw", [64, 512], F32)
            k.dma('sp', nw[:], m_norm_w[0:1, :].partition_broadcast(64), writes=[nw])
            k.op('pool', lambda e: e.memset(ones_f[:], 1.0), writes=[ones_f])
            GA = k.sb("GA", [64, NCH, 48], F32)
            gT = k.sb("gT", [32, T], F32)
            G = k.sb("G", [64, 32], F32)
            qh = k.sb("qh", [64, 2, T], BF16)
            kh = k.sb("kh", [64, 2, T], BF16)
            vT = k.sb("vT", [128, T], BF16)
            moT = k.sb("moT", [128, T], F32)
            Hf = k.sb("Hf", [64, NCH, 2, 64], F32)
            class _M:
                pass
            MS = []
            for si in range(2):
                M = _M()
                M.i = si
                M.Ktm = k.sb("Ktm", [64, 2, 64], BF16)
                M.Vaug = k.sb("Vaug", [64, 2, 66], BF16)
                M.PTm = k.sb("PTm", [64, 2, 64], BF16)
                M.Cst = k.sb("Cst", [64, 2, 66], F32)
                M.Cbf = k.sb("Cbf", [64, 2, 66], BF16)
                M.dn = k.sb("dn", [64, 2], F32)
                M.ff = k.sb("ff", [64, 2], F32)
                M.hs = k.sb("hs", [64, 2, 64], F32)
                M.st2 = k.sb("st2", [64, 2, 6], F32)
                M.mv2 = k.sb("mv2", [64, 2, 2], F32)
                M.rs2 = k.sb("rs2", [64, 2], F32)
                M.om = k.sb("om", [64, 128], F32)
                M.bA = k.ps("p2A", [64, 512])
                M.bB = k.ps("p2B", [64, 512])
                M.bA.excl = True
                M.bB.excl = True
                MS.append(M)
            pg = k.ps("p2g", [64, 32])
            pbb = k.ps("p2b", [64, 32])
            for b in range(NB):
                k.dma('sp', gT[:], FM[b][3584:3616, :], reads=[FM[b]], writes=[gT])
                for c in range(NCH):
                    k.op('pe', lambda e: e.transpose(pg[:], gT[:, c * 64:(c + 1) * 64], ident[0:32, 0:32]), reads=[gT, ident], writes=[pg])
                    k.op('dve', lambda e: e.tensor_copy(G[:], pg[:]), reads=[pg], writes=[G])
                    k.op('pe', lambda e: e.matmul(pbb[:, 0:8], cm[:, 0, :], G[:, 8:16], start=True, stop=True), reads=[cm, G], writes=[pbb], inc=False)
                    k.op('pe', lambda e: e.matmul(pbb[:, 8:16], cm[:, 1, :], G[:, 24:32], start=True, stop=True), reads=[cm, G], inc=False)
                    k.op('pe', lambda e: e.matmul(pbb[:, 16:24], ones_f[0:64, 0:64], G[:, 8:16], start=True, stop=True), reads=[ones_f, G], inc=False)
                    k.op('pe', lambda e: e.matmul(pbb[:, 24:32], ones_f[0:64, 0:64], G[:, 24:32], start=True, stop=True), reads=[ones_f, G])
                    k.op('act', lambda e: e.activation(GA[:, c, 0:32], pbb[:], AF.Exp), reads=[pbb], writes=[GA])
                    k.op('dve', lambda e: e.tensor_tensor(G[:, 0:8], G[:, 0:8], pbb[:, 0:8], ALU.subtract), reads=[pbb, G], writes=[G])
                    k.op('dve', lambda e: e.tensor_tensor(G[:, 16:24], G[:, 16:24], pbb[:, 8:16], ALU.subtract), reads=[pbb, G], writes=[G])
                    k.op('act', lambda e: e.activation(GA[:, c, 32:40], G[:, 0:8], AF.Exp), reads=[G], writes=[GA])
                    k.op('act', lambda e: e.activation(GA[:, c, 40:48], G[:, 16:24], AF.Exp), reads=[G], writes=[GA])
                for hp in range(4):
                    for h in range(2):
                        r0 = hp * 128 + h * 64
                        for q4 in range(4):
                            t0 = q4 * 1088
                            k.dma('pool', qh[:, h, t0:t0 + 1088], FM[b][r0:r0 + 64, t0:t0 + 1088], reads=[FM[b]], pw=[qh])
                            k.dma('pool', kh[:, h, t0:t0 + 1088], FM[b][512 + r0:512 + r0 + 64, t0:t0 + 1088], reads=[FM[b]], pw=[kh])
                    for q4 in range(4):
                        t0 = q4 * 1088
                        k.dma('pool', vT[:, t0:t0 + 1088], FM[b][2560 + hp * 128:2560 + (hp + 1) * 128, t0:t0 + 1088], reads=[FM[b]], pw=[vT])
                    k.dma('sp', moT[:], FM[b][3072 + hp * 128:3072 + (hp + 1) * 128, :], reads=[FM[b]], writes=[moT])
                    def mstream(M, d, done):
                        Ktm, Vaug, PTm, Cst, Cbf, dn, ff, hs, st2, mv2, rs2, om = M.Ktm, M.Vaug, M.PTm, M.Cst, M.Cbf, M.dn, M.ff, M.hs, M.st2, M.mv2, M.rs2, M.om
                        bA, bB = M.bA, M.bB
                        pk = bA[:, 0:64].bitcast(BF16).rearrange("p (h e) -> p h e", h=2)
                        pv = bA[:, 64:128].bitcast(BF16)
                        pp = bA[:, 128:256].rearrange("p (h e) -> p h e", h=2)
                        po = bB[:, 0:132].rearrange("p (h e) -> p h e", h=2)
                        pc = bB[:, 132:264].rearrange("p (h e) -> p h e", h=2)
                        pmo = bB[:, 264:392]
                        order = list(range(NCH)) if d == 0 else [3, 2, 1, 0] + list(range(NCH - 1, 3, -1))
                        k.op('pool', lambda e: e.memset(Cst[:], 0.0), writes=[Cst])
                        k.op('pool', lambda e: e.memset(Cbf[:], 0.0), writes=[Cbf])
                        for c in order:
                            cs = slice(c * 64, (c + 1) * 64)
                            hh = 2 * hp
                            a_ap = GA[:, c, d * 8 + hh:d * 8 + hh + 2]
                            e_ap = GA[:, c, 16 + d * 8 + hh:16 + d * 8 + hh + 2]
                            c_ap = GA[:, c, 32 + d * 8 + hh:32 + d * 8 + hh + 2]
                            needy = c >= 4
                            second = c in done
                            fin = needy and second
                            for h in range(2):
                                k.op('pe', lambda e: e.transpose(pk[:, h, :], kh[:, h, cs], identb[0:64, 0:64]), reads=[kh, identb], writes=[bA] if h == 0 else [], inc=False)
                            k.op('pe', lambda e: e.transpose(pv, vT[:, cs], identb[:]), reads=[vT, identb], inc=False)
                            for h in range(2):
                                k.op('pe', lambda e: e.matmul(pp[:, h, :], kh[:, h, cs], qh[:, h, cs], start=True, stop=True), reads=[kh, qh], inc=(h == 1))
                            k.op('act', lambda e: e.activation(Ktm[:], pk, AF.Identity), reads=[bA], writes=[Ktm])
                            k.op('dve', lambda e: e.tensor_tensor(Vaug[:, :, 0:64], pv.rearrange("p (h e) -> p h e", h=2),
                                                                  c_ap.unsqueeze(2).to_broadcast([64, 2, 64]), ALU.mult), reads=[bA, GA], writes=[Vaug])
                            k.op('act', lambda e: e.activation(Vaug[:, :, 64:65], c_ap.unsqueeze(2), AF.Identity), reads=[GA, Vaug], writes=[Vaug])
                            k.op('dve', lambda e: e.tensor_tensor(PTm[:], pp, cm[:, d:d + 1, :].to_broadcast([64, 2, 64]), ALU.mult), reads=[bA, cm], writes=[PTm])
                            yield
                            for h in range(2):
                                k.op('pe', lambda e: e.matmul(po[:, h, 0:65], PTm[:, h, :], Vaug[:, h, 0:65], start=True, stop=False), reads=[PTm, Vaug], writes=[bB] if h == 0 else [], inc=False)
                                k.op('pe', lambda e: e.matmul(po[:, h, 0:65], qh[:, h, cs], Cbf[:, h, 0:65], start=False, stop=True), reads=[qh, Cbf], inc=False)
                            if fin:
                                k.op('pe', lambda e: e.transpose(pmo, moT[:, cs], ident[:]), reads=[moT, ident], inc=False)
                            for h in range(2):
                                k.op('pe', lambda e: e.matmul(pc[:, h, 0:65], Ktm[:, h, :], Vaug[:, h, 0:65], start=True, stop=True), reads=[Ktm, Vaug], inc=(h == 1))
                            yield
                            k.op('dve', lambda e: e.tensor_tensor(Cst[:, :, 0:65], Cst[:, :, 0:65], pc[:, :, 0:65], ALU.add), reads=[bB, Cst], writes=[Cst])
                            k.op('dve', lambda e: e.tensor_tensor(Cst[:, :, 0:65], Cst[:, :, 0:65], e_ap.unsqueeze(2).to_broadcast([64, 2, 65]), ALU.mult), reads=[GA, Cst], writes=[Cst])
                            k.op('act', lambda e: e.activation(Cbf[:, :, 0:65], Cst[:, :, 0:65], AF.Identity), reads=[Cst], writes=[Cbf])
                            if needy:
                                k.op('dve', lambda e: e.tensor_tensor(dn[:], po[:, :, 64], a_ap, ALU.mult), reads=[bB, GA], writes=[dn])
                                k.op('act', lambda e: e.activation(dn[:], dn[:], AF.Abs), reads=[dn], writes=[dn])
                                k.op('dve', lambda e: e.tensor_scalar(dn[:], dn[:], 1.0, None, ALU.max), reads=[dn], writes=[dn])
                                k.op('dve', lambda e: e.reciprocal(dn[:], dn[:]), reads=[dn], writes=[dn])
                                k.op('dve', lambda e: e.tensor_tensor(ff[:], dn[:], a_ap, ALU.mult), reads=[dn, GA], writes=[ff])
                                if not second:
                                    k.op('dve', lambda e: e.tensor_tensor(Hf[:, c, :, :], po[:, :, 0:64], ff[:].unsqueeze(2).to_broadcast([64, 2, 64]), ALU.mult), reads=[bB, ff], pw_=[Hf])
                                    done[c] = M.i
                                else:
                                    k.op('dve', lambda e: e.tensor_tensor(hs[:], po[:, :, 0:64], ff[:].unsqueeze(2).to_broadcast([64, 2, 64]), ALU.mult), reads=[bB, ff], writes=[hs])
                                    k.op('dve', lambda e: e.tensor_tensor(hs[:], hs[:], Hf[:, c, :, :], ALU.add), reads=[hs, Hf], writes=[hs])
                            if fin:
                                for h in range(2):
                                    k.op('dve', lambda e: e.bn_stats(st2[:, h, :], hs[:, h, :]), reads=[hs], writes=[st2])
                                for h in range(2):
                                    k.op('dve', lambda e: e.bn_aggr(mv2[:, h, :], st2[:, h, :]), reads=[st2], writes=[mv2])
                                k.op('act', lambda e: e.activation(rs2[:], mv2[:, :, 1], AF.Sqrt, bias=LN_EPS), reads=[mv2], writes=[rs2])
                                k.op('dve', lambda e: e.reciprocal(rs2[:], rs2[:]), reads=[rs2], writes=[rs2])
                                for h in range(2):
                                    k.op('dve', lambda e: e.tensor_scalar(hs[:, h, :], hs[:, h, :], mv2[:, h, 0:1], rs2[:, h:h + 1], ALU.subtract, ALU.mult),
                                         reads=[hs, mv2, rs2], writes=[hs])
                                k.op('dve', lambda e: e.tensor_tensor(om[:], hs[:].rearrange("p h e -> p (h e)"), nw[:, hp * 128:(hp + 1) * 128], ALU.mult),
                                     reads=[hs, nw], writes=[om])
                                k.op('dve', lambda e: e.tensor_tensor(om[:], om[:], pmo, ALU.mult), reads=[om, bB], writes=[om])
                                k.dma('sp', MIX[b][(c - 4) * 64:(c - 3) * 64, hp * 128:(hp + 1) * 128], om[:], reads=[om], pw=[MIX[b]])
                            yield

                    done = {}
                    gens = [mstream(MS[0], 0, done), mstream(MS[1], 1, done)]
                    alive = [True, True]
                    while any(alive):
                        for gi in range(2):
                            if alive[gi]:
                                try:
                                    next(gens[gi])
                                except StopIteration:
                                    alive[gi] = False
        k.barrier()

        with ExitStack() as es:
          if 3 in phases:
            k.es = es
            NBK = 512
            NBC = 8
            rp1 = k.sb("rp1", [128, 4, 8], F32)
            k.dma('sp', rp1[:, :, 0:7], rp_in[:, :, :], writes=[rp1])
            k.op('dve', lambda e: e.tensor_scalar(rp1[:, :, 7], rp1[:, :, 4], -1.0, 1.0, ALU.mult, ALU.add), reads=[rp1], writes=[rp1])
            smask = k.sb("smask", [128, NBK], F32)
            k.dma('sp', smask[:], smask_in[:, 0:NBK], writes=[smask])
            onesbd = k.sb("onesbd", [128, 128], F32)
            k.dma('sp', onesbd[:], onesbd_in[:, :], writes=[onesbd])
            ARs = k.sb("ARs", [128, NBC, 128], BF16)
            ZTs = k.sb("ZTs", [128, NBC, 128], BF16)
            PRs = k.sb("PRs", [128, NBK], BF16)
            GLs = k.sb("GLs", [128, NBC], F32)
            rmask = k.sb("rmask", [128, 2, 128], F32)
            k.dma('sp', rmask[:], rmask_in[:, :, :], writes=[rmask])
            nmask = k.sb("nmask", [64, 2, 64], F32)
            k.dma('sp', nmask[:], nmask_in[:, :, :], writes=[nmask])
            gnw = k.sb("gnw", [64, 2, 512], F32)
            k.dma('sp', gnw[:, 0, :], r_norm_w[0:1, :].partition_broadcast(64), pw=[gnw])
            k.dma('sp', gnw[:, 1, :], r_norm_b[0:1, :].partition_broadcast(64), pw=[gnw])
            wBb = k.sb("wBb", [64, 2, 512], BF16)
            aBb = k.sb("aBb", [64, 512], BF16)
            gBb = k.sb("gBb", [128, 512], BF16)
            k.dma('pool', wBb[:, 0, :], r_wB[0, :, :], pw=[wBb])
            k.dma('pool', wBb[:, 1, :], r_wB[1, :, :], pw=[wBb])
            k.dma('pool', aBb[:], r_aB[:, :], writes=[aBb])
            k.dma('pool', gBb[:], r_gB[:, :], writes=[gBb])
            k.op('pool', lambda e: e.memset(ones_f[:], 1.0), writes=[ones_f])
            onesb = k.sb("onesb", [64, 2], BF16)
            k.op('pool', lambda e: e.memset(onesb[:], 1.0), writes=[onesb])
            lgb = k.sb("lgb", [128, T], BF16)
            lab = k.sb("lab", [64, NBK], BF16)
            lwb_ = k.sb("lwb_", [64, NBK], BF16)
            rr = k.sb("rr", [128, NBK], F32)
            rk = k.sb("rk", [128, NBK], F32)
            aa = k.sb("aa", [128, NBK], F32)
            t1 = k.sb("t1", [128, NBK], F32)
            t2 = k.sb("t2", [128, NBK], F32)
            khat = k.sb("khat", [128, NBK], F32)
            kmod = k.sb("kmod", [128, NBK], F32)
            beta = k.sb("beta", [128, NBK], F32)
            lgw = k.sb("lgw", [128, NBK], F32)
            lam = k.sb("lam", [128, NBK], F32)
            ee = k.sb("ee", [128, NBK], F32)
            class _S:
                pass
            SS = []
            for si in range(2):
                S = _S()
                S.i = si
                S.VTb = k.sb("VTb", [64, 2, 64 + NBK], BF16)
                S.PRb = k.sb("PRb", [64, 2, NBK], BF16)
                S.AR = k.sb("AR", [64, 2, NBC, 128], BF16)
                S.ZT = k.sb("ZT", [64, 2, NBC, 128], BF16)
                S.GL = k.sb("GL", [64, 2, NBC], F32)
                S.MmA = k.sb("MmA", [128, 2, NBC, 128], BF16)
                S.XLA = k.sb("XLA", [128, 2, NBC, 64], BF16)
                S.SWA = k.sb("SWA", [128, 2, NBC + 1, 64], BF16)
                S.WA = k.sb("WA", [128, 2, NBC, 64], BF16)
                S.ZtA = k.sb("ZtA", [128, 2, NBC, 64], BF16)
                S.TTA = k.sb("TTA", [64, 2, NBC, 64], BF16)
                S.Nt = [k.sb("Nt%d" % j, [64, 8, 64], BF16) for j in range(2)]
                S.GTt = [k.sb("GTt%d" % j, [64, 8, 2, 64], BF16) for j in range(2)]
                S.Xb = k.sb("Xb", [64, 2, 64], BF16)
                S.ST = k.sb("ST", [64, 2, 64], F32)
                S.tS = k.sb("tS", [64, 2, 64], F32)
                S.ys = k.sb("ys", [64, 2, 64], F32)
                S.bon = k.sb("bon", [64, 2], F32)
                S.om3 = k.sb("om3", [64, 128], F32)
                S.st2 = k.sb("st2r", [64, 2, 6], F32)
                S.mv2 = k.sb("mv2r", [64, 2, 2], F32)
                S.rs2 = k.sb("rs2r", [64, 2], F32)
                S.pXU = k.ps("p3x", [64, 2, 2, 64])
                S.pYS = k.ps("p3y", [64, 512])
                SS.append(S)
            Yf = k.sb("Yf", [64, NCH, 2, 64], F32)
            identg = k.sb("identg", [64, 8, 64], F32)
            pMg = k.ps("p3m", [128, 8, 128])
            pSg = k.ps("p3s", [128, 2, 512])
            pA = pMg
            for S in SS:
                k.op('pool', lambda e: e.memset(S.VTb[:], 0.0), writes=[S.VTb])
                k.op('pool', lambda e: e.memset(S.SWA[:], 0.0), writes=[S.SWA])
            for m in range(8):
                k.op('dve', lambda e: e.tensor_copy(identg[:, m, :], ident[0:64, 0:64]), reads=[ident], writes=[identg])

            prep_lock = [False]

            def prep(S, b, hp, tok0, ntok, d):
                VTb, PRb, AR, ZT, GL = S.VTb, S.PRb, S.AR, S.ZT, S.GL
                while prep_lock[0]:
                    yield
                prep_lock[0] = True
                nch = ntok // 64
                n = ntok
                NS = slice(0, ntok)
                r0 = hp * 128
                k.dma('pool', lab[:, NS], FM[b][3744:3808, tok0:tok0 + ntok], reads=[FM[b]], writes=[lab])
                lo = 3616 + 64 * d
                k.dma('pool', lwb_[:, NS], FM[b][lo:lo + 64, tok0:tok0 + ntok], reads=[FM[b]], writes=[lwb_])
                k.dma('sp', rr[:, NS], FM[b][1024 + r0:1024 + r0 + 128, tok0:tok0 + ntok], reads=[FM[b]], writes=[rr])
                k.dma('sp', rk[:, NS], FM[b][1536 + r0:1536 + r0 + 128, tok0:tok0 + ntok], reads=[FM[b]], writes=[rk])
                for h in range(2):
                    k.dma('pool', VTb[:, h, 64:64 + ntok], FM[b][2048 + r0 + h * 64:2048 + r0 + (h + 1) * 64, tok0:tok0 + ntok], reads=[FM[b]], pw=[VTb])
                yield
                pA0 = pMg[:, 0:4, :].rearrange("p a b -> p (a b)")[:, 0:n]
                pA1 = pMg[:, 4:8, :].rearrange("p a b -> p (a b)")[:, 0:n]
                k.op('pe', lambda e: e.matmul(pA0, aBb[:, hp * 128:(hp + 1) * 128], lab[:, NS], start=True, stop=True), reads=[aBb, lab], writes=[pMg], inc=False)
                k.op('pe', lambda e: e.matmul(pA1, wBb[:, d, hp * 128:(hp + 1) * 128], lwb_[:, NS], start=True, stop=True), reads=[wBb, lwb_])
                k.op('act', lambda e: e.activation(aa[:, NS], pA0, AF.Sigmoid, bias=rp1[:, hp, 2:3]), reads=[pMg, rp1], writes=[aa])
                k.op('act', lambda e: e.activation(lgw[:, NS], pA1, AF.Sigmoid, bias=rp1[:, hp, d:d + 1]), reads=[pMg, rp1], writes=[lgw])
                k.op('dve', lambda e: e.tensor_scalar(lgw[:, NS], lgw[:, NS], -DS, None, ALU.mult), reads=[lgw], writes=[lgw])
                yield
                k.op('dve', lambda e: e.tensor_scalar(t1[:, NS], rk[:, NS], rp1[:, hp, 3:4], None, ALU.mult), reads=[rk, rp1], writes=[t1])
                k.op('dve', lambda e: e.tensor_tensor(t2[:, NS], t1[:, NS], t1[:, NS], ALU.mult), reads=[t1], writes=[t2])
                k.op('pe', lambda e: e.matmul(pA0, onesbd[:], t2[:, NS], start=True, stop=True), reads=[onesbd, t2], writes=[pMg])
                k.op('act', lambda e: e.activation(khat[:, NS], pA0, AF.Sqrt), reads=[pMg], writes=[khat])
                yield
                k.op('dve', lambda e: e.tensor_scalar(khat[:, NS], khat[:, NS], 1e-12, None, ALU.max), reads=[khat], writes=[khat])
                k.op('dve', lambda e: e.reciprocal(khat[:, NS], khat[:, NS]), reads=[khat], writes=[khat])
                k.op('dve', lambda e: e.tensor_tensor(khat[:, NS], khat[:, NS], t1[:, NS], ALU.mult), reads=[khat, t1], writes=[khat])
                yield
                k.op('dve', lambda e: e.tensor_scalar(t1[:, NS], aa[:, NS], rp1[:, hp, 4:5], rp1[:, hp, 7:8], ALU.mult, ALU.add), reads=[aa, rp1], writes=[t1])
                k.op('dve', lambda e: e.tensor_tensor(kmod[:, NS], rk[:, NS], t1[:, NS], ALU.mult), reads=[rk, t1], writes=[kmod])
                k.op('dve', lambda e: e.tensor_tensor(beta[:, NS], khat[:, NS], aa[:, NS], ALU.mult), reads=[khat, aa], writes=[beta])
                yield
                k.op('dve', lambda e: e.scalar_tensor_tensor(PRs[:, NS], rr[:, NS], rp1[:, hp, 6:7], kmod[:, NS], ALU.mult, ALU.mult), reads=[rr, rp1, kmod], writes=[PRs])
                k.op('dve', lambda e: e.tensor_tensor_scan(lam[:, NS], smask[:, NS], lgw[:, NS], 0.0, ALU.mult, ALU.add), reads=[smask, lgw], writes=[lam])
                yield
                v3 = lambda t_: t_[:, NS].rearrange("p (c t) -> p c t", t=64)
                if d == 1:
                    k.op('dve', lambda e: e.tensor_tensor(t1[:, NS], lgw[:, NS], lam[:, NS], ALU.subtract), reads=[lgw, lam], writes=[t1])
                    k.op('dve', lambda e: e.tensor_tensor(v3(t2), v3(t1), v3(lam)[:, :, 63:64].to_broadcast([128, nch, 64]), ALU.add), reads=[t1, lam], writes=[t2])
                    k.op('act', lambda e: e.activation(lam[:, NS], t2[:, NS], AF.Identity), reads=[t2], writes=[lam])
                k.op('dve', lambda e: e.tensor_tensor(t1[:, NS], lam[:, NS], lgw[:, NS], ALU.subtract), reads=[lam, lgw], writes=[t1])
                k.op('act', lambda e: e.activation(ee[:, NS], t1[:, NS], AF.Exp), reads=[t1], writes=[ee])
                yield
                k.op('dve', lambda e: e.scalar_tensor_tensor(ARs[:, 0:nch, 0:64], v3(khat), -1.0, v3(ee), ALU.mult, ALU.mult), reads=[khat, ee], writes=[ARs])
                k.op('act', lambda e: e.activation(ee[:, NS], lam[:, NS], AF.Exp), reads=[lam], writes=[ee])
                k.op('dve', lambda e: e.tensor_tensor(ARs[:, 0:nch, 64:128], v3(rr), v3(ee), ALU.mult), reads=[rr, ee], writes=[ARs])
                yield
                gcol = 63 if d == 0 else 0
                k.op('act', lambda e: e.activation(GLs[:, 0:nch], v3(ee)[:, :, gcol], AF.Identity), reads=[ee], writes=[GLs])
                k.op('act', lambda e: e.activation(t1[:, NS], lam[:, NS], AF.Exp, scale=-1.0), reads=[lam], writes=[t1])
                yield
                k.op('dve', lambda e: e.tensor_tensor(ZTs[:, 0:nch, 0:64], v3(beta), v3(t1), ALU.mult), reads=[beta, t1], writes=[ZTs])
                k.op('dve', lambda e: e.tensor_tensor(ZTs[:, 0:nch, 64:128], v3(kmod), v3(t1), ALU.mult), reads=[kmod, t1], writes=[ZTs])
                yield
                k.op('act', lambda e: e.activation(AR[:, 0, 0:nch, :], ARs[0:64, 0:nch, :], AF.Identity), reads=[ARs], writes=[AR])
                k.op('act', lambda e: e.activation(ZT[:, 0, 0:nch, :], ZTs[0:64, 0:nch, :], AF.Identity), reads=[ZTs], writes=[ZT])
                k.op('act', lambda e: e.activation(PRb[:, 0, NS], PRs[0:64, NS], AF.Identity), reads=[PRs], writes=[PRb])
                k.op('act', lambda e: e.activation(GL[:, 0, 0:nch], GLs[0:64, 0:nch], AF.Identity), reads=[GLs], writes=[GL])
                k.dma('sp', AR[:, 1, 0:nch, :], ARs[64:128, 0:nch, :], reads=[ARs], pw=[AR])
                k.dma('sp', ZT[:, 1, 0:nch, :], ZTs[64:128, 0:nch, :], reads=[ZTs], pw=[ZT])
                k.dma('sp', PRb[:, 1, NS], PRs[64:128, NS], reads=[PRs], pw=[PRb])
                k.dma('sp', GL[:, 1, 0:nch], GLs[64:128, 0:nch], reads=[GLs], pw=[GL])
                prep_lock[0] = False

            pSf = lambda: pSg[:].rearrange("p a b -> p (a b)")

            def precompute(S, l0, d):
                VTb, AR, ZT, MmA, XLA, SWA, WA, ZtA, TTA = S.VTb, S.AR, S.ZT, S.MmA, S.XLA, S.SWA, S.WA, S.ZtA, S.TTA
                G4 = slice(l0, l0 + 4)
                pTb = pSg[:, 0, :].bitcast(BF16)
                for h in range(2):
                    for j in range(4):
                        m = h * 4 + j
                        k.op('pe', lambda e: e.transpose(pTb[:, m * 64:(m + 1) * 64], ZT[:, h, l0 + j, :], identb[0:64, 0:64]), reads=[ZT, identb], writes=[pSg], inc=False)
                for h in range(2):
                    for j in range(4):
                        m = 8 + h * 4 + j
                        k.op('pe', lambda e: e.transpose(pTb[:, m * 64:(m + 1) * 64], VTb[:, h, (l0 + j) * 64:(l0 + j) * 64 + 128], identb[0:64, 0:64]), reads=[VTb, identb], inc=(h == 1 and j == 3))
                zsrc = pTb[:, 0:512].rearrange("p (h j e) -> p h j e", h=2, j=4)
                vsrc = pTb[64:128, 512:1024].rearrange("p (h j e) -> p h j e", h=2, j=4)
                k.op('act', lambda e: e.activation(ZtA[:, :, G4, :], zsrc, AF.Identity), reads=[pSg], writes=[ZtA])
                k.op('act', lambda e: e.activation(SWA[64:128, :, G4, :], vsrc, AF.Identity), reads=[pSg], writes=[SWA])
                k.op('act', lambda e: e.activation(WA[64:128, :, G4, :], vsrc, AF.Identity), reads=[pSg], writes=[WA])
                yield
                for h in range(2):
                    for j in range(4):
                        m = h * 4 + j
                        k.op('pe', lambda e: e.matmul(pMg[:, m, :], ZT[:, h, l0 + j, :], AR[:, h, l0 + j, :], start=True, stop=True), reads=[ZT, AR], writes=[pMg], inc=(m == 7))
                for h in range(2):
                    for j in range(4):
                        m = h * 4 + j
                        k.op('pe', lambda e: e.matmul(pSg[0:64, 1, m * 64:(m + 1) * 64], AR[:, h, l0 + j, 0:64], ZT[:, h, l0 + j, 0:64], start=True, stop=True), reads=[ZT, AR], writes=[pSg], inc=(m == 7))
                for h in range(2):
                    k.op('dve', lambda e: e.tensor_tensor(MmA[:, h, G4, :], pMg[:, h * 4:(h + 1) * 4, :], rmask[:, d:d + 1, :].to_broadcast([128, 4, 128]), ALU.mult),
                         reads=[pMg, rmask], writes=[MmA])
                Nt, GTt = S.Nt, S.GTt
                k.op('dve', lambda e: e.tensor_tensor(Nt[0][:], pSg[0:64, 1, :].rearrange("p (m e) -> p m e", e=64), nmask[:, d:d + 1, :].to_broadcast([64, 8, 64]), ALU.mult),
                     reads=[pSg, nmask], writes=[Nt[0]])
                k.op('act', lambda e: e.activation(GTt[0][:, :, 0, :].rearrange("p (h j) e -> p h j e", h=2), MmA[0:64, :, G4, 0:64], AF.Identity), reads=[MmA], writes=[GTt[0]])
                k.op('act', lambda e: e.activation(GTt[0][:, :, 1, :], identg[:], AF.Identity), reads=[identg, GTt[0]], writes=[GTt[0]])
                k.op('act', lambda e: e.activation(XLA[64:128, :, G4, :], MmA[64:128, :, G4, 0:64], AF.Identity), reads=[MmA], writes=[XLA])
                k.op('act', lambda e: e.activation(XLA[0:64, :, G4, :], AR[:, :, G4, 0:64], AF.Identity), reads=[AR], writes=[XLA])
                cur = 0
                for lv in range(5):
                    yield
                    nsrc, gsrc, ndst, gdst = Nt[cur], GTt[cur], Nt[1 - cur], GTt[1 - cur]
                    for m in range(8):
                        k.op('pe', lambda e: e.matmul(pSg[0:64, 0, m * 64:(m + 1) * 64], gsrc[:, m, 0, :], nsrc[:, m, :], start=True, stop=True), reads=[gsrc, nsrc], writes=[pSg] if m == 0 else [], inc=False)
                    for m in range(8):
                        k.op('pe', lambda e: e.matmul(pMg[0:64, m, :], nsrc[:, m, :], gsrc[:, m, :, :].rearrange("p a e -> p (a e)"), start=True, stop=True), reads=[nsrc, gsrc], writes=[pMg] if m == 0 else [], inc=(m == 7))
                    k.op('act', lambda e: e.activation(ndst[:].rearrange("p m e -> p (m e)"), pSg[0:64, 0, :], AF.Identity), reads=[pSg], writes=[ndst])
                    k.op('act', lambda e: e.activation(gdst[:, :, 0, :], pMg[0:64, :, 0:64], AF.Identity), reads=[pMg], writes=[gdst])
                    k.op('dve', lambda e: e.tensor_tensor(gdst[:, :, 1, :], pMg[0:64, :, 64:128], gsrc[:, :, 1, :], ALU.add), reads=[pMg, gsrc, gdst], writes=[gdst])
                    cur = 1 - cur
                yield
                nsrc, gsrc = Nt[cur], GTt[cur]
                for m in range(8):
                    k.op('pe', lambda e: e.matmul(pSg[0:64, 1, m * 64:(m + 1) * 64], nsrc[:, m, :], gsrc[:, m, 1, :], start=True, stop=True), reads=[nsrc, gsrc], writes=[pSg] if m == 0 else [], inc=(m == 7))
                k.op('dve', lambda e: e.tensor_tensor(TTA[:, :, G4, :], pSg[0:64, 1, :].rearrange("p (h j e) -> p h j e", h=2, j=4), gsrc[:, :, 1, :].rearrange("p (h j) e -> p h j e", h=2), ALU.add),
                     reads=[pSg, gsrc], writes=[TTA])

            def step(S, b, hp, c, lc, lnext, d, done):
                VTb, PRb, AR, GL, MmA, XLA, SWA, WA, ZtA, TTA = S.VTb, S.PRb, S.AR, S.GL, S.MmA, S.XLA, S.SWA, S.WA, S.ZtA, S.TTA
                Xb, ST, tS, ys, bon, om3, st2, mv2, rs2, pXU, pYS = S.Xb, S.ST, S.tS, S.ys, S.bon, S.om3, S.st2, S.mv2, S.rs2, S.pXU, S.pYS
                pX = pXU[:, 0, :, :]
                pU = pXU[:, 1, :, :]
                pY = pYS[:, 0:128].rearrange("p (h e) -> p h e", h=2)
                pS_ = pYS[:, 128:256].rearrange("p (h e) -> p h e", h=2)
                for h in range(2):
                    k.op('pe', lambda e: e.matmul(pXU[:, 0, h, :], XLA[:, h, lc, :], SWA[:, h, lc, :], start=True, stop=True), reads=[XLA, SWA], writes=[pXU], inc=(h == 1))
                k.op('act', lambda e: e.activation(Xb[:], pX, AF.Identity), reads=[pXU], writes=[Xb])
                yield
                for h in range(2):
                    k.op('pe', lambda e: e.matmul(pXU[:, 1, h, :], TTA[:, h, lc, :], Xb[:, h, :], start=True, stop=True), reads=[TTA, Xb], writes=[pXU], inc=(h == 1))
                k.op('dve', lambda e: e.tensor_copy(WA[0:64, :, lc, :], pU), reads=[pXU], writes=[WA])
                yield
                second = (c in done)
                needy = (c >= 4)
                wfirst = [pYS]
                if needy:
                    for h in range(2):
                        k.op('pe', lambda e: e.matmul(pYS[:, h * 64:(h + 1) * 64], AR[:, h, lc, 64:128], SWA[0:64, h, lc, :], start=True, stop=False), reads=[AR, SWA], writes=wfirst, inc=False)
                        wfirst = []
                        k.op('pe', lambda e: e.matmul(pYS[:, h * 64:(h + 1) * 64], MmA[:, h, lc, 64:128], WA[:, h, lc, :], start=False, stop=True), reads=[MmA, WA], inc=False)
                fin = (needy and second)
                if fin:
                    ts = slice(lc * 64, (lc + 1) * 64)
                    for h in range(2):
                        k.op('pe', lambda e: e.matmul(pYS[:, 384 + 2 * h:386 + 2 * h], PRb[:, h, ts], onesb[:, 0:2], start=True, stop=True), reads=[PRb, onesb], inc=False)
                    k.op('pe', lambda e: e.matmul(pYS[:, 256:384], lgb[:, c * 64:(c + 1) * 64], gBb[:, hp * 128:(hp + 1) * 128], start=True, stop=True), reads=[lgb, gBb], inc=False)
                    pvt = pYS[:, 448:512].bitcast(BF16)
                    for h in range(2):
                        k.op('pe', lambda e: e.transpose(pvt[:, h * 64:(h + 1) * 64], VTb[:, h, 64 + lc * 64:128 + lc * 64], identb[0:64, 0:64]), reads=[VTb, identb], inc=False)
                for h in range(2):
                    k.op('pe', lambda e: e.matmul(pYS[:, 128 + h * 64:128 + (h + 1) * 64], ZtA[:, h, lc, :], WA[:, h, lc, :], start=True, stop=True), reads=[ZtA, WA], writes=wfirst, inc=(h == 1))
                    wfirst = []
                k.op('dve', lambda e: e.tensor_tensor(tS[:], pS_, ST[:], ALU.add), reads=[pYS, ST], writes=[tS])
                k.op('dve', lambda e: e.tensor_tensor(ST[:], tS[:], GL[:, :, lc:lc + 1].to_broadcast([64, 2, 64]), ALU.mult), reads=[tS, GL], writes=[ST])
                k.op('act', lambda e: e.activation(SWA[0:64, :, lnext, :], ST[:], AF.Identity), reads=[ST], writes=[SWA])
                if needy and not second:
                    k.op('dve', lambda e: e.tensor_copy(Yf[:, c, :, :], pY), reads=[pYS], pw_=[Yf])
                    done[c] = S.i
                elif needy:
                    k.op('dve', lambda e: e.tensor_tensor(ys[:], pY, Yf[:, c, :, :], ALU.add), reads=[pYS, Yf], writes=[ys])
                if fin:
                    for h in range(2):
                        k.op('dve', lambda e: e.bn_stats(st2[:, h, :], ys[:, h, :]), reads=[ys], writes=[st2])
                    for h in range(2):
                        k.op('dve', lambda e: e.bn_aggr(mv2[:, h, :], st2[:, h, :]), reads=[st2], writes=[mv2])
                    k.op('act', lambda e: e.activation(rs2[:], mv2[:, :, 1], AF.Sqrt, bias=GN_EPS), reads=[mv2], writes=[rs2])
                    k.op('dve', lambda e: e.reciprocal(rs2[:], rs2[:]), reads=[rs2], writes=[rs2])
                    for h in range(2):
                        k.op('dve', lambda e: e.tensor_scalar(ys[:, h, :], ys[:, h, :], mv2[:, h, 0:1], rs2[:, h:h + 1], ALU.subtract, ALU.mult), reads=[ys, mv2, rs2], writes=[ys])
                    ysf = ys[:].rearrange("p h e -> p (h e)")
                    k.op('dve', lambda e: e.tensor_tensor(om3[:], ysf, gnw[:, 0, hp * 128:(hp + 1) * 128], ALU.mult), reads=[ys, gnw], writes=[om3])
                    k.op('dve', lambda e: e.tensor_tensor(om3[:], om3[:], gnw[:, 1, hp * 128:(hp + 1) * 128], ALU.add), reads=[om3, gnw], writes=[om3])
                    k.op('dve', lambda e: e.tensor_copy(bon[:], pYS[:, 384:388].rearrange("p (h two) -> p h two", two=2)[:, :, 0]), reads=[pYS], writes=[bon])
                    for h in range(2):
                        k.op('dve', lambda e: e.scalar_tensor_tensor(om3[:, h * 64:(h + 1) * 64], pvt[:, h * 64:(h + 1) * 64], bon[:, h:h + 1], om3[:, h * 64:(h + 1) * 64], ALU.mult, ALU.add),
                             reads=[pYS, bon, om3], writes=[om3])
                    k.op('dve', lambda e: e.tensor_tensor(om3[:], om3[:], pYS[:, 256:384], ALU.mult), reads=[om3, pYS], writes=[om3])
                    k.dma('sp', MIX[b][(c - 4) * 64:(c - 3) * 64, 512 + hp * 128:512 + (hp + 1) * 128], om3[:], reads=[om3], pw=[MIX[b]])

            blocks = [(0, 256)] + [(256 + i * 512, 512) for i in range(8)]

            def stream(S, b, hp, d, done):
                k.op('pool', lambda e: e.memset(S.ST[:], 0.0), writes=[S.ST])
                border = list(range(9)) if d == 0 else [0] + list(range(8, 0, -1))
                first = True
                for bi in border:
                    tok0, ntok = blocks[bi]
                    nch = ntok // 64
                    yield from prep(S, b, hp, tok0, ntok, d)
                    lcs = list(range(nch)) if d == 0 else list(range(nch - 1, -1, -1))
                    if first:
                        k.op('pool', lambda e: e.memset(S.SWA[0:64, :, lcs[0], :], 0.0), writes=[S.SWA])
                        first = False
                    else:
                        k.op('act', lambda e: e.activation(S.SWA[0:64, :, lcs[0], :], S.ST[:], AF.Identity), reads=[S.ST], writes=[S.SWA])
                    yield
                    for g0 in range(0, nch, 4):
                        if os.environ.get('RW_SKIP_PRE'):
                            break
                        yield from precompute(S, g0, d)
                        yield
                    for ii, lc in enumerate(lcs):
                        if os.environ.get('RW_SKIP_STEP'):
                            break
                        lnext = lcs[ii + 1] if ii + 1 < len(lcs) else NBC
                        yield from step(S, b, hp, tok0 // 64 + lc, lc, lnext, d, done)
                        yield

            for b in range(NB):
                for q4 in range(4):
                    k.dma('pool', lgb[:, q4 * 1088:(q4 + 1) * 1088], FM[b][3808:3936, q4 * 1088:(q4 + 1) * 1088], reads=[FM[b]], pw=[lgb])
                for hp in range(int(os.environ.get('P3H', 4))):
                    done = {}
                    gens = [stream(SS[0], b, hp, 0, done), stream(SS[1], b, hp, 1, done)]
                    alive = [True, True]
                    for _ in range(int(os.environ.get('RW_OFF', 28))):
                        next(gens[0])
                    while any(alive):
                        for gi in range(2):
                            if alive[gi]:
                                try:
                                    next(gens[gi])
                                except StopIteration:
                                    alive[gi] = False
        k.barrier()

        with ExitStack() as es:
          if 4 in phases:
           try:
            P4S = int(os.environ.get('P4S', 9))
            k.es = es
            NT = NB * SEQ // 128
            NBLK = NBLK_
            SUB = BS // 128
            LG = k.sb("LG", [128, NT, 36], F32)
            OH1 = k.sb("OH1", [128, NT, 32], F32)
            OH2 = k.sb("OH2", [128, NT, 32], F32)
            W1 = k.sb("W1", [128, NT], F32)
            W2 = k.sb("W2", [128, NT], F32)
            DST = k.sb("DST", [128, NT, 2], I32)
            WIDX = k.sb("WIDX", [128, NBLK, 12], I32)
            g2b = k.sb("g2b", [128, NB, D], F32)
            lnp = k.sb("lnp", [128, 4, D], F32)
            for j, src in enumerate((ln1_g, ln1_b, ln2_g, ln2_b)):
                k.dma('sp', lnp[:, j, :], src[0:1, :].partition_broadcast(128), pw=[lnp])
            for b in range(NB):
                k.dma('sp', g2b[:, b, :], MODD[b:b + 1, 5 * D:6 * D].partition_broadcast(128), reads=[MODD], pw=[g2b])
            with ExitStack() as es4:
                k.es = es4
                wob = k.sb("wob", [128, 8, D], BF16)
                for kc in range(8):
                    k.dma('pool', wob[:, kc, :], w_out[kc * 128:(kc + 1) * 128, :], pw=[wob])
                rt = k.sb("rt", [128, 8, 36], F32)
                k.dma('sp', rt[:], rt_in[:, :].rearrange("(kc p) n -> p kc n", p=128), writes=[rt])
                rtbb = k.sb("rtbb", [128, 36], F32)
                k.dma('sp', rtbb[:], rtb_in[0:1, :].partition_broadcast(128), writes=[rtbb])
                mb4 = k.sb("mb4", [128, 3, D], F32)
                class _A:
                    pass
                AS = []
                for si in range(2):
                    A = _A()
                    A.mxb = k.sb("mxb", [128, D], BF16)
                    A.mT = k.sb("mT", [128, 8, 128], BF16)
                    A.x4 = k.sb("x4", [128, D], F32)
                    A.t4 = k.sb("t4", [128, D], F32)
                    A.y4 = k.sb("y4", [128, D], F32)
                    A.h4 = k.sb("h4", [128, D], F32)
                    A.h4b = k.sb("h4b", [128, D], BF16)
                    A.h4T = k.sb("h4T", [128, 8, 128], F32)
                    A.st4 = k.sb("st4", [128, 2, 6], F32)
                    A.mv4 = k.sb("mv4", [128, 2], F32)
                    A.rs4 = k.sb("rs4", [128, 1], F32)
                    A.nb4 = k.sb("nb4", [128, 1], F32)
                    A.P01 = k.ps("p4o", [128, 2, 512])
                    A.P01.excl = True
                    A.plg = k.ps("p4lg", [128, 36])
                    AS.append(A)

                def ln_stats(A, src):
                    for hf in range(2):
                        k.op('dve', lambda e: e.bn_stats(A.st4[:, hf, :], src[:, hf * 512:(hf + 1) * 512]), reads=[src], writes=[A.st4])
                    k.op('dve', lambda e: e.bn_aggr(A.mv4[:], A.st4[:].rearrange("p a b -> p (a b)")), reads=[A.st4], writes=[A.mv4])
                    k.op('act', lambda e: e.activation(A.rs4[:], A.mv4[:, 1:2], AF.Sqrt, bias=LN_EPS), reads=[A.mv4], writes=[A.rs4])
                    k.op('dve', lambda e: e.reciprocal(A.rs4[:], A.rs4[:]), reads=[A.rs4], writes=[A.rs4])
                    k.op('dve', lambda e: e.scalar_tensor_tensor(A.nb4[:], A.mv4[:, 0:1], -1.0, A.rs4[:], ALU.mult, ALU.mult), reads=[A.mv4, A.rs4], writes=[A.nb4])

                def tile4(A, b, i):
                    gi = b * (SEQ // 128) + i
                    P01 = A.P01
                    k.dma('pool', A.mxb[:], MIX[b][i * 128:(i + 1) * 128, :], reads=[MIX[b]], writes=[A.mxb])
                    k.dma('sp', A.x4[:], x_in[b, i * 128:(i + 1) * 128, :], writes=[A.x4])
                    yield
                    ptm = P01[:, 0, :].bitcast(BF16)
                    for kc in range(8):
                        k.op('pe', lambda e: e.transpose(ptm[:, kc * 128:(kc + 1) * 128], A.mxb[:, kc * 128:(kc + 1) * 128], identb[:]), reads=[A.mxb, identb], writes=[P01] if kc == 0 else [], inc=(kc == 7))
                    k.op('act', lambda e: e.activation(A.mT[:].rearrange("p a b -> p (a b)"), ptm, AF.Identity), reads=[P01], writes=[A.mT])
                    yield
                    for n in range(2):
                        for kc in range(8):
                            k.op('pe', lambda e: e.matmul(P01[:, n, :], A.mT[:, kc, :], wob[:, kc, n * 512:(n + 1) * 512], start=(kc == 0), stop=(kc == 7)),
                                 reads=[A.mT, wob], writes=[P01] if (kc == 0 and n == 0) else [], inc=(kc == 7 and n == 1))
                    k.op('dve', lambda e: e.tensor_tensor(A.t4[:], P01[:].rearrange("p a b -> p (a b)"), mb4[:, 0, :], ALU.mult), reads=[P01, mb4], writes=[A.t4])
                    k.op('dve', lambda e: e.scalar_tensor_tensor(A.y4[:], A.x4[:], ALPHA, A.t4[:], ALU.mult, ALU.add), reads=[A.x4, A.t4], writes=[A.y4])
                    yield
                    ln_stats(A, A.y4)
                    k.op('act', lambda e: e.activation(A.y4[:], A.y4[:], AF.Identity, bias=A.nb4[:, 0:1], scale=A.rs4[:, 0:1]), reads=[A.y4, A.nb4, A.rs4], writes=[A.y4])
                    yield
                    k.op('dve', lambda e: e.tensor_tensor(A.y4[:], A.y4[:], lnp[:, 0, :], ALU.mult), reads=[A.y4, lnp], writes=[A.y4])
                    k.op('dve', lambda e: e.tensor_tensor(A.y4[:], A.y4[:], lnp[:, 1, :], ALU.add), reads=[A.y4, lnp], writes=[A.y4])
                    k.dma('sp', X1[gi * 128:(gi + 1) * 128, :], A.y4[:], reads=[A.y4], pw=[X1])
                    yield
                    ln_stats(A, A.y4)
                    k.op('act', lambda e: e.activation(A.h4[:], A.y4[:], AF.Identity, bias=A.nb4[:, 0:1], scale=A.rs4[:, 0:1]), reads=[A.y4, A.nb4, A.rs4], writes=[A.h4])
                    yield
                    k.op('dve', lambda e: e.tensor_tensor(A.h4[:], A.h4[:], mb4[:, 1, :], ALU.mult), reads=[A.h4, mb4], writes=[A.h4])
                    k.op('dve', lambda e: e.tensor_tensor(A.h4[:], A.h4[:], mb4[:, 2, :], ALU.add), reads=[A.h4, mb4], writes=[A.h4])
                    k.op('act', lambda e: e.activation(A.h4b[:], A.h4[:], AF.Identity), reads=[A.h4], writes=[A.h4b])
                    k.dma('sp', H2[gi * 128:(gi + 1) * 128, :], A.h4b[:], reads=[A.h4b], pw=[H2])
                    yield
                    pth = P01[:].rearrange("p a b -> p (a b)")
                    for kc in range(8):
                        k.op('pe', lambda e: e.transpose(pth[:, kc * 128:(kc + 1) * 128], A.h4[:, kc * 128:(kc + 1) * 128], ident[:]), reads=[A.h4, ident], writes=[P01] if kc == 0 else [], inc=(kc == 7))
                    k.op('act', lambda e: e.activation(A.h4T[:].rearrange("p a b -> p (a b)"), pth, AF.Identity), reads=[P01], writes=[A.h4T])
                    yield
                    for kc in range(8):
                        k.op('pe', lambda e: e.matmul(A.plg[:], A.h4T[:, kc, :], rt[:, kc, :], start=(kc == 0), stop=(kc == 7)),
                             reads=[A.h4T, rt], writes=[A.plg] if kc == 0 else [], inc=(kc == 7))
                    k.op('dve', lambda e: e.tensor_tensor(LG[:, gi, :], A.plg[:], rtbb[:], ALU.add), reads=[A.plg, rtbb], pw_=[LG])

                for b in range(NB):
                    for j, c0 in enumerate((2 * D, 4 * D, 3 * D)):
                        k.dma('sp', mb4[:, j, :], MODD[b:b + 1, c0:c0 + D].partition_broadcast(128), reads=[MODD], writes=[mb4] if j == 0 else [], pw=[] if j == 0 else [mb4])
                    k.op('dve', lambda e: e.tensor_scalar(mb4[:, 1, :], mb4[:, 1, :], 1.0, None, ALU.add), reads=[mb4], writes=[mb4])

                    def astream(si):
                        for i in range(si, SEQ // 128, 2):
                            yield from tile4(AS[si], b, i)
                            yield
                    gens = [astream(0), astream(1)]
                    alive = [True, True]
                    while any(alive):
                        for gq in range(2):
                            if alive[gq]:
                                try:
                                    next(gens[gq])
                                except StopIteration:
                                    alive[gq] = False
            k.barrier()
            with ExitStack() as es5:
                k.es = es5
                if P4S < 1:
                    raise _Stop()
                gmx = k.sb("gmx", [128, NT], F32)
                goh = k.sb("goh", [128, NT, 4], F32)
                tg = k.sb("tg", [128, NT, 4], F32)
                ptop = k.sb("ptop", [128, NT], F32)
                lem = k.sb("lem", [128, NT, 32], F32)
                v1 = k.sb("v1", [128, NT], F32)
                v2 = k.sb("v2", [128, NT], F32)
                lgv = LG[:, :, 0:4]
                lev = LG[:, :, 4:36]
                k.op('dve', lambda e: e.tensor_reduce(gmx[:], lgv, AX.X, ALU.max), reads=[LG], writes=[gmx])
                k.op('dve', lambda e: e.tensor_tensor(goh[:], lgv, gmx[:].unsqueeze(2).to_broadcast([128, NT, 4]), ALU.is_equal), reads=[LG, gmx], writes=[goh])
                k.op('dve', lambda e: e.tensor_tensor(tg[:], lgv, gmx[:].unsqueeze(2).to_broadcast([128, NT, 4]), ALU.subtract), reads=[LG, gmx], writes=[tg])
                k.op('act', lambda e: e.activation(tg[:], tg[:], AF.Exp), reads=[tg], writes=[tg])
                k.op('dve', lambda e: e.tensor_reduce(ptop[:], tg[:], AX.X, ALU.add), reads=[tg], writes=[ptop])
                k.op('dve', lambda e: e.reciprocal(ptop[:], ptop[:]), reads=[ptop], writes=[ptop])
                k.op('dve', lambda e: e.tensor_scalar(goh[:], goh[:], -1.0, 1e30, ALU.add, ALU.mult), reads=[goh], writes=[goh])
                for g in range(4):
                    k.op('dve', lambda e: e.tensor_tensor(lem[:, :, g * 8:(g + 1) * 8], LG[:, :, 4 + g * 8:12 + g * 8],
                                                          goh[:, :, g:g + 1].to_broadcast([128, NT, 8]), ALU.add), reads=[LG, goh], writes=[lem])
                k.op('dve', lambda e: e.tensor_reduce(v1[:], lem[:], AX.X, ALU.max), reads=[lem], writes=[v1])
                k.op('dve', lambda e: e.tensor_tensor(OH1[:], lem[:], v1[:].unsqueeze(2).to_broadcast([128, NT, 32]), ALU.is_equal), reads=[lem, v1], writes=[OH1])
                k.op('dve', lambda e: e.scalar_tensor_tensor(lem[:], OH1[:], -1e30, lem[:], ALU.mult, ALU.add), reads=[OH1, lem], writes=[lem])
                k.op('dve', lambda e: e.tensor_reduce(v2[:], lem[:], AX.X, ALU.max), reads=[lem], writes=[v2])
                k.op('dve', lambda e: e.tensor_tensor(OH2[:], lem[:], v2[:].unsqueeze(2).to_broadcast([128, NT, 32]), ALU.is_equal), reads=[lem, v2], writes=[OH2])
                k.op('dve', lambda e: e.tensor_tensor(v2[:], v2[:], v1[:], ALU.subtract), reads=[v1, v2], writes=[v2])
                k.op('act', lambda e: e.activation(v2[:], v2[:], AF.Exp), reads=[v2], writes=[v2])
                k.op('dve', lambda e: e.tensor_scalar(v2[:], v2[:], 1.0, None, ALU.add), reads=[v2], writes=[v2])
                k.op('dve', lambda e: e.reciprocal(v2[:], v2[:]), reads=[v2], writes=[v2])
                k.op('dve', lambda e: e.tensor_tensor(W1[:], v2[:], ptop[:], ALU.mult), reads=[v2, ptop], writes=[W1])
                k.op('dve', lambda e: e.tensor_tensor(W2[:], ptop[:], W1[:], ALU.subtract), reads=[W1, ptop], writes=[W2])
            k.barrier()
            with ExitStack() as es6:
                k.es = es6
                if P4S < 2:
                    raise _Stop()
                OHb = k.sb("OHb", [128, NT, 32], BF16)
                triS = k.sb("triS", [128, 128], BF16)
                onb = k.sb("onb", [128, 128], BF16)
                thr = k.sb("thr", [128, 128], F32)
                blki = k.sb("blki", [128, NBLK], F32)
                kcp = k.sb("kcp", [128, 12], F32)
                cnt = k.sb("cnt", [128, 32], F32)
                big = k.sb("big", [128, 32, 128], F32)
                nbk = k.sb("nbk", [128, 32], F32)
                pend = k.sb("pend", [128, 32], F32)
                pst = k.sb("pst", [128, 32], F32)
                run = k.sb("run", [128, 32], F32)
                RK = k.sb("RK", [128, NT, 32], F32)
                dsf = k.sb("dsf", [128, NT, 2], F32)
                bexp = k.sb("bexp", [128, NBLK], F32)
                bigb = k.sb("bigb", [128, NBLK, 32], F32)
                widxf = k.sb("widxf", [128, NBLK, 12], F32)
                tokid = k.sb("tokid", [128, NT, 16], I32)
                zt = k.sb("zt", [128, 16], I32)
                pcn = k.ps("p5c", [128, 32])
                prk = k.ps("p5r", [128, 32])
                ptt = k.ps("p5t", [128, 32])
                stg = k.sb("stg", [128, 128], F32)
                k.dma('sp', stg[:], tris_in[:, :], writes=[stg])
                k.op('dve', lambda e: e.tensor_copy(triS[:], stg[:]), reads=[stg], writes=[triS])
                k.op('pool', lambda e: e.memset(onb[:], 1.0), writes=[onb])
                k.dma('sp', thr[:], thr_in[:, :], writes=[thr])
                k.dma('sp', blki[:], blki_in[:, 0:NBLK], writes=[blki])
                k.dma('sp', kcp[:], kcp_in[:, :], writes=[kcp])
                k.dma('sp', tokid[:], tokid_in[:, 0:NT, :], writes=[tokid])
                k.op('pool', lambda e: e.memset(zt[:], 0), writes=[zt])
                k.dma('sp', TOKB[:, :].rearrange("(b p) c -> p b c", p=128), zt[:].unsqueeze(1).to_broadcast([128, NBLK * SUB, 16]), reads=[zt], writes=[TOKB])
                k.op('dve', lambda e: e.tensor_tensor(OHb[:], OH1[:], OH2[:], ALU.add), reads=[OH1, OH2], writes=[OHb])
                for i in range(NT):
                    k.op('pe', lambda e: e.matmul(pcn[:], onb[:], OHb[:, i, :], start=(i == 0), stop=(i == NT - 1)), reads=[onb, OHb], writes=[pcn] if i == 0 else [], inc=(i == NT - 1))
                k.op('dve', lambda e: e.tensor_copy(cnt[:], pcn[:]), reads=[pcn], writes=[cnt])
                k.op('dve', lambda e: e.tensor_tensor(big[:], cnt[:].unsqueeze(2).to_broadcast([128, 32, 128]), thr[:].unsqueeze(1).to_broadcast([128, 32, 128]), ALU.is_gt),
                     reads=[cnt, thr], writes=[big])
                k.op('dve', lambda e: e.tensor_reduce(nbk[:], big[:], AX.X, ALU.add), reads=[big], writes=[nbk])
                k.op('pool', lambda e: e.memset(run[:], 1.0), writes=[run])
                k.op('dve', lambda e: e.tensor_tensor_scan(pend[:], run[:], nbk[:], 0.0, ALU.mult, ALU.add), reads=[run, nbk], writes=[pend])
                k.op('dve', lambda e: e.tensor_tensor(pst[:], pend[:], nbk[:], ALU.subtract), reads=[pend, nbk], writes=[pst])
                k.op('dve', lambda e: e.tensor_scalar(pst[:], pst[:], float(BS), None, ALU.mult), reads=[pst], writes=[pst])
                k.op('pool', lambda e: e.memset(run[:], 0.0), reads=[run], writes=[run])
                for i in range(NT):
                    k.op('pe', lambda e: e.matmul(prk[:], triS[:], OHb[:, i, :], start=True, stop=True), reads=[triS, OHb], writes=[prk])
                    k.op('pe', lambda e: e.matmul(ptt[:], onb[:], OHb[:, i, :], start=True, stop=True), reads=[onb, OHb], writes=[ptt])
                    k.op('dve', lambda e: e.tensor_tensor(RK[:, i, :], prk[:], run[:], ALU.add), reads=[prk, run], writes=[RK])
                    k.op('dve', lambda e: e.tensor_tensor(run[:], run[:], ptt[:], ALU.add), reads=[run, ptt], writes=[run])
                k.op('dve', lambda e: e.tensor_tensor(RK[:], RK[:], pst[:].unsqueeze(1).to_broadcast([128, NT, 32]), ALU.add), reads=[RK, pst], writes=[RK])
                for j, OH in enumerate((OH1, OH2)):
                    k.op('dve', lambda e: e.tensor_tensor(OH[:], OH[:], RK[:], ALU.mult), reads=[OH, RK], writes=[OH])
                    k.op('dve', lambda e: e.tensor_reduce(dsf[:, :, j], OH[:], AX.X, ALU.add), reads=[OH], writes=[dsf])
                k.op('dve', lambda e: e.tensor_copy(DST[:], dsf[:]), reads=[dsf], writes=[DST])
                k.op('dve', lambda e: e.tensor_tensor(bigb[:], pend[:].unsqueeze(1).to_broadcast([128, NBLK, 32]), blki[:].unsqueeze(2).to_broadcast([128, NBLK, 32]), ALU.is_le),
                     reads=[pend, blki], writes=[bigb])
                k.op('dve', lambda e: e.tensor_reduce(bexp[:], bigb[:], AX.X, ALU.add), reads=[bigb], writes=[bexp])
                k.op('dve', lambda e: e.tensor_scalar(bexp[:], bexp[:], 31.0, None, ALU.min), reads=[bexp], writes=[bexp])
                k.op('dve', lambda e: e.tensor_scalar(widxf[:, :, 0:8], bexp[:].unsqueeze(2).to_broadcast([128, NBLK, 8]), 256.0, None, ALU.mult), reads=[bexp], writes=[widxf])
                k.op('dve', lambda e: e.tensor_scalar(widxf[:, :, 8:12], bexp[:].unsqueeze(2).to_broadcast([128, NBLK, 4]), 256.0, None, ALU.mult), reads=[bexp], writes=[widxf])
                k.op('dve', lambda e: e.tensor_tensor(widxf[:], widxf[:], kcp[:].unsqueeze(1).to_broadcast([128, NBLK, 12]), ALU.add), reads=[widxf, kcp], writes=[widxf])
                k.op('dve', lambda e: e.tensor_copy(WIDX[:], widxf[:]), reads=[widxf], writes=[WIDX])
                for i in range(NT):
                    for j in range(2):
                        k.dma('pool', TOKB[:, :], tokid[:, i, :], reads=[tokid, DST], pw=[TOKB],
                              indirect=(bass.IndirectOffsetOnAxis(ap=DST[:, i, j:j + 1], axis=0), None))
            k.barrier()
            with ExitStack() as es7:
                k.es = es7
                if P4S < 3:
                    raise _Stop()
                wg = [k.sb("wg%d" % i, [128, 8, 512], BF16) for i in range(3)]
                wu = [k.sb("wu%d" % i, [128, 8, 512], BF16) for i in range(3)]
                wd = [k.sb("wd%d" % i, [128, 4, D], BF16) for i in range(3)]
                class _E:
                    pass
                ES = []
                NES = 4
                for si in range(NES):
                    E = _E()
                    E.tki = k.sb("tki", [128, 16], I32)
                    E.xg = k.sb("xg", [128, D], BF16)
                    E.xgT = k.sb("xgT", [128, 8, 128], BF16)
                    E.gs = k.sb("gs", [128, 512], F32)
                    E.hb = k.sb("hb", [128, 512], BF16)
                    E.hbT = k.sb("hbT", [128, 4, 128], BF16)
                    E.yb = k.sb("yb", [128, D], F32)
                    E.bG = k.ps("p6g", [128, 512])
                    E.bU = k.ps("p6u", [128, 512])
                    E.bY = E.bG
                    ES.append(E)

                def load_w(bk):
                    q = bk % 3
                    for hf in range(2):
                        ix = bass.IndirectOffsetOnAxis(ap=WIDX[:, bk, hf:hf + 1], axis=0)
                        k.dma('pool', wg[q][:, hf * 4:(hf + 1) * 4, :].rearrange("p a b -> p (a b)"), ex_gate[:, :], reads=[WIDX], pw=[wg[q]], indirect=(None, ix))
                        k.dma('pool', wu[q][:, hf * 4:(hf + 1) * 4, :].rearrange("p a b -> p (a b)"), ex_up[:, :], reads=[WIDX], pw=[wu[q]], indirect=(None, ix))
                        k.dma('pool', wd[q][:, hf * 2:(hf + 1) * 2, :].rearrange("p a b -> p (a b)"), ex_down[:, :], reads=[WIDX], pw=[wd[q]], indirect=(None, ix))

                def subtile(E, bk, sub):
                    q = bk % 3
                    r0 = (bk * SUB + sub) * 128
                    k.dma('sp', E.tki[:], TOKB[r0:r0 + 128, :], reads=[TOKB], writes=[E.tki])
                    k.dma('pool', E.xg[:], H2[:, :], reads=[H2, E.tki], writes=[E.xg],
                          indirect=(None, bass.IndirectOffsetOnAxis(ap=E.tki[:, 0:1], axis=0)))
                    yield
                    pxt = E.bU[:].bitcast(BF16)
                    xv = E.xg[:].rearrange("p (j kc) -> p kc j", kc=8)
                    for kc in range(8):
                        k.op('pe', lambda e: e.transpose(pxt[:, kc * 128:(kc + 1) * 128], xv[:, kc, :], identb[:]), reads=[E.xg, identb], writes=[E.bU] if kc == 0 else [], inc=(kc == 7))
                    k.op('act', lambda e: e.activation(E.xgT[:].rearrange("p a b -> p (a b)"), pxt, AF.Identity), reads=[E.bU], writes=[E.xgT])
                    yield
                    for kc in range(8):
                        k.op('pe', lambda e: e.matmul(E.bG[:], E.xgT[:, kc, :], wg[q][:, kc, :], start=(kc == 0), stop=(kc == 7)), reads=[E.xgT, wg[q]], writes=[E.bG] if kc == 0 else [], inc=(kc == 7))
                    for kc in range(8):
                        k.op('pe', lambda e: e.matmul(E.bU[:], E.xgT[:, kc, :], wu[q][:, kc, :], start=(kc == 0), stop=(kc == 7)), reads=[E.xgT, wu[q]], writes=[E.bU] if kc == 0 else [], inc=(kc == 7))
                    yield
                    k.op('act', lambda e: e.activation(E.gs[:], E.bG[:], AF.Silu), reads=[E.bG], writes=[E.gs])
                    k.op('dve', lambda e: e.tensor_tensor(E.hb[:], E.gs[:], E.bU[:], ALU.mult), reads=[E.gs, E.bU], writes=[E.hb])
                    yield
                    pht = E.bG[:].bitcast(BF16)
                    hv = E.hb[:].rearrange("p (j fc) -> p fc j", fc=4)
                    for fc in range(4):
                        k.op('pe', lambda e: e.transpose(pht[:, fc * 128:(fc + 1) * 128], hv[:, fc, :], identb[:]), reads=[E.hb, identb], writes=[E.bG] if fc == 0 else [], inc=(fc == 3))
                    k.op('dve', lambda e: e.tensor_copy(E.hbT[:].rearrange("p a b -> p (a b)"), pht[:, 0:512]), reads=[E.bG], writes=[E.hbT])
                    yield
                    for n in range(2):
                        for fc in range(4):
                            k.op('pe', lambda e: e.matmul(E.bY[:], E.hbT[:, fc, :], wd[q][:, fc, n * 512:(n + 1) * 512], start=(fc == 0), stop=(fc == 3)),
                                 reads=[E.hbT, wd[q]], writes=[E.bY] if fc == 0 else [], inc=(fc == 3))
                        k.op('act', lambda e: e.activation(E.yb[:, n * 512:(n + 1) * 512], E.bY[:], AF.Identity), reads=[E.bY], writes=[E.yb])
                        yield
                    k.dma('sp', YB[r0:r0 + 128, :], E.yb[:], reads=[E.yb], pw=[YB])

                def estream(si):
                    for gsub in range(si, NBLK * SUB, NES):
                        bk, sub = gsub // SUB, gsub % SUB
                        if True:
                            for bb in (bk, bk + 1):
                                if bb < NBLK and bb not in loaded:
                                    loaded.add(bb)
                                    load_w(bb)
                        yield from subtile(ES[si], bk, sub)
                        yield

                loaded = set()
                gens = [estream(i) for i in range(NES)]
                alive = [True] * NES
                while any(alive):
                    for gi in range(NES):
                        if alive[gi]:
                            try:
                                next(gens[gi])
                            except StopIteration:
                                alive[gi] = False
            k.barrier()
            with ExitStack() as es8:
                k.es = es8
                if P4S < 4:
                    raise _Stop()
                x6 = [k.sb("x6%d" % i, [128, D], F32) for i in range(2)]
                y0 = [k.sb("y0%d" % i, [128, D], F32) for i in range(2)]
                y1 = [k.sb("y1%d" % i, [128, D], F32) for i in range(2)]
                o6 = [k.sb("o6%d" % i, [128, D], F32) for i in range(2)]
                st6 = k.sb("st6", [128, 2, 6], F32)
                mv6 = k.sb("mv6", [128, 2], F32)
                rs6 = k.sb("rs6", [128, 1], F32)
                for gi in range(NT):
                    b, i = gi // (SEQ // 128), gi % (SEQ // 128)
                    q = gi % 2
                    k.dma('sp', x6[q][:], X1[gi * 128:(gi + 1) * 128, :], reads=[X1], writes=[x6[q]])
                    k.dma('pool', y0[q][:], YB[:, :], reads=[YB, DST], writes=[y0[q]],
                          indirect=(None, bass.IndirectOffsetOnAxis(ap=DST[:, gi, 0:1], axis=0)))
                    k.dma('pool', y1[q][:], YB[:, :], reads=[YB, DST], writes=[y1[q]],
                          indirect=(None, bass.IndirectOffsetOnAxis(ap=DST[:, gi, 1:2], axis=0)))
                    o = o6[q]
                    k.op('dve', lambda e: e.tensor_scalar(y0[q][:], y0[q][:], W1[:, gi:gi + 1], None, ALU.mult), reads=[y0[q], W1], writes=[y0[q]])
                    k.op('dve', lambda e: e.scalar_tensor_tensor(y0[q][:], y1[q][:], W2[:, gi:gi + 1], y0[q][:], ALU.mult, ALU.add), reads=[y1[q], W2, y0[q]], writes=[y0[q]])
                    k.op('dve', lambda e: e.tensor_tensor(y0[q][:], y0[q][:], g2b[:, b, :], ALU.mult), reads=[y0[q], g2b], writes=[y0[q]])
                    k.op('dve', lambda e: e.scalar_tensor_tensor(o[:], x6[q][:], ALPHA, y0[q][:], ALU.mult, ALU.add), reads=[x6[q], y0[q]], writes=[o])
                    for hf in range(2):
                        k.op('dve', lambda e: e.bn_stats(st6[:, hf, :], o[:, hf * 512:(hf + 1) * 512]), reads=[o], writes=[st6])
                    k.op('dve', lambda e: e.bn_aggr(mv6[:], st6[:].rearrange("p a b -> p (a b)")), reads=[st6], writes=[mv6])
                    k.op('act', lambda e: e.activation(rs6[:], mv6[:, 1:2], AF.Sqrt, bias=LN_EPS), reads=[mv6], writes=[rs6])
                    k.op('dve', lambda e: e.reciprocal(rs6[:], rs6[:]), reads=[rs6], writes=[rs6])
                    k.op('dve', lambda e: e.tensor_scalar(o[:], o[:], mv6[:, 0:1], rs6[:, 0:1], ALU.subtract, ALU.mult), reads=[o, mv6, rs6], writes=[o])
                    k.op('dve', lambda e: e.tensor_tensor(o[:], o[:], lnp[:, 2, :], ALU.mult), reads=[o, lnp], writes=[o])
                    k.op('dve', lambda e: e.tensor_tensor(o[:], o[:], lnp[:, 3, :], ALU.add), reads=[o, lnp], writes=[o])
                    k.dma('sp', out_d[b, i * 128:(i + 1) * 128, :], o[:], reads=[o])
           except _Stop:
            pass
        k.barrier()

        k.barrier()
    return nc


def host_inputs(inputs, batches, NB):
    f = lambda a: np.ascontiguousarray(a, dtype=np.float32)
    bs = list(batches)
    m = {}
    m["x"] = f(inputs["x"][bs])
    m["ctx"] = f(inputs["ctx"][bs])
    cc = np.zeros((3, D), np.float32)
    for i, b in enumerate(bs):
        cc[i] = inputs["c"][b]
    cc[2] = inputs["c_ctx"]
    m["cc"] = cc
    m["w_ada"] = f(inputs["w_ada"][0])
    m["b_ada"] = f(inputs["b_ada"][0][None, :])
    m["w_in"] = f(inputs["w_in"][0])
    m["conv_w"] = f(inputs["conv_w"][0].reshape(9, 2560))
    bi, bf = inputs["m_bias_i"][0], inputs["m_bias_f"][0]
    m["m_bias"] = f(np.concatenate([bi[0], bf[0], bi[1], bf[1]])[:, None])
    m["ident"] = np.eye(128, dtype=np.float32)
    gm = np.zeros((32, 2), np.float32)
    gm[0:8, 0] = 1; gm[16:24, 0] = 1; gm[8:16, 1] = -1; gm[24:32, 1] = -1
    m["gmask"] = gm
    ii = np.arange(64)
    m["cmask"] = np.stack([(ii[:, None] <= ii[None, :]), (ii[:, None] >= ii[None, :])], axis=1).astype(np.float32)
    m["m_norm_w"] = f(inputs["m_norm_w"][0][None, :])
    hk = lambda v: np.asarray(v, np.float32).reshape(4, 128).T
    m["rp"] = f(np.stack([hk(inputs["r_w0"][0][0]), hk(inputs["r_w0"][0][1]), hk(inputs["r_a0"][0]), hk(inputs["r_kk"][0]),
                          hk(inputs["r_ka"][0]), hk(inputs["r_ka"][0]), hk(inputs["r_bonus"][0].reshape(-1))], axis=2))
    sm = np.ones((128, 1088), np.float32); sm[:, ::64] = 0
    obd = np.zeros((128, 128), np.float32); obd[:64, :64] = 1; obd[64:, 64:] = 1
    m["onesbd"] = obd
    m["smask"] = sm
    jj = np.arange(128) % 64
    tt = np.arange(128)
    rm = np.zeros((128, 2, 128), np.float32)
    for dd in range(2):
        for col in range(128):
            tq = col % 64
            if col < 64:
                rm[:, dd, col] = (jj < tq) if dd == 0 else (jj > tq)
            else:
                rm[:, dd, col] = (jj <= tq) if dd == 0 else (jj >= tq)
    m["rmask"] = rm
    m["nmask"] = np.stack([(ii[None, :] < ii[:, None]), (ii[None, :] > ii[:, None])], axis=1).astype(np.float32)
    m["r_norm_w"] = f(inputs["r_norm_w"][0][None, :])
    m["r_norm_b"] = f(inputs["r_norm_b"][0][None, :])
    m["r_wB"] = f(inputs["r_wB"][0])
    m["r_aB"] = f(inputs["r_aB"][0])
    m["r_gB"] = f(inputs["r_gB"][0])
    m["w_out"] = f(inputs["w_out"][0])
    for nm in ("ln1_g", "ln1_b", "ln2_g", "ln2_b"):
        m[nm] = f(inputs[nm][0][None, :])
    m["rt"] = f(np.concatenate([inputs["rt_g"][0], inputs["rt_e"][0]], axis=1))
    m["rtb"] = f(np.concatenate([inputs["rt_g_b"][0], inputs["rt_e_b"][0]])[None, :])
    m["ex_gate"] = f(inputs["ex_gate"][0].reshape(8192, 2048))
    m["ex_up"] = f(inputs["ex_up"][0].reshape(8192, 2048))
    m["ex_down"] = f(inputs["ex_down"][0].reshape(8192, 2048))
    pp = np.arange(128)
    m["tris"] = (pp[:, None] < pp[None, :]).astype(np.float32)
    m["thr"] = np.broadcast_to((512.0 * pp)[None, :], (128, 128)).astype(np.float32).copy()
    m["blki"] = np.broadcast_to(np.arange(160, dtype=np.float32)[None, :], (128, 160)).copy()
    m["kcp"] = (2 * pp[:, None] + (np.arange(12) % 2)[None, :]).astype(np.float32)
    m["tokid"] = np.broadcast_to((np.arange(64)[None, :] * 128 + pp[:, None])[:, :, None], (128, 64, 16)).astype(np.int32).copy()
    return m


_NC_CACHE = {}


def kernel(**inputs):
    inputs = {k_: np.asarray(v) for k_, v in inputs.items()}
    NB = 2
    n_cores = 8
    if NB not in _NC_CACHE:
        _NC_CACHE[NB] = build(NB=NB)
    nc = _NC_CACHE[NB]
    in_maps = [host_inputs(inputs, [NB * c + j for j in range(NB)], NB) for c in range(n_cores)]
    res = run_bass_kernel_spmd(nc, in_maps, core_ids=list(range(n_cores)))
    out = np.concatenate([np.asarray(r["out"]) for r in res.results], axis=0)
    return np.ascontiguousarray(out, dtype=np.float32)
```

```python
import math, os
from contextlib import ExitStack
import numpy as np
import concourse.bass as bass
import concourse.mybir as mybir
from concourse.bass_utils import run_bass_kernel_spmd

F32 = mybir.dt.float32
BF16 = mybir.dt.bfloat16
I32 = mybir.dt.int32
AF = mybir.ActivationFunctionType
ALU = mybir.AluOpType
AX = mybir.AxisListType

D = 1024
SEQ = 4096
CTX = 256
T = SEQ + CTX
NCH = T // 64
INC = 3936
DS = math.exp(-0.5)
ALPHA = 2.0 ** 0.25
LN_EPS = 1e-6
GN_EPS = 64e-5
SEC = dict(mq=0, mk=512, rr=1024, rk=1536, rv=2048, mv=2560, mo=3072, gates=3584,
           lwf=3616, lwb=3680, la=3744, lg=3808)
NDS = 40


class _Stop(Exception):
    pass


class Buf:
    def __init__(self, t):
        self.t = t
        self.w = {}
        self.r = {}

    def __getitem__(self, k):
        return self.t[k]


def _merge(d, s):
    for k, v in s.items():
        if d.get(k, 0) < v:
            d[k] = v


class KB:
    def __init__(self, nc):
        self.nc = nc
        self.engs = {'pe': nc.tensor, 'dve': nc.vector, 'act': nc.scalar, 'pool': nc.gpsimd, 'sp': nc.sync}
        self.esem = {e: nc.alloc_semaphore('es_' + e) for e in self.engs}
        self.ecnt = {e: 0 for e in self.engs}
        self.pending = {e: False for e in self.engs}
        self.seen = {e: {} for e in self.engs}
        self.dsem = [nc.alloc_semaphore('ds%d' % i) for i in range(NDS)]
        self.dcnt = [0] * NDS
        self.dnext = 0
        self.es = None
        self.uid = 0

    def semh(self, key):
        return self.esem[key] if isinstance(key, str) else self.dsem[key[1]]

    def _wait(self, eng, need):
        for key, cnt in need.items():
            if self.seen[eng].get(key, 0) >= cnt:
                continue
            if key == eng and eng in ('pe',):
                continue
            self.engs[eng].wait_ge(self.semh(key), cnt)
            self.seen[eng][key] = cnt

    def op(self, eng, fn, reads=(), writes=(), inc=True, pw_=()):
        need = {}
        for b in pw_:
            _merge(need, b.r)
        for b in reads:
            _merge(need, b.w)
            if getattr(b, 'excl', False):
                _merge(need, {kk: vv for kk, vv in b.r.items() if kk != eng})
        for b in writes:
            _merge(need, b.w)
            _merge(need, b.r)
        self._wait(eng, need)
        ins = fn(self.engs[eng])
        cnt = self.ecnt[eng] + 1
        if inc:
            ins.then_inc(self.esem[eng], 1)
            self.ecnt[eng] = cnt
        for b in reads:
            b.r[eng] = cnt
        for b in writes:
            b.w = {eng: cnt}
            b.r = {}
        for b in pw_:
            b.w[eng] = cnt
        return ins

    def dma(self, q, out, in_, reads=(), writes=(), pw=(), indirect=None, **kw):
        i = self.dnext
        self.dnext = (i + 1) % NDS
        need = {}
        if self.dcnt[i]:
            need[('d', i)] = self.dcnt[i]
        for b in reads:
            _merge(need, b.w)
        for b in writes:
            _merge(need, b.w)
            _merge(need, b.r)
        for b in pw:
            _merge(need, b.r)
        self._wait(q, need)
        if indirect is None:
            ins = self.engs[q].dma_start(out=out, in_=in_, **kw)
        else:
            ins = self.engs[q].indirect_dma_start(out, indirect[0], in_, indirect[1], **kw)
        self.dcnt[i] += 16
        ins.then_inc(self.dsem[i], 16)
        key = ('d', i)
        cnt = self.dcnt[i]
        for b in reads:
            b.r[key] = cnt
        for b in writes:
            b.w = {key: cnt}
            b.r = {}
        for b in pw:
            b.w[key] = cnt
        return ins

    def barrier(self):
        need = {e: c for e, c in self.ecnt.items() if c}
        for i in range(NDS):
            if self.dcnt[i]:
                need[('d', i)] = self.dcnt[i]
        for e in self.engs:
            self._wait(e, need)

    def sb(self, name, shape, dt):
        self.uid += 1
        return Buf(self.es.enter_context(self.nc.sbuf_tensor("s%d_%s" % (self.uid, name), list(shape), dt)))

    def ps(self, name, shape, dt=F32):
        self.uid += 1
        return Buf(self.es.enter_context(self.nc.psum_tensor("p%d_%s" % (self.uid, name), list(shape), dt)))


def build(NB=2, debug=None, phases=(0, 1, 2, 3, 4, 5, 6)):
    nc = bass.Bass("TRN2", target_bir_lowering=False)
    k = KB(nc)

    def din(name, shape):
        return nc.dram_tensor(name, list(shape), F32, kind="ExternalInput").ap()

    x_in = din("x", [NB, SEQ, D])
    ctx_in = din("ctx", [NB, CTX, D])
    cc_in = din("cc", [3, D])
    w_ada = din("w_ada", [D, 6 * D])
    b_ada = din("b_ada", [1, 6 * D])
    w_in = din("w_in", [D, INC])
    conv_w = din("conv_w", [9, 2560])
    m_bias = din("m_bias", [32, 1])
    ident_in = din("ident", [128, 128])
    gmask_in = din("gmask", [32, 2])
    cmask_in = din("cmask", [64, 2, 64])
    m_norm_w = din("m_norm_w", [1, 512])
    rp_in = din("rp", [128, 4, 7])
    smask_in = din("smask", [128, 1088])
    onesbd_in = din("onesbd", [128, 128])
    rmask_in = din("rmask", [128, 2, 128])
    nmask_in = din("nmask", [64, 2, 64])
    r_norm_w = din("r_norm_w", [1, 512])
    r_norm_b = din("r_norm_b", [1, 512])
    r_wB = din("r_wB", [2, 64, 512])
    r_aB = din("r_aB", [64, 512])
    r_gB = din("r_gB", [128, 512])
    w_out = din("w_out", [D, D])
    ln1_g = din("ln1_g", [1, D]); ln1_b = din("ln1_b", [1, D]); ln2_g = din("ln2_g", [1, D]); ln2_b = din("ln2_b", [1, D])
    rt_in = din("rt", [D, 36]); rtb_in = din("rtb", [1, 36])
    ex_gate = din("ex_gate", [8192, 2048]); ex_up = din("ex_up", [8192, 2048]); ex_down = din("ex_down", [8192, 2048])
    tris_in = din("tris", [128, 128]); thr_in = din("thr", [128, 128]); blki_in = din("blki", [128, 160]); kcp_in = din("kcp", [128, 12])
    tokid_in = nc.dram_tensor("tokid", [128, 64, 16], I32, kind="ExternalInput").ap()
    out_d = nc.dram_tensor("out", [NB, SEQ, D], F32, kind="ExternalOutput").ap()

    def dscr(name, shape, dt=F32):
        kind = "ExternalOutput" if (debug and name in debug) else "Internal"
        return Buf(nc.dram_tensor(name, list(shape), dt, kind=kind).ap())

    MODD = dscr("MODD", [3, 6 * D])
    FM = [dscr("FM%d" % b, [INC, T]) for b in range(NB)]
    MIX = [dscr("MIX%d" % b, [SEQ, D]) for b in range(NB)]
    BS = 512
    NBLK_ = NB * SEQ * 2 // BS + 32
    X1 = dscr("X1", [NB * SEQ, D])
    H2 = dscr("H2", [NB * SEQ, D], BF16)
    TOKB = dscr("TOKB", [NBLK_ * BS, 16], I32)
    YB = dscr("YB", [NBLK_ * BS, D])

    with ExitStack() as es0:
        k.es = es0
        ident = k.sb("ident", [128, 128], F32)
        identb = k.sb("identb", [128, 128], BF16)
        ones_f = k.sb("ones_f", [128, 128], F32)
        modT = k.sb("modT", [128, 48, 3], F32)
        k.dma('sp', ident[:], ident_in[:, :], writes=[ident])
        k.op('dve', lambda e: e.tensor_copy(identb[:], ident[:]), reads=[ident], writes=[identb])

        with ExitStack() as es:
            k.es = es
            cc = k.sb("cc", [3, D], F32)
            scT = k.sb("scT", [128, 8, 3], F32)
            bada = k.sb("bada", [3, 6 * D], F32)
            mods = k.sb("mods", [3, 6 * D], F32)
            wa = [k.sb("wa%d" % i, [128, 8, 512], F32) for i in range(2)]
            pst = k.ps("p0t", [128, 8, 3])
            psm = [k.ps("p0m%d" % i, [3, 512]) for i in range(2)]
            pmt = k.ps("p0mt", [128, 48, 3])
            k.dma('sp', cc[:], cc_in[:, :], writes=[cc])
            k.dma('sp', bada[:], b_ada[0:1, :].partition_broadcast(3), writes=[bada])
            k.op('act', lambda e: e.activation(cc[:], cc[:], AF.Silu), reads=[cc], writes=[cc])
            for kc in range(8):
                k.op('pe', lambda e: e.transpose(pst[:, kc, :], cc[:, kc * 128:(kc + 1) * 128], ident[0:3, 0:3]),
                     reads=[cc, ident], writes=[pst], inc=(kc == 7))
            k.op('dve', lambda e: e.tensor_copy(scT[:], pst[:]), reads=[pst], writes=[scT])
            for n in range(12):
                wb = wa[n % 2]
                k.dma('sp', wb[:], w_ada[:, n * 512:(n + 1) * 512].rearrange("(kc p) n -> p kc n", p=128), writes=[wb])
                pm = psm[n % 2]
                for kc in range(8):
                    k.op('pe', lambda e: e.matmul(pm[:], scT[:, kc, :], wb[:, kc, :], start=(kc == 0), stop=(kc == 7)),
                         reads=[scT, wb], writes=[pm] if kc == 0 else [], inc=(kc == 7))
                k.op('dve', lambda e: e.tensor_tensor(mods[:, n * 512:(n + 1) * 512], pm[:], bada[:, n * 512:(n + 1) * 512], ALU.add),
                     reads=[pm, bada], writes=[mods])
            k.dma('sp', MODD[:, :], mods[:], reads=[mods], writes=[MODD])
            for j in range(48):
                k.op('pe', lambda e: e.transpose(pmt[:, j, :], mods[:, j * 128:(j + 1) * 128], ident[0:3, 0:3]),
                     reads=[mods, ident], writes=[pmt], inc=(j == 47))
            k.op('dve', lambda e: e.tensor_copy(modT[:], pmt[:]), reads=[pmt], writes=[modT])
            for j0 in (8, 32):
                k.op('dve', lambda e: e.tensor_scalar(modT[:, j0:j0 + 8, :], modT[:, j0:j0 + 8, :], 1.0, None, ALU.add),
                     reads=[modT], writes=[modT])
        k.barrier()

        with ExitStack() as es:
          if 1 in phases:
              k.es = es
              wbf = k.sb("wbf", [128, 8, INC], BF16)
              hT = k.sb("hT", [128, 8, T], BF16)
              xt = [k.sb("xt%d" % i, [128, D], F32) for i in range(2)]
              xn = [k.sb("xn%d" % i, [128, D], BF16) for i in range(2)]
              st = k.sb("st", [128, 2, 6], F32)
              mv = k.sb("mv", [128, 2], F32)
              rstd = k.sb("rstd", [128, 1], F32)
              pT = [k.sb("pT%d" % i, [128, T], F32) for i in range(2)]
              acc = k.sb("acc", [128, T], F32)
              cw = k.sb("cw", [128, 20, 9], F32)
              mb = k.sb("mb", [32, 4], F32)
              ptr = [k.ps("p1t%d" % i, [128, 8, 128], BF16) for i in range(2)]
              pmm = [k.ps("p1m%d" % i, [128, 512]) for i in range(3)]
              pcw = k.ps("p1cw", [128, 20, 9])
              for kc in range(8):
                  for hf in range(2):
                      k.dma('pool', wbf[:, kc, hf * 1968:(hf + 1) * 1968],
                            w_in[kc * 128:(kc + 1) * 128, hf * 1968:(hf + 1) * 1968], pw=[wbf])
              crow = k.sb("crow", [9, 2560], F32)
              k.dma('sp', crow[:], conv_w[:, :], writes=[crow])
              for c in range(20):
                  k.op('pe', lambda e: e.transpose(pcw[:, c, :], crow[:, c * 128:(c + 1) * 128], ident[0:9, 0:9]),
                       reads=[crow, ident], writes=[pcw], inc=(c == 19))
              k.op('dve', lambda e: e.tensor_copy(cw[:], pcw[:]), reads=[pcw], writes=[cw])
              k.dma('sp', mb[:, 0:1], m_bias[:, :], writes=[mb])
              k.op('dve', lambda e: e.tensor_scalar(mb[:, 1:2], mb[:, 0:1], -1.0, None, ALU.mult), reads=[mb], writes=[mb])
              k.dma('sp', mb[:, 2:4], gmask_in[:, :], pw=[mb])
              for b in range(NB):
                  for i in range(int(os.environ.get('P1A', T // 128))):
                      xb, xnb, pt = xt[i % 2], xn[i % 2], ptr[i % 2]
                      src = ctx_in[b, i * 128:(i + 1) * 128, :] if i < 2 else x_in[b, (i - 2) * 128:(i - 1) * 128, :]
                      r = 2 if i < 2 else b
                      k.dma('sp', xb[:], src, writes=[xb])
                      S1 = int(os.environ.get('P1S', 9))
                      for hf in range(2):
                          k.op('dve', lambda e: e.bn_stats(st[:, hf, :], xb[:, hf * 512:(hf + 1) * 512]), reads=[xb], writes=[st])
                      if S1 >= 2: k.op('dve', lambda e: e.bn_aggr(mv[:], st[:].rearrange("p a b -> p (a b)")), reads=[st], writes=[mv])
                      if S1 >= 3: k.op('act', lambda e: e.activation(rstd[:], mv[:, 1:2], AF.Sqrt, bias=LN_EPS), reads=[mv], writes=[rstd])
                      if S1 >= 4: k.op('dve', lambda e: e.reciprocal(rstd[:], rstd[:]), reads=[rstd], writes=[rstd])
                      if S1 >= 5: k.op('dve', lambda e: e.tensor_scalar(xnb[:], xb[:], mv[:, 0:1], rstd[:, 0:1], ALU.subtract, ALU.mult),
                           reads=[xb, mv, rstd], writes=[xnb])
                      for kc in range(8 if S1 >= 6 else 0):
                          k.op('pe', lambda e: e.transpose(pt[:, kc, :], xnb[:, kc * 128:(kc + 1) * 128], identb[:]),
                               reads=[xnb, identb], writes=[pt] if kc == 0 else [], inc=(kc == 7))
                      for kc in range(8 if S1 >= 7 else 0):
                          if i % 2 == 0:
                              k.op('act', lambda e: e.activation(hT[:, kc, i * 128:(i + 1) * 128], pt[:, kc, :], AF.Identity,
                                                                 bias=modT[:, kc, r:r + 1], scale=modT[:, 8 + kc, r:r + 1]),
                                   reads=[pt, modT], writes=[] if (i or kc) else [hT])
                          else:
                              k.op('dve', lambda e: e.tensor_scalar(hT[:, kc, i * 128:(i + 1) * 128], pt[:, kc, :],
                                                                    modT[:, 8 + kc, r:r + 1], modT[:, kc, r:r + 1], ALU.mult, ALU.add),
                                   reads=[pt, modT], writes=[])
                      hT.w['act'] = k.ecnt['act']
                      hT.w['dve'] = k.ecnt['dve']
                  chunks = [(c * 128, 128) for c in range(28)] + [(3584, 32), (3616, 64), (3680, 64), (3744, 64), (3808, 128)]
                  for ci, (c0, M) in enumerate(chunks[:int(os.environ.get('P1C', 99))]):
                      pb = pT[ci % 2]
                      for g in range(9):
                          t0 = g * 512
                          n = min(512, T - t0)
                          pm = pmm[(ci * 9 + g) % 3]
                          for kc in range(8):
                              k.op('pe', lambda e: e.matmul(pm[0:M, 0:n], wbf[:, kc, c0:c0 + M], hT[:, kc, t0:t0 + n],
                                                            start=(kc == 0), stop=(kc == 7)),
                                   reads=[wbf, hT], writes=[pm] if kc == 0 else [], inc=(kc == 7))
                          k.op('act', lambda e: e.activation(pb[0:M, t0:t0 + n], pm[0:M, 0:n], AF.Identity),
                               reads=[pm], writes=[pb] if g == 0 else [])
                          pb.w['act'] = k.ecnt['act']
                      src = pb
                      if c0 < 2560:
                          c = c0 // 128
                          k.op('act', lambda e: e.activation(acc[:, :], pb[:, :], AF.Identity, scale=cw[:, c, 4:5]),
                               reads=[pb, cw], writes=[acc])
                          k.op('dve', lambda e: e.scalar_tensor_tensor(acc[:, 1:CTX], pb[:, 0:CTX - 1], cw[:, c, 3:4], acc[:, 1:CTX], ALU.mult, ALU.add),
                               reads=[pb, cw, acc], writes=[acc])
                          k.op('dve', lambda e: e.scalar_tensor_tensor(acc[:, 0:CTX - 1], pb[:, 1:CTX], cw[:, c, 5:6], acc[:, 0:CTX - 1], ALU.mult, ALU.add),
                               reads=[pb, cw, acc], writes=[acc])
                          a3 = acc[:, CTX:T].rearrange("p (r c) -> p r c", c=64)
                          p3 = pb[:, CTX:T].rearrange("p (r c) -> p r c", c=64)
                          for ky in range(3):
                              for kx in range(3):
                                  if ky == 1 and kx == 1:
                                      continue
                                  dy, dx = ky - 1, kx - 1
                                  oy0, oy1 = max(0, -dy), 64 - max(0, dy)
                                  ox0, ox1 = max(0, -dx), 64 - max(0, dx)
                                  k.op('dve', lambda e: e.scalar_tensor_tensor(
                                      a3[:, oy0:oy1, ox0:ox1], p3[:, oy0 + dy:oy1 + dy, ox0 + dx:ox1 + dx],
                                      cw[:, c, ky * 3 + kx:ky * 3 + kx + 1], a3[:, oy0:oy1, ox0:ox1], ALU.mult, ALU.add),
                                      reads=[pb, cw, acc], writes=[acc])
                          src = acc
                      sec = [s for s, v in SEC.items() if v <= c0][-1]
                      if sec in ('mq', 'mk'):
                          k.op('act', lambda e: e.activation(acc[:, :], src[:, :], AF.Silu), reads=[src], writes=[acc])
                          if sec == 'mk':
                              k.op('dve', lambda e: e.tensor_scalar(acc[:, :], acc[:, :], 0.125, None, ALU.mult), reads=[acc], writes=[acc])
                          src = acc
                      elif sec in ('mo', 'lg'):
                          k.op('act', lambda e: e.activation(acc[0:M, :], src[0:M, :], AF.Sigmoid), reads=[src], writes=[acc])
                          src = acc
                      elif sec in ('lwf', 'lwb'):
                          k.op('act', lambda e: e.activation(acc[0:M, :], src[0:M, :], AF.Tanh), reads=[src], writes=[acc])
                          src = acc
                      elif sec == 'gates':
                          tmp = pT[1 - ci % 2]
                          k.op('act', lambda e: e.activation(tmp[0:32, :], pb[0:32, :], AF.Exp, bias=mb[:, 1:2], scale=-1.0),
                               reads=[pb, mb], writes=[tmp])
                          k.op('act', lambda e: e.activation(tmp[0:32, :], tmp[0:32, :], AF.Ln, bias=1.0), reads=[tmp], writes=[tmp])
                          k.op('dve', lambda e: e.tensor_scalar(tmp[0:32, :], tmp[0:32, :], mb[:, 3:4], None, ALU.mult), reads=[tmp, mb], writes=[tmp])
                          k.op('dve', lambda e: e.tensor_scalar(acc[0:32, :], pb[0:32, :], mb[:, 0:1], mb[:, 2:3], ALU.add, ALU.mult),
                               reads=[pb, mb], writes=[acc])
                          k.op('dve', lambda e: e.tensor_tensor(acc[0:32, :], acc[0:32, :], tmp[0:32, :], ALU.add), reads=[acc, tmp], writes=[acc])
                          src = acc
                      k.dma('sp', FM[b][c0:c0 + M, :], src[0:M, :], reads=[src], pw=[FM[b]])
        k.barrier()


        with ExitStack() as es:
          if 2 in phases:
            k.es = es
            cm = k.sb("cm", [64, 2, 64], F32)
            k.dma('sp', cm[:], cmask_in[:, :, :], writes=[cm])
            nw = k.sb("nw", [64, 512], F32)
            k.dma('sp', nw[:], m_norm_w[0:1, :].partition_broadcast(64), writes=[nw])
            k.op('pool', lambda e: e.memset(ones_f[:], 1.0), writes=[ones_f])
            GA = k.sb("GA", [64, NCH, 48], F32)
            gT = k.sb("gT", [32, T], F32)
            G = k.sb("G", [64, 32], F32)
            qh = k.sb("qh", [64, 2, T], BF16)
            kh = k.sb("kh", [64, 2, T], BF16)
            vT = k.sb("vT", [128, T], BF16)
            moT = k.sb("moT", [128, T], F32)
            Hf = k.sb("Hf", [64, NCH, 2, 64], F32)
            class _M:
                pass
            MS = []
            for si in range(2):
                M = _M()
                M.i = si
                M.Ktm = k.sb("Ktm", [64, 2, 64], BF16)
                M.Vaug = k.sb("Vaug", [64, 2, 66], BF16)
                M.PTm = k.sb("PTm", [64, 2, 64], BF16)
                M.Cst = k.sb("Cst", [64, 2, 66], F32)
                M.Cbf = k.sb("Cbf", [64, 2, 66], BF16)
                M.dn = k.sb("dn", [64, 2], F32)
                M.ff = k.sb("ff", [64, 2], F32)
                M.hs = k.sb("hs", [64, 2, 64], F32)
                M.st2 = k.sb("st2", [64, 2, 6], F32)
                M.mv2 = k.sb("mv2", [64, 2, 2], F32)
                M.rs2 = k.sb("rs2", [64, 2], F32)
                M.om = k.sb("om", [64, 128], F32)
                M.bA = k.ps("p2A", [64, 512])
                M.bB = k.ps("p2B", [64, 512])
                M.bA.excl = True
                M.bB.excl = True
                MS.append(M)
            pg = k.ps("p2g", [64, 32])
            pbb = k.ps("p2b", [64, 32])
            for b in range(NB):
                k.dma('sp', gT[:], FM[b][3584:3616, :], reads=[FM[b]], writes=[gT])
                for c in range(NCH):
                    k.op('pe', lambda e: e.transpose(pg[:], gT[:, c * 64:(c + 1) * 64], ident[0:32, 0:32]), reads=[gT, ident], writes=[pg])
                    k.op('dve', lambda e: e.tensor_copy(G[:], pg[:]), reads=[pg], writes=[G])
                    k.op('pe', lambda e: e.matmul(pbb[:, 0:8], cm[:, 0, :], G[:, 8:16], start=True, stop=True), reads=[cm, G], writes=[pbb], inc=False)
                    k.op('pe', lambda e: e.matmul(pbb[:, 8:16], cm[:, 1, :], G[:, 24:32], start=True, stop=True), reads=[cm, G], inc=False)
                    k.op('pe', lambda e: e.matmul(pbb[:, 16:24], ones_f[0:64, 0:64], G[:, 8:16], start=True, stop=True), reads=[ones_f, G], inc=False)
                    k.op('pe', lambda e: e.matmul(pbb[:, 24:32], ones_f[0:64, 0:64], G[:, 24:32], start=True, stop=True), reads=[ones_f, G])
                    k.op('act', lambda e: e.activation(GA[:, c, 0:32], pbb[:], AF.Exp), reads=[pbb], writes=[GA])
                    k.op('dve', lambda e: e.tensor_tensor(G[:, 0:8], G[:, 0:8], pbb[:, 0:8], ALU.subtract), reads=[pbb, G], writes=[G])
                    k.op('dve', lambda e: e.tensor_tensor(G[:, 16:24], G[:, 16:24], pbb[:, 8:16], ALU.subtract), reads=[pbb, G], writes=[G])
                    k.op('act', lambda e: e.activation(GA[:, c, 32:40], G[:, 0:8], AF.Exp), reads=[G], writes=[GA])
                    k.op('act', lambda e: e.activation(GA[:, c, 40:48], G[:, 16:24], AF.Exp), reads=[G], writes=[GA])
                for hp in range(4):
                    for h in range(2):
                        r0 = hp * 128 + h * 64
                        for q4 in range(4):
                            t0 = q4 * 1088
                            k.dma('pool', qh[:, h, t0:t0 + 1088], FM[b][r0:r0 + 64, t0:t0 + 1088], reads=[FM[b]], pw=[qh])
                            k.dma('pool', kh[:, h, t0:t0 + 1088], FM[b][512 + r0:512 + r0 + 64, t0:t0 + 1088], reads=[FM[b]], pw=[kh])
                    for q4 in range(4):
                        t0 = q4 * 1088
                        k.dma('pool', vT[:, t0:t0 + 1088], FM[b][2560 + hp * 128:2560 + (hp + 1) * 128, t0:t0 + 1088], reads=[FM[b]], pw=[vT])
                    k.dma('sp', moT[:], FM[b][3072 + hp * 128:3072 + (hp + 1) * 128, :], reads=[FM[b]], writes=[moT])
                    def mstream(M, d, done):
                        Ktm, Vaug, PTm, Cst, Cbf, dn, ff, hs, st2, mv2, rs2, om = M.Ktm, M.Vaug, M.PTm, M.Cst, M.Cbf, M.dn, M.ff, M.hs, M.st2, M.mv2, M.rs2, M.om
                        bA, bB = M.bA, M.bB
                        pk = bA[:, 0:64].bitcast(BF16).rearrange("p (h e) -> p h e", h=2)
                        pv = bA[:, 64:128].bitcast(BF16)
                        pp = bA[:, 128:256].rearrange("p (h e) -> p h e", h=2)
                        po = bB[:, 0:132].rearrange("p (h e) -> p h e", h=2)
                        pc = bB[:, 132:264].rearrange("p (h e) -> p h e", h=2)
                        pmo = bB[:, 264:392]
                        order = list(range(NCH)) if d == 0 else [3, 2, 1, 0] + list(range(NCH - 1, 3, -1))
                        k.op('pool', lambda e: e.memset(Cst[:], 0.0), writes=[Cst])
                        k.op('pool', lambda e: e.memset(Cbf[:], 0.0), writes=[Cbf])
                        for c in order:
                            cs = slice(c * 64, (c + 1) * 64)
                            hh = 2 * hp
                            a_ap = GA[:, c, d * 8 + hh:d * 8 + hh + 2]
                            e_ap = GA[:, c, 16 + d * 8 + hh:16 + d * 8 + hh + 2]
                            c_ap = GA[:, c, 32 + d * 8 + hh:32 + d * 8 + hh + 2]
                            needy = c >= 4
                            second = c in done
                            fin = needy and second
                            for h in range(2):
                                k.op('pe', lambda e: e.transpose(pk[:, h, :], kh[:, h, cs], identb[0:64, 0:64]), reads=[kh, identb], writes=[bA] if h == 0 else [], inc=False)
                            k.op('pe', lambda e: e.transpose(pv, vT[:, cs], identb[:]), reads=[vT, identb], inc=False)
                            for h in range(2):
                                k.op('pe', lambda e: e.matmul(pp[:, h, :], kh[:, h, cs], qh[:, h, cs], start=True, stop=True), reads=[kh, qh], inc=(h == 1))
                            k.op('act', lambda e: e.activation(Ktm[:], pk, AF.Identity), reads=[bA], writes=[Ktm])
                            k.op('dve', lambda e: e.tensor_tensor(Vaug[:, :, 0:64], pv.rearrange("p (h e) -> p h e", h=2),
                                                                  c_ap.unsqueeze(2).to_broadcast([64, 2, 64]), ALU.mult), reads=[bA, GA], writes=[Vaug])
                            k.op('act', lambda e: e.activation(Vaug[:, :, 64:65], c_ap.unsqueeze(2), AF.Identity), reads=[GA, Vaug], writes=[Vaug])
                            k.op('dve', lambda e: e.tensor_tensor(PTm[:], pp, cm[:, d:d + 1, :].to_broadcast([64, 2, 64]), ALU.mult), reads=[bA, cm], writes=[PTm])
                            yield
                            for h in range(2):
                                k.op('pe', lambda e: e.matmul(po[:, h, 0:65], PTm[:, h, :], Vaug[:, h, 0:65], start=True, stop=False), reads=[PTm, Vaug], writes=[bB] if h == 0 else [], inc=False)
                                k.op('pe', lambda e: e.matmul(po[:, h, 0:65], qh[:, h, cs], Cbf[:, h, 0:65], start=False, stop=True), reads=[qh, Cbf], inc=False)
                            if fin:
                                k.op('pe', lambda e: e.transpose(pmo, moT[:, cs], ident[:]), reads=[moT, ident], inc=False)
                            for h in range(2):
                                k.op('pe', lambda e: e.matmul(pc[:, h, 0:65], Ktm[:, h, :], Vaug[:, h, 0:65], start=True, stop=True), reads=[Ktm, Vaug], inc=(h == 1))
                            yield
                            k.op('dve', lambda e: e.tensor_tensor(Cst[:, :, 0:65], Cst[:, :, 0:65], pc[:, :, 0:65], ALU.add), reads=[bB, Cst], writes=[Cst])
                            k.op('dve', lambda e: e.tensor_tensor(Cst[:, :, 0:65], Cst[:, :, 0:65], e_ap.unsqueeze(2).to_broadcast([64, 2, 65]), ALU.mult), reads=[GA, Cst], writes=[Cst])
                            k.op('act', lambda e: e.activation(Cbf[:, :, 0:65], Cst[:, :, 0:65], AF.Identity), reads=[Cst], writes=[Cbf])
                            if needy:
                                k.op('dve', lambda e: e.tensor_tensor(dn[:], po[:, :, 64], a_ap, ALU.mult), reads=[bB, GA], writes=[dn])
                                k.op('act', lambda e: e.activation(dn[:], dn[:], AF.Abs), reads=[dn], writes=[dn])
                                k.op('dve', lambda e: e.tensor_scalar(dn[:], dn[:], 1.0, None, ALU.max), reads=[dn], writes=[dn])
                                k.op('dve', lambda e: e.reciprocal(dn[:], dn[:]), reads=[dn], writes=[dn])
                                k.op('dve', lambda e: e.tensor_tensor(ff[:], dn[:], a_ap, ALU.mult), reads=[dn, GA], writes=[ff])
                                if not second:
                                    k.op('dve', lambda e: e.tensor_tensor(Hf[:, c, :, :], po[:, :, 0:64], ff[:].unsqueeze(2).to_broadcast([64, 2, 64]), ALU.mult), reads=[bB, ff], pw_=[Hf])
                                    done[c] = M.i
                                else:
                                    k.op('dve', lambda e: e.tensor_tensor(hs[:], po[:, :, 0:64], ff[:].unsqueeze(2).to_broadcast([64, 2, 64]), ALU.mult), reads=[bB, ff], writes=[hs])
                                    k.op('dve', lambda e: e.tensor_tensor(hs[:], hs[:], Hf[:, c, :, :], ALU.add), reads=[hs, Hf], writes=[hs])
                            if fin:
                                for h in range(2):
                                    k.op('dve', lambda e: e.bn_stats(st2[:, h, :], hs[:, h, :]), reads=[hs], writes=[st2])
                                for h in range(2):
                                    k.op('dve', lambda e: e.bn_aggr(mv2[:, h, :], st2[:, h, :]), reads=[st2], writes=[mv2])
                                k.op('act', lambda e: e.activation(rs2[:], mv2[:, :, 1], AF.Sqrt, bias=LN_EPS), reads=[mv2], writes=[rs2])
                                k.op('dve', lambda e: e.reciprocal(rs2[:], rs2[:]), reads=[rs2], writes=[rs2])
                                for h in range(2):
                                    k.op('dve', lambda e: e.tensor_scalar(hs[:, h, :], hs[:, h, :], mv2[:, h, 0:1], rs2[:, h:h + 1], ALU.subtract, ALU.mult),
                                         reads=[hs, mv2, rs2], writes=[hs])
                                k.op('dve', lambda e: e.tensor_tensor(om[:], hs[:].rearrange("p h e -> p (h e)"), nw[:, hp * 128:(hp + 1) * 128], ALU.mult),
                                     reads=[hs, nw], writes=[om])
                                k.op('dve', lambda e: e.tensor_tensor(om[:], om[:], pmo, ALU.mult), reads=[om, bB], writes=[om])
                                k.dma('sp', MIX[b][(c - 4) * 64:(c - 3) * 64, hp * 128:(hp + 1) * 128], om[:], reads=[om], pw=[MIX[b]])
                            yield

                    done = {}
                    gens = [mstream(MS[0], 0, done), mstream(MS[1], 1, done)]
                    alive = [True, True]
                    while any(alive):
                        for gi in range(2):
                            if alive[gi]:
                                try:
                                    next(gens[gi])
                                except StopIteration:
                                    alive[gi] = False
        k.barrier()

        with ExitStack() as es:
          if 3 in phases:
            k.es = es
            NBK = 512
            NBC = 8
            rp1 = k.sb("rp1", [128, 4, 8], F32)
            k.dma('sp', rp1[:, :, 0:7], rp_in[:, :, :], writes=[rp1])
            k.op('dve', lambda e: e.tensor_scalar(rp1[:, :, 7], rp1[:, :, 4], -1.0, 1.0, ALU.mult, ALU.add), reads=[rp1], writes=[rp1])
            smask = k.sb("smask", [128, NBK], F32)
            k.dma('sp', smask[:], smask_in[:, 0:NBK], writes=[smask])
            onesbd = k.sb("onesbd", [128, 128], F32)
            k.dma('sp', onesbd[:], onesbd_in[:, :], writes=[onesbd])
            ARs = k.sb("ARs", [128, NBC, 128], BF16)
            ZTs = k.sb("ZTs", [128, NBC, 128], BF16)
            PRs = k.sb("PRs", [128, NBK], BF16)
            GLs = k.sb("GLs", [128, NBC], F32)
            rmask = k.sb("rmask", [128, 2, 128], F32)
            k.dma('sp', rmask[:], rmask_in[:, :, :], writes=[rmask])
            nmask = k.sb("nmask", [64, 2, 64], F32)
            k.dma('sp', nmask[:], nmask_in[:, :, :], writes=[nmask])
            gnw = k.sb("gnw", [64, 2, 512], F32)
            k.dma('sp', gnw[:, 0, :], r_norm_w[0:1, :].partition_broadcast(64), pw=[gnw])
            k.dma('sp', gnw[:, 1, :], r_norm_b[0:1, :].partition_broadcast(64), pw=[gnw])
            wBb = k.sb("wBb", [64, 2, 512], BF16)
            aBb = k.sb("aBb", [64, 512], BF16)
            gBb = k.sb("gBb", [128, 512], BF16)
            k.dma('pool', wBb[:, 0, :], r_wB[0, :, :], pw=[wBb])
            k.dma('pool', wBb[:, 1, :], r_wB[1, :, :], pw=[wBb])
            k.dma('pool', aBb[:], r_aB[:, :], writes=[aBb])
            k.dma('pool', gBb[:], r_gB[:, :], writes=[gBb])
            k.op('pool', lambda e: e.memset(ones_f[:], 1.0), writes=[ones_f])
            onesb = k.sb("onesb", [64, 2], BF16)
            k.op('pool', lambda e: e.memset(onesb[:], 1.0), writes=[onesb])
            lgb = k.sb("lgb", [128, T], BF16)
            lab = k.sb("lab", [64, NBK], BF16)
            lwb_ = k.sb("lwb_", [64, NBK], BF16)
            rr = k.sb("rr", [128, NBK], F32)
            rk = k.sb("rk", [128, NBK], F32)
            aa = k.sb("aa", [128, NBK], F32)
            t1 = k.sb("t1", [128, NBK], F32)
            t2 = k.sb("t2", [128, NBK], F32)
            khat = k.sb("khat", [128, NBK], F32)
            kmod = k.sb("kmod", [128, NBK], F32)
            beta = k.sb("beta", [128, NBK], F32)
            lgw = k.sb("lgw", [128, NBK], F32)
            lam = k.sb("lam", [128, NBK], F32)
            ee = k.sb("ee", [128, NBK], F32)
            class _S:
                pass
            SS = []
            for si in range(2):
                S = _S()
                S.i = si
                S.VTb = k.sb("VTb", [64, 2, 64 + NBK], BF16)
                S.PRb = k.sb("PRb", [64, 2, NBK], BF16)
                S.AR = k.sb("AR", [64, 2, NBC, 128], BF16)
                S.ZT = k.sb("ZT", [64, 2, NBC, 128], BF16)
                S.GL = k.sb("GL", [64, 2, NBC], F32)
                S.MmA = k.sb("MmA", [128, 2, NBC, 128], BF16)
                S.XLA = k.sb("XLA", [128, 2, NBC, 64], BF16)
                S.SWA = k.sb("SWA", [128, 2, NBC + 1, 64], BF16)
                S.WA = k.sb("WA", [128, 2, NBC, 64], BF16)
                S.ZtA = k.sb("ZtA", [128, 2, NBC, 64], BF16)
                S.TTA = k.sb("TTA", [64, 2, NBC, 64], BF16)
                S.Nt = [k.sb("Nt%d" % j, [64, 8, 64], BF16) for j in range(2)]
                S.GTt = [k.sb("GTt%d" % j, [64, 8, 2, 64], BF16) for j in range(2)]
                S.Xb = k.sb("Xb", [64, 2, 64], BF16)
                S.ST = k.sb("ST", [64, 2, 64], F32)
                S.tS = k.sb("tS", [64, 2, 64], F32)
                S.ys = k.sb("ys", [64, 2, 64], F32)
                S.bon = k.sb("bon", [64, 2], F32)
                S.om3 = k.sb("om3", [64, 128], F32)
                S.st2 = k.sb("st2r", [64, 2, 6], F32)
                S.mv2 = k.sb("mv2r", [64, 2, 2], F32)
                S.rs2 = k.sb("rs2r", [64, 2], F32)
                S.pXU = k.ps("p3x", [64, 2, 2, 64])
                S.pYS = k.ps("p3y", [64, 512])
                SS.append(S)
            Yf = k.sb("Yf", [64, NCH, 2, 64], F32)
            identg = k.sb("identg", [64, 8, 64], F32)
            pMg = k.ps("p3m", [128, 8, 128])
            pSg = k.ps("p3s", [128, 2, 512])
            pA = pMg
            for S in SS:
                k.op('pool', lambda e: e.memset(S.VTb[:], 0.0), writes=[S.VTb])
                k.op('pool', lambda e: e.memset(S.SWA[:], 0.0), writes=[S.SWA])
            for m in range(8):
                k.op('dve', lambda e: e.tensor_copy(identg[:, m, :], ident[0:64, 0:64]), reads=[ident], writes=[identg])

            prep_lock = [False]

            def prep(S, b, hp, tok0, ntok, d):
                VTb, PRb, AR, ZT, GL = S.VTb, S.PRb, S.AR, S.ZT, S.GL
                while prep_lock[0]:
                    yield
                prep_lock[0] = True
                nch = ntok // 64
                n = ntok
                NS = slice(0, ntok)
                r0 = hp * 128
                k.dma('pool', lab[:, NS], FM[b][3744:3808, tok0:tok0 + ntok], reads=[FM[b]], writes=[lab])
                lo = 3616 + 64 * d
                k.dma('pool', lwb_[:, NS], FM[b][lo:lo + 64, tok0:tok0 + ntok], reads=[FM[b]], writes=[lwb_])
                k.dma('sp', rr[:, NS], FM[b][1024 + r0:1024 + r0 + 128, tok0:tok0 + ntok], reads=[FM[b]], writes=[rr])
                k.dma('sp', rk[:, NS], FM[b][1536 + r0:1536 + r0 + 128, tok0:tok0 + ntok], reads=[FM[b]], writes=[rk])
                for h in range(2):
                    k.dma('pool', VTb[:, h, 64:64 + ntok], FM[b][2048 + r0 + h * 64:2048 + r0 + (h + 1) * 64, tok0:tok0 + ntok], reads=[FM[b]], pw=[VTb])
                yield
                pA0 = pMg[:, 0:4, :].rearrange("p a b -> p (a b)")[:, 0:n]
                pA1 = pMg[:, 4:8, :].rearrange("p a b -> p (a b)")[:, 0:n]
                k.op('pe', lambda e: e.matmul(pA0, aBb[:, hp * 128:(hp + 1) * 128], lab[:, NS], start=True, stop=True), reads=[aBb, lab], writes=[pMg], inc=False)
                k.op('pe', lambda e: e.matmul(pA1, wBb[:, d, hp * 128:(hp + 1) * 128], lwb_[:, NS], start=True, stop=True), reads=[wBb, lwb_])
                k.op('act', lambda e: e.activation(aa[:, NS], pA0, AF.Sigmoid, bias=rp1[:, hp, 2:3]), reads=[pMg, rp1], writes=[aa])
                k.op('act', lambda e: e.activation(lgw[:, NS], pA1, AF.Sigmoid, bias=rp1[:, hp, d:d + 1]), reads=[pMg, rp1], writes=[lgw])
                k.op('dve', lambda e: e.tensor_scalar(lgw[:, NS], lgw[:, NS], -DS, None, ALU.mult), reads=[lgw], writes=[lgw])
                yield
                k.op('dve', lambda e: e.tensor_scalar(t1[:, NS], rk[:, NS], rp1[:, hp, 3:4], None, ALU.mult), reads=[rk, rp1], writes=[t1])
                k.op('dve', lambda e: e.tensor_tensor(t2[:, NS], t1[:, NS], t1[:, NS], ALU.mult), reads=[t1], writes=[t2])
                k.op('pe', lambda e: e.matmul(pA0, onesbd[:], t2[:, NS], start=True, stop=True), reads=[onesbd, t2], writes=[pMg])
                k.op('act', lambda e: e.activation(khat[:, NS], pA0, AF.Sqrt), reads=[pMg], writes=[khat])
                yield
                k.op('dve', lambda e: e.tensor_scalar(khat[:, NS], khat[:, NS], 1e-12, None, ALU.max), reads=[khat], writes=[khat])
                k.op('dve', lambda e: e.reciprocal(khat[:, NS], khat[:, NS]), reads=[khat], writes=[khat])
                k.op('dve', lambda e: e.tensor_tensor(khat[:, NS], khat[:, NS], t1[:, NS], ALU.mult), reads=[khat, t1], writes=[khat])
                yield
                k.op('dve', lambda e: e.tensor_scalar(t1[:, NS], aa[:, NS], rp1[:, hp, 4:5], rp1[:, hp, 7:8], ALU.mult, ALU.add), reads=[aa, rp1], writes=[t1])
                k.op('dve', lambda e: e.tensor_tensor(kmod[:, NS], rk[:, NS], t1[:, NS], ALU.mult), reads=[rk, t1], writes=[kmod])
                k.op('dve', lambda e: e.tensor_tensor(beta[:, NS], khat[:, NS], aa[:, NS], ALU.mult), reads=[khat, aa], writes=[beta])
                yield
                k.op('dve', lambda e: e.scalar_tensor_tensor(PRs[:, NS], rr[:, NS], rp1[:, hp, 6:7], kmod[:, NS], ALU.mult, ALU.mult), reads=[rr, rp1, kmod], writes=[PRs])
                k.op('dve', lambda e: e.tensor_tensor_scan(lam[:, NS], smask[:, NS], lgw[:, NS], 0.0, ALU.mult, ALU.add), reads=[smask, lgw], writes=[lam])
                yield
                v3 = lambda t_: t_[:, NS].rearrange("p (c t) -> p c t", t=64)
                if d == 1:
                    k.op('dve', lambda e: e.tensor_tensor(t1[:, NS], lgw[:, NS], lam[:, NS], ALU.subtract), reads=[lgw, lam], writes=[t1])
                    k.op('dve', lambda e: e.tensor_tensor(v3(t2), v3(t1), v3(lam)[:, :, 63:64].to_broadcast([128, nch, 64]), ALU.add), reads=[t1, lam], writes=[t2])
                    k.op('act', lambda e: e.activation(lam[:, NS], t2[:, NS], AF.Identity), reads=[t2], writes=[lam])
                k.op('dve', lambda e: e.tensor_tensor(t1[:, NS], lam[:, NS], lgw[:, NS], ALU.subtract), reads=[lam, lgw], writes=[t1])
                k.op('act', lambda e: e.activation(ee[:, NS], t1[:, NS], AF.Exp), reads=[t1], writes=[ee])
                yield
                k.op('dve', lambda e: e.scalar_tensor_tensor(ARs[:, 0:nch, 0:64], v3(khat), -1.0, v3(ee), ALU.mult, ALU.mult), reads=[khat, ee], writes=[ARs])
                k.op('act', lambda e: e.activation(ee[:, NS], lam[:, NS], AF.Exp), reads=[lam], writes=[ee])
                k.op('dve', lambda e: e.tensor_tensor(ARs[:, 0:nch, 64:128], v3(rr), v3(ee), ALU.mult), reads=[rr, ee], writes=[ARs])
                yield
                gcol = 63 if d == 0 else 0
                k.op('act', lambda e: e.activation(GLs[:, 0:nch], v3(ee)[:, :, gcol], AF.Identity), reads=[ee], writes=[GLs])
                k.op('act', lambda e: e.activation(t1[:, NS], lam[:, NS], AF.Exp, scale=-1.0), reads=[lam], writes=[t1])
                yield
                k.op('dve', lambda e: e.tensor_tensor(ZTs[:, 0:nch, 0:64], v3(beta), v3(t1), ALU.mult), reads=[beta, t1], writes=[ZTs])
                k.op('dve', lambda e: e.tensor_tensor(ZTs[:, 0:nch, 64:128], v3(kmod), v3(t1), ALU.mult), reads=[kmod, t1], writes=[ZTs])
                yield
                k.op('act', lambda e: e.activation(AR[:, 0, 0:nch, :], ARs[0:64, 0:nch, :], AF.Identity), reads=[ARs], writes=[AR])
                k.op('act', lambda e: e.activation(ZT[:, 0, 0:nch, :], ZTs[0:64, 0:nch, :], AF.Identity), reads=[ZTs], writes=[ZT])
                k.op('act', lambda e: e.activation(PRb[:, 0, NS], PRs[0:64, NS], AF.Identity), reads=[PRs], writes=[PRb])
                k.op('act', lambda e: e.activation(GL[:, 0, 0:nch], GLs[0:64, 0:nch], AF.Identity), reads=[GLs], writes=[GL])
                k.dma('sp', AR[:, 1, 0:nch, :], ARs[64:128, 0:nch, :], reads=[ARs], pw=[AR])
                k.dma('sp', ZT[:, 1, 0:nch, :], ZTs[64:128, 0:nch, :], reads=[ZTs], pw=[ZT])
                k.dma('sp', PRb[:, 1, NS], PRs[64:128, NS], reads=[PRs], pw=[PRb])
                k.dma('sp', GL[:, 1, 0:nch], GLs[64:128, 0:nch], reads=[GLs], pw=[GL])
                prep_lock[0] = False

            pSf = lambda: pSg[:].rearrange("p a b -> p (a b)")

            def precompute(S, l0, d):
                VTb, AR, ZT, MmA, XLA, SWA, WA, ZtA, TTA = S.VTb, S.AR, S.ZT, S.MmA, S.XLA, S.SWA, S.WA, S.ZtA, S.TTA
                G4 = slice(l0, l0 + 4)
                pTb = pSg[:, 0, :].bitcast(BF16)
                for h in range(2):
                    for j in range(4):
                        m = h * 4 + j
                        k.op('pe', lambda e: e.transpose(pTb[:, m * 64:(m + 1) * 64], ZT[:, h, l0 + j, :], identb[0:64, 0:64]), reads=[ZT, identb], writes=[pSg], inc=False)
                for h in range(2):
                    for j in range(4):
                        m = 8 + h * 4 + j
                        k.op('pe', lambda e: e.transpose(pTb[:, m * 64:(m + 1) * 64], VTb[:, h, (l0 + j) * 64:(l0 + j) * 64 + 128], identb[0:64, 0:64]), reads=[VTb, identb], inc=(h == 1 and j == 3))
                zsrc = pTb[:, 0:512].rearrange("p (h j e) -> p h j e", h=2, j=4)
                vsrc = pTb[64:128, 512:1024].rearrange("p (h j e) -> p h j e", h=2, j=4)
                k.op('act', lambda e: e.activation(ZtA[:, :, G4, :], zsrc, AF.Identity), reads=[pSg], writes=[ZtA])
                k.op('act', lambda e: e.activation(SWA[64:128, :, G4, :], vsrc, AF.Identity), reads=[pSg], writes=[SWA])
                k.op('act', lambda e: e.activation(WA[64:128, :, G4, :], vsrc, AF.Identity), reads=[pSg], writes=[WA])
                yield
                for h in range(2):
                    for j in range(4):
                        m = h * 4 + j
                        k.op('pe', lambda e: e.matmul(pMg[:, m, :], ZT[:, h, l0 + j, :], AR[:, h, l0 + j, :], start=True, stop=True), reads=[ZT, AR], writes=[pMg], inc=(m == 7))
                for h in range(2):
                    for j in range(4):
                        m = h * 4 + j
                        k.op('pe', lambda e: e.matmul(pSg[0:64, 1, m * 64:(m + 1) * 64], AR[:, h, l0 + j, 0:64], ZT[:, h, l0 + j, 0:64], start=True, stop=True), reads=[ZT, AR], writes=[pSg], inc=(m == 7))
                for h in range(2):
                    k.op('dve', lambda e: e.tensor_tensor(MmA[:, h, G4, :], pMg[:, h * 4:(h + 1) * 4, :], rmask[:, d:d + 1, :].to_broadcast([128, 4, 128]), ALU.mult),
                         reads=[pMg, rmask], writes=[MmA])
                Nt, GTt = S.Nt, S.GTt
                k.op('dve', lambda e: e.tensor_tensor(Nt[0][:], pSg[0:64, 1, :].rearrange("p (m e) -> p m e", e=64), nmask[:, d:d + 1, :].to_broadcast([64, 8, 64]), ALU.mult),
                     reads=[pSg, nmask], writes=[Nt[0]])
                k.op('act', lambda e: e.activation(GTt[0][:, :, 0, :].rearrange("p (h j) e -> p h j e", h=2), MmA[0:64, :, G4, 0:64], AF.Identity), reads=[MmA], writes=[GTt[0]])
                k.op('act', lambda e: e.activation(GTt[0][:, :, 1, :], identg[:], AF.Identity), reads=[identg, GTt[0]], writes=[GTt[0]])
                k.op('act', lambda e: e.activation(XLA[64:128, :, G4, :], MmA[64:128, :, G4, 0:64], AF.Identity), reads=[MmA], writes=[XLA])
                k.op('act', lambda e: e.activation(XLA[0:64, :, G4, :], AR[:, :, G4, 0:64], AF.Identity), reads=[AR], writes=[XLA])
                cur = 0
                for lv in range(5):
                    yield
                    nsrc, gsrc, ndst, gdst = Nt[cur], GTt[cur], Nt[1 - cur], GTt[1 - cur]
                    for m in range(8):
                        k.op('pe', lambda e: e.matmul(pSg[0:64, 0, m * 64:(m + 1) * 64], gsrc[:, m, 0, :], nsrc[:, m, :], start=True, stop=True), reads=[gsrc, nsrc], writes=[pSg] if m == 0 else [], inc=False)
                    for m in range(8):
                        k.op('pe', lambda e: e.matmul(pMg[0:64, m, :], nsrc[:, m, :], gsrc[:, m, :, :].rearrange("p a e -> p (a e)"), start=True, stop=True), reads=[nsrc, gsrc], writes=[pMg] if m == 0 else [], inc=(m == 7))
                    k.op('act', lambda e: e.activation(ndst[:].rearrange("p m e -> p (m e)"), pSg[0:64, 0, :], AF.Identity), reads=[pSg], writes=[ndst])
                    k.op('act', lambda e: e.activation(gdst[:, :, 0, :], pMg[0:64, :, 0:64], AF.Identity), reads=[pMg], writes=[gdst])
                    k.op('dve', lambda e: e.tensor_tensor(gdst[:, :, 1, :], pMg[0:64, :, 64:128], gsrc[:, :, 1, :], ALU.add), reads=[pMg, gsrc, gdst], writes=[gdst])
                    cur = 1 - cur
                yield
                nsrc, gsrc = Nt[cur], GTt[cur]
                for m in range(8):
                    k.op('pe', lambda e: e.matmul(pSg[0:64, 1, m * 64:(m + 1) * 64], nsrc[:, m, :], gsrc[:, m, 1, :], start=True, stop=True), reads=[nsrc, gsrc], writes=[pSg] if m == 0 else [], inc=(m == 7))
                k.op('dve', lambda e: e.tensor_tensor(TTA[:, :, G4, :], pSg[0:64, 1, :].rearrange("p (h j e) -> p h j e", h=2, j=4), gsrc[:, :, 1, :].rearrange("p (h j) e -> p h j e", h=2), ALU.add),
                     reads=[pSg, gsrc], writes=[TTA])

            def step(S, b, hp, c, lc, lnext, d, done):
                VTb, PRb, AR, GL, MmA, XLA, SWA, WA, ZtA, TTA = S.VTb, S.PRb, S.AR, S.GL, S.MmA, S.XLA, S.SWA, S.WA, S.ZtA, S.TTA
                Xb, ST, tS, ys, bon, om3, st2, mv2, rs2, pXU, pYS = S.Xb, S.ST, S.tS, S.ys, S.bon, S.om3, S.st2, S.mv2, S.rs2, S.pXU, S.pYS
                pX = pXU[:, 0, :, :]
                pU = pXU[:, 1, :, :]
                pY = pYS[:, 0:128].rearrange("p (h e) -> p h e", h=2)
                pS_ = pYS[:, 128:256].rearrange("p (h e) -> p h e", h=2)
                for h in range(2):
                    k.op('pe', lambda e: e.matmul(pXU[:, 0, h, :], XLA[:, h, lc, :], SWA[:, h, lc, :], start=True, stop=True), reads=[XLA, SWA], writes=[pXU], inc=(h == 1))
                k.op('act', lambda e: e.activation(Xb[:], pX, AF.Identity), reads=[pXU], writes=[Xb])
                yield
                for h in range(2):
                    k.op('pe', lambda e: e.matmul(pXU[:, 1, h, :], TTA[:, h, lc, :], Xb[:, h, :], start=True, stop=True), reads=[TTA, Xb], writes=[pXU], inc=(h == 1))
                k.op('dve', lambda e: e.tensor_copy(WA[0:64, :, lc, :], pU), reads=[pXU], writes=[WA])
                yield
                second = (c in done)
                needy = (c >= 4)
                wfirst = [pYS]
                if needy:
                    for h in range(2):
                        k.op('pe', lambda e: e.matmul(pYS[:, h * 64:(h + 1) * 64], AR[:, h, lc, 64:128], SWA[0:64, h, lc, :], start=True, stop=False), reads=[AR, SWA], writes=wfirst, inc=False)
                        wfirst = []
                        k.op('pe', lambda e: e.matmul(pYS[:, h * 64:(h + 1) * 64], MmA[:, h, lc, 64:128], WA[:, h, lc, :], start=False, stop=True), reads=[MmA, WA], inc=False)
                fin = (needy and second)
                if fin:
                    ts = slice(lc * 64, (lc + 1) * 64)
                    for h in range(2):
                        k.op('pe', lambda e: e.matmul(pYS[:, 384 + 2 * h:386 + 2 * h], PRb[:, h, ts], onesb[:, 0:2], start=True, stop=True), reads=[PRb, onesb], inc=False)
                    k.op('pe', lambda e: e.matmul(pYS[:, 256:384], lgb[:, c * 64:(c + 1) * 64], gBb[:, hp * 128:(hp + 1) * 128], start=True, stop=True), reads=[lgb, gBb], inc=False)
                    pvt = pYS[:, 448:512].bitcast(BF16)
                    for h in range(2):
                        k.op('pe', lambda e: e.transpose(pvt[:, h * 64:(h + 1) * 64], VTb[:, h, 64 + lc * 64:128 + lc * 64], identb[0:64, 0:64]), reads=[VTb, identb], inc=False)
                for h in range(2):
                    k.op('pe', lambda e: e.matmul(pYS[:, 128 + h * 64:128 + (h + 1) * 64], ZtA[:, h, lc, :], WA[:, h, lc, :], start=True, stop=True), reads=[ZtA, WA], writes=wfirst, inc=(h == 1))
                    wfirst = []
                k.op('dve', lambda e: e.tensor_tensor(tS[:], pS_, ST[:], ALU.add), reads=[pYS, ST], writes=[tS])
                k.op('dve', lambda e: e.tensor_tensor(ST[:], tS[:], GL[:, :, lc:lc + 1].to_broadcast([64, 2, 64]), ALU.mult), reads=[tS, GL], writes=[ST])
                k.op('act', lambda e: e.activation(SWA[0:64, :, lnext, :], ST[:], AF.Identity), reads=[ST], writes=[SWA])
                if needy and not second:
                    k.op('dve', lambda e: e.tensor_copy(Yf[:, c, :, :], pY), reads=[pYS], pw_=[Yf])
                    done[c] = S.i
                elif needy:
                    k.op('dve', lambda e: e.tensor_tensor(ys[:], pY, Yf[:, c, :, :], ALU.add), reads=[pYS, Yf], writes=[ys])
                if fin:
                    for h in range(2):
                        k.op('dve', lambda e: e.bn_stats(st2[:, h, :], ys[:, h, :]), reads=[ys], writes=[st2])
                    for h in range(2):
                        k.op('dve', lambda e: e.bn_aggr(mv2[:, h, :], st2[:, h, :]), reads=[st2], writes=[mv2])
                    k.op('act', lambda e: e.activation(rs2[:], mv2[:, :, 1], AF.Sqrt, bias=GN_EPS), reads=[mv2], writes=[rs2])
                    k.op('dve', lambda e: e.reciprocal(rs2[:], rs2[:]), reads=[rs2], writes=[rs2])
                    for h in range(2):
                        k.op('dve', lambda e: e.tensor_scalar(ys[:, h, :], ys[:, h, :], mv2[:, h, 0:1], rs2[:, h:h + 1], ALU.subtract, ALU.mult), reads=[ys, mv2, rs2], writes=[ys])
                    ysf = ys[:].rearrange("p h e -> p (h e)")
                    k.op('dve', lambda e: e.tensor_tensor(om3[:], ysf, gnw[:, 0, hp * 128:(hp + 1) * 128], ALU.mult), reads=[ys, gnw], writes=[om3])
                    k.op('dve', lambda e: e.tensor_tensor(om3[:], om3[:], gnw[:, 1, hp * 128:(hp + 1) * 128], ALU.add), reads=[om3, gnw], writes=[om3])
                    k.op('dve', lambda e: e.tensor_copy(bon[:], pYS[:, 384:388].rearrange("p (h two) -> p h two", two=2)[:, :, 0]), reads=[pYS], writes=[bon])
                    for h in range(2):
                        k.op('dve', lambda e: e.scalar_tensor_tensor(om3[:, h * 64:(h + 1) * 64], pvt[:, h * 64:(h + 1) * 64], bon[:, h:h + 1], om3[:, h * 64:(h + 1) * 64], ALU.mult, ALU.add),
                             reads=[pYS, bon, om3], writes=[om3])
                    k.op('dve', lambda e: e.tensor_tensor(om3[:], om3[:], pYS[:, 256:384], ALU.mult), reads=[om3, pYS], writes=[om3])
                    k.dma('sp', MIX[b][(c - 4) * 64:(c - 3) * 64, 512 + hp * 128:512 + (hp + 1) * 128], om3[:], reads=[om3], pw=[MIX[b]])

            blocks = [(0, 256)] + [(256 + i * 512, 512) for i in range(8)]

            def stream(S, b, hp, d, done):
                k.op('pool', lambda e: e.memset(S.ST[:], 0.0), writes=[S.ST])
                border = list(range(9)) if d == 0 else [0] + list(range(8, 0, -1))
                first = True
                for bi in border:
                    tok0, ntok = blocks[bi]
                    nch = ntok // 64
                    yield from prep(S, b, hp, tok0, ntok, d)
                    lcs = list(range(nch)) if d == 0 else list(range(nch - 1, -1, -1))
                    if first:
                        k.op('pool', lambda e: e.memset(S.SWA[0:64, :, lcs[0], :], 0.0), writes=[S.SWA])
                        first = False
                    else:
                        k.op('act', lambda e: e.activation(S.SWA[0:64, :, lcs[0], :], S.ST[:], AF.Identity), reads=[S.ST], writes=[S.SWA])
                    yield
                    for g0 in range(0, nch, 4):
                        if os.environ.get('RW_SKIP_PRE'):
                            break
                        yield from precompute(S, g0, d)
                        yield
                    for ii, lc in enumerate(lcs):
                        if os.environ.get('RW_SKIP_STEP'):
                            break
                        lnext = lcs[ii + 1] if ii + 1 < len(lcs) else NBC
                        yield from step(S, b, hp, tok0 // 64 + lc, lc, lnext, d, done)
                        yield

            for b in range(NB):
                for q4 in range(4):
                    k.dma('pool', lgb[:, q4 * 1088:(q4 + 1) * 1088], FM[b][3808:3936, q4 * 1088:(q4 + 1) * 1088], reads=[FM[b]], pw=[lgb])
                for hp in range(int(os.environ.get('P3H', 4))):
                    done = {}
                    gens = [stream(SS[0], b, hp, 0, done), stream(SS[1], b, hp, 1, done)]
                    alive = [True, True]
                    for _ in range(int(os.environ.get('RW_OFF', 28))):
                        next(gens[0])
                    while any(alive):
                        for gi in range(2):
                            if alive[gi]:
                                try:
                                    next(gens[gi])
                                except StopIteration:
                                    alive[gi] = False
        k.barrier()

        with ExitStack() as es:
          if 4 in phases:
           try:
            P4S = int(os.environ.get('P4S', 9))
            k.es = es
            NT = NB * SEQ // 128
            NBLK = NBLK_
            SUB = BS // 128
            LG = k.sb("LG", [128, NT, 36], F32)
            OH1 = k.sb("OH1", [128, NT, 32], F32)
            OH2 = k.sb("OH2", [128, NT, 32], F32)
            W1 = k.sb("W1", [128, NT], F32)
            W2 = k.sb("W2", [128, NT], F32)
            DST = k.sb("DST", [128, NT, 2], I32)
            WIDX = k.sb("WIDX", [128, NBLK, 12], I32)
            g2b = k.sb("g2b", [128, NB, D], F32)
            lnp = k.sb("lnp", [128, 4, D], F32)
            for j, src in enumerate((ln1_g, ln1_b, ln2_g, ln2_b)):
                k.dma('sp', lnp[:, j, :], src[0:1, :].partition_broadcast(128), pw=[lnp])
            for b in range(NB):
                k.dma('sp', g2b[:, b, :], MODD[b:b + 1, 5 * D:6 * D].partition_broadcast(128), reads=[MODD], pw=[g2b])
            with ExitStack() as es4:
                k.es = es4
                wob = k.sb("wob", [128, 8, D], BF16)
                for kc in range(8):
                    k.dma('pool', wob[:, kc, :], w_out[kc * 128:(kc + 1) * 128, :], pw=[wob])
                rt = k.sb("rt", [128, 8, 36], F32)
                k.dma('sp', rt[:], rt_in[:, :].rearrange("(kc p) n -> p kc n", p=128), writes=[rt])
                rtbb = k.sb("rtbb", [128, 36], F32)
                k.dma('sp', rtbb[:], rtb_in[0:1, :].partition_broadcast(128), writes=[rtbb])
                mb4 = k.sb("mb4", [128, 3, D], F32)
                class _A:
                    pass
                AS = []
                for si in range(2):
                    A = _A()
                    A.mxb = k.sb("mxb", [128, D], BF16)
                    A.mT = k.sb("mT", [128, 8, 128], BF16)
                    A.x4 = k.sb("x4", [128, D], F32)
                    A.t4 = k.sb("t4", [128, D], F32)
                    A.y4 = k.sb("y4", [128, D], F32)
                    A.h4 = k.sb("h4", [128, D], F32)
                    A.h4b = k.sb("h4b", [128, D], BF16)
                    A.h4T = k.sb("h4T", [128, 8, 128], F32)
                    A.st4 = k.sb("st4", [128, 2, 6], F32)
                    A.mv4 = k.sb("mv4", [128, 2], F32)
                    A.rs4 = k.sb("rs4", [128, 1], F32)
                    A.nb4 = k.sb("nb4", [128, 1], F32)
                    A.P01 = k.ps("p4o", [128, 2, 512])
                    A.P01.excl = True
                    A.plg = k.ps("p4lg", [128, 36])
                    AS.append(A)

                def ln_stats(A, src):
                    for hf in range(2):
                        k.op('dve', lambda e: e.bn_stats(A.st4[:, hf, :], src[:, hf * 512:(hf + 1) * 512]), reads=[src], writes=[A.st4])
                    k.op('dve', lambda e: e.bn_aggr(A.mv4[:], A.st4[:].rearrange("p a b -> p (a b)")), reads=[A.st4], writes=[A.mv4])
                    k.op('act', lambda e: e.activation(A.rs4[:], A.mv4[:, 1:2], AF.Sqrt, bias=LN_EPS), reads=[A.mv4], writes=[A.rs4])
                    k.op('dve', lambda e: e.reciprocal(A.rs4[:], A.rs4[:]), reads=[A.rs4], writes=[A.rs4])
                    k.op('dve', lambda e: e.scalar_tensor_tensor(A.nb4[:], A.mv4[:, 0:1], -1.0, A.rs4[:], ALU.mult, ALU.mult), reads=[A.mv4, A.rs4], writes=[A.nb4])

                def tile4(A, b, i):
                    gi = b * (SEQ // 128) + i
                    P01 = A.P01
                    k.dma('pool', A.mxb[:], MIX[b][i * 128:(i + 1) * 128, :], reads=[MIX[b]], writes=[A.mxb])
                    k.dma('sp', A.x4[:], x_in[b, i * 128:(i + 1) * 128, :], writes=[A.x4])
                    yield
                    ptm = P01[:, 0, :].bitcast(BF16)
                    for kc in range(8):
                        k.op('pe', lambda e: e.transpose(ptm[:, kc * 128:(kc + 1) * 128], A.mxb[:, kc * 128:(kc + 1) * 128], identb[:]), reads=[A.mxb, identb], writes=[P01] if kc == 0 else [], inc=(kc == 7))
                    k.op('act', lambda e: e.activation(A.mT[:].rearrange("p a b -> p (a b)"), ptm, AF.Identity), reads=[P01], writes=[A.mT])
                    yield
                    for n in range(2):
                        for kc in range(8):
                            k.op('pe', lambda e: e.matmul(P01[:, n, :], A.mT[:, kc, :], wob[:, kc, n * 512:(n + 1) * 512], start=(kc == 0), stop=(kc == 7)),
                                 reads=[A.mT, wob], writes=[P01] if (kc == 0 and n == 0) else [], inc=(kc == 7 and n == 1))
                    k.op('dve', lambda e: e.tensor_tensor(A.t4[:], P01[:].rearrange("p a b -> p (a b)"), mb4[:, 0, :], ALU.mult), reads=[P01, mb4], writes=[A.t4])
                    k.op('dve', lambda e: e.scalar_tensor_tensor(A.y4[:], A.x4[:], ALPHA, A.t4[:], ALU.mult, ALU.add), reads=[A.x4, A.t4], writes=[A.y4])
                    yield
                    ln_stats(A, A.y4)
                    k.op('act', lambda e: e.activation(A.y4[:], A.y4[:], AF.Identity, bias=A.nb4[:, 0:1], scale=A.rs4[:, 0:1]), reads=[A.y4, A.nb4, A.rs4], writes=[A.y4])
                    yield
                    k.op('dve', lambda e: e.tensor_tensor(A.y4[:], A.y4[:], lnp[:, 0, :], ALU.mult), reads=[A.y4, lnp], writes=[A.y4])
                    k.op('dve', lambda e: e.tensor_tensor(A.y4[:], A.y4[:], lnp[:, 1, :], ALU.add), reads=[A.y4, lnp], writes=[A.y4])
                    k.dma('sp', X1[gi * 128:(gi + 1) * 128, :], A.y4[:], reads=[A.y4], pw=[X1])
                    yield
                    ln_stats(A, A.y4)
                    k.op('act', lambda e: e.activation(A.h4[:], A.y4[:], AF.Identity, bias=A.nb4[:, 0:1], scale=A.rs4[:, 0:1]), reads=[A.y4, A.nb4, A.rs4], writes=[A.h4])
                    yield
                    k.op('dve', lambda e: e.tensor_tensor(A.h4[:], A.h4[:], mb4[:, 1, :], ALU.mult), reads=[A.h4, mb4], writes=[A.h4])
                    k.op('dve', lambda e: e.tensor_tensor(A.h4[:], A.h4[:], mb4[:, 2, :], ALU.add), reads=[A.h4, mb4], writes=[A.h4])
                    k.op('act', lambda e: e.activation(A.h4b[:], A.h4[:], AF.Identity), reads=[A.h4], writes=[A.h4b])
                    k.dma('sp', H2[gi * 128:(gi + 1) * 128, :], A.h4b[:], reads=[A.h4b], pw=[H2])
                    yield
                    pth = P01[:].rearrange("p a b -> p (a b)")
                    for kc in range(8):
                        k.op('pe', lambda e: e.transpose(pth[:, kc * 128:(kc + 1) * 128], A.h4[:, kc * 128:(kc + 1) * 128], ident[:]), reads=[A.h4, ident], writes=[P01] if kc == 0 else [], inc=(kc == 7))
                    k.op('act', lambda e: e.activation(A.h4T[:].rearrange("p a b -> p (a b)"), pth, AF.Identity), reads=[P01], writes=[A.h4T])
                    yield
                    for kc in range(8):
                        k.op('pe', lambda e: e.matmul(A.plg[:], A.h4T[:, kc, :], rt[:, kc, :], start=(kc == 0), stop=(kc == 7)),
                             reads=[A.h4T, rt], writes=[A.plg] if kc == 0 else [], inc=(kc == 7))
                    k.op('dve', lambda e: e.tensor_tensor(LG[:, gi, :], A.plg[:], rtbb[:], ALU.add), reads=[A.plg, rtbb], pw_=[LG])

                for b in range(NB):
                    for j, c0 in enumerate((2 * D, 4 * D, 3 * D)):
                        k.dma('sp', mb4[:, j, :], MODD[b:b + 1, c0:c0 + D].partition_broadcast(128), reads=[MODD], writes=[mb4] if j == 0 else [], pw=[] if j == 0 else [mb4])
                    k.op('dve', lambda e: e.tensor_scalar(mb4[:, 1, :], mb4[:, 1, :], 1.0, None, ALU.add), reads=[mb4], writes=[mb4])

                    def astream(si):
                        for i in range(si, SEQ // 128, 2):
                            yield from tile4(AS[si], b, i)
                            yield
                    gens = [astream(0), astream(1)]
                    alive = [True, True]
                    while any(alive):
                        for gq in range(2):
                            if alive[gq]:
                                try:
                                    next(gens[gq])
                                except StopIteration:
                                    alive[gq] = False
            k.barrier()
            with ExitStack() as es5:
                k.es = es5
                if P4S < 1:
                    raise _Stop()
                gmx = k.sb("gmx", [128, NT], F32)
                goh = k.sb("goh", [128, NT, 4], F32)
                tg = k.sb("tg", [128, NT, 4], F32)
                ptop = k.sb("ptop", [128, NT], F32)
                lem = k.sb("lem", [128, NT, 32], F32)
                v1 = k.sb("v1", [128, NT], F32)
                v2 = k.sb("v2", [128, NT], F32)
                lgv = LG[:, :, 0:4]
                lev = LG[:, :, 4:36]
                k.op('dve', lambda e: e.tensor_reduce(gmx[:], lgv, AX.X, ALU.max), reads=[LG], writes=[gmx])
                k.op('dve', lambda e: e.tensor_tensor(goh[:], lgv, gmx[:].unsqueeze(2).to_broadcast([128, NT, 4]), ALU.is_equal), reads=[LG, gmx], writes=[goh])
                k.op('dve', lambda e: e.tensor_tensor(tg[:], lgv, gmx[:].unsqueeze(2).to_broadcast([128, NT, 4]), ALU.subtract), reads=[LG, gmx], writes=[tg])
                k.op('act', lambda e: e.activation(tg[:], tg[:], AF.Exp), reads=[tg], writes=[tg])
                k.op('dve', lambda e: e.tensor_reduce(ptop[:], tg[:], AX.X, ALU.add), reads=[tg], writes=[ptop])
                k.op('dve', lambda e: e.reciprocal(ptop[:], ptop[:]), reads=[ptop], writes=[ptop])
                k.op('dve', lambda e: e.tensor_scalar(goh[:], goh[:], -1.0, 1e30, ALU.add, ALU.mult), reads=[goh], writes=[goh])
                for g in range(4):
                    k.op('dve', lambda e: e.tensor_tensor(lem[:, :, g * 8:(g + 1) * 8], LG[:, :, 4 + g * 8:12 + g * 8],
                                                          goh[:, :, g:g + 1].to_broadcast([128, NT, 8]), ALU.add), reads=[LG, goh], writes=[lem])
                k.op('dve', lambda e: e.tensor_reduce(v1[:], lem[:], AX.X, ALU.max), reads=[lem], writes=[v1])
                k.op('dve', lambda e: e.tensor_tensor(OH1[:], lem[:], v1[:].unsqueeze(2).to_broadcast([128, NT, 32]), ALU.is_equal), reads=[lem, v1], writes=[OH1])
                k.op('dve', lambda e: e.scalar_tensor_tensor(lem[:], OH1[:], -1e30, lem[:], ALU.mult, ALU.add), reads=[OH1, lem], writes=[lem])
                k.op('dve', lambda e: e.tensor_reduce(v2[:], lem[:], AX.X, ALU.max), reads=[lem], writes=[v2])
                k.op('dve', lambda e: e.tensor_tensor(OH2[:], lem[:], v2[:].unsqueeze(2).to_broadcast([128, NT, 32]), ALU.is_equal), reads=[lem, v2], writes=[OH2])
                k.op('dve', lambda e: e.tensor_tensor(v2[:], v2[:], v1[:], ALU.subtract), reads=[v1, v2], writes=[v2])
                k.op('act', lambda e: e.activation(v2[:], v2[:], AF.Exp), reads=[v2], writes=[v2])
                k.op('dve', lambda e: e.tensor_scalar(v2[:], v2[:], 1.0, None, ALU.add), reads=[v2], writes=[v2])
                k.op('dve', lambda e: e.reciprocal(v2[:], v2[:]), reads=[v2], writes=[v2])
                k.op('dve', lambda e: e.tensor_tensor(W1[:], v2[:], ptop[:], ALU.mult), reads=[v2, ptop], writes=[W1])
                k.op('dve', lambda e: e.tensor_tensor(W2[:], ptop[:], W1[:], ALU.subtract), reads=[W1, ptop], writes=[W2])
            k.barrier()
            with ExitStack() as es6:
                k.es = es6
                if P4S < 2:
                    raise _Stop()
                OHb = k.sb("OHb", [128, NT, 32], BF16)
                triS = k.sb("triS", [128, 128], BF16)
                onb = k.sb("onb", [128, 128], BF16)
                thr = k.sb("thr", [128, 128], F32)
                blki = k.sb("blki", [128, NBLK], F32)
                kcp = k.sb("kcp", [128, 12], F32)
                cnt = k.sb("cnt", [128, 32], F32)
                big = k.sb("big", [128, 32, 128], F32)
                nbk = k.sb("nbk", [128, 32], F32)
                pend = k.sb("pend", [128, 32], F32)
                pst = k.sb("pst", [128, 32], F32)
                run = k.sb("run", [128, 32], F32)
                RK = k.sb("RK", [128, NT, 32], F32)
                dsf = k.sb("dsf", [128, NT, 2], F32)
                bexp = k.sb("bexp", [128, NBLK], F32)
                bigb = k.sb("bigb", [128, NBLK, 32], F32)
                widxf = k.sb("widxf", [128, NBLK, 12], F32)
                tokid = k.sb("tokid", [128, NT, 16], I32)
                zt = k.sb("zt", [128, 16], I32)
                pcn = k.ps("p5c", [128, 32])
                prk = k.ps("p5r", [128, 32])
                ptt = k.ps("p5t", [128, 32])
                stg = k.sb("stg", [128, 128], F32)
                k.dma('sp', stg[:], tris_in[:, :], writes=[stg])
                k.op('dve', lambda e: e.tensor_copy(triS[:], stg[:]), reads=[stg], writes=[triS])
                k.op('pool', lambda e: e.memset(onb[:], 1.0), writes=[onb])
                k.dma('sp', thr[:], thr_in[:, :], writes=[thr])
                k.dma('sp', blki[:], blki_in[:, 0:NBLK], writes=[blki])
                k.dma('sp', kcp[:], kcp_in[:, :], writes=[kcp])
                k.dma('sp', tokid[:], tokid_in[:, 0:NT, :], writes=[tokid])
                k.op('pool', lambda e: e.memset(zt[:], 0), writes=[zt])
                k.dma('sp', TOKB[:, :].rearrange("(b p) c -> p b c", p=128), zt[:].unsqueeze(1).to_broadcast([128, NBLK * SUB, 16]), reads=[zt], writes=[TOKB])
                k.op('dve', lambda e: e.tensor_tensor(OHb[:], OH1[:], OH2[:], ALU.add), reads=[OH1, OH2], writes=[OHb])
                for i in range(NT):
                    k.op('pe', lambda e: e.matmul(pcn[:], onb[:], OHb[:, i, :], start=(i == 0), stop=(i == NT - 1)), reads=[onb, OHb], writes=[pcn] if i == 0 else [], inc=(i == NT - 1))
                k.op('dve', lambda e: e.tensor_copy(cnt[:], pcn[:]), reads=[pcn], writes=[cnt])
                k.op('dve', lambda e: e.tensor_tensor(big[:], cnt[:].unsqueeze(2).to_broadcast([128, 32, 128]), thr[:].unsqueeze(1).to_broadcast([128, 32, 128]), ALU.is_gt),
                     reads=[cnt, thr], writes=[big])
                k.op('dve', lambda e: e.tensor_reduce(nbk[:], big[:], AX.X, ALU.add), reads=[big], writes=[nbk])
                k.op('pool', lambda e: e.memset(run[:], 1.0), writes=[run])
                k.op('dve', lambda e: e.tensor_tensor_scan(pend[:], run[:], nbk[:], 0.0, ALU.mult, ALU.add), reads=[run, nbk], writes=[pend])
                k.op('dve', lambda e: e.tensor_tensor(pst[:], pend[:], nbk[:], ALU.subtract), reads=[pend, nbk], writes=[pst])
                k.op('dve', lambda e: e.tensor_scalar(pst[:], pst[:], float(BS), None, ALU.mult), reads=[pst], writes=[pst])
                k.op('pool', lambda e: e.memset(run[:], 0.0), reads=[run], writes=[run])
                for i in range(NT):
                    k.op('pe', lambda e: e.matmul(prk[:], triS[:], OHb[:, i, :], start=True, stop=True), reads=[triS, OHb], writes=[prk])
                    k.op('pe', lambda e: e.matmul(ptt[:], onb[:], OHb[:, i, :], start=True, stop=True), reads=[onb, OHb], writes=[ptt])
                    k.op('dve', lambda e: e.tensor_tensor(RK[:, i, :], prk[:], run[:], ALU.add), reads=[prk, run], writes=[RK])
                    k.op('dve', lambda e: e.tensor_tensor(run[:], run[:], ptt[:], ALU.add), reads=[run, ptt], writes=[run])
                k.op('dve', lambda e: e.tensor_tensor(RK[:], RK[:], pst[:].unsqueeze(1).to_broadcast([128, NT, 32]), ALU.add), reads=[RK, pst], writes=[RK])
                for j, OH in enumerate((OH1, OH2)):
                    k.op('dve', lambda e: e.tensor_tensor(OH[:], OH[:], RK[:], ALU.mult), reads=[OH, RK], writes=[OH])
                    k.op('dve', lambda e: e.tensor_reduce(dsf[:, :, j], OH[:], AX.X, ALU.add), reads=[OH], writes=[dsf])
                k.op('dve', lambda e: e.tensor_copy(DST[:], dsf[:]), reads=[dsf], writes=[DST])
                k.op('dve', lambda e: e.tensor_tensor(bigb[:], pend[:].unsqueeze(1).to_broadcast([128, NBLK, 32]), blki[:].unsqueeze(2).to_broadcast([128, NBLK, 32]), ALU.is_le),
                     reads=[pend, blki], writes=[bigb])
                k.op('dve', lambda e: e.tensor_reduce(bexp[:], bigb[:], AX.X, ALU.add), reads=[bigb], writes=[bexp])
                k.op('dve', lambda e: e.tensor_scalar(bexp[:], bexp[:], 31.0, None, ALU.min), reads=[bexp], writes=[bexp])
                k.op('dve', lambda e: e.tensor_scalar(widxf[:, :, 0:8], bexp[:].unsqueeze(2).to_broadcast([128, NBLK, 8]), 256.0, None, ALU.mult), reads=[bexp], writes=[widxf])
                k.op('dve', lambda e: e.tensor_scalar(widxf[:, :, 8:12], bexp[:].unsqueeze(2).to_broadcast([128, NBLK, 4]), 256.0, None, ALU.mult), reads=[bexp], writes=[widxf])
                k.op('dve', lambda e: e.tensor_tensor(widxf[:], widxf[:], kcp[:].unsqueeze(1).to_broadcast([128, NBLK, 12]), ALU.add), reads=[widxf, kcp], writes=[widxf])
                k.op('dve', lambda e: e.tensor_copy(WIDX[:], widxf[:]), reads=[widxf], writes=[WIDX])
                for i in range(NT):
                    for j in range(2):
                        k.dma('pool', TOKB[:, :], tokid[:, i, :], reads=[tokid, DST], pw=[TOKB],
                              indirect=(bass.IndirectOffsetOnAxis(ap=DST[:, i, j:j + 1], axis=0), None))
            k.barrier()
            with ExitStack() as es7:
                k.es = es7
                if P4S < 3:
                    raise _Stop()
                wg = [k.sb("wg%d" % i, [128, 8, 512], BF16) for i in range(3)]
                wu = [k.sb("wu%d" % i, [128, 8, 512], BF16) for i in range(3)]
                wd = [k.sb("wd%d" % i, [128, 4, D], BF16) for i in range(3)]
                class _E:
                    pass
                ES = []
                NES = 4
                for si in range(NES):
                    E = _E()
                    E.tki = k.sb("tki", [128, 16], I32)
                    E.xg = k.sb("xg", [128, D], BF16)
                    E.xgT = k.sb("xgT", [128, 8, 128], BF16)
                    E.gs = k.sb("gs", [128, 512], F32)
                    E.hb = k.sb("hb", [128, 512], BF16)
                    E.hbT = k.sb("hbT", [128, 4, 128], BF16)
                    E.yb = k.sb("yb", [128, D], F32)
                    E.bG = k.ps("p6g", [128, 512])
                    E.bU = k.ps("p6u", [128, 512])
                    E.bY = E.bG
                    ES.append(E)

                def load_w(bk):
                    q = bk % 3
                    for hf in range(2):
                        ix = bass.IndirectOffsetOnAxis(ap=WIDX[:, bk, hf:hf + 1], axis=0)
                        k.dma('pool', wg[q][:, hf * 4:(hf + 1) * 4, :].rearrange("p a b -> p (a b)"), ex_gate[:, :], reads=[WIDX], pw=[wg[q]], indirect=(None, ix))
                        k.dma('pool', wu[q][:, hf * 4:(hf + 1) * 4, :].rearrange("p a b -> p (a b)"), ex_up[:, :], reads=[WIDX], pw=[wu[q]], indirect=(None, ix))
                        k.dma('pool', wd[q][:, hf * 2:(hf + 1) * 2, :].rearrange("p a b -> p (a b)"), ex_down[:, :], reads=[WIDX], pw=[wd[q]], indirect=(None, ix))

                def subtile(E, bk, sub):
                    q = bk % 3
                    r0 = (bk * SUB + sub) * 128
                    k.dma('sp', E.tki[:], TOKB[r0:r0 + 128, :], reads=[TOKB], writes=[E.tki])
                    k.dma('pool', E.xg[:], H2[:, :], reads=[H2, E.tki], writes=[E.xg],
                          indirect=(None, bass.IndirectOffsetOnAxis(ap=E.tki[:, 0:1], axis=0)))
                    yield
                    pxt = E.bU[:].bitcast(BF16)
                    xv = E.xg[:].rearrange("p (j kc) -> p kc j", kc=8)
                    for kc in range(8):
                        k.op('pe', lambda e: e.transpose(pxt[:, kc * 128:(kc + 1) * 128], xv[:, kc, :], identb[:]), reads=[E.xg, identb], writes=[E.bU] if kc == 0 else [], inc=(kc == 7))
                    k.op('act', lambda e: e.activation(E.xgT[:].rearrange("p a b -> p (a b)"), pxt, AF.Identity), reads=[E.bU], writes=[E.xgT])
                    yield
                    for kc in range(8):
                        k.op('pe', lambda e: e.matmul(E.bG[:], E.xgT[:, kc, :], wg[q][:, kc, :], start=(kc == 0), stop=(kc == 7)), reads=[E.xgT, wg[q]], writes=[E.bG] if kc == 0 else [], inc=(kc == 7))
                    for kc in range(8):
                        k.op('pe', lambda e: e.matmul(E.bU[:], E.xgT[:, kc, :], wu[q][:, kc, :], start=(kc == 0), stop=(kc == 7)), reads=[E.xgT, wu[q]], writes=[E.bU] if kc == 0 else [], inc=(kc == 7))
                    yield
                    k.op('act', lambda e: e.activation(E.gs[:], E.bG[:], AF.Silu), reads=[E.bG], writes=[E.gs])
                    k.op('dve', lambda e: e.tensor_tensor(E.hb[:], E.gs[:], E.bU[:], ALU.mult), reads=[E.gs, E.bU], writes=[E.hb])
                    yield
                    pht = E.bG[:].bitcast(BF16)
                    hv = E.hb[:].rearrange("p (j fc) -> p fc j", fc=4)
                    for fc in range(4):
                        k.op('pe', lambda e: e.transpose(pht[:, fc * 128:(fc + 1) * 128], hv[:, fc, :], identb[:]), reads=[E.hb, identb], writes=[E.bG] if fc == 0 else [], inc=(fc == 3))
                    k.op('dve', lambda e: e.tensor_copy(E.hbT[:].rearrange("p a b -> p (a b)"), pht[:, 0:512]), reads=[E.bG], writes=[E.hbT])
                    yield
                    for n in range(2):
                        for fc in range(4):
                            k.op('pe', lambda e: e.matmul(E.bY[:], E.hbT[:, fc, :], wd[q][:, fc, n * 512:(n + 1) * 512], start=(fc == 0), stop=(fc == 3)),
                                 reads=[E.hbT, wd[q]], writes=[E.bY] if fc == 0 else [], inc=(fc == 3))
                        k.op('act', lambda e: e.activation(E.yb[:, n * 512:(n + 1) * 512], E.bY[:], AF.Identity), reads=[E.bY], writes=[E.yb])
                        yield
                    k.dma('sp', YB[r0:r0 + 128, :], E.yb[:], reads=[E.yb], pw=[YB])

                def estream(si):
                    for gsub in range(si, NBLK * SUB, NES):
                        bk, sub = gsub // SUB, gsub % SUB
                        if True:
                            for bb in (bk, bk + 1):
                                if bb < NBLK and bb not in loaded:
                                    loaded.add(bb)
                                    load_w(bb)
                        yield from subtile(ES[si], bk, sub)
                        yield

                loaded = set()
                gens = [estream(i) for i in range(NES)]
                alive = [True] * NES
                while any(alive):
                    for gi in range(NES):
                        if alive[gi]:
                            try:
                                next(gens[gi])
                            except StopIteration:
                                alive[gi] = False
            k.barrier()
            with ExitStack() as es8:
                k.es = es8
                if P4S < 4:
                    raise _Stop()
                class _C:
                    pass
                CS = []
                for si in range(2):
                    C = _C()
                    C.x6 = k.sb("x6", [128, D], F32)
                    C.y0 = k.sb("y0", [128, D], F32)
                    C.y1 = k.sb("y1", [128, D], F32)
                    C.o = k.sb("o6", [128, D], F32)
                    C.st6 = k.sb("st6", [128, 2, 6], F32)
                    C.mv6 = k.sb("mv6", [128, 2], F32)
                    C.rs6 = k.sb("rs6", [128, 1], F32)
                    C.nb6 = k.sb("nb6", [128, 1], F32)
                    CS.append(C)

                def ctile(C, gi):
                    b, i = gi // (SEQ // 128), gi % (SEQ // 128)
                    x6, y0, y1, o, st6, mv6, rs6, nb6 = C.x6, C.y0, C.y1, C.o, C.st6, C.mv6, C.rs6, C.nb6
                    k.dma('sp', x6[:], X1[gi * 128:(gi + 1) * 128, :], reads=[X1], writes=[x6])
                    k.dma('pool', y0[:], YB[:, :], reads=[YB, DST], writes=[y0],
                          indirect=(None, bass.IndirectOffsetOnAxis(ap=DST[:, gi, 0:1], axis=0)))
                    k.dma('pool', y1[:], YB[:, :], reads=[YB, DST], writes=[y1],
                          indirect=(None, bass.IndirectOffsetOnAxis(ap=DST[:, gi, 1:2], axis=0)))
                    yield
                    k.op('act', lambda e: e.activation(y0[:], y0[:], AF.Identity, scale=W1[:, gi:gi + 1]), reads=[y0, W1], writes=[y0])
                    k.op('dve', lambda e: e.scalar_tensor_tensor(y0[:], y1[:], W2[:, gi:gi + 1], y0[:], ALU.mult, ALU.add), reads=[y1, W2, y0], writes=[y0])
                    yield
                    k.op('dve', lambda e: e.tensor_tensor(y0[:], y0[:], g2b[:, b, :], ALU.mult), reads=[y0, g2b], writes=[y0])
                    k.op('dve', lambda e: e.scalar_tensor_tensor(o[:], x6[:], ALPHA, y0[:], ALU.mult, ALU.add), reads=[x6, y0], writes=[o])
                    yield
                    for hf in range(2):
                        k.op('dve', lambda e: e.bn_stats(st6[:, hf, :], o[:, hf * 512:(hf + 1) * 512]), reads=[o], writes=[st6])
                    k.op('dve', lambda e: e.bn_aggr(mv6[:], st6[:].rearrange("p a b -> p (a b)")), reads=[st6], writes=[mv6])
                    k.op('act', lambda e: e.activation(rs6[:], mv6[:, 1:2], AF.Sqrt, bias=LN_EPS), reads=[mv6], writes=[rs6])
                    k.op('dve', lambda e: e.reciprocal(rs6[:], rs6[:]), reads=[rs6], writes=[rs6])
                    k.op('dve', lambda e: e.scalar_tensor_tensor(nb6[:], mv6[:, 0:1], -1.0, rs6[:], ALU.mult, ALU.mult), reads=[mv6, rs6], writes=[nb6])
                    yield
                    k.op('act', lambda e: e.activation(o[:], o[:], AF.Identity, bias=nb6[:, 0:1], scale=rs6[:, 0:1]), reads=[o, nb6, rs6], writes=[o])
                    yield
                    k.op('dve', lambda e: e.tensor_tensor(o[:], o[:], lnp[:, 2, :], ALU.mult), reads=[o, lnp], writes=[o])
                    k.op('dve', lambda e: e.tensor_tensor(o[:], o[:], lnp[:, 3, :], ALU.add), reads=[o, lnp], writes=[o])
                    k.dma('sp', out_d[b, i * 128:(i + 1) * 128, :], o[:], reads=[o])

                def cstream(si):
                    for gi in range(si, NT, 2):
                        yield from ctile(CS[si], gi)
                        yield
                gens = [cstream(0), cstream(1)]
                alive = [True, True]
                while any(alive):
                    for gq in range(2):
                        if alive[gq]:
                            try:
                                next(gens[gq])
                            except StopIteration:
                                alive[gq] = False
           except _Stop:
            pass
        k.barrier()

        k.barrier()
    return nc


def host_inputs(inputs, batches, NB):
    f = lambda a: np.ascontiguousarray(a, dtype=np.float32)
    bs = list(batches)
    m = {}
    m["x"] = f(inputs["x"][bs])
    m["ctx"] = f(inputs["ctx"][bs])
    cc = np.zeros((3, D), np.float32)
    for i, b in enumerate(bs):
        cc[i] = inputs["c"][b]
    cc[2] = inputs["c_ctx"]
    m["cc"] = cc
    m["w_ada"] = f(inputs["w_ada"][0])
    m["b_ada"] = f(inputs["b_ada"][0][None, :])
    m["w_in"] = f(inputs["w_in"][0])
    m["conv_w"] = f(inputs["conv_w"][0].reshape(9, 2560))
    bi, bf = inputs["m_bias_i"][0], inputs["m_bias_f"][0]
    m["m_bias"] = f(np.concatenate([bi[0], bf[0], bi[1], bf[1]])[:, None])
    m["ident"] = np.eye(128, dtype=np.float32)
    gm = np.zeros((32, 2), np.float32)
    gm[0:8, 0] = 1; gm[16:24, 0] = 1; gm[8:16, 1] = -1; gm[24:32, 1] = -1
    m["gmask"] = gm
    ii = np.arange(64)
    m["cmask"] = np.stack([(ii[:, None] <= ii[None, :]), (ii[:, None] >= ii[None, :])], axis=1).astype(np.float32)
    m["m_norm_w"] = f(inputs["m_norm_w"][0][None, :])
    hk = lambda v: np.asarray(v, np.float32).reshape(4, 128).T
    m["rp"] = f(np.stack([hk(inputs["r_w0"][0][0]), hk(inputs["r_w0"][0][1]), hk(inputs["r_a0"][0]), hk(inputs["r_kk"][0]),
                          hk(inputs["r_ka"][0]), hk(inputs["r_ka"][0]), hk(inputs["r_bonus"][0].reshape(-1))], axis=2))
    sm = np.ones((128, 1088), np.float32); sm[:, ::64] = 0
    obd = np.zeros((128, 128), np.float32); obd[:64, :64] = 1; obd[64:, 64:] = 1
    m["onesbd"] = obd
    m["smask"] = sm
    jj = np.arange(128) % 64
    tt = np.arange(128)
    rm = np.zeros((128, 2, 128), np.float32)
    for dd in range(2):
        for col in range(128):
            tq = col % 64
            if col < 64:
                rm[:, dd, col] = (jj < tq) if dd == 0 else (jj > tq)
            else:
                rm[:, dd, col] = (jj <= tq) if dd == 0 else (jj >= tq)
    m["rmask"] = rm
    m["nmask"] = np.stack([(ii[None, :] < ii[:, None]), (ii[None, :] > ii[:, None])], axis=1).astype(np.float32)
    m["r_norm_w"] = f(inputs["r_norm_w"][0][None, :])
    m["r_norm_b"] = f(inputs["r_norm_b"][0][None, :])
    m["r_wB"] = f(inputs["r_wB"][0])
    m["r_aB"] = f(inputs["r_aB"][0])
    m["r_gB"] = f(inputs["r_gB"][0])
    m["w_out"] = f(inputs["w_out"][0])
    for nm in ("ln1_g", "ln1_b", "ln2_g", "ln2_b"):
        m[nm] = f(inputs[nm][0][None, :])
    m["rt"] = f(np.concatenate([inputs["rt_g"][0], inputs["rt_e"][0]], axis=1))
    m["rtb"] = f(np.concatenate([inputs["rt_g_b"][0], inputs["rt_e_b"][0]])[None, :])
    m["ex_gate"] = f(inputs["ex_gate"][0].reshape(8192, 2048))
    m["ex_up"] = f(inputs["ex_up"][0].reshape(8192, 2048))
    m["ex_down"] = f(inputs["ex_down"][0].reshape(8192, 2048))
    pp = np.arange(128)
    m["tris"] = (pp[:, None] < pp[None, :]).astype(np.float32)
    m["thr"] = np.broadcast_to((512.0 * pp)[None, :], (128, 128)).astype(np.float32).copy()
    m["blki"] = np.broadcast_to(np.arange(160, dtype=np.float32)[None, :], (128, 160)).copy()
    m["kcp"] = (2 * pp[:, None] + (np.arange(12) % 2)[None, :]).astype(np.float32)
    m["tokid"] = np.broadcast_to((np.arange(64)[None, :] * 128 + pp[:, None])[:, :, None], (128, 64, 16)).astype(np.int32).copy()
    return m


_NC_CACHE = {}


def kernel(**inputs):
    inputs = {k_: np.asarray(v) for k_, v in inputs.items()}
    NB = 2
    n_cores = 8
    if NB not in _NC_CACHE:
        _NC_CACHE[NB] = build(NB=NB)
    nc = _NC_CACHE[NB]
    in_maps = [host_inputs(inputs, [NB * c + j for j in range(NB)], NB) for c in range(n_cores)]
    res = run_bass_kernel_spmd(nc, in_maps, core_ids=list(range(n_cores)))
    out = np.concatenate([np.asarray(r["out"]) for r in res.results], axis=0)
    return np.ascontiguousarray(out, dtype=np.float32)
```

```python
import math, os
from contextlib import ExitStack
import numpy as np
import concourse.bass as bass
import concourse.mybir as mybir
from concourse.bass_utils import run_bass_kernel_spmd

F32 = mybir.dt.float32
BF16 = mybir.dt.bfloat16
I32 = mybir.dt.int32
AF = mybir.ActivationFunctionType
ALU = mybir.AluOpType
AX = mybir.AxisListType

D = 1024
SEQ = 4096
CTX = 256
T = SEQ + CTX
NCH = T // 64
INC = 3936
DS = math.exp(-0.5)
ALPHA = 2.0 ** 0.25
LN_EPS = 1e-6
GN_EPS = 64e-5
SEC = dict(mq=0, mk=512, rr=1024, rk=1536, rv=2048, mv=2560, mo=3072, gates=3584,
           lwf=3616, lwb=3680, la=3744, lg=3808)
NDS = 40


class _Stop(Exception):
    pass


class Buf:
    def __init__(self, t):
        self.t = t
        self.w = {}
        self.r = {}

    def __getitem__(self, k):
        return self.t[k]


def _merge(d, s):
    for k, v in s.items():
        if d.get(k, 0) < v:
            d[k] = v


class KB:
    def __init__(self, nc):
        self.nc = nc
        self.engs = {'pe': nc.tensor, 'dve': nc.vector, 'act': nc.scalar, 'pool': nc.gpsimd, 'sp': nc.sync}
        self.esem = {e: nc.alloc_semaphore('es_' + e) for e in self.engs}
        self.ecnt = {e: 0 for e in self.engs}
        self.pending = {e: False for e in self.engs}
        self.seen = {e: {} for e in self.engs}
        self.dsem = [nc.alloc_semaphore('ds%d' % i) for i in range(NDS)]
        self.dcnt = [0] * NDS
        self.dnext = 0
        self.es = None
        self.uid = 0

    def semh(self, key):
        return self.esem[key] if isinstance(key, str) else self.dsem[key[1]]

    def _wait(self, eng, need):
        for key, cnt in need.items():
            if self.seen[eng].get(key, 0) >= cnt:
                continue
            if key == eng and eng in ('pe',):
                continue
            self.engs[eng].wait_ge(self.semh(key), cnt)
            self.seen[eng][key] = cnt

    def op(self, eng, fn, reads=(), writes=(), inc=True, pw_=()):
        need = {}
        for b in pw_:
            _merge(need, b.r)
        for b in reads:
            _merge(need, b.w)
            if getattr(b, 'excl', False):
                _merge(need, {kk: vv for kk, vv in b.r.items() if kk != eng})
        for b in writes:
            _merge(need, b.w)
            _merge(need, b.r)
        self._wait(eng, need)
        ins = fn(self.engs[eng])
        cnt = self.ecnt[eng] + 1
        if inc:
            ins.then_inc(self.esem[eng], 1)
            self.ecnt[eng] = cnt
        for b in reads:
            b.r[eng] = cnt
        for b in writes:
            b.w = {eng: cnt}
            b.r = {}
        for b in pw_:
            b.w[eng] = cnt
        return ins

    def dma(self, q, out, in_, reads=(), writes=(), pw=(), indirect=None, **kw):
        i = self.dnext
        self.dnext = (i + 1) % NDS
        need = {}
        if self.dcnt[i]:
            need[('d', i)] = self.dcnt[i]
        for b in reads:
            _merge(need, b.w)
        for b in writes:
            _merge(need, b.w)
            _merge(need, b.r)
        for b in pw:
            _merge(need, b.r)
        self._wait(q, need)
        if indirect is None:
            ins = self.engs[q].dma_start(out=out, in_=in_, **kw)
        else:
            ins = self.engs[q].indirect_dma_start(out, indirect[0], in_, indirect[1], **kw)
        self.dcnt[i] += 16
        ins.then_inc(self.dsem[i], 16)
        key = ('d', i)
        cnt = self.dcnt[i]
        for b in reads:
            b.r[key] = cnt
        for b in writes:
            b.w = {key: cnt}
            b.r = {}
        for b in pw:
            b.w[key] = cnt
        return ins

    def barrier(self):
        need = {e: c for e, c in self.ecnt.items() if c}
        for i in range(NDS):
            if self.dcnt[i]:
                need[('d', i)] = self.dcnt[i]
        for e in self.engs:
            self._wait(e, need)

    def sb(self, name, shape, dt):
        self.uid += 1
        return Buf(self.es.enter_context(self.nc.sbuf_tensor("s%d_%s" % (self.uid, name), list(shape), dt)))

    def ps(self, name, shape, dt=F32):
        self.uid += 1
        return Buf(self.es.enter_context(self.nc.psum_tensor("p%d_%s" % (self.uid, name), list(shape), dt)))


def build(NB=2, debug=None, phases=(0, 1, 2, 3, 4, 5, 6)):
    nc = bass.Bass("TRN2", target_bir_lowering=False)
    k = KB(nc)

    def din(name, shape):
        return nc.dram_tensor(name, list(shape), F32, kind="ExternalInput").ap()

    x_in = din("x", [NB, SEQ, D])
    ctx_in = din("ctx", [NB, CTX, D])
    cc_in = din("cc", [3, D])
    w_ada = din("w_ada", [D, 6 * D])
    b_ada = din("b_ada", [1, 6 * D])
    w_in = din("w_in", [D, INC])
    conv_w = din("conv_w", [9, 2560])
    m_bias = din("m_bias", [32, 1])
    ident_in = din("ident", [128, 128])
    gmask_in = din("gmask", [32, 2])
    cmask_in = din("cmask", [64, 2, 64])
    m_norm_w = din("m_norm_w", [1, 512])
    rp_in = din("rp", [128, 4, 7])
    smask_in = din("smask", [128, 1088])
    onesbd_in = din("onesbd", [128, 128])
    rmask_in = din("rmask", [128, 2, 128])
    nmask_in = din("nmask", [64, 2, 64])
    r_norm_w = din("r_norm_w", [1, 512])
    r_norm_b = din("r_norm_b", [1, 512])
    r_wB = din("r_wB", [2, 64, 512])
    r_aB = din("r_aB", [64, 512])
    r_gB = din("r_gB", [128, 512])
    w_out = din("w_out", [D, D])
    ln1_g = din("ln1_g", [1, D]); ln1_b = din("ln1_b", [1, D]); ln2_g = din("ln2_g", [1, D]); ln2_b = din("ln2_b", [1, D])
    rt_in = din("rt", [D, 36]); rtb_in = din("rtb", [1, 36])
    ex_gate = din("ex_gate", [8192, 2048]); ex_up = din("ex_up", [8192, 2048]); ex_down = din("ex_down", [8192, 2048])
    tris_in = din("tris", [128, 128]); thr_in = din("thr", [128, 128]); blki_in = din("blki", [128, 160]); kcp_in = din("kcp", [128, 12])
    tokid_in = nc.dram_tensor("tokid", [128, 64, 16], I32, kind="ExternalInput").ap()
    out_d = nc.dram_tensor("out", [NB, SEQ, D], F32, kind="ExternalOutput").ap()

    def dscr(name, shape, dt=F32):
        kind = "ExternalOutput" if (debug and name in debug) else "Internal"
        return Buf(nc.dram_tensor(name, list(shape), dt, kind=kind).ap())

    MODD = dscr("MODD", [3, 6 * D])
    FM = [dscr("FM%d" % b, [INC, T]) for b in range(NB)]
    MIX = [dscr("MIX%d" % b, [SEQ, D]) for b in range(NB)]
    BS = 512
    NBLK_ = NB * SEQ * 2 // BS + 32
    X1 = dscr("X1", [NB * SEQ, D])
    H2 = dscr("H2", [NB * SEQ, D], BF16)
    TOKB = dscr("TOKB", [NBLK_ * BS, 16], I32)
    YB = dscr("YB", [NBLK_ * BS, D])

    with ExitStack() as es0:
        k.es = es0
        ident = k.sb("ident", [128, 128], F32)
        identb = k.sb("identb", [128, 128], BF16)
        ones_f = k.sb("ones_f", [128, 128], F32)
        modT = k.sb("modT", [128, 48, 3], F32)
        k.dma('sp', ident[:], ident_in[:, :], writes=[ident])
        k.op('dve', lambda e: e.tensor_copy(identb[:], ident[:]), reads=[ident], writes=[identb])

        with ExitStack() as es:
            k.es = es
            cc = k.sb("cc", [3, D], F32)
            scT = k.sb("scT", [128, 8, 3], F32)
            bada = k.sb("bada", [3, 6 * D], F32)
            mods = k.sb("mods", [3, 6 * D], F32)
            wa = [k.sb("wa%d" % i, [128, 8, 512], F32) for i in range(2)]
            pst = k.ps("p0t", [128, 8, 3])
            psm = [k.ps("p0m%d" % i, [3, 512]) for i in range(2)]
            pmt = k.ps("p0mt", [128, 48, 3])
            k.dma('sp', cc[:], cc_in[:, :], writes=[cc])
            k.dma('sp', bada[:], b_ada[0:1, :].partition_broadcast(3), writes=[bada])
            k.op('act', lambda e: e.activation(cc[:], cc[:], AF.Silu), reads=[cc], writes=[cc])
            for kc in range(8):
                k.op('pe', lambda e: e.transpose(pst[:, kc, :], cc[:, kc * 128:(kc + 1) * 128], ident[0:3, 0:3]),
                     reads=[cc, ident], writes=[pst], inc=(kc == 7))
            k.op('dve', lambda e: e.tensor_copy(scT[:], pst[:]), reads=[pst], writes=[scT])
            for n in range(12):
                wb = wa[n % 2]
                k.dma('sp', wb[:], w_ada[:, n * 512:(n + 1) * 512].rearrange("(kc p) n -> p kc n", p=128), writes=[wb])
                pm = psm[n % 2]
                for kc in range(8):
                    k.op('pe', lambda e: e.matmul(pm[:], scT[:, kc, :], wb[:, kc, :], start=(kc == 0), stop=(kc == 7)),
                         reads=[scT, wb], writes=[pm] if kc == 0 else [], inc=(kc == 7))
                k.op('dve', lambda e: e.tensor_tensor(mods[:, n * 512:(n + 1) * 512], pm[:], bada[:, n * 512:(n + 1) * 512], ALU.add),
                     reads=[pm, bada], writes=[mods])
            k.dma('sp', MODD[:, :], mods[:], reads=[mods], writes=[MODD])
            for j in range(48):
                k.op('pe', lambda e: e.transpose(pmt[:, j, :], mods[:, j * 128:(j + 1) * 128], ident[0:3, 0:3]),
                     reads=[mods, ident], writes=[pmt], inc=(j == 47))
            k.op('dve', lambda e: e.tensor_copy(modT[:], pmt[:]), reads=[pmt], writes=[modT])
            for j0 in (8, 32):
                k.op('dve', lambda e: e.tensor_scalar(modT[:, j0:j0 + 8, :], modT[:, j0:j0 + 8, :], 1.0, None, ALU.add),
                     reads=[modT], writes=[modT])
        k.barrier()

        with ExitStack() as es:
          if 1 in phases:
              k.es = es
              wbf = k.sb("wbf", [128, 8, INC], BF16)
              hT = k.sb("hT", [128, 8, T], BF16)
              xt = [k.sb("xt%d" % i, [128, D], F32) for i in range(2)]
              xn = [k.sb("xn%d" % i, [128, D], BF16) for i in range(2)]
              st = k.sb("st", [128, 2, 6], F32)
              mv = k.sb("mv", [128, 2], F32)
              rstd = k.sb("rstd", [128, 1], F32)
              pT = [k.sb("pT%d" % i, [128, T], F32) for i in range(2)]
              acc = k.sb("acc", [128, T], F32)
              cw = k.sb("cw", [128, 20, 9], F32)
              mb = k.sb("mb", [32, 4], F32)
              ptr = [k.ps("p1t%d" % i, [128, 8, 128], BF16) for i in range(2)]
              pmm = [k.ps("p1m%d" % i, [128, 512]) for i in range(3)]
              pcw = k.ps("p1cw", [128, 20, 9])
              for kc in range(8):
                  for hf in range(2):
                      k.dma('pool', wbf[:, kc, hf * 1968:(hf + 1) * 1968],
                            w_in[kc * 128:(kc + 1) * 128, hf * 1968:(hf + 1) * 1968], pw=[wbf])
              crow = k.sb("crow", [9, 2560], F32)
              k.dma('sp', crow[:], conv_w[:, :], writes=[crow])
              for c in range(20):
                  k.op('pe', lambda e: e.transpose(pcw[:, c, :], crow[:, c * 128:(c + 1) * 128], ident[0:9, 0:9]),
                       reads=[crow, ident], writes=[pcw], inc=(c == 19))
              k.op('dve', lambda e: e.tensor_copy(cw[:], pcw[:]), reads=[pcw], writes=[cw])
              k.dma('sp', mb[:, 0:1], m_bias[:, :], writes=[mb])
              k.op('dve', lambda e: e.tensor_scalar(mb[:, 1:2], mb[:, 0:1], -1.0, None, ALU.mult), reads=[mb], writes=[mb])
              k.dma('sp', mb[:, 2:4], gmask_in[:, :], pw=[mb])
              for b in range(NB):
                  for i in range(int(os.environ.get('P1A', T // 128))):
                      xb, xnb, pt = xt[i % 2], xn[i % 2], ptr[i % 2]
                      src = ctx_in[b, i * 128:(i + 1) * 128, :] if i < 2 else x_in[b, (i - 2) * 128:(i - 1) * 128, :]
                      r = 2 if i < 2 else b
                      k.dma('sp', xb[:], src, writes=[xb])
                      S1 = int(os.environ.get('P1S', 9))
                      for hf in range(2):
                          k.op('dve', lambda e: e.bn_stats(st[:, hf, :], xb[:, hf * 512:(hf + 1) * 512]), reads=[xb], writes=[st])
                      if S1 >= 2: k.op('dve', lambda e: e.bn_aggr(mv[:], st[:].rearrange("p a b -> p (a b)")), reads=[st], writes=[mv])
                      if S1 >= 3: k.op('act', lambda e: e.activation(rstd[:], mv[:, 1:2], AF.Sqrt, bias=LN_EPS), reads=[mv], writes=[rstd])
                      if S1 >= 4: k.op('dve', lambda e: e.reciprocal(rstd[:], rstd[:]), reads=[rstd], writes=[rstd])
                      if S1 >= 5: k.op('dve', lambda e: e.tensor_scalar(xnb[:], xb[:], mv[:, 0:1], rstd[:, 0:1], ALU.subtract, ALU.mult),
                           reads=[xb, mv, rstd], writes=[xnb])
                      for kc in range(8 if S1 >= 6 else 0):
                          k.op('pe', lambda e: e.transpose(pt[:, kc, :], xnb[:, kc * 128:(kc + 1) * 128], identb[:]),
                               reads=[xnb, identb], writes=[pt] if kc == 0 else [], inc=(kc == 7))
                      for kc in range(8 if S1 >= 7 else 0):
                          if i % 2 == 0:
                              k.op('act', lambda e: e.activation(hT[:, kc, i * 128:(i + 1) * 128], pt[:, kc, :], AF.Identity,
                                                                 bias=modT[:, kc, r:r + 1], scale=modT[:, 8 + kc, r:r + 1]),
                                   reads=[pt, modT], writes=[] if (i or kc) else [hT])
                          else:
                              k.op('dve', lambda e: e.tensor_scalar(hT[:, kc, i * 128:(i + 1) * 128], pt[:, kc, :],
                                                                    modT[:, 8 + kc, r:r + 1], modT[:, kc, r:r + 1], ALU.mult, ALU.add),
                                   reads=[pt, modT], writes=[])
                      hT.w['act'] = k.ecnt['act']
                      hT.w['dve'] = k.ecnt['dve']
                  chunks = [(c * 128, 128) for c in range(28)] + [(3584, 32), (3616, 64), (3680, 64), (3744, 64), (3808, 128)]
                  for ci, (c0, M) in enumerate(chunks[:int(os.environ.get('P1C', 99))]):
                      pb = pT[ci % 2]
                      for g in range(9):
                          t0 = g * 512
                          n = min(512, T - t0)
                          pm = pmm[(ci * 9 + g) % 3]
                          for kc in range(8):
                              k.op('pe', lambda e: e.matmul(pm[0:M, 0:n], wbf[:, kc, c0:c0 + M], hT[:, kc, t0:t0 + n],
                                                            start=(kc == 0), stop=(kc == 7)),
                                   reads=[wbf, hT], writes=[pm] if kc == 0 else [], inc=(kc == 7))
                          k.op('act', lambda e: e.activation(pb[0:M, t0:t0 + n], pm[0:M, 0:n], AF.Identity),
                               reads=[pm], writes=[pb] if g == 0 else [])
                          pb.w['act'] = k.ecnt['act']
                      src = pb
                      if c0 < 2560:
                          c = c0 // 128
                          k.op('act', lambda e: e.activation(acc[:, :], pb[:, :], AF.Identity, scale=cw[:, c, 4:5]),
                               reads=[pb, cw], writes=[acc])
                          k.op('dve', lambda e: e.scalar_tensor_tensor(acc[:, 1:CTX], pb[:, 0:CTX - 1], cw[:, c, 3:4], acc[:, 1:CTX], ALU.mult, ALU.add),
                               reads=[pb, cw, acc], writes=[acc])
                          k.op('dve', lambda e: e.scalar_tensor_tensor(acc[:, 0:CTX - 1], pb[:, 1:CTX], cw[:, c, 5:6], acc[:, 0:CTX - 1], ALU.mult, ALU.add),
                               reads=[pb, cw, acc], writes=[acc])
                          a3 = acc[:, CTX:T].rearrange("p (r c) -> p r c", c=64)
                          p3 = pb[:, CTX:T].rearrange("p (r c) -> p r c", c=64)
                          for ky in range(3):
                              for kx in range(3):
                                  if ky == 1 and kx == 1:
                                      continue
                                  dy, dx = ky - 1, kx - 1
                                  oy0, oy1 = max(0, -dy), 64 - max(0, dy)
                                  ox0, ox1 = max(0, -dx), 64 - max(0, dx)
                                  k.op('dve', lambda e: e.scalar_tensor_tensor(
                                      a3[:, oy0:oy1, ox0:ox1], p3[:, oy0 + dy:oy1 + dy, ox0 + dx:ox1 + dx],
                                      cw[:, c, ky * 3 + kx:ky * 3 + kx + 1], a3[:, oy0:oy1, ox0:ox1], ALU.mult, ALU.add),
                                      reads=[pb, cw, acc], writes=[acc])
                          src = acc
                      sec = [s for s, v in SEC.items() if v <= c0][-1]
                      if sec in ('mq', 'mk'):
                          k.op('act', lambda e: e.activation(acc[:, :], src[:, :], AF.Silu), reads=[src], writes=[acc])
                          if sec == 'mk':
                              k.op('dve', lambda e: e.tensor_scalar(acc[:, :], acc[:, :], 0.125, None, ALU.mult), reads=[acc], writes=[acc])
                          src = acc
                      elif sec in ('mo', 'lg'):
                          k.op('act', lambda e: e.activation(acc[0:M, :], src[0:M, :], AF.Sigmoid), reads=[src], writes=[acc])
                          src = acc
                      elif sec in ('lwf', 'lwb'):
                          k.op('act', lambda e: e.activation(acc[0:M, :], src[0:M, :], AF.Tanh), reads=[src], writes=[acc])
                          src = acc
                      elif sec == 'gates':
                          tmp = pT[1 - ci % 2]
                          k.op('act', lambda e: e.activation(tmp[0:32, :], pb[0:32, :], AF.Exp, bias=mb[:, 1:2], scale=-1.0),
                               reads=[pb, mb], writes=[tmp])
                          k.op('act', lambda e: e.activation(tmp[0:32, :], tmp[0:32, :], AF.Ln, bias=1.0), reads=[tmp], writes=[tmp])
                          k.op('dve', lambda e: e.tensor_scalar(tmp[0:32, :], tmp[0:32, :], mb[:, 3:4], None, ALU.mult), reads=[tmp, mb], writes=[tmp])
                          k.op('dve', lambda e: e.tensor_scalar(acc[0:32, :], pb[0:32, :], mb[:, 0:1], mb[:, 2:3], ALU.add, ALU.mult),
                               reads=[pb, mb], writes=[acc])
                          k.op('dve', lambda e: e.tensor_tensor(acc[0:32, :], acc[0:32, :], tmp[0:32, :], ALU.add), reads=[acc, tmp], writes=[acc])
                          src = acc
                      k.dma('sp', FM[b][c0:c0 + M, :], src[0:M, :], reads=[src], pw=[FM[b]])
        k.barrier()


        with ExitStack() as es:
          if 2 in phases:
            k.es = es
            cm = k.sb("cm", [64, 2, 64], F32)
            k.dma('sp', cm[:], cmask_in[:, :, :], writes=[cm])
            nw = k.sb("nw", [64, 512], F32)
            k.dma('sp', nw[:], m_norm_w[0:1, :].partition_broadcast(64), writes=[nw])
            k.op('pool', lambda e: e.memset(ones_f[:], 1.0), writes=[ones_f])
            GA = k.sb("GA", [64, NCH, 48], F32)
            gT = k.sb("gT", [32, T], F32)
            G = k.sb("G", [64, 32], F32)
            qh = k.sb("qh", [64, 2, T], BF16)
            kh = k.sb("kh", [64, 2, T], BF16)
            vT = k.sb("vT", [128, T], BF16)
            moT = k.sb("moT", [128, T], F32)
            Hf = k.sb("Hf", [64, NCH, 2, 64], F32)
            class _M:
                pass
            MS = []
            for si in range(2):
                M = _M()
                M.i = si
                M.Ktm = k.sb("Ktm", [64, 2, 64], BF16)
                M.Vaug = k.sb("Vaug", [64, 2, 66], BF16)
                M.PTm = k.sb("PTm", [64, 2, 64], BF16)
                M.Cst = k.sb("Cst", [64, 2, 66], F32)
                M.Cbf = k.sb("Cbf", [64, 2, 66], BF16)
                M.dn = k.sb("dn", [64, 2], F32)
                M.ff = k.sb("ff", [64, 2], F32)
                M.hs = k.sb("hs", [64, 2, 64], F32)
                M.st2 = k.sb("st2", [64, 2, 6], F32)
                M.mv2 = k.sb("mv2", [64, 2, 2], F32)
                M.rs2 = k.sb("rs2", [64, 2], F32)
                M.om = k.sb("om", [64, 128], F32)
                M.bA = k.ps("p2A", [64, 512])
                M.bB = k.ps("p2B", [64, 512])
                M.bA.excl = True
                M.bB.excl = True
                MS.append(M)
            pg = k.ps("p2g", [64, 32])
            pbb = k.ps("p2b", [64, 32])
            for b in range(NB):
                k.dma('sp', gT[:], FM[b][3584:3616, :], reads=[FM[b]], writes=[gT])
                for c in range(NCH):
                    k.op('pe', lambda e: e.transpose(pg[:], gT[:, c * 64:(c + 1) * 64], ident[0:32, 0:32]), reads=[gT, ident], writes=[pg])
                    k.op('dve', lambda e: e.tensor_copy(G[:], pg[:]), reads=[pg], writes=[G])
                    k.op('pe', lambda e: e.matmul(pbb[:, 0:8], cm[:, 0, :], G[:, 8:16], start=True, stop=True), reads=[cm, G], writes=[pbb], inc=False)
                    k.op('pe', lambda e: e.matmul(pbb[:, 8:16], cm[:, 1, :], G[:, 24:32], start=True, stop=True), reads=[cm, G], inc=False)
                    k.op('pe', lambda e: e.matmul(pbb[:, 16:24], ones_f[0:64, 0:64], G[:, 8:16], start=True, stop=True), reads=[ones_f, G], inc=False)
                    k.op('pe', lambda e: e.matmul(pbb[:, 24:32], ones_f[0:64, 0:64], G[:, 24:32], start=True, stop=True), reads=[ones_f, G])
                    k.op('act', lambda e: e.activation(GA[:, c, 0:32], pbb[:], AF.Exp), reads=[pbb], writes=[GA])
                    k.op('dve', lambda e: e.tensor_tensor(G[:, 0:8], G[:, 0:8], pbb[:, 0:8], ALU.subtract), reads=[pbb, G], writes=[G])
                    k.op('dve', lambda e: e.tensor_tensor(G[:, 16:24], G[:, 16:24], pbb[:, 8:16], ALU.subtract), reads=[pbb, G], writes=[G])
                    k.op('act', lambda e: e.activation(GA[:, c, 32:40], G[:, 0:8], AF.Exp), reads=[G], writes=[GA])
                    k.op('act', lambda e: e.activation(GA[:, c, 40:48], G[:, 16:24], AF.Exp), reads=[G], writes=[GA])
                for hp in range(4):
                    for h in range(2):
                        r0 = hp * 128 + h * 64
                        for q4 in range(4):
                            t0 = q4 * 1088
                            k.dma('pool', qh[:, h, t0:t0 + 1088], FM[b][r0:r0 + 64, t0:t0 + 1088], reads=[FM[b]], pw=[qh])
                            k.dma('pool', kh[:, h, t0:t0 + 1088], FM[b][512 + r0:512 + r0 + 64, t0:t0 + 1088], reads=[FM[b]], pw=[kh])
                    for q4 in range(4):
                        t0 = q4 * 1088
                        k.dma('pool', vT[:, t0:t0 + 1088], FM[b][2560 + hp * 128:2560 + (hp + 1) * 128, t0:t0 + 1088], reads=[FM[b]], pw=[vT])
                    k.dma('sp', moT[:], FM[b][3072 + hp * 128:3072 + (hp + 1) * 128, :], reads=[FM[b]], writes=[moT])
                    def mstream(M, d, done):
                        Ktm, Vaug, PTm, Cst, Cbf, dn, ff, hs, st2, mv2, rs2, om = M.Ktm, M.Vaug, M.PTm, M.Cst, M.Cbf, M.dn, M.ff, M.hs, M.st2, M.mv2, M.rs2, M.om
                        bA, bB = M.bA, M.bB
                        pk = bA[:, 0:64].bitcast(BF16).rearrange("p (h e) -> p h e", h=2)
                        pv = bA[:, 64:128].bitcast(BF16)
                        pp = bA[:, 128:256].rearrange("p (h e) -> p h e", h=2)
                        po = bB[:, 0:132].rearrange("p (h e) -> p h e", h=2)
                        pc = bB[:, 132:264].rearrange("p (h e) -> p h e", h=2)
                        pmo = bB[:, 264:392]
                        order = list(range(NCH)) if d == 0 else [3, 2, 1, 0] + list(range(NCH - 1, 3, -1))
                        k.op('pool', lambda e: e.memset(Cst[:], 0.0), writes=[Cst])
                        k.op('pool', lambda e: e.memset(Cbf[:], 0.0), writes=[Cbf])
                        for c in order:
                            cs = slice(c * 64, (c + 1) * 64)
                            hh = 2 * hp
                            a_ap = GA[:, c, d * 8 + hh:d * 8 + hh + 2]
                            e_ap = GA[:, c, 16 + d * 8 + hh:16 + d * 8 + hh + 2]
                            c_ap = GA[:, c, 32 + d * 8 + hh:32 + d * 8 + hh + 2]
                            needy = c >= 4
                            second = c in done
                            fin = needy and second
                            for h in range(2):
                                k.op('pe', lambda e: e.transpose(pk[:, h, :], kh[:, h, cs], identb[0:64, 0:64]), reads=[kh, identb], writes=[bA] if h == 0 else [], inc=False)
                            k.op('pe', lambda e: e.transpose(pv, vT[:, cs], identb[:]), reads=[vT, identb], inc=False)
                            for h in range(2):
                                k.op('pe', lambda e: e.matmul(pp[:, h, :], kh[:, h, cs], qh[:, h, cs], start=True, stop=True), reads=[kh, qh], inc=(h == 1))
                            k.op('act', lambda e: e.activation(Ktm[:], pk, AF.Identity), reads=[bA], writes=[Ktm])
                            k.op('dve', lambda e: e.tensor_tensor(Vaug[:, :, 0:64], pv.rearrange("p (h e) -> p h e", h=2),
                                                                  c_ap.unsqueeze(2).to_broadcast([64, 2, 64]), ALU.mult), reads=[bA, GA], writes=[Vaug])
                            k.op('act', lambda e: e.activation(Vaug[:, :, 64:65], c_ap.unsqueeze(2), AF.Identity), reads=[GA, Vaug], writes=[Vaug])
                            k.op('dve', lambda e: e.tensor_tensor(PTm[:], pp, cm[:, d:d + 1, :].to_broadcast([64, 2, 64]), ALU.mult), reads=[bA, cm], writes=[PTm])
                            yield
                            for h in range(2):
                                k.op('pe', lambda e: e.matmul(po[:, h, 0:65], PTm[:, h, :], Vaug[:, h, 0:65], start=True, stop=False), reads=[PTm, Vaug], writes=[bB] if h == 0 else [], inc=False)
                                k.op('pe', lambda e: e.matmul(po[:, h, 0:65], qh[:, h, cs], Cbf[:, h, 0:65], start=False, stop=True), reads=[qh, Cbf], inc=False)
                            if fin:
                                k.op('pe', lambda e: e.transpose(pmo, moT[:, cs], ident[:]), reads=[moT, ident], inc=False)
                            for h in range(2):
                                k.op('pe', lambda e: e.matmul(pc[:, h, 0:65], Ktm[:, h, :], Vaug[:, h, 0:65], start=True, stop=True), reads=[Ktm, Vaug], inc=(h == 1))
                            yield
                            k.op('dve', lambda e: e.tensor_tensor(Cst[:, :, 0:65], Cst[:, :, 0:65], pc[:, :, 0:65], ALU.add), reads=[bB, Cst], writes=[Cst])
                            k.op('dve', lambda e: e.tensor_tensor(Cst[:, :, 0:65], Cst[:, :, 0:65], e_ap.unsqueeze(2).to_broadcast([64, 2, 65]), ALU.mult), reads=[GA, Cst], writes=[Cst])
                            k.op('act', lambda e: e.activation(Cbf[:, :, 0:65], Cst[:, :, 0:65], AF.Identity), reads=[Cst], writes=[Cbf])
                            if needy:
                                yield
                                k.op('dve', lambda e: e.tensor_tensor(dn[:], po[:, :, 64], a_ap, ALU.mult), reads=[bB, GA], writes=[dn])
                                k.op('act', lambda e: e.activation(dn[:], dn[:], AF.Abs), reads=[dn], writes=[dn])
                                k.op('dve', lambda e: e.tensor_scalar(dn[:], dn[:], 1.0, None, ALU.max), reads=[dn], writes=[dn])
                                yield
                                k.op('dve', lambda e: e.reciprocal(dn[:], dn[:]), reads=[dn], writes=[dn])
                                k.op('dve', lambda e: e.tensor_tensor(ff[:], dn[:], a_ap, ALU.mult), reads=[dn, GA], writes=[ff])
                                yield
                                if not second:
                                    k.op('dve', lambda e: e.tensor_tensor(Hf[:, c, :, :], po[:, :, 0:64], ff[:].unsqueeze(2).to_broadcast([64, 2, 64]), ALU.mult), reads=[bB, ff], pw_=[Hf])
                                    done[c] = M.i
                                else:
                                    k.op('dve', lambda e: e.tensor_tensor(hs[:], po[:, :, 0:64], ff[:].unsqueeze(2).to_broadcast([64, 2, 64]), ALU.mult), reads=[bB, ff], writes=[hs])
                                    k.op('dve', lambda e: e.tensor_tensor(hs[:], hs[:], Hf[:, c, :, :], ALU.add), reads=[hs, Hf], writes=[hs])
                            if fin:
                                yield
                                for h in range(2):
                                    k.op('dve', lambda e: e.bn_stats(st2[:, h, :], hs[:, h, :]), reads=[hs], writes=[st2])
                                for h in range(2):
                                    k.op('dve', lambda e: e.bn_aggr(mv2[:, h, :], st2[:, h, :]), reads=[st2], writes=[mv2])
                                yield
                                k.op('act', lambda e: e.activation(rs2[:], mv2[:, :, 1], AF.Sqrt, bias=LN_EPS), reads=[mv2], writes=[rs2])
                                k.op('dve', lambda e: e.reciprocal(rs2[:], rs2[:]), reads=[rs2], writes=[rs2])
                                yield
                                for h in range(2):
                                    k.op('dve', lambda e: e.tensor_scalar(hs[:, h, :], hs[:, h, :], mv2[:, h, 0:1], rs2[:, h:h + 1], ALU.subtract, ALU.mult),
                                         reads=[hs, mv2, rs2], writes=[hs])
                                k.op('dve', lambda e: e.tensor_tensor(om[:], hs[:].rearrange("p h e -> p (h e)"), nw[:, hp * 128:(hp + 1) * 128], ALU.mult),
                                     reads=[hs, nw], writes=[om])
                                k.op('dve', lambda e: e.tensor_tensor(om[:], om[:], pmo, ALU.mult), reads=[om, bB], writes=[om])
                                k.dma('sp', MIX[b][(c - 4) * 64:(c - 3) * 64, hp * 128:(hp + 1) * 128], om[:], reads=[om], pw=[MIX[b]])
                            yield

                    done = {}
                    gens = [mstream(MS[0], 0, done), mstream(MS[1], 1, done)]
                    alive = [True, True]
                    while any(alive):
                        for gi in range(2):
                            if alive[gi]:
                                try:
                                    next(gens[gi])
                                except StopIteration:
                                    alive[gi] = False
        k.barrier()

        with ExitStack() as es:
          if 3 in phases:
            k.es = es
            NBK = 512
            NBC = 8
            rp1 = k.sb("rp1", [128, 4, 8], F32)
            k.dma('sp', rp1[:, :, 0:7], rp_in[:, :, :], writes=[rp1])
            k.op('dve', lambda e: e.tensor_scalar(rp1[:, :, 7], rp1[:, :, 4], -1.0, 1.0, ALU.mult, ALU.add), reads=[rp1], writes=[rp1])
            smask = k.sb("smask", [128, NBK], F32)
            k.dma('sp', smask[:], smask_in[:, 0:NBK], writes=[smask])
            onesbd = k.sb("onesbd", [128, 128], F32)
            k.dma('sp', onesbd[:], onesbd_in[:, :], writes=[onesbd])
            ARs = k.sb("ARs", [128, NBC, 128], BF16)
            ZTs = k.sb("ZTs", [128, NBC, 128], BF16)
            PRs = k.sb("PRs", [128, NBK], BF16)
            GLs = k.sb("GLs", [128, NBC], F32)
            rmask = k.sb("rmask", [128, 2, 128], F32)
            k.dma('sp', rmask[:], rmask_in[:, :, :], writes=[rmask])
            nmask = k.sb("nmask", [64, 2, 64], F32)
            k.dma('sp', nmask[:], nmask_in[:, :, :], writes=[nmask])
            gnw = k.sb("gnw", [64, 2, 512], F32)
            k.dma('sp', gnw[:, 0, :], r_norm_w[0:1, :].partition_broadcast(64), pw=[gnw])
            k.dma('sp', gnw[:, 1, :], r_norm_b[0:1, :].partition_broadcast(64), pw=[gnw])
            wBb = k.sb("wBb", [64, 2, 512], BF16)
            aBb = k.sb("aBb", [64, 512], BF16)
            gBb = k.sb("gBb", [128, 512], BF16)
            k.dma('pool', wBb[:, 0, :], r_wB[0, :, :], pw=[wBb])
            k.dma('pool', wBb[:, 1, :], r_wB[1, :, :], pw=[wBb])
            k.dma('pool', aBb[:], r_aB[:, :], writes=[aBb])
            k.dma('pool', gBb[:], r_gB[:, :], writes=[gBb])
            k.op('pool', lambda e: e.memset(ones_f[:], 1.0), writes=[ones_f])
            onesb = k.sb("onesb", [64, 2], BF16)
            k.op('pool', lambda e: e.memset(onesb[:], 1.0), writes=[onesb])
            lgb = k.sb("lgb", [128, T], BF16)
            lab = k.sb("lab", [64, NBK], BF16)
            lwb_ = k.sb("lwb_", [64, NBK], BF16)
            rr = k.sb("rr", [128, NBK], F32)
            rk = k.sb("rk", [128, NBK], F32)
            aa = k.sb("aa", [128, NBK], F32)
            t1 = k.sb("t1", [128, NBK], F32)
            t2 = k.sb("t2", [128, NBK], F32)
            khat = k.sb("khat", [128, NBK], F32)
            kmod = k.sb("kmod", [128, NBK], F32)
            beta = k.sb("beta", [128, NBK], F32)
            lgw = k.sb("lgw", [128, NBK], F32)
            lam = k.sb("lam", [128, NBK], F32)
            ee = k.sb("ee", [128, NBK], F32)
            class _S:
                pass
            SS = []
            for si in range(2):
                S = _S()
                S.i = si
                S.VTb = k.sb("VTb", [64, 2, 64 + NBK], BF16)
                S.PRb = k.sb("PRb", [64, 2, NBK], BF16)
                S.AR = k.sb("AR", [64, 2, NBC, 128], BF16)
                S.ZT = k.sb("ZT", [64, 2, NBC, 128], BF16)
                S.GL = k.sb("GL", [64, 2, NBC], F32)
                S.MmA = k.sb("MmA", [128, 2, NBC, 128], BF16)
                S.XLA = k.sb("XLA", [128, 2, NBC, 64], BF16)
                S.SWA = k.sb("SWA", [128, 2, NBC + 1, 64], BF16)
                S.WA = k.sb("WA", [128, 2, NBC, 64], BF16)
                S.ZtA = k.sb("ZtA", [128, 2, NBC, 64], BF16)
                S.TTA = k.sb("TTA", [64, 2, NBC, 64], BF16)
                S.Nt = [k.sb("Nt%d" % j, [64, 8, 64], BF16) for j in range(2)]
                S.GTt = [k.sb("GTt%d" % j, [64, 8, 2, 64], BF16) for j in range(2)]
                S.Xb = k.sb("Xb", [64, 2, 64], BF16)
                S.ST = k.sb("ST", [64, 2, 64], F32)
                S.tS = k.sb("tS", [64, 2, 64], F32)
                S.ys = k.sb("ys", [64, 2, 64], F32)
                S.bon = k.sb("bon", [64, 2], F32)
                S.om3 = k.sb("om3", [64, 128], F32)
                S.st2 = k.sb("st2r", [64, 2, 6], F32)
                S.mv2 = k.sb("mv2r", [64, 2, 2], F32)
                S.rs2 = k.sb("rs2r", [64, 2], F32)
                S.pXU = k.ps("p3x", [64, 2, 2, 64])
                S.pYS = k.ps("p3y", [64, 512])
                SS.append(S)
            Yf = k.sb("Yf", [64, NCH, 2, 64], F32)
            identg = k.sb("identg", [64, 8, 64], F32)
            pMg = k.ps("p3m", [128, 8, 128])
            pSg = k.ps("p3s", [128, 2, 512])
            pA = pMg
            for S in SS:
                k.op('pool', lambda e: e.memset(S.VTb[:], 0.0), writes=[S.VTb])
                k.op('pool', lambda e: e.memset(S.SWA[:], 0.0), writes=[S.SWA])
            for m in range(8):
                k.op('dve', lambda e: e.tensor_copy(identg[:, m, :], ident[0:64, 0:64]), reads=[ident], writes=[identg])

            prep_lock = [False]

            def prep(S, b, hp, tok0, ntok, d):
                VTb, PRb, AR, ZT, GL = S.VTb, S.PRb, S.AR, S.ZT, S.GL
                while prep_lock[0]:
                    yield
                prep_lock[0] = True
                nch = ntok // 64
                n = ntok
                NS = slice(0, ntok)
                r0 = hp * 128
                k.dma('pool', lab[:, NS], FM[b][3744:3808, tok0:tok0 + ntok], reads=[FM[b]], writes=[lab])
                lo = 3616 + 64 * d
                k.dma('pool', lwb_[:, NS], FM[b][lo:lo + 64, tok0:tok0 + ntok], reads=[FM[b]], writes=[lwb_])
                k.dma('sp', rr[:, NS], FM[b][1024 + r0:1024 + r0 + 128, tok0:tok0 + ntok], reads=[FM[b]], writes=[rr])
                k.dma('sp', rk[:, NS], FM[b][1536 + r0:1536 + r0 + 128, tok0:tok0 + ntok], reads=[FM[b]], writes=[rk])
                for h in range(2):
                    k.dma('pool', VTb[:, h, 64:64 + ntok], FM[b][2048 + r0 + h * 64:2048 + r0 + (h + 1) * 64, tok0:tok0 + ntok], reads=[FM[b]], pw=[VTb])
                yield
                pA0 = pMg[:, 0:4, :].rearrange("p a b -> p (a b)")[:, 0:n]
                pA1 = pMg[:, 4:8, :].rearrange("p a b -> p (a b)")[:, 0:n]
                k.op('pe', lambda e: e.matmul(pA0, aBb[:, hp * 128:(hp + 1) * 128], lab[:, NS], start=True, stop=True), reads=[aBb, lab], writes=[pMg], inc=False)
                k.op('pe', lambda e: e.matmul(pA1, wBb[:, d, hp * 128:(hp + 1) * 128], lwb_[:, NS], start=True, stop=True), reads=[wBb, lwb_])
                k.op('act', lambda e: e.activation(aa[:, NS], pA0, AF.Sigmoid, bias=rp1[:, hp, 2:3]), reads=[pMg, rp1], writes=[aa])
                k.op('act', lambda e: e.activation(lgw[:, NS], pA1, AF.Sigmoid, bias=rp1[:, hp, d:d + 1]), reads=[pMg, rp1], writes=[lgw])
                k.op('dve', lambda e: e.tensor_scalar(lgw[:, NS], lgw[:, NS], -DS, None, ALU.mult), reads=[lgw], writes=[lgw])
                yield
                k.op('dve', lambda e: e.tensor_scalar(t1[:, NS], rk[:, NS], rp1[:, hp, 3:4], None, ALU.mult), reads=[rk, rp1], writes=[t1])
                k.op('dve', lambda e: e.tensor_tensor(t2[:, NS], t1[:, NS], t1[:, NS], ALU.mult), reads=[t1], writes=[t2])
                k.op('pe', lambda e: e.matmul(pA0, onesbd[:], t2[:, NS], start=True, stop=True), reads=[onesbd, t2], writes=[pMg])
                k.op('act', lambda e: e.activation(khat[:, NS], pA0, AF.Sqrt), reads=[pMg], writes=[khat])
                yield
                k.op('dve', lambda e: e.tensor_scalar(khat[:, NS], khat[:, NS], 1e-12, None, ALU.max), reads=[khat], writes=[khat])
                k.op('dve', lambda e: e.reciprocal(khat[:, NS], khat[:, NS]), reads=[khat], writes=[khat])
                k.op('dve', lambda e: e.tensor_tensor(khat[:, NS], khat[:, NS], t1[:, NS], ALU.mult), reads=[khat, t1], writes=[khat])
                yield
                k.op('dve', lambda e: e.tensor_scalar(t1[:, NS], aa[:, NS], rp1[:, hp, 4:5], rp1[:, hp, 7:8], ALU.mult, ALU.add), reads=[aa, rp1], writes=[t1])
                k.op('dve', lambda e: e.tensor_tensor(kmod[:, NS], rk[:, NS], t1[:, NS], ALU.mult), reads=[rk, t1], writes=[kmod])
                k.op('dve', lambda e: e.tensor_tensor(beta[:, NS], khat[:, NS], aa[:, NS], ALU.mult), reads=[khat, aa], writes=[beta])
                yield
                k.op('dve', lambda e: e.scalar_tensor_tensor(PRs[:, NS], rr[:, NS], rp1[:, hp, 6:7], kmod[:, NS], ALU.mult, ALU.mult), reads=[rr, rp1, kmod], writes=[PRs])
                k.op('dve', lambda e: e.tensor_tensor_scan(lam[:, NS], smask[:, NS], lgw[:, NS], 0.0, ALU.mult, ALU.add), reads=[smask, lgw], writes=[lam])
                yield
                v3 = lambda t_: t_[:, NS].rearrange("p (c t) -> p c t", t=64)
                if d == 1:
                    k.op('dve', lambda e: e.tensor_tensor(t1[:, NS], lgw[:, NS], lam[:, NS], ALU.subtract), reads=[lgw, lam], writes=[t1])
                    k.op('dve', lambda e: e.tensor_tensor(v3(t2), v3(t1), v3(lam)[:, :, 63:64].to_broadcast([128, nch, 64]), ALU.add), reads=[t1, lam], writes=[t2])
                    k.op('act', lambda e: e.activation(lam[:, NS], t2[:, NS], AF.Identity), reads=[t2], writes=[lam])
                k.op('dve', lambda e: e.tensor_tensor(t1[:, NS], lam[:, NS], lgw[:, NS], ALU.subtract), reads=[lam, lgw], writes=[t1])
                k.op('act', lambda e: e.activation(ee[:, NS], t1[:, NS], AF.Exp), reads=[t1], writes=[ee])
                yield
                k.op('dve', lambda e: e.scalar_tensor_tensor(ARs[:, 0:nch, 0:64], v3(khat), -1.0, v3(ee), ALU.mult, ALU.mult), reads=[khat, ee], writes=[ARs])
                k.op('act', lambda e: e.activation(ee[:, NS], lam[:, NS], AF.Exp), reads=[lam], writes=[ee])
                k.op('dve', lambda e: e.tensor_tensor(ARs[:, 0:nch, 64:128], v3(rr), v3(ee), ALU.mult), reads=[rr, ee], writes=[ARs])
                yield
                gcol = 63 if d == 0 else 0
                k.op('act', lambda e: e.activation(GLs[:, 0:nch], v3(ee)[:, :, gcol], AF.Identity), reads=[ee], writes=[GLs])
                k.op('act', lambda e: e.activation(t1[:, NS], lam[:, NS], AF.Exp, scale=-1.0), reads=[lam], writes=[t1])
                yield
                k.op('dve', lambda e: e.tensor_tensor(ZTs[:, 0:nch, 0:64], v3(beta), v3(t1), ALU.mult), reads=[beta, t1], writes=[ZTs])
                k.op('dve', lambda e: e.tensor_tensor(ZTs[:, 0:nch, 64:128], v3(kmod), v3(t1), ALU.mult), reads=[kmod, t1], writes=[ZTs])
                yield
                k.op('act', lambda e: e.activation(AR[:, 0, 0:nch, :], ARs[0:64, 0:nch, :], AF.Identity), reads=[ARs], writes=[AR])
                k.op('act', lambda e: e.activation(ZT[:, 0, 0:nch, :], ZTs[0:64, 0:nch, :], AF.Identity), reads=[ZTs], writes=[ZT])
                k.op('act', lambda e: e.activation(PRb[:, 0, NS], PRs[0:64, NS], AF.Identity), reads=[PRs], writes=[PRb])
                k.op('act', lambda e: e.activation(GL[:, 0, 0:nch], GLs[0:64, 0:nch], AF.Identity), reads=[GLs], writes=[GL])
                k.dma('sp', AR[:, 1, 0:nch, :], ARs[64:128, 0:nch, :], reads=[ARs], pw=[AR])
                k.dma('sp', ZT[:, 1, 0:nch, :], ZTs[64:128, 0:nch, :], reads=[ZTs], pw=[ZT])
                k.dma('sp', PRb[:, 1, NS], PRs[64:128, NS], reads=[PRs], pw=[PRb])
                k.dma('sp', GL[:, 1, 0:nch], GLs[64:128, 0:nch], reads=[GLs], pw=[GL])
                prep_lock[0] = False

            pSf = lambda: pSg[:].rearrange("p a b -> p (a b)")

            def precompute(S, l0, d):
                VTb, AR, ZT, MmA, XLA, SWA, WA, ZtA, TTA = S.VTb, S.AR, S.ZT, S.MmA, S.XLA, S.SWA, S.WA, S.ZtA, S.TTA
                G4 = slice(l0, l0 + 4)
                pTb = pSg[:, 0, :].bitcast(BF16)
                for h in range(2):
                    for j in range(4):
                        m = h * 4 + j
                        k.op('pe', lambda e: e.transpose(pTb[:, m * 64:(m + 1) * 64], ZT[:, h, l0 + j, :], identb[0:64, 0:64]), reads=[ZT, identb], writes=[pSg], inc=False)
                for h in range(2):
                    for j in range(4):
                        m = 8 + h * 4 + j
                        k.op('pe', lambda e: e.transpose(pTb[:, m * 64:(m + 1) * 64], VTb[:, h, (l0 + j) * 64:(l0 + j) * 64 + 128], identb[0:64, 0:64]), reads=[VTb, identb], inc=(h == 1 and j == 3))
                zsrc = pTb[:, 0:512].rearrange("p (h j e) -> p h j e", h=2, j=4)
                vsrc = pTb[64:128, 512:1024].rearrange("p (h j e) -> p h j e", h=2, j=4)
                k.op('act', lambda e: e.activation(ZtA[:, :, G4, :], zsrc, AF.Identity), reads=[pSg], writes=[ZtA])
                k.op('act', lambda e: e.activation(SWA[64:128, :, G4, :], vsrc, AF.Identity), reads=[pSg], writes=[SWA])
                k.op('act', lambda e: e.activation(WA[64:128, :, G4, :], vsrc, AF.Identity), reads=[pSg], writes=[WA])
                yield
                for h in range(2):
                    for j in range(4):
                        m = h * 4 + j
                        k.op('pe', lambda e: e.matmul(pMg[:, m, :], ZT[:, h, l0 + j, :], AR[:, h, l0 + j, :], start=True, stop=True), reads=[ZT, AR], writes=[pMg], inc=(m == 7))
                for h in range(2):
                    for j in range(4):
                        m = h * 4 + j
                        k.op('pe', lambda e: e.matmul(pSg[0:64, 1, m * 64:(m + 1) * 64], AR[:, h, l0 + j, 0:64], ZT[:, h, l0 + j, 0:64], start=True, stop=True), reads=[ZT, AR], writes=[pSg], inc=(m == 7))
                for h in range(2):
                    k.op('dve', lambda e: e.tensor_tensor(MmA[:, h, G4, :], pMg[:, h * 4:(h + 1) * 4, :], rmask[:, d:d + 1, :].to_broadcast([128, 4, 128]), ALU.mult),
                         reads=[pMg, rmask], writes=[MmA])
                Nt, GTt = S.Nt, S.GTt
                k.op('dve', lambda e: e.tensor_tensor(Nt[0][:], pSg[0:64, 1, :].rearrange("p (m e) -> p m e", e=64), nmask[:, d:d + 1, :].to_broadcast([64, 8, 64]), ALU.mult),
                     reads=[pSg, nmask], writes=[Nt[0]])
                k.op('act', lambda e: e.activation(GTt[0][:, :, 0, :].rearrange("p (h j) e -> p h j e", h=2), MmA[0:64, :, G4, 0:64], AF.Identity), reads=[MmA], writes=[GTt[0]])
                k.op('act', lambda e: e.activation(GTt[0][:, :, 1, :], identg[:], AF.Identity), reads=[identg, GTt[0]], writes=[GTt[0]])
                k.op('act', lambda e: e.activation(XLA[64:128, :, G4, :], MmA[64:128, :, G4, 0:64], AF.Identity), reads=[MmA], writes=[XLA])
                k.op('act', lambda e: e.activation(XLA[0:64, :, G4, :], AR[:, :, G4, 0:64], AF.Identity), reads=[AR], writes=[XLA])
                cur = 0
                for lv in range(5):
                    yield
                    nsrc, gsrc, ndst, gdst = Nt[cur], GTt[cur], Nt[1 - cur], GTt[1 - cur]
                    for m in range(8):
                        k.op('pe', lambda e: e.matmul(pSg[0:64, 0, m * 64:(m + 1) * 64], gsrc[:, m, 0, :], nsrc[:, m, :], start=True, stop=True), reads=[gsrc, nsrc], writes=[pSg] if m == 0 else [], inc=False)
                    for m in range(8):
                        k.op('pe', lambda e: e.matmul(pMg[0:64, m, :], nsrc[:, m, :], gsrc[:, m, :, :].rearrange("p a e -> p (a e)"), start=True, stop=True), reads=[nsrc, gsrc], writes=[pMg] if m == 0 else [], inc=(m == 7))
                    k.op('act', lambda e: e.activation(ndst[:].rearrange("p m e -> p (m e)"), pSg[0:64, 0, :], AF.Identity), reads=[pSg], writes=[ndst])
                    k.op('act', lambda e: e.activation(gdst[:, :, 0, :], pMg[0:64, :, 0:64], AF.Identity), reads=[pMg], writes=[gdst])
                    k.op('dve', lambda e: e.tensor_tensor(gdst[:, :, 1, :], pMg[0:64, :, 64:128], gsrc[:, :, 1, :], ALU.add), reads=[pMg, gsrc, gdst], writes=[gdst])
                    cur = 1 - cur
                yield
                nsrc, gsrc = Nt[cur], GTt[cur]
                for m in range(8):
                    k.op('pe', lambda e: e.matmul(pSg[0:64, 1, m * 64:(m + 1) * 64], nsrc[:, m, :], gsrc[:, m, 1, :], start=True, stop=True), reads=[nsrc, gsrc], writes=[pSg] if m == 0 else [], inc=(m == 7))
                k.op('dve', lambda e: e.tensor_tensor(TTA[:, :, G4, :], pSg[0:64, 1, :].rearrange("p (h j e) -> p h j e", h=2, j=4), gsrc[:, :, 1, :].rearrange("p (h j) e -> p h j e", h=2), ALU.add),
                     reads=[pSg, gsrc], writes=[TTA])

            def step(S, b, hp, c, lc, lnext, d, done):
                VTb, PRb, AR, GL, MmA, XLA, SWA, WA, ZtA, TTA = S.VTb, S.PRb, S.AR, S.GL, S.MmA, S.XLA, S.SWA, S.WA, S.ZtA, S.TTA
                Xb, ST, tS, ys, bon, om3, st2, mv2, rs2, pXU, pYS = S.Xb, S.ST, S.tS, S.ys, S.bon, S.om3, S.st2, S.mv2, S.rs2, S.pXU, S.pYS
                pX = pXU[:, 0, :, :]
                pU = pXU[:, 1, :, :]
                pY = pYS[:, 0:128].rearrange("p (h e) -> p h e", h=2)
                pS_ = pYS[:, 128:256].rearrange("p (h e) -> p h e", h=2)
                for h in range(2):
                    k.op('pe', lambda e: e.matmul(pXU[:, 0, h, :], XLA[:, h, lc, :], SWA[:, h, lc, :], start=True, stop=True), reads=[XLA, SWA], writes=[pXU], inc=(h == 1))
                k.op('act', lambda e: e.activation(Xb[:], pX, AF.Identity), reads=[pXU], writes=[Xb])
                yield
                for h in range(2):
                    k.op('pe', lambda e: e.matmul(pXU[:, 1, h, :], TTA[:, h, lc, :], Xb[:, h, :], start=True, stop=True), reads=[TTA, Xb], writes=[pXU], inc=(h == 1))
                k.op('dve', lambda e: e.tensor_copy(WA[0:64, :, lc, :], pU), reads=[pXU], writes=[WA])
                yield
                second = (c in done)
                needy = (c >= 4)
                wfirst = [pYS]
                if needy:
                    for h in range(2):
                        k.op('pe', lambda e: e.matmul(pYS[:, h * 64:(h + 1) * 64], AR[:, h, lc, 64:128], SWA[0:64, h, lc, :], start=True, stop=False), reads=[AR, SWA], writes=wfirst, inc=False)
                        wfirst = []
                        k.op('pe', lambda e: e.matmul(pYS[:, h * 64:(h + 1) * 64], MmA[:, h, lc, 64:128], WA[:, h, lc, :], start=False, stop=True), reads=[MmA, WA], inc=False)
                fin = (needy and second)
                if fin:
                    ts = slice(lc * 64, (lc + 1) * 64)
                    for h in range(2):
                        k.op('pe', lambda e: e.matmul(pYS[:, 384 + 2 * h:386 + 2 * h], PRb[:, h, ts], onesb[:, 0:2], start=True, stop=True), reads=[PRb, onesb], inc=False)
                    k.op('pe', lambda e: e.matmul(pYS[:, 256:384], lgb[:, c * 64:(c + 1) * 64], gBb[:, hp * 128:(hp + 1) * 128], start=True, stop=True), reads=[lgb, gBb], inc=False)
                    pvt = pYS[:, 448:512].bitcast(BF16)
                    for h in range(2):
                        k.op('pe', lambda e: e.transpose(pvt[:, h * 64:(h + 1) * 64], VTb[:, h, 64 + lc * 64:128 + lc * 64], identb[0:64, 0:64]), reads=[VTb, identb], inc=False)
                for h in range(2):
                    k.op('pe', lambda e: e.matmul(pYS[:, 128 + h * 64:128 + (h + 1) * 64], ZtA[:, h, lc, :], WA[:, h, lc, :], start=True, stop=True), reads=[ZtA, WA], writes=wfirst, inc=(h == 1))
                    wfirst = []
                k.op('dve', lambda e: e.tensor_tensor(tS[:], pS_, ST[:], ALU.add), reads=[pYS, ST], writes=[tS])
                k.op('dve', lambda e: e.tensor_tensor(ST[:], tS[:], GL[:, :, lc:lc + 1].to_broadcast([64, 2, 64]), ALU.mult), reads=[tS, GL], writes=[ST])
                k.op('act', lambda e: e.activation(SWA[0:64, :, lnext, :], ST[:], AF.Identity), reads=[ST], writes=[SWA])
                if needy and not second:
                    k.op('dve', lambda e: e.tensor_copy(Yf[:, c, :, :], pY), reads=[pYS], pw_=[Yf])
                    done[c] = S.i
                elif needy:
                    k.op('dve', lambda e: e.tensor_tensor(ys[:], pY, Yf[:, c, :, :], ALU.add), reads=[pYS, Yf], writes=[ys])
                if fin:
                    yield
                    for h in range(2):
                        k.op('dve', lambda e: e.bn_stats(st2[:, h, :], ys[:, h, :]), reads=[ys], writes=[st2])
                    for h in range(2):
                        k.op('dve', lambda e: e.bn_aggr(mv2[:, h, :], st2[:, h, :]), reads=[st2], writes=[mv2])
                    yield
                    k.op('act', lambda e: e.activation(rs2[:], mv2[:, :, 1], AF.Sqrt, bias=GN_EPS), reads=[mv2], writes=[rs2])
                    k.op('dve', lambda e: e.reciprocal(rs2[:], rs2[:]), reads=[rs2], writes=[rs2])
                    yield
                    for h in range(2):
                        k.op('dve', lambda e: e.tensor_scalar(ys[:, h, :], ys[:, h, :], mv2[:, h, 0:1], rs2[:, h:h + 1], ALU.subtract, ALU.mult), reads=[ys, mv2, rs2], writes=[ys])
                    ysf = ys[:].rearrange("p h e -> p (h e)")
                    k.op('dve', lambda e: e.tensor_tensor(om3[:], ysf, gnw[:, 0, hp * 128:(hp + 1) * 128], ALU.mult), reads=[ys, gnw], writes=[om3])
                    yield
                    k.op('dve', lambda e: e.tensor_tensor(om3[:], om3[:], gnw[:, 1, hp * 128:(hp + 1) * 128], ALU.add), reads=[om3, gnw], writes=[om3])
                    k.op('dve', lambda e: e.tensor_copy(bon[:], pYS[:, 384:388].rearrange("p (h two) -> p h two", two=2)[:, :, 0]), reads=[pYS], writes=[bon])
                    yield
                    for h in range(2):
                        k.op('dve', lambda e: e.scalar_tensor_tensor(om3[:, h * 64:(h + 1) * 64], pvt[:, h * 64:(h + 1) * 64], bon[:, h:h + 1], om3[:, h * 64:(h + 1) * 64], ALU.mult, ALU.add),
                             reads=[pYS, bon, om3], writes=[om3])
                    k.op('dve', lambda e: e.tensor_tensor(om3[:], om3[:], pYS[:, 256:384], ALU.mult), reads=[om3, pYS], writes=[om3])
                    k.dma('sp', MIX[b][(c - 4) * 64:(c - 3) * 64, 512 + hp * 128:512 + (hp + 1) * 128], om3[:], reads=[om3], pw=[MIX[b]])

            blocks = [(0, 256)] + [(256 + i * 512, 512) for i in range(8)]

            def stream(S, b, hp, d, done):
                k.op('pool', lambda e: e.memset(S.ST[:], 0.0), writes=[S.ST])
                border = list(range(9)) if d == 0 else [0] + list(range(8, 0, -1))
                first = True
                for bi in border:
                    tok0, ntok = blocks[bi]
                    nch = ntok // 64
                    yield from prep(S, b, hp, tok0, ntok, d)
                    lcs = list(range(nch)) if d == 0 else list(range(nch - 1, -1, -1))
                    if first:
                        k.op('pool', lambda e: e.memset(S.SWA[0:64, :, lcs[0], :], 0.0), writes=[S.SWA])
                        first = False
                    else:
                        k.op('act', lambda e: e.activation(S.SWA[0:64, :, lcs[0], :], S.ST[:], AF.Identity), reads=[S.ST], writes=[S.SWA])
                    yield
                    for g0 in range(0, nch, 4):
                        if os.environ.get('RW_SKIP_PRE'):
                            break
                        yield from precompute(S, g0, d)
                        yield
                    for ii, lc in enumerate(lcs):
                        if os.environ.get('RW_SKIP_STEP'):
                            break
                        lnext = lcs[ii + 1] if ii + 1 < len(lcs) else NBC
                        yield from step(S, b, hp, tok0 // 64 + lc, lc, lnext, d, done)
                        yield

            for b in range(NB):
                for q4 in range(4):
                    k.dma('pool', lgb[:, q4 * 1088:(q4 + 1) * 1088], FM[b][3808:3936, q4 * 1088:(q4 + 1) * 1088], reads=[FM[b]], pw=[lgb])
                for hp in range(int(os.environ.get('P3H', 4))):
                    done = {}
                    gens = [stream(SS[0], b, hp, 0, done), stream(SS[1], b, hp, 1, done)]
                    alive = [True, True]
                    for _ in range(int(os.environ.get('RW_OFF', 22))):
                        next(gens[0])
                    while any(alive):
                        for gi in range(2):
                            if alive[gi]:
                                try:
                                    next(gens[gi])
                                except StopIteration:
                                    alive[gi] = False
        k.barrier()

        with ExitStack() as es:
          if 4 in phases:
           try:
            P4S = int(os.environ.get('P4S', 9))
            k.es = es
            NT = NB * SEQ // 128
            NBLK = NBLK_
            SUB = BS // 128
            LG = k.sb("LG", [128, NT, 36], F32)
            OH1 = k.sb("OH1", [128, NT, 32], F32)
            OH2 = k.sb("OH2", [128, NT, 32], F32)
            W1 = k.sb("W1", [128, NT], F32)
            W2 = k.sb("W2", [128, NT], F32)
            DST = k.sb("DST", [128, NT, 2], I32)
            WIDX = k.sb("WIDX", [128, NBLK, 12], I32)
            g2b = k.sb("g2b", [128, NB, D], F32)
            lnp = k.sb("lnp", [128, 4, D], F32)
            for j, src in enumerate((ln1_g, ln1_b, ln2_g, ln2_b)):
                k.dma('sp', lnp[:, j, :], src[0:1, :].partition_broadcast(128), pw=[lnp])
            for b in range(NB):
                k.dma('sp', g2b[:, b, :], MODD[b:b + 1, 5 * D:6 * D].partition_broadcast(128), reads=[MODD], pw=[g2b])
            with ExitStack() as es4:
                k.es = es4
                wob = k.sb("wob", [128, 8, D], BF16)
                for kc in range(8):
                    k.dma('pool', wob[:, kc, :], w_out[kc * 128:(kc + 1) * 128, :], pw=[wob])
                rt = k.sb("rt", [128, 8, 36], F32)
                k.dma('sp', rt[:], rt_in[:, :].rearrange("(kc p) n -> p kc n", p=128), writes=[rt])
                rtbb = k.sb("rtbb", [128, 36], F32)
                k.dma('sp', rtbb[:], rtb_in[0:1, :].partition_broadcast(128), writes=[rtbb])
                mb4 = k.sb("mb4", [128, 3, D], F32)
                class _A:
                    pass
                AS = []
                for si in range(2):
                    A = _A()
                    A.mxb = k.sb("mxb", [128, D], BF16)
                    A.mT = k.sb("mT", [128, 8, 128], BF16)
                    A.x4 = k.sb("x4", [128, D], F32)
                    A.t4 = k.sb("t4", [128, D], F32)
                    A.y4 = k.sb("y4", [128, D], F32)
                    A.h4 = k.sb("h4", [128, D], F32)
                    A.h4b = k.sb("h4b", [128, D], BF16)
                    A.h4T = k.sb("h4T", [128, 8, 128], F32)
                    A.st4 = k.sb("st4", [128, 2, 6], F32)
                    A.mv4 = k.sb("mv4", [128, 2], F32)
                    A.rs4 = k.sb("rs4", [128, 1], F32)
                    A.nb4 = k.sb("nb4", [128, 1], F32)
                    A.P01 = k.ps("p4o", [128, 2, 512])
                    A.P01.excl = True
                    A.plg = k.ps("p4lg", [128, 36])
                    AS.append(A)

                def ln_stats(A, src):
                    for hf in range(2):
                        k.op('dve', lambda e: e.bn_stats(A.st4[:, hf, :], src[:, hf * 512:(hf + 1) * 512]), reads=[src], writes=[A.st4])
                    k.op('dve', lambda e: e.bn_aggr(A.mv4[:], A.st4[:].rearrange("p a b -> p (a b)")), reads=[A.st4], writes=[A.mv4])
                    k.op('act', lambda e: e.activation(A.rs4[:], A.mv4[:, 1:2], AF.Sqrt, bias=LN_EPS), reads=[A.mv4], writes=[A.rs4])
                    k.op('dve', lambda e: e.reciprocal(A.rs4[:], A.rs4[:]), reads=[A.rs4], writes=[A.rs4])
                    k.op('dve', lambda e: e.scalar_tensor_tensor(A.nb4[:], A.mv4[:, 0:1], -1.0, A.rs4[:], ALU.mult, ALU.mult), reads=[A.mv4, A.rs4], writes=[A.nb4])

                def tile4(A, b, i):
                    gi = b * (SEQ // 128) + i
                    P01 = A.P01
                    k.dma('pool', A.mxb[:], MIX[b][i * 128:(i + 1) * 128, :], reads=[MIX[b]], writes=[A.mxb])
                    k.dma('sp', A.x4[:], x_in[b, i * 128:(i + 1) * 128, :], writes=[A.x4])
                    yield
                    ptm = P01[:, 0, :].bitcast(BF16)
                    for kc in range(8):
                        k.op('pe', lambda e: e.transpose(ptm[:, kc * 128:(kc + 1) * 128], A.mxb[:, kc * 128:(kc + 1) * 128], identb[:]), reads=[A.mxb, identb], writes=[P01] if kc == 0 else [], inc=(kc == 7))
                    k.op('act', lambda e: e.activation(A.mT[:].rearrange("p a b -> p (a b)"), ptm, AF.Identity), reads=[P01], writes=[A.mT])
                    yield
                    for n in range(2):
                        for kc in range(8):
                            k.op('pe', lambda e: e.matmul(P01[:, n, :], A.mT[:, kc, :], wob[:, kc, n * 512:(n + 1) * 512], start=(kc == 0), stop=(kc == 7)),
                                 reads=[A.mT, wob], writes=[P01] if (kc == 0 and n == 0) else [], inc=(kc == 7 and n == 1))
                    k.op('dve', lambda e: e.tensor_tensor(A.t4[:], P01[:].rearrange("p a b -> p (a b)"), mb4[:, 0, :], ALU.mult), reads=[P01, mb4], writes=[A.t4])
                    k.op('dve', lambda e: e.scalar_tensor_tensor(A.y4[:], A.x4[:], ALPHA, A.t4[:], ALU.mult, ALU.add), reads=[A.x4, A.t4], writes=[A.y4])
                    yield
                    ln_stats(A, A.y4)
                    k.op('act', lambda e: e.activation(A.y4[:], A.y4[:], AF.Identity, bias=A.nb4[:, 0:1], scale=A.rs4[:, 0:1]), reads=[A.y4, A.nb4, A.rs4], writes=[A.y4])
                    yield
                    k.op('dve', lambda e: e.tensor_tensor(A.y4[:], A.y4[:], lnp[:, 0, :], ALU.mult), reads=[A.y4, lnp], writes=[A.y4])
                    k.op('dve', lambda e: e.tensor_tensor(A.y4[:], A.y4[:], lnp[:, 1, :], ALU.add), reads=[A.y4, lnp], writes=[A.y4])
                    k.dma('sp', X1[gi * 128:(gi + 1) * 128, :], A.y4[:], reads=[A.y4], pw=[X1])
                    yield
                    ln_stats(A, A.y4)
                    k.op('act', lambda e: e.activation(A.h4[:], A.y4[:], AF.Identity, bias=A.nb4[:, 0:1], scale=A.rs4[:, 0:1]), reads=[A.y4, A.nb4, A.rs4], writes=[A.h4])
                    yield
                    k.op('dve', lambda e: e.tensor_tensor(A.h4[:], A.h4[:], mb4[:, 1, :], ALU.mult), reads=[A.h4, mb4], writes=[A.h4])
                    k.op('dve', lambda e: e.tensor_tensor(A.h4[:], A.h4[:], mb4[:, 2, :], ALU.add), reads=[A.h4, mb4], writes=[A.h4])
                    k.op('act', lambda e: e.activation(A.h4b[:], A.h4[:], AF.Identity), reads=[A.h4], writes=[A.h4b])
                    k.dma('sp', H2[gi * 128:(gi + 1) * 128, :], A.h4b[:], reads=[A.h4b], pw=[H2])
                    yield
                    pth = P01[:].rearrange("p a b -> p (a b)")
                    for kc in range(8):
                        k.op('pe', lambda e: e.transpose(pth[:, kc * 128:(kc + 1) * 128], A.h4[:, kc * 128:(kc + 1) * 128], ident[:]), reads=[A.h4, ident], writes=[P01] if kc == 0 else [], inc=(kc == 7))
                    k.op('act', lambda e: e.activation(A.h4T[:].rearrange("p a b -> p (a b)"), pth, AF.Identity), reads=[P01], writes=[A.h4T])
                    yield
                    for kc in range(8):
                        k.op('pe', lambda e: e.matmul(A.plg[:], A.h4T[:, kc, :], rt[:, kc, :], start=(kc == 0), stop=(kc == 7)),
                             reads=[A.h4T, rt], writes=[A.plg] if kc == 0 else [], inc=(kc == 7))
                    k.op('dve', lambda e: e.tensor_tensor(LG[:, gi, :], A.plg[:], rtbb[:], ALU.add), reads=[A.plg, rtbb], pw_=[LG])

                for b in range(NB):
                    for j, c0 in enumerate((2 * D, 4 * D, 3 * D)):
                        k.dma('sp', mb4[:, j, :], MODD[b:b + 1, c0:c0 + D].partition_broadcast(128), reads=[MODD], writes=[mb4] if j == 0 else [], pw=[] if j == 0 else [mb4])
                    k.op('dve', lambda e: e.tensor_scalar(mb4[:, 1, :], mb4[:, 1, :], 1.0, None, ALU.add), reads=[mb4], writes=[mb4])

                    def astream(si):
                        for i in range(si, SEQ // 128, 2):
                            yield from tile4(AS[si], b, i)
                            yield
                    gens = [astream(0), astream(1)]
                    alive = [True, True]
                    while any(alive):
                        for gq in range(2):
                            if alive[gq]:
                                try:
                                    next(gens[gq])
                                except StopIteration:
                                    alive[gq] = False
            k.barrier()
            with ExitStack() as es5:
                k.es = es5
                if P4S < 1:
                    raise _Stop()
                gmx = k.sb("gmx", [128, NT], F32)
                goh = k.sb("goh", [128, NT, 4], F32)
                tg = k.sb("tg", [128, NT, 4], F32)
                ptop = k.sb("ptop", [128, NT], F32)
                lem = k.sb("lem", [128, NT, 32], F32)
                v1 = k.sb("v1", [128, NT], F32)
                v2 = k.sb("v2", [128, NT], F32)
                lgv = LG[:, :, 0:4]
                lev = LG[:, :, 4:36]
                k.op('dve', lambda e: e.tensor_reduce(gmx[:], lgv, AX.X, ALU.max), reads=[LG], writes=[gmx])
                k.op('dve', lambda e: e.tensor_tensor(goh[:], lgv, gmx[:].unsqueeze(2).to_broadcast([128, NT, 4]), ALU.is_equal), reads=[LG, gmx], writes=[goh])
                k.op('dve', lambda e: e.tensor_tensor(tg[:], lgv, gmx[:].unsqueeze(2).to_broadcast([128, NT, 4]), ALU.subtract), reads=[LG, gmx], writes=[tg])
                k.op('act', lambda e: e.activation(tg[:], tg[:], AF.Exp), reads=[tg], writes=[tg])
                k.op('dve', lambda e: e.tensor_reduce(ptop[:], tg[:], AX.X, ALU.add), reads=[tg], writes=[ptop])
                k.op('dve', lambda e: e.reciprocal(ptop[:], ptop[:]), reads=[ptop], writes=[ptop])
                k.op('dve', lambda e: e.tensor_scalar(goh[:], goh[:], -1.0, 1e30, ALU.add, ALU.mult), reads=[goh], writes=[goh])
                for g in range(4):
                    k.op('dve', lambda e: e.tensor_tensor(lem[:, :, g * 8:(g + 1) * 8], LG[:, :, 4 + g * 8:12 + g * 8],
                                                          goh[:, :, g:g + 1].to_broadcast([128, NT, 8]), ALU.add), reads=[LG, goh], writes=[lem])
                k.op('dve', lambda e: e.tensor_reduce(v1[:], lem[:], AX.X, ALU.max), reads=[lem], writes=[v1])
                k.op('dve', lambda e: e.tensor_tensor(OH1[:], lem[:], v1[:].unsqueeze(2).to_broadcast([128, NT, 32]), ALU.is_equal), reads=[lem, v1], writes=[OH1])
                k.op('dve', lambda e: e.scalar_tensor_tensor(lem[:], OH1[:], -1e30, lem[:], ALU.mult, ALU.add), reads=[OH1, lem], writes=[lem])
                k.op('dve', lambda e: e.tensor_reduce(v2[:], lem[:], AX.X, ALU.max), reads=[lem], writes=[v2])
                k.op('dve', lambda e: e.tensor_tensor(OH2[:], lem[:], v2[:].unsqueeze(2).to_broadcast([128, NT, 32]), ALU.is_equal), reads=[lem, v2], writes=[OH2])
                k.op('dve', lambda e: e.tensor_tensor(v2[:], v2[:], v1[:], ALU.subtract), reads=[v1, v2], writes=[v2])
                k.op('act', lambda e: e.activation(v2[:], v2[:], AF.Exp), reads=[v2], writes=[v2])
                k.op('dve', lambda e: e.tensor_scalar(v2[:], v2[:], 1.0, None, ALU.add), reads=[v2], writes=[v2])
                k.op('dve', lambda e: e.reciprocal(v2[:], v2[:]), reads=[v2], writes=[v2])
                k.op('dve', lambda e: e.tensor_tensor(W1[:], v2[:], ptop[:], ALU.mult), reads=[v2, ptop], writes=[W1])
                k.op('dve', lambda e: e.tensor_tensor(W2[:], ptop[:], W1[:], ALU.subtract), reads=[W1, ptop], writes=[W2])
            k.barrier()
            with ExitStack() as es6:
                k.es = es6
                if P4S < 2:
                    raise _Stop()
                OHb = k.sb("OHb", [128, NT, 32], BF16)
                triS = k.sb("triS", [128, 128], BF16)
                onb = k.sb("onb", [128, 128], BF16)
                thr = k.sb("thr", [128, 128], F32)
                blki = k.sb("blki", [128, NBLK], F32)
                kcp = k.sb("kcp", [128, 12], F32)
                cnt = k.sb("cnt", [128, 32], F32)
                big = k.sb("big", [128, 32, 128], F32)
                nbk = k.sb("nbk", [128, 32], F32)
                pend = k.sb("pend", [128, 32], F32)
                pst = k.sb("pst", [128, 32], F32)
                run = k.sb("run", [128, 32], F32)
                RK = k.sb("RK", [128, NT, 32], F32)
                dsf = k.sb("dsf", [128, NT, 2], F32)
                bexp = k.sb("bexp", [128, NBLK], F32)
                bigb = k.sb("bigb", [128, NBLK, 32], F32)
                widxf = k.sb("widxf", [128, NBLK, 12], F32)
                tokid = k.sb("tokid", [128, NT, 16], I32)
                zt = k.sb("zt", [128, 16], I32)
                pcn = k.ps("p5c", [128, 32])
                prk = k.ps("p5r", [128, 32])
                ptt = k.ps("p5t", [128, 32])
                stg = k.sb("stg", [128, 128], F32)
                k.dma('sp', stg[:], tris_in[:, :], writes=[stg])
                k.op('dve', lambda e: e.tensor_copy(triS[:], stg[:]), reads=[stg], writes=[triS])
                k.op('pool', lambda e: e.memset(onb[:], 1.0), writes=[onb])
                k.dma('sp', thr[:], thr_in[:, :], writes=[thr])
                k.dma('sp', blki[:], blki_in[:, 0:NBLK], writes=[blki])
                k.dma('sp', kcp[:], kcp_in[:, :], writes=[kcp])
                k.dma('sp', tokid[:], tokid_in[:, 0:NT, :], writes=[tokid])
                k.op('pool', lambda e: e.memset(zt[:], 0), writes=[zt])
                k.dma('sp', TOKB[:, :].rearrange("(b p) c -> p b c", p=128), zt[:].unsqueeze(1).to_broadcast([128, NBLK * SUB, 16]), reads=[zt], writes=[TOKB])
                k.op('dve', lambda e: e.tensor_tensor(OHb[:], OH1[:], OH2[:], ALU.add), reads=[OH1, OH2], writes=[OHb])
                for i in range(NT):
                    k.op('pe', lambda e: e.matmul(pcn[:], onb[:], OHb[:, i, :], start=(i == 0), stop=(i == NT - 1)), reads=[onb, OHb], writes=[pcn] if i == 0 else [], inc=(i == NT - 1))
                k.op('dve', lambda e: e.tensor_copy(cnt[:], pcn[:]), reads=[pcn], writes=[cnt])
                k.op('dve', lambda e: e.tensor_tensor(big[:], cnt[:].unsqueeze(2).to_broadcast([128, 32, 128]), thr[:].unsqueeze(1).to_broadcast([128, 32, 128]), ALU.is_gt),
                     reads=[cnt, thr], writes=[big])
                k.op('dve', lambda e: e.tensor_reduce(nbk[:], big[:], AX.X, ALU.add), reads=[big], writes=[nbk])
                k.op('pool', lambda e: e.memset(run[:], 1.0), writes=[run])
                k.op('dve', lambda e: e.tensor_tensor_scan(pend[:], run[:], nbk[:], 0.0, ALU.mult, ALU.add), reads=[run, nbk], writes=[pend])
                k.op('dve', lambda e: e.tensor_tensor(pst[:], pend[:], nbk[:], ALU.subtract), reads=[pend, nbk], writes=[pst])
                k.op('dve', lambda e: e.tensor_scalar(pst[:], pst[:], float(BS), None, ALU.mult), reads=[pst], writes=[pst])
                k.op('pool', lambda e: e.memset(run[:], 0.0), reads=[run], writes=[run])
                for i in range(NT):
                    k.op('pe', lambda e: e.matmul(prk[:], triS[:], OHb[:, i, :], start=True, stop=True), reads=[triS, OHb], writes=[prk])
                    k.op('pe', lambda e: e.matmul(ptt[:], onb[:], OHb[:, i, :], start=True, stop=True), reads=[onb, OHb], writes=[ptt])
                    k.op('dve', lambda e: e.tensor_tensor(RK[:, i, :], prk[:], run[:], ALU.add), reads=[prk, run], writes=[RK])
                    k.op('dve', lambda e: e.tensor_tensor(run[:], run[:], ptt[:], ALU.add), reads=[run, ptt], writes=[run])
                k.op('dve', lambda e: e.tensor_tensor(RK[:], RK[:], pst[:].unsqueeze(1).to_broadcast([128, NT, 32]), ALU.add), reads=[RK, pst], writes=[RK])
                for j, OH in enumerate((OH1, OH2)):
                    k.op('dve', lambda e: e.tensor_tensor(OH[:], OH[:], RK[:], ALU.mult), reads=[OH, RK], writes=[OH])
                    k.op('dve', lambda e: e.tensor_reduce(dsf[:, :, j], OH[:], AX.X, ALU.add), reads=[OH], writes=[dsf])
                k.op('dve', lambda e: e.tensor_copy(DST[:], dsf[:]), reads=[dsf], writes=[DST])
                k.op('dve', lambda e: e.tensor_tensor(bigb[:], pend[:].unsqueeze(1).to_broadcast([128, NBLK, 32]), blki[:].unsqueeze(2).to_broadcast([128, NBLK, 32]), ALU.is_le),
                     reads=[pend, blki], writes=[bigb])
                k.op('dve', lambda e: e.tensor_reduce(bexp[:], bigb[:], AX.X, ALU.add), reads=[bigb], writes=[bexp])
                k.op('dve', lambda e: e.tensor_scalar(bexp[:], bexp[:], 31.0, None, ALU.min), reads=[bexp], writes=[bexp])
                k.op('dve', lambda e: e.tensor_scalar(widxf[:, :, 0:8], bexp[:].unsqueeze(2).to_broadcast([128, NBLK, 8]), 256.0, None, ALU.mult), reads=[bexp], writes=[widxf])
                k.op('dve', lambda e: e.tensor_scalar(widxf[:, :, 8:12], bexp[:].unsqueeze(2).to_broadcast([128, NBLK, 4]), 256.0, None, ALU.mult), reads=[bexp], writes=[widxf])
                k.op('dve', lambda e: e.tensor_tensor(widxf[:], widxf[:], kcp[:].unsqueeze(1).to_broadcast([128, NBLK, 12]), ALU.add), reads=[widxf, kcp], writes=[widxf])
                k.op('dve', lambda e: e.tensor_copy(WIDX[:], widxf[:]), reads=[widxf], writes=[WIDX])
                for i in range(NT):
                    for j in range(2):
                        k.dma('pool', TOKB[:, :], tokid[:, i, :], reads=[tokid, DST], pw=[TOKB],
                              indirect=(bass.IndirectOffsetOnAxis(ap=DST[:, i, j:j + 1], axis=0), None))
            k.barrier()
            with ExitStack() as es7:
                k.es = es7
                if P4S < 3:
                    raise _Stop()
                wg = [k.sb("wg%d" % i, [128, 8, 512], BF16) for i in range(3)]
                wu = [k.sb("wu%d" % i, [128, 8, 512], BF16) for i in range(3)]
                wd = [k.sb("wd%d" % i, [128, 4, D], BF16) for i in range(3)]
                class _E:
                    pass
                ES = []
                NES = 4
                for si in range(NES):
                    E = _E()
                    E.tki = k.sb("tki", [128, 16], I32)
                    E.xg = k.sb("xg", [128, D], BF16)
                    E.xgT = k.sb("xgT", [128, 8, 128], BF16)
                    E.gs = k.sb("gs", [128, 512], F32)
                    E.hb = k.sb("hb", [128, 512], BF16)
                    E.hbT = k.sb("hbT", [128, 4, 128], BF16)
                    E.yb = k.sb("yb", [128, D], F32)
                    E.bG = k.ps("p6g", [128, 512])
                    E.bU = k.ps("p6u", [128, 512])
                    E.bY = E.bG
                    ES.append(E)

                def load_w(bk):
                    q = bk % 3
                    for hf in range(2):
                        ix = bass.IndirectOffsetOnAxis(ap=WIDX[:, bk, hf:hf + 1], axis=0)
                        k.dma('pool', wg[q][:, hf * 4:(hf + 1) * 4, :].rearrange("p a b -> p (a b)"), ex_gate[:, :], reads=[WIDX], pw=[wg[q]], indirect=(None, ix))
                        k.dma('pool', wu[q][:, hf * 4:(hf + 1) * 4, :].rearrange("p a b -> p (a b)"), ex_up[:, :], reads=[WIDX], pw=[wu[q]], indirect=(None, ix))
                        k.dma('pool', wd[q][:, hf * 2:(hf + 1) * 2, :].rearrange("p a b -> p (a b)"), ex_down[:, :], reads=[WIDX], pw=[wd[q]], indirect=(None, ix))

                def subtile(E, bk, sub):
                    q = bk % 3
                    r0 = (bk * SUB + sub) * 128
                    k.dma('sp', E.tki[:], TOKB[r0:r0 + 128, :], reads=[TOKB], writes=[E.tki])
                    k.dma('pool', E.xg[:], H2[:, :], reads=[H2, E.tki], writes=[E.xg],
                          indirect=(None, bass.IndirectOffsetOnAxis(ap=E.tki[:, 0:1], axis=0)))
                    yield
                    pxt = E.bU[:].bitcast(BF16)
                    xv = E.xg[:].rearrange("p (j kc) -> p kc j", kc=8)
                    for kc in range(8):
                        k.op('pe', lambda e: e.transpose(pxt[:, kc * 128:(kc + 1) * 128], xv[:, kc, :], identb[:]), reads=[E.xg, identb], writes=[E.bU] if kc == 0 else [], inc=(kc == 7))
                    k.op('act', lambda e: e.activation(E.xgT[:].rearrange("p a b -> p (a b)"), pxt, AF.Identity), reads=[E.bU], writes=[E.xgT])
                    yield
                    for kc in range(8):
                        k.op('pe', lambda e: e.matmul(E.bG[:], E.xgT[:, kc, :], wg[q][:, kc, :], start=(kc == 0), stop=(kc == 7)), reads=[E.xgT, wg[q]], writes=[E.bG] if kc == 0 else [], inc=(kc == 7))
                    for kc in range(8):
                        k.op('pe', lambda e: e.matmul(E.bU[:], E.xgT[:, kc, :], wu[q][:, kc, :], start=(kc == 0), stop=(kc == 7)), reads=[E.xgT, wu[q]], writes=[E.bU] if kc == 0 else [], inc=(kc == 7))
                    yield
                    k.op('act', lambda e: e.activation(E.gs[:], E.bG[:], AF.Silu), reads=[E.bG], writes=[E.gs])
                    k.op('dve', lambda e: e.tensor_tensor(E.hb[:], E.gs[:], E.bU[:], ALU.mult), reads=[E.gs, E.bU], writes=[E.hb])
                    yield
                    pht = E.bG[:].bitcast(BF16)
                    hv = E.hb[:].rearrange("p (j fc) -> p fc j", fc=4)
                    for fc in range(4):
                        k.op('pe', lambda e: e.transpose(pht[:, fc * 128:(fc + 1) * 128], hv[:, fc, :], identb[:]), reads=[E.hb, identb], writes=[E.bG] if fc == 0 else [], inc=(fc == 3))
                    k.op('dve', lambda e: e.tensor_copy(E.hbT[:].rearrange("p a b -> p (a b)"), pht[:, 0:512]), reads=[E.bG], writes=[E.hbT])
                    yield
                    for n in range(2):
                        for fc in range(4):
                            k.op('pe', lambda e: e.matmul(E.bY[:], E.hbT[:, fc, :], wd[q][:, fc, n * 512:(n + 1) * 512], start=(fc == 0), stop=(fc == 3)),
                                 reads=[E.hbT, wd[q]], writes=[E.bY] if fc == 0 else [], inc=(fc == 3))
                        k.op('act', lambda e: e.activation(E.yb[:, n * 512:(n + 1) * 512], E.bY[:], AF.Identity), reads=[E.bY], writes=[E.yb])
                        yield
                    k.dma('sp', YB[r0:r0 + 128, :], E.yb[:], reads=[E.yb], pw=[YB])

                def estream(si):
                    for gsub in range(si, NBLK * SUB, NES):
                        bk, sub = gsub // SUB, gsub % SUB
                        if True:
                            for bb in (bk, bk + 1):
                                if bb < NBLK and bb not in loaded:
                                    loaded.add(bb)
                                    load_w(bb)
                        yield from subtile(ES[si], bk, sub)
                        yield

                loaded = set()
                gens = [estream(i) for i in range(NES)]
                alive = [True] * NES
                while any(alive):
                    for gi in range(NES):
                        if alive[gi]:
                            try:
                                next(gens[gi])
                            except StopIteration:
                                alive[gi] = False
            k.barrier()
            with ExitStack() as es8:
                k.es = es8
                if P4S < 4:
                    raise _Stop()
                class _C:
                    pass
                CS = []
                for si in range(2):
                    C = _C()
                    C.x6 = k.sb("x6", [128, D], F32)
                    C.y0 = k.sb("y0", [128, D], F32)
                    C.y1 = k.sb("y1", [128, D], F32)
                    C.o = k.sb("o6", [128, D], F32)
                    C.st6 = k.sb("st6", [128, 2, 6], F32)
                    C.mv6 = k.sb("mv6", [128, 2], F32)
                    C.rs6 = k.sb("rs6", [128, 1], F32)
                    C.nb6 = k.sb("nb6", [128, 1], F32)
                    CS.append(C)

                def ctile(C, gi):
                    b, i = gi // (SEQ // 128), gi % (SEQ // 128)
                    x6, y0, y1, o, st6, mv6, rs6, nb6 = C.x6, C.y0, C.y1, C.o, C.st6, C.mv6, C.rs6, C.nb6
                    k.dma('sp', x6[:], X1[gi * 128:(gi + 1) * 128, :], reads=[X1], writes=[x6])
                    k.dma('pool', y0[:], YB[:, :], reads=[YB, DST], writes=[y0],
                          indirect=(None, bass.IndirectOffsetOnAxis(ap=DST[:, gi, 0:1], axis=0)))
                    k.dma('pool', y1[:], YB[:, :], reads=[YB, DST], writes=[y1],
                          indirect=(None, bass.IndirectOffsetOnAxis(ap=DST[:, gi, 1:2], axis=0)))
                    yield
                    k.op('act', lambda e: e.activation(y0[:], y0[:], AF.Identity, scale=W1[:, gi:gi + 1]), reads=[y0, W1], writes=[y0])
                    k.op('dve', lambda e: e.scalar_tensor_tensor(y0[:], y1[:], W2[:, gi:gi + 1], y0[:], ALU.mult, ALU.add), reads=[y1, W2, y0], writes=[y0])
                    yield
                    k.op('dve', lambda e: e.tensor_tensor(y0[:], y0[:], g2b[:, b, :], ALU.mult), reads=[y0, g2b], writes=[y0])
                    k.op('dve', lambda e: e.scalar_tensor_tensor(o[:], x6[:], ALPHA, y0[:], ALU.mult, ALU.add), reads=[x6, y0], writes=[o])
                    yield
                    for hf in range(2):
                        k.op('dve', lambda e: e.bn_stats(st6[:, hf, :], o[:, hf * 512:(hf + 1) * 512]), reads=[o], writes=[st6])
                    k.op('dve', lambda e: e.bn_aggr(mv6[:], st6[:].rearrange("p a b -> p (a b)")), reads=[st6], writes=[mv6])
                    k.op('act', lambda e: e.activation(rs6[:], mv6[:, 1:2], AF.Sqrt, bias=LN_EPS), reads=[mv6], writes=[rs6])
                    k.op('dve', lambda e: e.reciprocal(rs6[:], rs6[:]), reads=[rs6], writes=[rs6])
                    k.op('dve', lambda e: e.scalar_tensor_tensor(nb6[:], mv6[:, 0:1], -1.0, rs6[:], ALU.mult, ALU.mult), reads=[mv6, rs6], writes=[nb6])
                    yield
                    k.op('act', lambda e: e.activation(o[:], o[:], AF.Identity, bias=nb6[:, 0:1], scale=rs6[:, 0:1]), reads=[o, nb6, rs6], writes=[o])
                    yield
                    k.op('dve', lambda e: e.tensor_tensor(o[:], o[:], lnp[:, 2, :], ALU.mult), reads=[o, lnp], writes=[o])
                    k.op('dve', lambda e: e.tensor_tensor(o[:], o[:], lnp[:, 3, :], ALU.add), reads=[o, lnp], writes=[o])
                    k.dma('sp', out_d[b, i * 128:(i + 1) * 128, :], o[:], reads=[o])

                def cstream(si):
                    for gi in range(si, NT, 2):
                        yield from ctile(CS[si], gi)
                        yield
                gens = [cstream(0), cstream(1)]
                alive = [True, True]
                while any(alive):
                    for gq in range(2):
                        if alive[gq]:
                            try:
                                next(gens[gq])
                            except StopIteration:
                                alive[gq] = False
           except _Stop:
            pass
        k.barrier()

        k.barrier()
    return nc


def host_inputs(inputs, batches, NB):
    f = lambda a: np.ascontiguousarray(a, dtype=np.float32)
    bs = list(batches)
    m = {}
    m["x"] = f(inputs["x"][bs])
    m["ctx"] = f(inputs["ctx"][bs])
    cc = np.zeros((3, D), np.float32)
    for i, b in enumerate(bs):
        cc[i] = inputs["c"][b]
    cc[2] = inputs["c_ctx"]
    m["cc"] = cc
    m["w_ada"] = f(inputs["w_ada"][0])
    m["b_ada"] = f(inputs["b_ada"][0][None, :])
    m["w_in"] = f(inputs["w_in"][0])
    m["conv_w"] = f(inputs["conv_w"][0].reshape(9, 2560))
    bi, bf = inputs["m_bias_i"][0], inputs["m_bias_f"][0]
    m["m_bias"] = f(np.concatenate([bi[0], bf[0], bi[1], bf[1]])[:, None])
    m["ident"] = np.eye(128, dtype=np.float32)
    gm = np.zeros((32, 2), np.float32)
    gm[0:8, 0] = 1; gm[16:24, 0] = 1; gm[8:16, 1] = -1; gm[24:32, 1] = -1
    m["gmask"] = gm
    ii = np.arange(64)
    m["cmask"] = np.stack([(ii[:, None] <= ii[None, :]), (ii[:, None] >= ii[None, :])], axis=1).astype(np.float32)
    m["m_norm_w"] = f(inputs["m_norm_w"][0][None, :])
    hk = lambda v: np.asarray(v, np.float32).reshape(4, 128).T
    m["rp"] = f(np.stack([hk(inputs["r_w0"][0][0]), hk(inputs["r_w0"][0][1]), hk(inputs["r_a0"][0]), hk(inputs["r_kk"][0]),
                          hk(inputs["r_ka"][0]), hk(inputs["r_ka"][0]), hk(inputs["r_bonus"][0].reshape(-1))], axis=2))
    sm = np.ones((128, 1088), np.float32); sm[:, ::64] = 0
    obd = np.zeros((128, 128), np.float32); obd[:64, :64] = 1; obd[64:, 64:] = 1
    m["onesbd"] = obd
    m["smask"] = sm
    jj = np.arange(128) % 64
    tt = np.arange(128)
    rm = np.zeros((128, 2, 128), np.float32)
    for dd in range(2):
        for col in range(128):
            tq = col % 64
            if col < 64:
                rm[:, dd, col] = (jj < tq) if dd == 0 else (jj > tq)
            else:
                rm[:, dd, col] = (jj <= tq) if dd == 0 else (jj >= tq)
    m["rmask"] = rm
    m["nmask"] = np.stack([(ii[None, :] < ii[:, None]), (ii[None, :] > ii[:, None])], axis=1).astype(np.float32)
    m["r_norm_w"] = f(inputs["r_norm_w"][0][None, :])
    m["r_norm_b"] = f(inputs["r_norm_b"][0][None, :])
    m["r_wB"] = f(inputs["r_wB"][0])
    m["r_aB"] = f(inputs["r_aB"][0])
    m["r_gB"] = f(inputs["r_gB"][0])
    m["w_out"] = f(inputs["w_out"][0])
    for nm in ("ln1_g", "ln1_b", "ln2_g", "ln2_b"):
        m[nm] = f(inputs[nm][0][None, :])
    m["rt"] = f(np.concatenate([inputs["rt_g"][0], inputs["rt_e"][0]], axis=1))
    m["rtb"] = f(np.concatenate([inputs["rt_g_b"][0], inputs["rt_e_b"][0]])[None, :])
    m["ex_gate"] = f(inputs["ex_gate"][0].reshape(8192, 2048))
    m["ex_up"] = f(inputs["ex_up"][0].reshape(8192, 2048))
    m["ex_down"] = f(inputs["ex_down"][0].reshape(8192, 2048))
    pp = np.arange(128)
    m["tris"] = (pp[:, None] < pp[None, :]).astype(np.float32)
    m["thr"] = np.broadcast_to((512.0 * pp)[None, :], (128, 128)).astype(np.float32).copy()
    m["blki"] = np.broadcast_to(np.arange(160, dtype=np.float32)[None, :], (128, 160)).copy()
    m["kcp"] = (2 * pp[:, None] + (np.arange(12) % 2)[None, :]).astype(np.float32)
    m["tokid"] = np.broadcast_to((np.arange(64)[None, :] * 128 + pp[:, None])[:, :, None], (128, 64, 16)).astype(np.int32).copy()
    return m


_NC_CACHE = {}


def kernel(**inputs):
    inputs = {k_: np.asarray(v) for k_, v in inputs.items()}
    NB = 2
    n_cores = 8
    if NB not in _NC_CACHE:
        _NC_CACHE[NB] = build(NB=NB)
    nc = _NC_CACHE[NB]
    in_maps = [host_inputs(inputs, [NB * c + j for j in range(NB)], NB) for c in range(n_cores)]
    res = run_bass_kernel_spmd(nc, in_maps, core_ids=list(range(n_cores)))
    out = np.concatenate([np.asarray(r["out"]) for r in res.results], axis=0)
    return np.ascontiguousarray(out, dtype=np.float32)
```

```python
import math, os
from contextlib import ExitStack
import numpy as np
import concourse.bass as bass
import concourse.mybir as mybir
from concourse.bass_utils import run_bass_kernel_spmd

F32 = mybir.dt.float32
BF16 = mybir.dt.bfloat16
I32 = mybir.dt.int32
AF = mybir.ActivationFunctionType
ALU = mybir.AluOpType
AX = mybir.AxisListType

D = 1024
SEQ = 4096
CTX = 256
T = SEQ + CTX
NCH = T // 64
INC = 3936
DS = math.exp(-0.5)
ALPHA = 2.0 ** 0.25
LN_EPS = 1e-6
GN_EPS = 64e-5
SEC = dict(mq=0, mk=512, rr=1024, rk=1536, rv=2048, mv=2560, mo=3072, gates=3584,
           lwf=3616, lwb=3680, la=3744, lg=3808)
NDS = 40


class _Stop(Exception):
    pass


class Buf:
    def __init__(self, t):
        self.t = t
        self.w = {}
        self.r = {}

    def __getitem__(self, k):
        return self.t[k]


def _merge(d, s):
    for k, v in s.items():
        if d.get(k, 0) < v:
            d[k] = v


class KB:
    def __init__(self, nc):
        self.nc = nc
        self.engs = {'pe': nc.tensor, 'dve': nc.vector, 'act': nc.scalar, 'pool': nc.gpsimd, 'sp': nc.sync}
        self.esem = {e: nc.alloc_semaphore('es_' + e) for e in self.engs}
        self.ecnt = {e: 0 for e in self.engs}
        self.pending = {e: False for e in self.engs}
        self.seen = {e: {} for e in self.engs}
        self.dsem = [nc.alloc_semaphore('ds%d' % i) for i in range(NDS)]
        self.dcnt = [0] * NDS
        self.dnext = 0
        self.es = None
        self.uid = 0

    def semh(self, key):
        return self.esem[key] if isinstance(key, str) else self.dsem[key[1]]

    def _wait(self, eng, need):
        for key, cnt in need.items():
            if self.seen[eng].get(key, 0) >= cnt:
                continue
            if key == eng and eng in ('pe',):
                continue
            self.engs[eng].wait_ge(self.semh(key), cnt)
            self.seen[eng][key] = cnt

    def op(self, eng, fn, reads=(), writes=(), inc=True, pw_=()):
        need = {}
        for b in pw_:
            _merge(need, b.r)
        for b in reads:
            _merge(need, b.w)
            if getattr(b, 'excl', False):
                _merge(need, {kk: vv for kk, vv in b.r.items() if kk != eng})
        for b in writes:
            _merge(need, b.w)
            _merge(need, b.r)
        self._wait(eng, need)
        ins = fn(self.engs[eng])
        cnt = self.ecnt[eng] + 1
        if inc:
            ins.then_inc(self.esem[eng], 1)
            self.ecnt[eng] = cnt
        for b in reads:
            b.r[eng] = cnt
        for b in writes:
            b.w = {eng: cnt}
            b.r = {}
        for b in pw_:
            b.w[eng] = cnt
        return ins

    def dma(self, q, out, in_, reads=(), writes=(), pw=(), indirect=None, **kw):
        i = self.dnext
        self.dnext = (i + 1) % NDS
        need = {}
        if self.dcnt[i]:
            need[('d', i)] = self.dcnt[i]
        for b in reads:
            _merge(need, b.w)
        for b in writes:
            _merge(need, b.w)
            _merge(need, b.r)
        for b in pw:
            _merge(need, b.r)
        self._wait(q, need)
        if indirect is None:
            ins = self.engs[q].dma_start(out=out, in_=in_, **kw)
        else:
            ins = self.engs[q].indirect_dma_start(out, indirect[0], in_, indirect[1], **kw)
        self.dcnt[i] += 16
        ins.then_inc(self.dsem[i], 16)
        key = ('d', i)
        cnt = self.dcnt[i]
        for b in reads:
            b.r[key] = cnt
        for b in writes:
            b.w = {key: cnt}
            b.r = {}
        for b in pw:
            b.w[key] = cnt
        return ins

    def barrier(self):
        need = {e: c for e, c in self.ecnt.items() if c}
        for i in range(NDS):
            if self.dcnt[i]:
                need[('d', i)] = self.dcnt[i]
        for e in self.engs:
            self._wait(e, need)

    def sb(self, name, shape, dt):
        self.uid += 1
        return Buf(self.es.enter_context(self.nc.sbuf_tensor("s%d_%s" % (self.uid, name), list(shape), dt)))

    def ps(self, name, shape, dt=F32):
        self.uid += 1
        return Buf(self.es.enter_context(self.nc.psum_tensor("p%d_%s" % (self.uid, name), list(shape), dt)))


def build(NB=2, debug=None, phases=(0, 1, 2, 3, 4, 5, 6)):
    nc = bass.Bass("TRN2", target_bir_lowering=False)
    k = KB(nc)

    def din(name, shape):
        return nc.dram_tensor(name, list(shape), F32, kind="ExternalInput").ap()

    x_in = din("x", [NB, SEQ, D])
    ctx_in = din("ctx", [NB, CTX, D])
    cc_in = din("cc", [3, D])
    w_ada = din("w_ada", [D, 6 * D])
    b_ada = din("b_ada", [1, 6 * D])
    w_in = din("w_in", [D, INC])
    conv_w = din("conv_w", [9, 2560])
    m_bias = din("m_bias", [32, 1])
    ident_in = din("ident", [128, 128])
    gmask_in = din("gmask", [32, 2])
    cmask_in = din("cmask", [64, 2, 64])
    m_norm_w = din("m_norm_w", [1, 512])
    rp_in = din("rp", [128, 4, 7])
    smask_in = din("smask", [128, 1088])
    onesbd_in = din("onesbd", [128, 128])
    rmask_in = din("rmask", [128, 2, 128])
    nmask_in = din("nmask", [64, 2, 64])
    r_norm_w = din("r_norm_w", [1, 512])
    r_norm_b = din("r_norm_b", [1, 512])
    r_wB = din("r_wB", [2, 64, 512])
    r_aB = din("r_aB", [64, 512])
    r_gB = din("r_gB", [128, 512])
    w_out = din("w_out", [D, D])
    ln1_g = din("ln1_g", [1, D]); ln1_b = din("ln1_b", [1, D]); ln2_g = din("ln2_g", [1, D]); ln2_b = din("ln2_b", [1, D])
    rt_in = din("rt", [D, 36]); rtb_in = din("rtb", [1, 36])
    ex_gate = din("ex_gate", [8192, 2048]); ex_up = din("ex_up", [8192, 2048]); ex_down = din("ex_down", [8192, 2048])
    tris_in = din("tris", [128, 128]); thr_in = din("thr", [128, 128]); blki_in = din("blki", [128, 160]); kcp_in = din("kcp", [128, 12])
    tokid_in = nc.dram_tensor("tokid", [128, 64, 16], I32, kind="ExternalInput").ap()
    out_d = nc.dram_tensor("out", [NB, SEQ, D], F32, kind="ExternalOutput").ap()

    def dscr(name, shape, dt=F32):
        kind = "ExternalOutput" if (debug and name in debug) else "Internal"
        return Buf(nc.dram_tensor(name, list(shape), dt, kind=kind).ap())

    MODD = dscr("MODD", [3, 6 * D])
    FM = [dscr("FM%d" % b, [INC, T]) for b in range(NB)]
    MIX = [dscr("MIX%d" % b, [SEQ, D]) for b in range(NB)]
    BS = 512
    NBLK_ = NB * SEQ * 2 // BS + 32
    X1 = dscr("X1", [NB * SEQ, D])
    H2 = dscr("H2", [NB * SEQ, D], BF16)
    TOKB = dscr("TOKB", [NBLK_ * BS, 16], I32)
    YB = dscr("YB", [NBLK_ * BS, D])

    with ExitStack() as es0:
        k.es = es0
        ident = k.sb("ident", [128, 128], F32)
        identb = k.sb("identb", [128, 128], BF16)
        ones_f = k.sb("ones_f", [128, 128], F32)
        modT = k.sb("modT", [128, 48, 3], F32)
        k.dma('sp', ident[:], ident_in[:, :], writes=[ident])
        k.op('dve', lambda e: e.tensor_copy(identb[:], ident[:]), reads=[ident], writes=[identb])

        with ExitStack() as es:
            k.es = es
            cc = k.sb("cc", [3, D], F32)
            scT = k.sb("scT", [128, 8, 3], F32)
            bada = k.sb("bada", [3, 6 * D], F32)
            mods = k.sb("mods", [3, 6 * D], F32)
            wa = [k.sb("wa%d" % i, [128, 8, 512], F32) for i in range(2)]
            pst = k.ps("p0t", [128, 8, 3])
            psm = [k.ps("p0m%d" % i, [3, 512]) for i in range(2)]
            pmt = k.ps("p0mt", [128, 48, 3])
            k.dma('sp', cc[:], cc_in[:, :], writes=[cc])
            k.dma('sp', bada[:], b_ada[0:1, :].partition_broadcast(3), writes=[bada])
            k.op('act', lambda e: e.activation(cc[:], cc[:], AF.Silu), reads=[cc], writes=[cc])
            for kc in range(8):
                k.op('pe', lambda e: e.transpose(pst[:, kc, :], cc[:, kc * 128:(kc + 1) * 128], ident[0:3, 0:3]),
                     reads=[cc, ident], writes=[pst], inc=(kc == 7))
            k.op('dve', lambda e: e.tensor_copy(scT[:], pst[:]), reads=[pst], writes=[scT])
            for n in range(12):
                wb = wa[n % 2]
                k.dma('sp', wb[:], w_ada[:, n * 512:(n + 1) * 512].rearrange("(kc p) n -> p kc n", p=128), writes=[wb])
                pm = psm[n % 2]
                for kc in range(8):
                    k.op('pe', lambda e: e.matmul(pm[:], scT[:, kc, :], wb[:, kc, :], start=(kc == 0), stop=(kc == 7)),
                         reads=[scT, wb], writes=[pm] if kc == 0 else [], inc=(kc == 7))
                k.op('dve', lambda e: e.tensor_tensor(mods[:, n * 512:(n + 1) * 512], pm[:], bada[:, n * 512:(n + 1) * 512], ALU.add),
                     reads=[pm, bada], writes=[mods])
            k.dma('sp', MODD[:, :], mods[:], reads=[mods], writes=[MODD])
            for j in range(48):
                k.op('pe', lambda e: e.transpose(pmt[:, j, :], mods[:, j * 128:(j + 1) * 128], ident[0:3, 0:3]),
                     reads=[mods, ident], writes=[pmt], inc=(j == 47))
            k.op('dve', lambda e: e.tensor_copy(modT[:], pmt[:]), reads=[pmt], writes=[modT])
            for j0 in (8, 32):
                k.op('dve', lambda e: e.tensor_scalar(modT[:, j0:j0 + 8, :], modT[:, j0:j0 + 8, :], 1.0, None, ALU.add),
                     reads=[modT], writes=[modT])
        k.barrier()

        with ExitStack() as es:
          if 1 in phases:
              k.es = es
              wbf = k.sb("wbf", [128, 8, INC], BF16)
              hT = k.sb("hT", [128, 8, T], BF16)
              xt = [k.sb("xt%d" % i, [128, D], F32) for i in range(2)]
              xn = [k.sb("xn%d" % i, [128, D], BF16) for i in range(2)]
              st = k.sb("st", [128, 2, 6], F32)
              mv = k.sb("mv", [128, 2], F32)
              rstd = k.sb("rstd", [128, 1], F32)
              pT = [k.sb("pT%d" % i, [128, T], F32) for i in range(2)]
              acc = k.sb("acc", [128, T], F32)
              cw = k.sb("cw", [128, 20, 9], F32)
              mb = k.sb("mb", [32, 4], F32)
              ptr = [k.ps("p1t%d" % i, [128, 8, 128], BF16) for i in range(2)]
              pmm = [k.ps("p1m%d" % i, [128, 512]) for i in range(3)]
              pcw = k.ps("p1cw", [128, 20, 9])
              for kc in range(8):
                  for hf in range(2):
                      k.dma('pool', wbf[:, kc, hf * 1968:(hf + 1) * 1968],
                            w_in[kc * 128:(kc + 1) * 128, hf * 1968:(hf + 1) * 1968], pw=[wbf])
              crow = k.sb("crow", [9, 2560], F32)
              k.dma('sp', crow[:], conv_w[:, :], writes=[crow])
              for c in range(20):
                  k.op('pe', lambda e: e.transpose(pcw[:, c, :], crow[:, c * 128:(c + 1) * 128], ident[0:9, 0:9]),
                       reads=[crow, ident], writes=[pcw], inc=(c == 19))
              k.op('dve', lambda e: e.tensor_copy(cw[:], pcw[:]), reads=[pcw], writes=[cw])
              k.dma('sp', mb[:, 0:1], m_bias[:, :], writes=[mb])
              k.op('dve', lambda e: e.tensor_scalar(mb[:, 1:2], mb[:, 0:1], -1.0, None, ALU.mult), reads=[mb], writes=[mb])
              k.dma('sp', mb[:, 2:4], gmask_in[:, :], pw=[mb])
              for b in range(NB):
                  for i in range(int(os.environ.get('P1A', T // 128))):
                      xb, xnb, pt = xt[i % 2], xn[i % 2], ptr[i % 2]
                      src = ctx_in[b, i * 128:(i + 1) * 128, :] if i < 2 else x_in[b, (i - 2) * 128:(i - 1) * 128, :]
                      r = 2 if i < 2 else b
                      k.dma('sp', xb[:], src, writes=[xb])
                      S1 = int(os.environ.get('P1S', 9))
                      for hf in range(2):
                          k.op('dve', lambda e: e.bn_stats(st[:, hf, :], xb[:, hf * 512:(hf + 1) * 512]), reads=[xb], writes=[st])
                      if S1 >= 2: k.op('dve', lambda e: e.bn_aggr(mv[:], st[:].rearrange("p a b -> p (a b)")), reads=[st], writes=[mv])
                      if S1 >= 3: k.op('act', lambda e: e.activation(rstd[:], mv[:, 1:2], AF.Sqrt, bias=LN_EPS), reads=[mv], writes=[rstd])
                      if S1 >= 4: k.op('dve', lambda e: e.reciprocal(rstd[:], rstd[:]), reads=[rstd], writes=[rstd])
                      if S1 >= 5: k.op('dve', lambda e: e.tensor_scalar(xnb[:], xb[:], mv[:, 0:1], rstd[:, 0:1], ALU.subtract, ALU.mult),
                           reads=[xb, mv, rstd], writes=[xnb])
                      for kc in range(8 if S1 >= 6 else 0):
                          k.op('pe', lambda e: e.transpose(pt[:, kc, :], xnb[:, kc * 128:(kc + 1) * 128], identb[:]),
                               reads=[xnb, identb], writes=[pt] if kc == 0 else [], inc=(kc == 7))
                      for kc in range(8 if S1 >= 7 else 0):
                          if i % 2 == 0:
                              k.op('act', lambda e: e.activation(hT[:, kc, i * 128:(i + 1) * 128], pt[:, kc, :], AF.Identity,
                                                                 bias=modT[:, kc, r:r + 1], scale=modT[:, 8 + kc, r:r + 1]),
                                   reads=[pt, modT], writes=[] if (i or kc) else [hT])
                          else:
                              k.op('dve', lambda e: e.tensor_scalar(hT[:, kc, i * 128:(i + 1) * 128], pt[:, kc, :],
                                                                    modT[:, 8 + kc, r:r + 1], modT[:, kc, r:r + 1], ALU.mult, ALU.add),
                                   reads=[pt, modT], writes=[])
                      hT.w['act'] = k.ecnt['act']
                      hT.w['dve'] = k.ecnt['dve']
                  chunks = [(c * 128, 128) for c in range(28)] + [(3584, 32), (3616, 64), (3680, 64), (3744, 64), (3808, 128)]
                  for ci, (c0, M) in enumerate(chunks[:int(os.environ.get('P1C', 99))]):
                      pb = pT[ci % 2]
                      for g in range(9):
                          t0 = g * 512
                          n = min(512, T - t0)
                          pm = pmm[(ci * 9 + g) % 3]
                          for kc in range(8):
                              k.op('pe', lambda e: e.matmul(pm[0:M, 0:n], wbf[:, kc, c0:c0 + M], hT[:, kc, t0:t0 + n],
                                                            start=(kc == 0), stop=(kc == 7)),
                                   reads=[wbf, hT], writes=[pm] if kc == 0 else [], inc=(kc == 7))
                          k.op('act', lambda e: e.activation(pb[0:M, t0:t0 + n], pm[0:M, 0:n], AF.Identity),
                               reads=[pm], writes=[pb] if g == 0 else [])
                          pb.w['act'] = k.ecnt['act']
                      src = pb
                      if c0 < 2560:
                          c = c0 // 128
                          k.op('act', lambda e: e.activation(acc[:, :], pb[:, :], AF.Identity, scale=cw[:, c, 4:5]),
                               reads=[pb, cw], writes=[acc])
                          k.op('dve', lambda e: e.scalar_tensor_tensor(acc[:, 1:CTX], pb[:, 0:CTX - 1], cw[:, c, 3:4], acc[:, 1:CTX], ALU.mult, ALU.add),
                               reads=[pb, cw, acc], writes=[acc])
                          k.op('dve', lambda e: e.scalar_tensor_tensor(acc[:, 0:CTX - 1], pb[:, 1:CTX], cw[:, c, 5:6], acc[:, 0:CTX - 1], ALU.mult, ALU.add),
                               reads=[pb, cw, acc], writes=[acc])
                          a3 = acc[:, CTX:T].rearrange("p (r c) -> p r c", c=64)
                          p3 = pb[:, CTX:T].rearrange("p (r c) -> p r c", c=64)
                          for ky in range(3):
                              for kx in range(3):
                                  if ky == 1 and kx == 1:
                                      continue
                                  dy, dx = ky - 1, kx - 1
                                  oy0, oy1 = max(0, -dy), 64 - max(0, dy)
                                  ox0, ox1 = max(0, -dx), 64 - max(0, dx)
                                  k.op('dve', lambda e: e.scalar_tensor_tensor(
                                      a3[:, oy0:oy1, ox0:ox1], p3[:, oy0 + dy:oy1 + dy, ox0 + dx:ox1 + dx],
                                      cw[:, c, ky * 3 + kx:ky * 3 + kx + 1], a3[:, oy0:oy1, ox0:ox1], ALU.mult, ALU.add),
                                      reads=[pb, cw, acc], writes=[acc])
                          src = acc
                      sec = [s for s, v in SEC.items() if v <= c0][-1]
                      if sec in ('mq', 'mk'):
                          k.op('act', lambda e: e.activation(acc[:, :], src[:, :], AF.Silu), reads=[src], writes=[acc])
                          if sec == 'mk':
                              k.op('dve', lambda e: e.tensor_scalar(acc[:, :], acc[:, :], 0.125, None, ALU.mult), reads=[acc], writes=[acc])
                          src = acc
                      elif sec in ('mo', 'lg'):
                          k.op('act', lambda e: e.activation(acc[0:M, :], src[0:M, :], AF.Sigmoid), reads=[src], writes=[acc])
                          src = acc
                      elif sec in ('lwf', 'lwb'):
                          k.op('act', lambda e: e.activation(acc[0:M, :], src[0:M, :], AF.Tanh), reads=[src], writes=[acc])
                          src = acc
                      elif sec == 'gates':
                          tmp = pT[1 - ci % 2]
                          k.op('act', lambda e: e.activation(tmp[0:32, :], pb[0:32, :], AF.Exp, bias=mb[:, 1:2], scale=-1.0),
                               reads=[pb, mb], writes=[tmp])
                          k.op('act', lambda e: e.activation(tmp[0:32, :], tmp[0:32, :], AF.Ln, bias=1.0), reads=[tmp], writes=[tmp])
                          k.op('dve', lambda e: e.tensor_scalar(tmp[0:32, :], tmp[0:32, :], mb[:, 3:4], None, ALU.mult), reads=[tmp, mb], writes=[tmp])
                          k.op('dve', lambda e: e.tensor_scalar(acc[0:32, :], pb[0:32, :], mb[:, 0:1], mb[:, 2:3], ALU.add, ALU.mult),
                               reads=[pb, mb], writes=[acc])
                          k.op('dve', lambda e: e.tensor_tensor(acc[0:32, :], acc[0:32, :], tmp[0:32, :], ALU.add), reads=[acc, tmp], writes=[acc])
                          src = acc
                      k.dma('sp', FM[b][c0:c0 + M, :], src[0:M, :], reads=[src], pw=[FM[b]])
        k.barrier()


        with ExitStack() as es:
          if 2 in phases:
            k.es = es
            cm = k.sb("cm", [64, 2, 64], F32)
            k.dma('sp', cm[:], cmask_in[:, :, :], writes=[cm])
            nw = k.sb("nw", [64, 512], F32)
            k.dma('sp', nw[:], m_norm_w[0:1, :].partition_broadcast(64), writes=[nw])
            k.op('pool', lambda e: e.memset(ones_f[:], 1.0), writes=[ones_f])
            GA = k.sb("GA", [64, NCH, 48], F32)
            gT = k.sb("gT", [32, T], F32)
            G = k.sb("G", [64, 32], F32)
            qh = k.sb("qh", [64, 2, T], BF16)
            kh = k.sb("kh", [64, 2, T], BF16)
            vT = k.sb("vT", [128, T], BF16)
            moT = k.sb("moT", [128, T], F32)
            Hf = k.sb("Hf", [64, NCH, 2, 64], F32)
            class _M:
                pass
            MS = []
            for si in range(2):
                M = _M()
                M.i = si
                M.Ktm = k.sb("Ktm", [64, 2, 64], BF16)
                M.Vaug = k.sb("Vaug", [64, 2, 66], BF16)
                M.PTm = k.sb("PTm", [64, 2, 64], BF16)
                M.Cst = k.sb("Cst", [64, 2, 66], F32)
                M.Cbf = k.sb("Cbf", [64, 2, 66], BF16)
                M.dn = k.sb("dn", [64, 2], F32)
                M.ff = k.sb("ff", [64, 2], F32)
                M.hs = k.sb("hs", [64, 2, 64], F32)
                M.st2 = k.sb("st2", [64, 2, 6], F32)
                M.mv2 = k.sb("mv2", [64, 2, 2], F32)
                M.rs2 = k.sb("rs2", [64, 2], F32)
                M.om = k.sb("om", [64, 128], F32)
                M.bA = k.ps("p2A", [64, 512])
                M.bB = k.ps("p2B", [64, 512])
                M.bA.excl = True
                M.bB.excl = True
                MS.append(M)
            pg = k.ps("p2g", [64, 32])
            pbb = k.ps("p2b", [64, 32])
            for b in range(NB):
                k.dma('sp', gT[:], FM[b][3584:3616, :], reads=[FM[b]], writes=[gT])
                for c in range(NCH):
                    k.op('pe', lambda e: e.transpose(pg[:], gT[:, c * 64:(c + 1) * 64], ident[0:32, 0:32]), reads=[gT, ident], writes=[pg])
                    k.op('dve', lambda e: e.tensor_copy(G[:], pg[:]), reads=[pg], writes=[G])
                    k.op('pe', lambda e: e.matmul(pbb[:, 0:8], cm[:, 0, :], G[:, 8:16], start=True, stop=True), reads=[cm, G], writes=[pbb], inc=False)
                    k.op('pe', lambda e: e.matmul(pbb[:, 8:16], cm[:, 1, :], G[:, 24:32], start=True, stop=True), reads=[cm, G], inc=False)
                    k.op('pe', lambda e: e.matmul(pbb[:, 16:24], ones_f[0:64, 0:64], G[:, 8:16], start=True, stop=True), reads=[ones_f, G], inc=False)
                    k.op('pe', lambda e: e.matmul(pbb[:, 24:32], ones_f[0:64, 0:64], G[:, 24:32], start=True, stop=True), reads=[ones_f, G])
                    k.op('act', lambda e: e.activation(GA[:, c, 0:32], pbb[:], AF.Exp), reads=[pbb], writes=[GA])
                    k.op('dve', lambda e: e.tensor_tensor(G[:, 0:8], G[:, 0:8], pbb[:, 0:8], ALU.subtract), reads=[pbb, G], writes=[G])
                    k.op('dve', lambda e: e.tensor_tensor(G[:, 16:24], G[:, 16:24], pbb[:, 8:16], ALU.subtract), reads=[pbb, G], writes=[G])
                    k.op('act', lambda e: e.activation(GA[:, c, 32:40], G[:, 0:8], AF.Exp), reads=[G], writes=[GA])
                    k.op('act', lambda e: e.activation(GA[:, c, 40:48], G[:, 16:24], AF.Exp), reads=[G], writes=[GA])
                for hp in range(4):
                    for h in range(2):
                        r0 = hp * 128 + h * 64
                        for q4 in range(4):
                            t0 = q4 * 1088
                            k.dma('pool', qh[:, h, t0:t0 + 1088], FM[b][r0:r0 + 64, t0:t0 + 1088], reads=[FM[b]], pw=[qh])
                            k.dma('pool', kh[:, h, t0:t0 + 1088], FM[b][512 + r0:512 + r0 + 64, t0:t0 + 1088], reads=[FM[b]], pw=[kh])
                    for q4 in range(4):
                        t0 = q4 * 1088
                        k.dma('pool', vT[:, t0:t0 + 1088], FM[b][2560 + hp * 128:2560 + (hp + 1) * 128, t0:t0 + 1088], reads=[FM[b]], pw=[vT])
                    k.dma('sp', moT[:], FM[b][3072 + hp * 128:3072 + (hp + 1) * 128, :], reads=[FM[b]], writes=[moT])
                    def mstream(M, d, done):
                        Ktm, Vaug, PTm, Cst, Cbf, dn, ff, hs, st2, mv2, rs2, om = M.Ktm, M.Vaug, M.PTm, M.Cst, M.Cbf, M.dn, M.ff, M.hs, M.st2, M.mv2, M.rs2, M.om
                        bA, bB = M.bA, M.bB
                        pk = bA[:, 0:64].bitcast(BF16).rearrange("p (h e) -> p h e", h=2)
                        pv = bA[:, 64:128].bitcast(BF16)
                        pp = bA[:, 128:256].rearrange("p (h e) -> p h e", h=2)
                        po = bB[:, 0:132].rearrange("p (h e) -> p h e", h=2)
                        pc = bB[:, 132:264].rearrange("p (h e) -> p h e", h=2)
                        pmo = bB[:, 264:392]
                        order = list(range(NCH)) if d == 0 else [3, 2, 1, 0] + list(range(NCH - 1, 3, -1))
                        k.op('pool', lambda e: e.memset(Cst[:], 0.0), writes=[Cst])
                        k.op('pool', lambda e: e.memset(Cbf[:], 0.0), writes=[Cbf])
                        for c in order:
                            cs = slice(c * 64, (c + 1) * 64)
                            hh = 2 * hp
                            a_ap = GA[:, c, d * 8 + hh:d * 8 + hh + 2]
                            e_ap = GA[:, c, 16 + d * 8 + hh:16 + d * 8 + hh + 2]
                            c_ap = GA[:, c, 32 + d * 8 + hh:32 + d * 8 + hh + 2]
                            needy = c >= 4
                            second = c in done
                            fin = needy and second
                            for h in range(2):
                                k.op('pe', lambda e: e.transpose(pk[:, h, :], kh[:, h, cs], identb[0:64, 0:64]), reads=[kh, identb], writes=[bA] if h == 0 else [], inc=False)
                            k.op('pe', lambda e: e.transpose(pv, vT[:, cs], identb[:]), reads=[vT, identb], inc=False)
                            for h in range(2):
                                k.op('pe', lambda e: e.matmul(pp[:, h, :], kh[:, h, cs], qh[:, h, cs], start=True, stop=True), reads=[kh, qh], inc=(h == 1))
                            k.op('act', lambda e: e.activation(Ktm[:], pk, AF.Identity), reads=[bA], writes=[Ktm])
                            k.op('dve', lambda e: e.tensor_tensor(Vaug[:, :, 0:64], pv.rearrange("p (h e) -> p h e", h=2),
                                                                  c_ap.unsqueeze(2).to_broadcast([64, 2, 64]), ALU.mult), reads=[bA, GA], writes=[Vaug])
                            k.op('act', lambda e: e.activation(Vaug[:, :, 64:65], c_ap.unsqueeze(2), AF.Identity), reads=[GA, Vaug], writes=[Vaug])
                            k.op('dve', lambda e: e.tensor_tensor(PTm[:], pp, cm[:, d:d + 1, :].to_broadcast([64, 2, 64]), ALU.mult), reads=[bA, cm], writes=[PTm])
                            yield
                            for h in range(2):
                                k.op('pe', lambda e: e.matmul(po[:, h, 0:65], PTm[:, h, :], Vaug[:, h, 0:65], start=True, stop=False), reads=[PTm, Vaug], writes=[bB] if h == 0 else [], inc=False)
                                k.op('pe', lambda e: e.matmul(po[:, h, 0:65], qh[:, h, cs], Cbf[:, h, 0:65], start=False, stop=True), reads=[qh, Cbf], inc=False)
                            if fin:
                                k.op('pe', lambda e: e.transpose(pmo, moT[:, cs], ident[:]), reads=[moT, ident], inc=False)
                            for h in range(2):
                                k.op('pe', lambda e: e.matmul(pc[:, h, 0:65], Ktm[:, h, :], Vaug[:, h, 0:65], start=True, stop=True), reads=[Ktm, Vaug], inc=(h == 1))
                            yield
                            k.op('dve', lambda e: e.tensor_tensor(Cst[:, :, 0:65], Cst[:, :, 0:65], pc[:, :, 0:65], ALU.add), reads=[bB, Cst], writes=[Cst])
                            k.op('dve', lambda e: e.tensor_tensor(Cst[:, :, 0:65], Cst[:, :, 0:65], e_ap.unsqueeze(2).to_broadcast([64, 2, 65]), ALU.mult), reads=[GA, Cst], writes=[Cst])
                            k.op('act', lambda e: e.activation(Cbf[:, :, 0:65], Cst[:, :, 0:65], AF.Identity), reads=[Cst], writes=[Cbf])
                            if needy:
                                yield
                                k.op('dve', lambda e: e.tensor_tensor(dn[:], po[:, :, 64], a_ap, ALU.mult), reads=[bB, GA], writes=[dn])
                                k.op('act', lambda e: e.activation(dn[:], dn[:], AF.Abs), reads=[dn], writes=[dn])
                                k.op('dve', lambda e: e.tensor_scalar(dn[:], dn[:], 1.0, None, ALU.max), reads=[dn], writes=[dn])
                                yield
                                k.op('dve', lambda e: e.reciprocal(dn[:], dn[:]), reads=[dn], writes=[dn])
                                k.op('dve', lambda e: e.tensor_tensor(ff[:], dn[:], a_ap, ALU.mult), reads=[dn, GA], writes=[ff])
                                yield
                                if not second:
                                    k.op('dve', lambda e: e.tensor_tensor(Hf[:, c, :, :], po[:, :, 0:64], ff[:].unsqueeze(2).to_broadcast([64, 2, 64]), ALU.mult), reads=[bB, ff], pw_=[Hf])
                                    done[c] = M.i
                                else:
                                    k.op('dve', lambda e: e.tensor_tensor(hs[:], po[:, :, 0:64], ff[:].unsqueeze(2).to_broadcast([64, 2, 64]), ALU.mult), reads=[bB, ff], writes=[hs])
                                    k.op('dve', lambda e: e.tensor_tensor(hs[:], hs[:], Hf[:, c, :, :], ALU.add), reads=[hs, Hf], writes=[hs])
                            if fin:
                                yield
                                for h in range(2):
                                    k.op('dve', lambda e: e.bn_stats(st2[:, h, :], hs[:, h, :]), reads=[hs], writes=[st2])
                                for h in range(2):
                                    k.op('dve', lambda e: e.bn_aggr(mv2[:, h, :], st2[:, h, :]), reads=[st2], writes=[mv2])
                                yield
                                k.op('act', lambda e: e.activation(rs2[:], mv2[:, :, 1], AF.Sqrt, bias=LN_EPS), reads=[mv2], writes=[rs2])
                                k.op('dve', lambda e: e.reciprocal(rs2[:], rs2[:]), reads=[rs2], writes=[rs2])
                                yield
                                for h in range(2):
                                    k.op('dve', lambda e: e.tensor_scalar(hs[:, h, :], hs[:, h, :], mv2[:, h, 0:1], rs2[:, h:h + 1], ALU.subtract, ALU.mult),
                                         reads=[hs, mv2, rs2], writes=[hs])
                                k.op('dve', lambda e: e.tensor_tensor(om[:], hs[:].rearrange("p h e -> p (h e)"), nw[:, hp * 128:(hp + 1) * 128], ALU.mult),
                                     reads=[hs, nw], writes=[om])
                                k.op('dve', lambda e: e.tensor_tensor(om[:], om[:], pmo, ALU.mult), reads=[om, bB], writes=[om])
                                k.dma('sp', MIX[b][(c - 4) * 64:(c - 3) * 64, hp * 128:(hp + 1) * 128], om[:], reads=[om], pw=[MIX[b]])
                            yield

                    done = {}
                    gens = [mstream(MS[0], 0, done), mstream(MS[1], 1, done)]
                    alive = [True, True]
                    while any(alive):
                        for gi in range(2):
                            if alive[gi]:
                                try:
                                    next(gens[gi])
                                except StopIteration:
                                    alive[gi] = False
        k.barrier()

        with ExitStack() as es:
          if 3 in phases:
            k.es = es
            NBK = 512
            NBC = 8
            rp1 = k.sb("rp1", [128, 4, 8], F32)
            k.dma('sp', rp1[:, :, 0:7], rp_in[:, :, :], writes=[rp1])
            k.op('dve', lambda e: e.tensor_scalar(rp1[:, :, 7], rp1[:, :, 4], -1.0, 1.0, ALU.mult, ALU.add), reads=[rp1], writes=[rp1])
            smask = k.sb("smask", [128, NBK], F32)
            k.dma('sp', smask[:], smask_in[:, 0:NBK], writes=[smask])
            onesbd = k.sb("onesbd", [128, 128], F32)
            k.dma('sp', onesbd[:], onesbd_in[:, :], writes=[onesbd])
            ARs = k.sb("ARs", [128, NBC, 128], BF16)
            ZTs = k.sb("ZTs", [128, NBC, 128], BF16)
            PRs = k.sb("PRs", [128, NBK], BF16)
            GLs = k.sb("GLs", [128, NBC], F32)
            rmask = k.sb("rmask", [128, 2, 128], F32)
            k.dma('sp', rmask[:], rmask_in[:, :, :], writes=[rmask])
            nmask = k.sb("nmask", [64, 2, 64], F32)
            k.dma('sp', nmask[:], nmask_in[:, :, :], writes=[nmask])
            gnw = k.sb("gnw", [64, 2, 512], F32)
            k.dma('sp', gnw[:, 0, :], r_norm_w[0:1, :].partition_broadcast(64), pw=[gnw])
            k.dma('sp', gnw[:, 1, :], r_norm_b[0:1, :].partition_broadcast(64), pw=[gnw])
            wBb = k.sb("wBb", [64, 2, 512], BF16)
            aBb = k.sb("aBb", [64, 512], BF16)
            gBb = k.sb("gBb", [128, 512], BF16)
            k.dma('pool', wBb[:, 0, :], r_wB[0, :, :], pw=[wBb])
            k.dma('pool', wBb[:, 1, :], r_wB[1, :, :], pw=[wBb])
            k.dma('pool', aBb[:], r_aB[:, :], writes=[aBb])
            k.dma('pool', gBb[:], r_gB[:, :], writes=[gBb])
            k.op('pool', lambda e: e.memset(ones_f[:], 1.0), writes=[ones_f])
            onesb = k.sb("onesb", [64, 2], BF16)
            k.op('pool', lambda e: e.memset(onesb[:], 1.0), writes=[onesb])
            lgb = k.sb("lgb", [128, T], BF16)
            lab = k.sb("lab", [64, NBK], BF16)
            lwb_ = k.sb("lwb_", [64, NBK], BF16)
            rr = k.sb("rr", [128, NBK], F32)
            rk = k.sb("rk", [128, NBK], F32)
            aa = k.sb("aa", [128, NBK], F32)
            t1 = k.sb("t1", [128, NBK], F32)
            t2 = k.sb("t2", [128, NBK], F32)
            khat = k.sb("khat", [128, NBK], F32)
            kmod = k.sb("kmod", [128, NBK], F32)
            beta = k.sb("beta", [128, NBK], F32)
            lgw = k.sb("lgw", [128, NBK], F32)
            lam = k.sb("lam", [128, NBK], F32)
            ee = k.sb("ee", [128, NBK], F32)
            class _S:
                pass
            SS = []
            for si in range(2):
                S = _S()
                S.i = si
                S.VTb = k.sb("VTb", [64, 2, 64 + NBK], BF16)
                S.PRb = k.sb("PRb", [64, 2, NBK], BF16)
                S.AR = k.sb("AR", [64, 2, NBC, 128], BF16)
                S.ZT = k.sb("ZT", [64, 2, NBC, 128], BF16)
                S.GL = k.sb("GL", [64, 2, NBC], F32)
                S.MmA = k.sb("MmA", [128, 2, NBC, 128], BF16)
                S.XLA = k.sb("XLA", [128, 2, NBC, 64], BF16)
                S.SWA = k.sb("SWA", [128, 2, NBC + 1, 64], BF16)
                S.WA = k.sb("WA", [128, 2, NBC, 64], BF16)
                S.ZtA = k.sb("ZtA", [128, 2, NBC, 64], BF16)
                S.TTA = k.sb("TTA", [64, 2, NBC, 64], BF16)
                S.Nt = [k.sb("Nt%d" % j, [64, 8, 64], BF16) for j in range(2)]
                S.GTt = [k.sb("GTt%d" % j, [64, 8, 2, 64], BF16) for j in range(2)]
                S.Xb = k.sb("Xb", [64, 2, 64], BF16)
                S.ST = k.sb("ST", [64, 2, 64], F32)
                S.tS = k.sb("tS", [64, 2, 64], F32)
                S.ys = k.sb("ys", [64, 2, 64], F32)
                S.bon = k.sb("bon", [64, 2], F32)
                S.om3 = k.sb("om3", [64, 128], F32)
                S.st2 = k.sb("st2r", [64, 2, 6], F32)
                S.mv2 = k.sb("mv2r", [64, 2, 2], F32)
                S.rs2 = k.sb("rs2r", [64, 2], F32)
                S.pXU = k.ps("p3x", [64, 2, 2, 64])
                S.pYS = k.ps("p3y", [64, 512])
                SS.append(S)
            Yf = k.sb("Yf", [64, NCH, 2, 64], F32)
            identg = k.sb("identg", [64, 8, 64], F32)
            pMg = k.ps("p3m", [128, 8, 128])
            pSg = k.ps("p3s", [128, 2, 512])
            pA = pMg
            for S in SS:
                k.op('pool', lambda e: e.memset(S.VTb[:], 0.0), writes=[S.VTb])
                k.op('pool', lambda e: e.memset(S.SWA[:], 0.0), writes=[S.SWA])
            for m in range(8):
                k.op('dve', lambda e: e.tensor_copy(identg[:, m, :], ident[0:64, 0:64]), reads=[ident], writes=[identg])

            prep_lock = [False]

            def prep(S, b, hp, tok0, ntok, d):
                VTb, PRb, AR, ZT, GL = S.VTb, S.PRb, S.AR, S.ZT, S.GL
                while prep_lock[0]:
                    yield
                prep_lock[0] = True
                nch = ntok // 64
                n = ntok
                NS = slice(0, ntok)
                r0 = hp * 128
                k.dma('pool', lab[:, NS], FM[b][3744:3808, tok0:tok0 + ntok], reads=[FM[b]], writes=[lab])
                lo = 3616 + 64 * d
                k.dma('pool', lwb_[:, NS], FM[b][lo:lo + 64, tok0:tok0 + ntok], reads=[FM[b]], writes=[lwb_])
                k.dma('sp', rr[:, NS], FM[b][1024 + r0:1024 + r0 + 128, tok0:tok0 + ntok], reads=[FM[b]], writes=[rr])
                k.dma('sp', rk[:, NS], FM[b][1536 + r0:1536 + r0 + 128, tok0:tok0 + ntok], reads=[FM[b]], writes=[rk])
                for h in range(2):
                    k.dma('pool', VTb[:, h, 64:64 + ntok], FM[b][2048 + r0 + h * 64:2048 + r0 + (h + 1) * 64, tok0:tok0 + ntok], reads=[FM[b]], pw=[VTb])
                yield
                pA0 = pMg[:, 0:4, :].rearrange("p a b -> p (a b)")[:, 0:n]
                pA1 = pMg[:, 4:8, :].rearrange("p a b -> p (a b)")[:, 0:n]
                k.op('pe', lambda e: e.matmul(pA0, aBb[:, hp * 128:(hp + 1) * 128], lab[:, NS], start=True, stop=True), reads=[aBb, lab], writes=[pMg], inc=False)
                k.op('pe', lambda e: e.matmul(pA1, wBb[:, d, hp * 128:(hp + 1) * 128], lwb_[:, NS], start=True, stop=True), reads=[wBb, lwb_])
                k.op('act', lambda e: e.activation(aa[:, NS], pA0, AF.Sigmoid, bias=rp1[:, hp, 2:3]), reads=[pMg, rp1], writes=[aa])
                k.op('act', lambda e: e.activation(lgw[:, NS], pA1, AF.Sigmoid, bias=rp1[:, hp, d:d + 1]), reads=[pMg, rp1], writes=[lgw])
                k.op('dve', lambda e: e.tensor_scalar(lgw[:, NS], lgw[:, NS], -DS, None, ALU.mult), reads=[lgw], writes=[lgw])
                yield
                k.op('dve', lambda e: e.tensor_scalar(t1[:, NS], rk[:, NS], rp1[:, hp, 3:4], None, ALU.mult), reads=[rk, rp1], writes=[t1])
                k.op('dve', lambda e: e.tensor_tensor(t2[:, NS], t1[:, NS], t1[:, NS], ALU.mult), reads=[t1], writes=[t2])
                k.op('pe', lambda e: e.matmul(pA0, onesbd[:], t2[:, NS], start=True, stop=True), reads=[onesbd, t2], writes=[pMg])
                k.op('act', lambda e: e.activation(khat[:, NS], pA0, AF.Sqrt), reads=[pMg], writes=[khat])
                yield
                k.op('dve', lambda e: e.tensor_scalar(khat[:, NS], khat[:, NS], 1e-12, None, ALU.max), reads=[khat], writes=[khat])
                k.op('dve', lambda e: e.reciprocal(khat[:, NS], khat[:, NS]), reads=[khat], writes=[khat])
                k.op('dve', lambda e: e.tensor_tensor(khat[:, NS], khat[:, NS], t1[:, NS], ALU.mult), reads=[khat, t1], writes=[khat])
                yield
                k.op('dve', lambda e: e.tensor_scalar(t1[:, NS], aa[:, NS], rp1[:, hp, 4:5], rp1[:, hp, 7:8], ALU.mult, ALU.add), reads=[aa, rp1], writes=[t1])
                k.op('dve', lambda e: e.tensor_tensor(kmod[:, NS], rk[:, NS], t1[:, NS], ALU.mult), reads=[rk, t1], writes=[kmod])
                k.op('dve', lambda e: e.tensor_tensor(beta[:, NS], khat[:, NS], aa[:, NS], ALU.mult), reads=[khat, aa], writes=[beta])
                yield
                k.op('dve', lambda e: e.scalar_tensor_tensor(PRs[:, NS], rr[:, NS], rp1[:, hp, 6:7], kmod[:, NS], ALU.mult, ALU.mult), reads=[rr, rp1, kmod], writes=[PRs])
                k.op('dve', lambda e: e.tensor_tensor_scan(lam[:, NS], smask[:, NS], lgw[:, NS], 0.0, ALU.mult, ALU.add), reads=[smask, lgw], writes=[lam])
                yield
                v3 = lambda t_: t_[:, NS].rearrange("p (c t) -> p c t", t=64)
                if d == 1:
                    k.op('dve', lambda e: e.tensor_tensor(t1[:, NS], lgw[:, NS], lam[:, NS], ALU.subtract), reads=[lgw, lam], writes=[t1])
                    k.op('dve', lambda e: e.tensor_tensor(v3(t2), v3(t1), v3(lam)[:, :, 63:64].to_broadcast([128, nch, 64]), ALU.add), reads=[t1, lam], writes=[t2])
                    k.op('act', lambda e: e.activation(lam[:, NS], t2[:, NS], AF.Identity), reads=[t2], writes=[lam])
                k.op('dve', lambda e: e.tensor_tensor(t1[:, NS], lam[:, NS], lgw[:, NS], ALU.subtract), reads=[lam, lgw], writes=[t1])
                k.op('act', lambda e: e.activation(ee[:, NS], t1[:, NS], AF.Exp), reads=[t1], writes=[ee])
                yield
                k.op('dve', lambda e: e.scalar_tensor_tensor(ARs[:, 0:nch, 0:64], v3(khat), -1.0, v3(ee), ALU.mult, ALU.mult), reads=[khat, ee], writes=[ARs])
                k.op('act', lambda e: e.activation(ee[:, NS], lam[:, NS], AF.Exp), reads=[lam], writes=[ee])
                k.op('dve', lambda e: e.tensor_tensor(ARs[:, 0:nch, 64:128], v3(rr), v3(ee), ALU.mult), reads=[rr, ee], writes=[ARs])
                yield
                gcol = 63 if d == 0 else 0
                k.op('act', lambda e: e.activation(GLs[:, 0:nch], v3(ee)[:, :, gcol], AF.Identity), reads=[ee], writes=[GLs])
                k.op('act', lambda e: e.activation(t1[:, NS], lam[:, NS], AF.Exp, scale=-1.0), reads=[lam], writes=[t1])
                yield
                k.op('dve', lambda e: e.tensor_tensor(ZTs[:, 0:nch, 0:64], v3(beta), v3(t1), ALU.mult), reads=[beta, t1], writes=[ZTs])
                k.op('dve', lambda e: e.tensor_tensor(ZTs[:, 0:nch, 64:128], v3(kmod), v3(t1), ALU.mult), reads=[kmod, t1], writes=[ZTs])
                yield
                k.op('act', lambda e: e.activation(AR[:, 0, 0:nch, :], ARs[0:64, 0:nch, :], AF.Identity), reads=[ARs], writes=[AR])
                k.op('act', lambda e: e.activation(ZT[:, 0, 0:nch, :], ZTs[0:64, 0:nch, :], AF.Identity), reads=[ZTs], writes=[ZT])
                k.op('act', lambda e: e.activation(PRb[:, 0, NS], PRs[0:64, NS], AF.Identity), reads=[PRs], writes=[PRb])
                k.op('act', lambda e: e.activation(GL[:, 0, 0:nch], GLs[0:64, 0:nch], AF.Identity), reads=[GLs], writes=[GL])
                k.dma('sp', AR[:, 1, 0:nch, :], ARs[64:128, 0:nch, :], reads=[ARs], pw=[AR])
                k.dma('sp', ZT[:, 1, 0:nch, :], ZTs[64:128, 0:nch, :], reads=[ZTs], pw=[ZT])
                k.dma('sp', PRb[:, 1, NS], PRs[64:128, NS], reads=[PRs], pw=[PRb])
                k.dma('sp', GL[:, 1, 0:nch], GLs[64:128, 0:nch], reads=[GLs], pw=[GL])
                prep_lock[0] = False

            pSf = lambda: pSg[:].rearrange("p a b -> p (a b)")

            def precompute(S, l0, d):
                VTb, AR, ZT, MmA, XLA, SWA, WA, ZtA, TTA = S.VTb, S.AR, S.ZT, S.MmA, S.XLA, S.SWA, S.WA, S.ZtA, S.TTA
                G4 = slice(l0, l0 + 4)
                pTb = pSg[:, 0, :].bitcast(BF16)
                for h in range(2):
                    for j in range(4):
                        m = h * 4 + j
                        k.op('pe', lambda e: e.transpose(pTb[:, m * 64:(m + 1) * 64], ZT[:, h, l0 + j, :], identb[0:64, 0:64]), reads=[ZT, identb], writes=[pSg], inc=False)
                for h in range(2):
                    for j in range(4):
                        m = 8 + h * 4 + j
                        k.op('pe', lambda e: e.transpose(pTb[:, m * 64:(m + 1) * 64], VTb[:, h, (l0 + j) * 64:(l0 + j) * 64 + 128], identb[0:64, 0:64]), reads=[VTb, identb], inc=(h == 1 and j == 3))
                zsrc = pTb[:, 0:512].rearrange("p (h j e) -> p h j e", h=2, j=4)
                vsrc = pTb[64:128, 512:1024].rearrange("p (h j e) -> p h j e", h=2, j=4)
                k.op('act', lambda e: e.activation(ZtA[:, :, G4, :], zsrc, AF.Identity), reads=[pSg], writes=[ZtA])
                k.op('act', lambda e: e.activation(SWA[64:128, :, G4, :], vsrc, AF.Identity), reads=[pSg], writes=[SWA])
                k.op('act', lambda e: e.activation(WA[64:128, :, G4, :], vsrc, AF.Identity), reads=[pSg], writes=[WA])
                yield
                for h in range(2):
                    for j in range(4):
                        m = h * 4 + j
                        k.op('pe', lambda e: e.matmul(pMg[:, m, :], ZT[:, h, l0 + j, :], AR[:, h, l0 + j, :], start=True, stop=True), reads=[ZT, AR], writes=[pMg], inc=(m == 7))
                for h in range(2):
                    for j in range(4):
                        m = h * 4 + j
                        k.op('pe', lambda e: e.matmul(pSg[0:64, 1, m * 64:(m + 1) * 64], AR[:, h, l0 + j, 0:64], ZT[:, h, l0 + j, 0:64], start=True, stop=True), reads=[ZT, AR], writes=[pSg], inc=(m == 7))
                for h in range(2):
                    k.op('dve', lambda e: e.tensor_tensor(MmA[:, h, G4, :], pMg[:, h * 4:(h + 1) * 4, :], rmask[:, d:d + 1, :].to_broadcast([128, 4, 128]), ALU.mult),
                         reads=[pMg, rmask], writes=[MmA])
                Nt, GTt = S.Nt, S.GTt
                k.op('dve', lambda e: e.tensor_tensor(Nt[0][:], pSg[0:64, 1, :].rearrange("p (m e) -> p m e", e=64), nmask[:, d:d + 1, :].to_broadcast([64, 8, 64]), ALU.mult),
                     reads=[pSg, nmask], writes=[Nt[0]])
                k.op('act', lambda e: e.activation(GTt[0][:, :, 0, :].rearrange("p (h j) e -> p h j e", h=2), MmA[0:64, :, G4, 0:64], AF.Identity), reads=[MmA], writes=[GTt[0]])
                k.op('act', lambda e: e.activation(GTt[0][:, :, 1, :], identg[:], AF.Identity), reads=[identg, GTt[0]], writes=[GTt[0]])
                k.op('act', lambda e: e.activation(XLA[64:128, :, G4, :], MmA[64:128, :, G4, 0:64], AF.Identity), reads=[MmA], writes=[XLA])
                k.op('act', lambda e: e.activation(XLA[0:64, :, G4, :], AR[:, :, G4, 0:64], AF.Identity), reads=[AR], writes=[XLA])
                cur = 0
                for lv in range(5):
                    yield
                    nsrc, gsrc, ndst, gdst = Nt[cur], GTt[cur], Nt[1 - cur], GTt[1 - cur]
                    for m in range(8):
                        k.op('pe', lambda e: e.matmul(pSg[0:64, 0, m * 64:(m + 1) * 64], gsrc[:, m, 0, :], nsrc[:, m, :], start=True, stop=True), reads=[gsrc, nsrc], writes=[pSg] if m == 0 else [], inc=False)
                    for m in range(8):
                        k.op('pe', lambda e: e.matmul(pMg[0:64, m, :], nsrc[:, m, :], gsrc[:, m, :, :].rearrange("p a e -> p (a e)"), start=True, stop=True), reads=[nsrc, gsrc], writes=[pMg] if m == 0 else [], inc=(m == 7))
                    k.op('act', lambda e: e.activation(ndst[:].rearrange("p m e -> p (m e)"), pSg[0:64, 0, :], AF.Identity), reads=[pSg], writes=[ndst])
                    k.op('act', lambda e: e.activation(gdst[:, :, 0, :], pMg[0:64, :, 0:64], AF.Identity), reads=[pMg], writes=[gdst])
                    k.op('dve', lambda e: e.tensor_tensor(gdst[:, :, 1, :], pMg[0:64, :, 64:128], gsrc[:, :, 1, :], ALU.add), reads=[pMg, gsrc, gdst], writes=[gdst])
                    cur = 1 - cur
                yield
                nsrc, gsrc = Nt[cur], GTt[cur]
                for m in range(8):
                    k.op('pe', lambda e: e.matmul(pSg[0:64, 1, m * 64:(m + 1) * 64], nsrc[:, m, :], gsrc[:, m, 1, :], start=True, stop=True), reads=[nsrc, gsrc], writes=[pSg] if m == 0 else [], inc=(m == 7))
                k.op('dve', lambda e: e.tensor_tensor(TTA[:, :, G4, :], pSg[0:64, 1, :].rearrange("p (h j e) -> p h j e", h=2, j=4), gsrc[:, :, 1, :].rearrange("p (h j) e -> p h j e", h=2), ALU.add),
                     reads=[pSg, gsrc], writes=[TTA])

            def step(S, b, hp, c, lc, lnext, d, done):
                VTb, PRb, AR, GL, MmA, XLA, SWA, WA, ZtA, TTA = S.VTb, S.PRb, S.AR, S.GL, S.MmA, S.XLA, S.SWA, S.WA, S.ZtA, S.TTA
                Xb, ST, tS, ys, bon, om3, st2, mv2, rs2, pXU, pYS = S.Xb, S.ST, S.tS, S.ys, S.bon, S.om3, S.st2, S.mv2, S.rs2, S.pXU, S.pYS
                pX = pXU[:, 0, :, :]
                pU = pXU[:, 1, :, :]
                pY = pYS[:, 0:128].rearrange("p (h e) -> p h e", h=2)
                pS_ = pYS[:, 128:256].rearrange("p (h e) -> p h e", h=2)
                for h in range(2):
                    k.op('pe', lambda e: e.matmul(pXU[:, 0, h, :], XLA[:, h, lc, :], SWA[:, h, lc, :], start=True, stop=True), reads=[XLA, SWA], writes=[pXU], inc=(h == 1))
                k.op('act', lambda e: e.activation(Xb[:], pX, AF.Identity), reads=[pXU], writes=[Xb])
                yield
                for h in range(2):
                    k.op('pe', lambda e: e.matmul(pXU[:, 1, h, :], TTA[:, h, lc, :], Xb[:, h, :], start=True, stop=True), reads=[TTA, Xb], writes=[pXU], inc=(h == 1))
                k.op('dve', lambda e: e.tensor_copy(WA[0:64, :, lc, :], pU), reads=[pXU], writes=[WA])
                yield
                second = (c in done)
                needy = (c >= 4)
                wfirst = [pYS]
                if needy:
                    for h in range(2):
                        k.op('pe', lambda e: e.matmul(pYS[:, h * 64:(h + 1) * 64], AR[:, h, lc, 64:128], SWA[0:64, h, lc, :], start=True, stop=False), reads=[AR, SWA], writes=wfirst, inc=False)
                        wfirst = []
                        k.op('pe', lambda e: e.matmul(pYS[:, h * 64:(h + 1) * 64], MmA[:, h, lc, 64:128], WA[:, h, lc, :], start=False, stop=True), reads=[MmA, WA], inc=False)
                fin = (needy and second)
                if fin:
                    ts = slice(lc * 64, (lc + 1) * 64)
                    for h in range(2):
                        k.op('pe', lambda e: e.matmul(pYS[:, 384 + 2 * h:386 + 2 * h], PRb[:, h, ts], onesb[:, 0:2], start=True, stop=True), reads=[PRb, onesb], inc=False)
                    k.op('pe', lambda e: e.matmul(pYS[:, 256:384], lgb[:, c * 64:(c + 1) * 64], gBb[:, hp * 128:(hp + 1) * 128], start=True, stop=True), reads=[lgb, gBb], inc=False)
                    pvt = pYS[:, 448:512].bitcast(BF16)
                    for h in range(2):
                        k.op('pe', lambda e: e.transpose(pvt[:, h * 64:(h + 1) * 64], VTb[:, h, 64 + lc * 64:128 + lc * 64], identb[0:64, 0:64]), reads=[VTb, identb], inc=False)
                for h in range(2):
                    k.op('pe', lambda e: e.matmul(pYS[:, 128 + h * 64:128 + (h + 1) * 64], ZtA[:, h, lc, :], WA[:, h, lc, :], start=True, stop=True), reads=[ZtA, WA], writes=wfirst, inc=(h == 1))
                    wfirst = []
                k.op('dve', lambda e: e.tensor_tensor(tS[:], pS_, ST[:], ALU.add), reads=[pYS, ST], writes=[tS])
                k.op('dve', lambda e: e.tensor_tensor(ST[:], tS[:], GL[:, :, lc:lc + 1].to_broadcast([64, 2, 64]), ALU.mult), reads=[tS, GL], writes=[ST])
                k.op('act', lambda e: e.activation(SWA[0:64, :, lnext, :], ST[:], AF.Identity), reads=[ST], writes=[SWA])
                if needy and not second:
                    k.op('dve', lambda e: e.tensor_copy(Yf[:, c, :, :], pY), reads=[pYS], pw_=[Yf])
                    done[c] = S.i
                elif needy:
                    k.op('dve', lambda e: e.tensor_tensor(ys[:], pY, Yf[:, c, :, :], ALU.add), reads=[pYS, Yf], writes=[ys])
                if fin:
                    yield
                    for h in range(2):
                        k.op('dve', lambda e: e.bn_stats(st2[:, h, :], ys[:, h, :]), reads=[ys], writes=[st2])
                    for h in range(2):
                        k.op('dve', lambda e: e.bn_aggr(mv2[:, h, :], st2[:, h, :]), reads=[st2], writes=[mv2])
                    yield
                    k.op('act', lambda e: e.activation(rs2[:], mv2[:, :, 1], AF.Sqrt, bias=GN_EPS), reads=[mv2], writes=[rs2])
                    k.op('dve', lambda e: e.reciprocal(rs2[:], rs2[:]), reads=[rs2], writes=[rs2])
                    yield
                    for h in range(2):
                        k.op('dve', lambda e: e.tensor_scalar(ys[:, h, :], ys[:, h, :], mv2[:, h, 0:1], rs2[:, h:h + 1], ALU.subtract, ALU.mult), reads=[ys, mv2, rs2], writes=[ys])
                    ysf = ys[:].rearrange("p h e -> p (h e)")
                    k.op('dve', lambda e: e.tensor_tensor(om3[:], ysf, gnw[:, 0, hp * 128:(hp + 1) * 128], ALU.mult), reads=[ys, gnw], writes=[om3])
                    yield
                    k.op('dve', lambda e: e.tensor_tensor(om3[:], om3[:], gnw[:, 1, hp * 128:(hp + 1) * 128], ALU.add), reads=[om3, gnw], writes=[om3])
                    k.op('dve', lambda e: e.tensor_copy(bon[:], pYS[:, 384:388].rearrange("p (h two) -> p h two", two=2)[:, :, 0]), reads=[pYS], writes=[bon])
                    yield
                    for h in range(2):
                        k.op('dve', lambda e: e.scalar_tensor_tensor(om3[:, h * 64:(h + 1) * 64], pvt[:, h * 64:(h + 1) * 64], bon[:, h:h + 1], om3[:, h * 64:(h + 1) * 64], ALU.mult, ALU.add),
                             reads=[pYS, bon, om3], writes=[om3])
                    k.op('dve', lambda e: e.tensor_tensor(om3[:], om3[:], pYS[:, 256:384], ALU.mult), reads=[om3, pYS], writes=[om3])
                    k.dma('sp', MIX[b][(c - 4) * 64:(c - 3) * 64, 512 + hp * 128:512 + (hp + 1) * 128], om3[:], reads=[om3], pw=[MIX[b]])

            blocks = [(0, 256)] + [(256 + i * 512, 512) for i in range(8)]

            def stream(S, b, hp, d, done):
                k.op('pool', lambda e: e.memset(S.ST[:], 0.0), writes=[S.ST])
                border = list(range(9)) if d == 0 else [0] + list(range(8, 0, -1))
                first = True
                for bi in border:
                    tok0, ntok = blocks[bi]
                    nch = ntok // 64
                    yield from prep(S, b, hp, tok0, ntok, d)
                    lcs = list(range(nch)) if d == 0 else list(range(nch - 1, -1, -1))
                    if first:
                        k.op('pool', lambda e: e.memset(S.SWA[0:64, :, lcs[0], :], 0.0), writes=[S.SWA])
                        first = False
                    else:
                        k.op('act', lambda e: e.activation(S.SWA[0:64, :, lcs[0], :], S.ST[:], AF.Identity), reads=[S.ST], writes=[S.SWA])
                    yield
                    for g0 in range(0, nch, 4):
                        if os.environ.get('RW_SKIP_PRE'):
                            break
                        yield from precompute(S, g0, d)
                        yield
                    for ii, lc in enumerate(lcs):
                        if os.environ.get('RW_SKIP_STEP'):
                            break
                        lnext = lcs[ii + 1] if ii + 1 < len(lcs) else NBC
                        yield from step(S, b, hp, tok0 // 64 + lc, lc, lnext, d, done)
                        yield

            for b in range(NB):
                for q4 in range(4):
                    k.dma('pool', lgb[:, q4 * 1088:(q4 + 1) * 1088], FM[b][3808:3936, q4 * 1088:(q4 + 1) * 1088], reads=[FM[b]], pw=[lgb])
                for hp in range(int(os.environ.get('P3H', 4))):
                    done = {}
                    gens = [stream(SS[0], b, hp, 0, done), stream(SS[1], b, hp, 1, done)]
                    alive = [True, True]
                    for _ in range(int(os.environ.get('RW_OFF', 22))):
                        next(gens[0])
                    while any(alive):
                        for gi in range(2):
                            if alive[gi]:
                                try:
                                    next(gens[gi])
                                except StopIteration:
                                    alive[gi] = False
        k.barrier()

        with ExitStack() as es:
          if 4 in phases:
           try:
            P4S = int(os.environ.get('P4S', 9))
            k.es = es
            NT = NB * SEQ // 128
            NBLK = NBLK_
            SUB = BS // 128
            LG = k.sb("LG", [128, NT, 36], F32)
            OH1 = k.sb("OH1", [128, NT, 32], F32)
            OH2 = k.sb("OH2", [128, NT, 32], F32)
            W1 = k.sb("W1", [128, NT], F32)
            W2 = k.sb("W2", [128, NT], F32)
            DST = k.sb("DST", [128, NT, 2], I32)
            WIDX = k.sb("WIDX", [128, NBLK, 12], I32)
            g2b = k.sb("g2b", [128, NB, D], F32)
            lnp = k.sb("lnp", [128, 4, D], F32)
            for j, src in enumerate((ln1_g, ln1_b, ln2_g, ln2_b)):
                k.dma('sp', lnp[:, j, :], src[0:1, :].partition_broadcast(128), pw=[lnp])
            for b in range(NB):
                k.dma('sp', g2b[:, b, :], MODD[b:b + 1, 5 * D:6 * D].partition_broadcast(128), reads=[MODD], pw=[g2b])
            with ExitStack() as es4:
                k.es = es4
                wob = k.sb("wob", [128, 8, D], BF16)
                for kc in range(8):
                    k.dma('pool', wob[:, kc, :], w_out[kc * 128:(kc + 1) * 128, :], pw=[wob])
                rt = k.sb("rt", [128, 8, 36], F32)
                k.dma('sp', rt[:], rt_in[:, :].rearrange("(kc p) n -> p kc n", p=128), writes=[rt])
                rtbb = k.sb("rtbb", [128, 36], F32)
                k.dma('sp', rtbb[:], rtb_in[0:1, :].partition_broadcast(128), writes=[rtbb])
                mb4 = k.sb("mb4", [128, 3, D], F32)
                class _A:
                    pass
                AS = []
                for si in range(2):
                    A = _A()
                    A.mxb = k.sb("mxb", [128, D], BF16)
                    A.mT = k.sb("mT", [128, 8, 128], BF16)
                    A.x4 = k.sb("x4", [128, D], F32)
                    A.t4 = k.sb("t4", [128, D], F32)
                    A.y4 = k.sb("y4", [128, D], F32)
                    A.h4 = k.sb("h4", [128, D], F32)
                    A.h4b = k.sb("h4b", [128, D], BF16)
                    A.h4T = k.sb("h4T", [128, 8, 128], F32)
                    A.st4 = k.sb("st4", [128, 2, 6], F32)
                    A.mv4 = k.sb("mv4", [128, 2], F32)
                    A.rs4 = k.sb("rs4", [128, 1], F32)
                    A.nb4 = k.sb("nb4", [128, 1], F32)
                    A.P01 = k.ps("p4o", [128, 2, 512])
                    A.P01.excl = True
                    A.plg = k.ps("p4lg", [128, 36])
                    AS.append(A)

                def ln_stats(A, src):
                    for hf in range(2):
                        k.op('dve', lambda e: e.bn_stats(A.st4[:, hf, :], src[:, hf * 512:(hf + 1) * 512]), reads=[src], writes=[A.st4])
                    k.op('dve', lambda e: e.bn_aggr(A.mv4[:], A.st4[:].rearrange("p a b -> p (a b)")), reads=[A.st4], writes=[A.mv4])
                    yield
                    k.op('act', lambda e: e.activation(A.rs4[:], A.mv4[:, 1:2], AF.Sqrt, bias=LN_EPS), reads=[A.mv4], writes=[A.rs4])
                    yield
                    k.op('dve', lambda e: e.reciprocal(A.rs4[:], A.rs4[:]), reads=[A.rs4], writes=[A.rs4])
                    k.op('dve', lambda e: e.scalar_tensor_tensor(A.nb4[:], A.mv4[:, 0:1], -1.0, A.rs4[:], ALU.mult, ALU.mult), reads=[A.mv4, A.rs4], writes=[A.nb4])

                def tile4(A, b, i):
                    gi = b * (SEQ // 128) + i
                    P01 = A.P01
                    k.dma('pool', A.mxb[:], MIX[b][i * 128:(i + 1) * 128, :], reads=[MIX[b]], writes=[A.mxb])
                    k.dma('sp', A.x4[:], x_in[b, i * 128:(i + 1) * 128, :], writes=[A.x4])
                    yield
                    ptm = P01[:, 0, :].bitcast(BF16)
                    for kc in range(8):
                        k.op('pe', lambda e: e.transpose(ptm[:, kc * 128:(kc + 1) * 128], A.mxb[:, kc * 128:(kc + 1) * 128], identb[:]), reads=[A.mxb, identb], writes=[P01] if kc == 0 else [], inc=(kc == 7))
                    k.op('act', lambda e: e.activation(A.mT[:].rearrange("p a b -> p (a b)"), ptm, AF.Identity), reads=[P01], writes=[A.mT])
                    yield
                    for n in range(2):
                        for kc in range(8):
                            k.op('pe', lambda e: e.matmul(P01[:, n, :], A.mT[:, kc, :], wob[:, kc, n * 512:(n + 1) * 512], start=(kc == 0), stop=(kc == 7)),
                                 reads=[A.mT, wob], writes=[P01] if (kc == 0 and n == 0) else [], inc=(kc == 7 and n == 1))
                    k.op('dve', lambda e: e.tensor_tensor(A.t4[:], P01[:].rearrange("p a b -> p (a b)"), mb4[:, 0, :], ALU.mult), reads=[P01, mb4], writes=[A.t4])
                    k.op('dve', lambda e: e.scalar_tensor_tensor(A.y4[:], A.x4[:], ALPHA, A.t4[:], ALU.mult, ALU.add), reads=[A.x4, A.t4], writes=[A.y4])
                    yield
                    yield from ln_stats(A, A.y4)
                    yield
                    k.op('act', lambda e: e.activation(A.y4[:], A.y4[:], AF.Identity, bias=A.nb4[:, 0:1], scale=A.rs4[:, 0:1]), reads=[A.y4, A.nb4, A.rs4], writes=[A.y4])
                    yield
                    k.op('dve', lambda e: e.tensor_tensor(A.y4[:], A.y4[:], lnp[:, 0, :], ALU.mult), reads=[A.y4, lnp], writes=[A.y4])
                    k.op('dve', lambda e: e.tensor_tensor(A.y4[:], A.y4[:], lnp[:, 1, :], ALU.add), reads=[A.y4, lnp], writes=[A.y4])
                    k.dma('sp', X1[gi * 128:(gi + 1) * 128, :], A.y4[:], reads=[A.y4], pw=[X1])
                    yield
                    yield from ln_stats(A, A.y4)
                    yield
                    k.op('act', lambda e: e.activation(A.h4[:], A.y4[:], AF.Identity, bias=A.nb4[:, 0:1], scale=A.rs4[:, 0:1]), reads=[A.y4, A.nb4, A.rs4], writes=[A.h4])
                    yield
                    k.op('dve', lambda e: e.tensor_tensor(A.h4[:], A.h4[:], mb4[:, 1, :], ALU.mult), reads=[A.h4, mb4], writes=[A.h4])
                    k.op('dve', lambda e: e.tensor_tensor(A.h4[:], A.h4[:], mb4[:, 2, :], ALU.add), reads=[A.h4, mb4], writes=[A.h4])
                    k.op('act', lambda e: e.activation(A.h4b[:], A.h4[:], AF.Identity), reads=[A.h4], writes=[A.h4b])
                    k.dma('sp', H2[gi * 128:(gi + 1) * 128, :], A.h4b[:], reads=[A.h4b], pw=[H2])
                    yield
                    pth = P01[:].rearrange("p a b -> p (a b)")
                    for kc in range(8):
                        k.op('pe', lambda e: e.transpose(pth[:, kc * 128:(kc + 1) * 128], A.h4[:, kc * 128:(kc + 1) * 128], ident[:]), reads=[A.h4, ident], writes=[P01] if kc == 0 else [], inc=(kc == 7))
                    k.op('act', lambda e: e.activation(A.h4T[:].rearrange("p a b -> p (a b)"), pth, AF.Identity), reads=[P01], writes=[A.h4T])
                    yield
                    for kc in range(8):
                        k.op('pe', lambda e: e.matmul(A.plg[:], A.h4T[:, kc, :], rt[:, kc, :], start=(kc == 0), stop=(kc == 7)),
                             reads=[A.h4T, rt], writes=[A.plg] if kc == 0 else [], inc=(kc == 7))
                    k.op('dve', lambda e: e.tensor_tensor(LG[:, gi, :], A.plg[:], rtbb[:], ALU.add), reads=[A.plg, rtbb], pw_=[LG])

                for b in range(NB):
                    for j, c0 in enumerate((2 * D, 4 * D, 3 * D)):
                        k.dma('sp', mb4[:, j, :], MODD[b:b + 1, c0:c0 + D].partition_broadcast(128), reads=[MODD], writes=[mb4] if j == 0 else [], pw=[] if j == 0 else [mb4])
                    k.op('dve', lambda e: e.tensor_scalar(mb4[:, 1, :], mb4[:, 1, :], 1.0, None, ALU.add), reads=[mb4], writes=[mb4])

                    def astream(si):
                        for i in range(si, SEQ // 128, 2):
                            yield from tile4(AS[si], b, i)
                            yield
                    gens = [astream(0), astream(1)]
                    alive = [True, True]
                    while any(alive):
                        for gq in range(2):
                            if alive[gq]:
                                try:
                                    next(gens[gq])
                                except StopIteration:
                                    alive[gq] = False
            k.barrier()
            with ExitStack() as es5:
                k.es = es5
                if P4S < 1:
                    raise _Stop()
                gmx = k.sb("gmx", [128, NT], F32)
                goh = k.sb("goh", [128, NT, 4], F32)
                tg = k.sb("tg", [128, NT, 4], F32)
                ptop = k.sb("ptop", [128, NT], F32)
                lem = k.sb("lem", [128, NT, 32], F32)
                v1 = k.sb("v1", [128, NT], F32)
                v2 = k.sb("v2", [128, NT], F32)
                lgv = LG[:, :, 0:4]
                lev = LG[:, :, 4:36]
                k.op('dve', lambda e: e.tensor_reduce(gmx[:], lgv, AX.X, ALU.max), reads=[LG], writes=[gmx])
                k.op('dve', lambda e: e.tensor_tensor(goh[:], lgv, gmx[:].unsqueeze(2).to_broadcast([128, NT, 4]), ALU.is_equal), reads=[LG, gmx], writes=[goh])
                k.op('dve', lambda e: e.tensor_tensor(tg[:], lgv, gmx[:].unsqueeze(2).to_broadcast([128, NT, 4]), ALU.subtract), reads=[LG, gmx], writes=[tg])
                k.op('act', lambda e: e.activation(tg[:], tg[:], AF.Exp), reads=[tg], writes=[tg])
                k.op('dve', lambda e: e.tensor_reduce(ptop[:], tg[:], AX.X, ALU.add), reads=[tg], writes=[ptop])
                k.op('dve', lambda e: e.reciprocal(ptop[:], ptop[:]), reads=[ptop], writes=[ptop])
                k.op('dve', lambda e: e.tensor_scalar(goh[:], goh[:], -1.0, 1e30, ALU.add, ALU.mult), reads=[goh], writes=[goh])
                for g in range(4):
                    k.op('dve', lambda e: e.tensor_tensor(lem[:, :, g * 8:(g + 1) * 8], LG[:, :, 4 + g * 8:12 + g * 8],
                                                          goh[:, :, g:g + 1].to_broadcast([128, NT, 8]), ALU.add), reads=[LG, goh], writes=[lem])
                k.op('dve', lambda e: e.tensor_reduce(v1[:], lem[:], AX.X, ALU.max), reads=[lem], writes=[v1])
                k.op('dve', lambda e: e.tensor_tensor(OH1[:], lem[:], v1[:].unsqueeze(2).to_broadcast([128, NT, 32]), ALU.is_equal), reads=[lem, v1], writes=[OH1])
                k.op('dve', lambda e: e.scalar_tensor_tensor(lem[:], OH1[:], -1e30, lem[:], ALU.mult, ALU.add), reads=[OH1, lem], writes=[lem])
                k.op('dve', lambda e: e.tensor_reduce(v2[:], lem[:], AX.X, ALU.max), reads=[lem], writes=[v2])
                k.op('dve', lambda e: e.tensor_tensor(OH2[:], lem[:], v2[:].unsqueeze(2).to_broadcast([128, NT, 32]), ALU.is_equal), reads=[lem, v2], writes=[OH2])
                k.op('dve', lambda e: e.tensor_tensor(v2[:], v2[:], v1[:], ALU.subtract), reads=[v1, v2], writes=[v2])
                k.op('act', lambda e: e.activation(v2[:], v2[:], AF.Exp), reads=[v2], writes=[v2])
                k.op('dve', lambda e: e.tensor_scalar(v2[:], v2[:], 1.0, None, ALU.add), reads=[v2], writes=[v2])
                k.op('dve', lambda e: e.reciprocal(v2[:], v2[:]), reads=[v2], writes=[v2])
                k.op('dve', lambda e: e.tensor_tensor(W1[:], v2[:], ptop[:], ALU.mult), reads=[v2, ptop], writes=[W1])
                k.op('dve', lambda e: e.tensor_tensor(W2[:], ptop[:], W1[:], ALU.subtract), reads=[W1, ptop], writes=[W2])
            k.barrier()
            with ExitStack() as es6:
                k.es = es6
                if P4S < 2:
                    raise _Stop()
                OHb = k.sb("OHb", [128, NT, 32], BF16)
                triS = k.sb("triS", [128, 128], BF16)
                onb = k.sb("onb", [128, 128], BF16)
                thr = k.sb("thr", [128, 128], F32)
                blki = k.sb("blki", [128, NBLK], F32)
                kcp = k.sb("kcp", [128, 12], F32)
                cnt = k.sb("cnt", [128, 32], F32)
                big = k.sb("big", [128, 32, 128], F32)
                nbk = k.sb("nbk", [128, 32], F32)
                pend = k.sb("pend", [128, 32], F32)
                pst = k.sb("pst", [128, 32], F32)
                run = k.sb("run", [128, 32], F32)
                RK = k.sb("RK", [128, NT, 32], F32)
                dsf = k.sb("dsf", [128, NT, 2], F32)
                bexp = k.sb("bexp", [128, NBLK], F32)
                bigb = k.sb("bigb", [128, NBLK, 32], F32)
                widxf = k.sb("widxf", [128, NBLK, 12], F32)
                tokid = k.sb("tokid", [128, NT, 16], I32)
                zt = k.sb("zt", [128, 16], I32)
                pcn = k.ps("p5c", [128, 32])
                prk = k.ps("p5r", [128, 32])
                ptt = k.ps("p5t", [128, 32])
                stg = k.sb("stg", [128, 128], F32)
                k.dma('sp', stg[:], tris_in[:, :], writes=[stg])
                k.op('dve', lambda e: e.tensor_copy(triS[:], stg[:]), reads=[stg], writes=[triS])
                k.op('pool', lambda e: e.memset(onb[:], 1.0), writes=[onb])
                k.dma('sp', thr[:], thr_in[:, :], writes=[thr])
                k.dma('sp', blki[:], blki_in[:, 0:NBLK], writes=[blki])
                k.dma('sp', kcp[:], kcp_in[:, :], writes=[kcp])
                k.dma('sp', tokid[:], tokid_in[:, 0:NT, :], writes=[tokid])
                k.op('pool', lambda e: e.memset(zt[:], 0), writes=[zt])
                k.dma('sp', TOKB[:, :].rearrange("(b p) c -> p b c", p=128), zt[:].unsqueeze(1).to_broadcast([128, NBLK * SUB, 16]), reads=[zt], writes=[TOKB])
                k.op('dve', lambda e: e.tensor_tensor(OHb[:], OH1[:], OH2[:], ALU.add), reads=[OH1, OH2], writes=[OHb])
                for i in range(NT):
                    k.op('pe', lambda e: e.matmul(pcn[:], onb[:], OHb[:, i, :], start=(i == 0), stop=(i == NT - 1)), reads=[onb, OHb], writes=[pcn] if i == 0 else [], inc=(i == NT - 1))
                k.op('dve', lambda e: e.tensor_copy(cnt[:], pcn[:]), reads=[pcn], writes=[cnt])
                k.op('dve', lambda e: e.tensor_tensor(big[:], cnt[:].unsqueeze(2).to_broadcast([128, 32, 128]), thr[:].unsqueeze(1).to_broadcast([128, 32, 128]), ALU.is_gt),
                     reads=[cnt, thr], writes=[big])
                k.op('dve', lambda e: e.tensor_reduce(nbk[:], big[:], AX.X, ALU.add), reads=[big], writes=[nbk])
                k.op('pool', lambda e: e.memset(run[:], 1.0), writes=[run])
                k.op('dve', lambda e: e.tensor_tensor_scan(pend[:], run[:], nbk[:], 0.0, ALU.mult, ALU.add), reads=[run, nbk], writes=[pend])
                k.op('dve', lambda e: e.tensor_tensor(pst[:], pend[:], nbk[:], ALU.subtract), reads=[pend, nbk], writes=[pst])
                k.op('dve', lambda e: e.tensor_scalar(pst[:], pst[:], float(BS), None, ALU.mult), reads=[pst], writes=[pst])
                k.op('pool', lambda e: e.memset(run[:], 0.0), reads=[run], writes=[run])
                for i in range(NT):
                    k.op('pe', lambda e: e.matmul(prk[:], triS[:], OHb[:, i, :], start=True, stop=True), reads=[triS, OHb], writes=[prk])
                    k.op('pe', lambda e: e.matmul(ptt[:], onb[:], OHb[:, i, :], start=True, stop=True), reads=[onb, OHb], writes=[ptt])
                    k.op('dve', lambda e: e.tensor_tensor(RK[:, i, :], prk[:], run[:], ALU.add), reads=[prk, run], writes=[RK])
                    k.op('dve', lambda e: e.tensor_tensor(run[:], run[:], ptt[:], ALU.add), reads=[run, ptt], writes=[run])
                k.op('dve', lambda e: e.tensor_tensor(RK[:], RK[:], pst[:].unsqueeze(1).to_broadcast([128, NT, 32]), ALU.add), reads=[RK, pst], writes=[RK])
                for j, OH in enumerate((OH1, OH2)):
                    k.op('dve', lambda e: e.tensor_tensor(OH[:], OH[:], RK[:], ALU.mult), reads=[OH, RK], writes=[OH])
                    k.op('dve', lambda e: e.tensor_reduce(dsf[:, :, j], OH[:], AX.X, ALU.add), reads=[OH], writes=[dsf])
                k.op('dve', lambda e: e.tensor_copy(DST[:], dsf[:]), reads=[dsf], writes=[DST])
                k.op('dve', lambda e: e.tensor_tensor(bigb[:], pend[:].unsqueeze(1).to_broadcast([128, NBLK, 32]), blki[:].unsqueeze(2).to_broadcast([128, NBLK, 32]), ALU.is_le),
                     reads=[pend, blki], writes=[bigb])
                k.op('dve', lambda e: e.tensor_reduce(bexp[:], bigb[:], AX.X, ALU.add), reads=[bigb], writes=[bexp])
                k.op('dve', lambda e: e.tensor_scalar(bexp[:], bexp[:], 31.0, None, ALU.min), reads=[bexp], writes=[bexp])
                k.op('dve', lambda e: e.tensor_scalar(widxf[:, :, 0:8], bexp[:].unsqueeze(2).to_broadcast([128, NBLK, 8]), 256.0, None, ALU.mult), reads=[bexp], writes=[widxf])
                k.op('dve', lambda e: e.tensor_scalar(widxf[:, :, 8:12], bexp[:].unsqueeze(2).to_broadcast([128, NBLK, 4]), 256.0, None, ALU.mult), reads=[bexp], writes=[widxf])
                k.op('dve', lambda e: e.tensor_tensor(widxf[:], widxf[:], kcp[:].unsqueeze(1).to_broadcast([128, NBLK, 12]), ALU.add), reads=[widxf, kcp], writes=[widxf])
                k.op('dve', lambda e: e.tensor_copy(WIDX[:], widxf[:]), reads=[widxf], writes=[WIDX])
                for i in range(NT):
                    for j in range(2):
                        k.dma('pool', TOKB[:, :], tokid[:, i, :], reads=[tokid, DST], pw=[TOKB],
                              indirect=(bass.IndirectOffsetOnAxis(ap=DST[:, i, j:j + 1], axis=0), None))
            k.barrier()
            with ExitStack() as es7:
                k.es = es7
                if P4S < 3:
                    raise _Stop()
                wg = [k.sb("wg%d" % i, [128, 8, 512], BF16) for i in range(3)]
                wu = [k.sb("wu%d" % i, [128, 8, 512], BF16) for i in range(3)]
                wd = [k.sb("wd%d" % i, [128, 4, D], BF16) for i in range(3)]
                class _E:
                    pass
                ES = []
                NES = 4
                for si in range(NES):
                    E = _E()
                    E.tki = k.sb("tki", [128, 16], I32)
                    E.xg = k.sb("xg", [128, D], BF16)
                    E.xgT = k.sb("xgT", [128, 8, 128], BF16)
                    E.gs = k.sb("gs", [128, 512], F32)
                    E.hb = k.sb("hb", [128, 512], BF16)
                    E.hbT = k.sb("hbT", [128, 4, 128], BF16)
                    E.yb = k.sb("yb", [128, D], F32)
                    E.bG = k.ps("p6g", [128, 512])
                    E.bU = k.ps("p6u", [128, 512])
                    E.bY = E.bG
                    ES.append(E)

                def load_w(bk):
                    q = bk % 3
                    for hf in range(2):
                        ix = bass.IndirectOffsetOnAxis(ap=WIDX[:, bk, hf:hf + 1], axis=0)
                        k.dma('pool', wg[q][:, hf * 4:(hf + 1) * 4, :].rearrange("p a b -> p (a b)"), ex_gate[:, :], reads=[WIDX], pw=[wg[q]], indirect=(None, ix))
                        k.dma('pool', wu[q][:, hf * 4:(hf + 1) * 4, :].rearrange("p a b -> p (a b)"), ex_up[:, :], reads=[WIDX], pw=[wu[q]], indirect=(None, ix))
                        k.dma('pool', wd[q][:, hf * 2:(hf + 1) * 2, :].rearrange("p a b -> p (a b)"), ex_down[:, :], reads=[WIDX], pw=[wd[q]], indirect=(None, ix))

                def subtile(E, bk, sub):
                    q = bk % 3
                    r0 = (bk * SUB + sub) * 128
                    k.dma('sp', E.tki[:], TOKB[r0:r0 + 128, :], reads=[TOKB], writes=[E.tki])
                    k.dma('pool', E.xg[:], H2[:, :], reads=[H2, E.tki], writes=[E.xg],
                          indirect=(None, bass.IndirectOffsetOnAxis(ap=E.tki[:, 0:1], axis=0)))
                    yield
                    pxt = E.bU[:].bitcast(BF16)
                    xv = E.xg[:].rearrange("p (j kc) -> p kc j", kc=8)
                    for kc in range(8):
                        k.op('pe', lambda e: e.transpose(pxt[:, kc * 128:(kc + 1) * 128], xv[:, kc, :], identb[:]), reads=[E.xg, identb], writes=[E.bU] if kc == 0 else [], inc=(kc == 7))
                    k.op('act', lambda e: e.activation(E.xgT[:].rearrange("p a b -> p (a b)"), pxt, AF.Identity), reads=[E.bU], writes=[E.xgT])
                    yield
                    for kc in range(8):
                        k.op('pe', lambda e: e.matmul(E.bG[:], E.xgT[:, kc, :], wg[q][:, kc, :], start=(kc == 0), stop=(kc == 7)), reads=[E.xgT, wg[q]], writes=[E.bG] if kc == 0 else [], inc=(kc == 7))
                    for kc in range(8):
                        k.op('pe', lambda e: e.matmul(E.bU[:], E.xgT[:, kc, :], wu[q][:, kc, :], start=(kc == 0), stop=(kc == 7)), reads=[E.xgT, wu[q]], writes=[E.bU] if kc == 0 else [], inc=(kc == 7))
                    yield
                    k.op('act', lambda e: e.activation(E.gs[:], E.bG[:], AF.Silu), reads=[E.bG], writes=[E.gs])
                    k.op('dve', lambda e: e.tensor_tensor(E.hb[:], E.gs[:], E.bU[:], ALU.mult), reads=[E.gs, E.bU], writes=[E.hb])
                    yield
                    pht = E.bG[:].bitcast(BF16)
                    hv = E.hb[:].rearrange("p (j fc) -> p fc j", fc=4)
                    for fc in range(4):
                        k.op('pe', lambda e: e.transpose(pht[:, fc * 128:(fc + 1) * 128], hv[:, fc, :], identb[:]), reads=[E.hb, identb], writes=[E.bG] if fc == 0 else [], inc=(fc == 3))
                    k.op('dve', lambda e: e.tensor_copy(E.hbT[:].rearrange("p a b -> p (a b)"), pht[:, 0:512]), reads=[E.bG], writes=[E.hbT])
                    yield
                    for n in range(2):
                        for fc in range(4):
                            k.op('pe', lambda e: e.matmul(E.bY[:], E.hbT[:, fc, :], wd[q][:, fc, n * 512:(n + 1) * 512], start=(fc == 0), stop=(fc == 3)),
                                 reads=[E.hbT, wd[q]], writes=[E.bY] if fc == 0 else [], inc=(fc == 3))
                        k.op('act', lambda e: e.activation(E.yb[:, n * 512:(n + 1) * 512], E.bY[:], AF.Identity), reads=[E.bY], writes=[E.yb])
                        yield
                    k.dma('sp', YB[r0:r0 + 128, :], E.yb[:], reads=[E.yb], pw=[YB])

                def estream(si):
                    for gsub in range(si, NBLK * SUB, NES):
                        bk, sub = gsub // SUB, gsub % SUB
                        if True:
                            for bb in (bk, bk + 1):
                                if bb < NBLK and bb not in loaded:
                                    loaded.add(bb)
                                    load_w(bb)
                        yield from subtile(ES[si], bk, sub)
                        yield

                loaded = set()
                gens = [estream(i) for i in range(NES)]
                alive = [True] * NES
                while any(alive):
                    for gi in range(NES):
                        if alive[gi]:
                            try:
                                next(gens[gi])
                            except StopIteration:
                                alive[gi] = False
            k.barrier()
            with ExitStack() as es8:
                k.es = es8
                if P4S < 4:
                    raise _Stop()
                class _C:
                    pass
                CS = []
                for si in range(2):
                    C = _C()
                    C.x6 = k.sb("x6", [128, D], F32)
                    C.y0 = k.sb("y0", [128, D], F32)
                    C.y1 = k.sb("y1", [128, D], F32)
                    C.o = k.sb("o6", [128, D], F32)
                    C.st6 = k.sb("st6", [128, 2, 6], F32)
                    C.mv6 = k.sb("mv6", [128, 2], F32)
                    C.rs6 = k.sb("rs6", [128, 1], F32)
                    C.nb6 = k.sb("nb6", [128, 1], F32)
                    CS.append(C)

                def ctile(C, gi):
                    b, i = gi // (SEQ // 128), gi % (SEQ // 128)
                    x6, y0, y1, o, st6, mv6, rs6, nb6 = C.x6, C.y0, C.y1, C.o, C.st6, C.mv6, C.rs6, C.nb6
                    k.dma('sp', x6[:], X1[gi * 128:(gi + 1) * 128, :], reads=[X1], writes=[x6])
                    k.dma('pool', y0[:], YB[:, :], reads=[YB, DST], writes=[y0],
                          indirect=(None, bass.IndirectOffsetOnAxis(ap=DST[:, gi, 0:1], axis=0)))
                    k.dma('pool', y1[:], YB[:, :], reads=[YB, DST], writes=[y1],
                          indirect=(None, bass.IndirectOffsetOnAxis(ap=DST[:, gi, 1:2], axis=0)))
                    yield
                    k.op('act', lambda e: e.activation(y0[:], y0[:], AF.Identity, scale=W1[:, gi:gi + 1]), reads=[y0, W1], writes=[y0])
                    k.op('dve', lambda e: e.scalar_tensor_tensor(y0[:], y1[:], W2[:, gi:gi + 1], y0[:], ALU.mult, ALU.add), reads=[y1, W2, y0], writes=[y0])
                    yield
                    k.op('dve', lambda e: e.tensor_tensor(y0[:], y0[:], g2b[:, b, :], ALU.mult), reads=[y0, g2b], writes=[y0])
                    k.op('dve', lambda e: e.scalar_tensor_tensor(o[:], x6[:], ALPHA, y0[:], ALU.mult, ALU.add), reads=[x6, y0], writes=[o])
                    yield
                    for hf in range(2):
                        k.op('dve', lambda e: e.bn_stats(st6[:, hf, :], o[:, hf * 512:(hf + 1) * 512]), reads=[o], writes=[st6])
                    k.op('dve', lambda e: e.bn_aggr(mv6[:], st6[:].rearrange("p a b -> p (a b)")), reads=[st6], writes=[mv6])
                    yield
                    k.op('act', lambda e: e.activation(rs6[:], mv6[:, 1:2], AF.Sqrt, bias=LN_EPS), reads=[mv6], writes=[rs6])
                    yield
                    k.op('dve', lambda e: e.reciprocal(rs6[:], rs6[:]), reads=[rs6], writes=[rs6])
                    k.op('dve', lambda e: e.scalar_tensor_tensor(nb6[:], mv6[:, 0:1], -1.0, rs6[:], ALU.mult, ALU.mult), reads=[mv6, rs6], writes=[nb6])
                    yield
                    k.op('act', lambda e: e.activation(o[:], o[:], AF.Identity, bias=nb6[:, 0:1], scale=rs6[:, 0:1]), reads=[o, nb6, rs6], writes=[o])
                    yield
                    k.op('dve', lambda e: e.tensor_tensor(o[:], o[:], lnp[:, 2, :], ALU.mult), reads=[o, lnp], writes=[o])
                    k.op('dve', lambda e: e.tensor_tensor(o[:], o[:], lnp[:, 3, :], ALU.add), reads=[o, lnp], writes=[o])
                    k.dma('sp', out_d[b, i * 128:(i + 1) * 128, :], o[:], reads=[o])

                def cstream(si):
                    for gi in range(si, NT, 2):
                        yield from ctile(CS[si], gi)
                        yield
                gens = [cstream(0), cstream(1)]
                alive = [True, True]
                while any(alive):
                    for gq in range(2):
                        if alive[gq]:
                            try:
                                next(gens[gq])
                            except StopIteration:
                                alive[gq] = False
           except _Stop:
            pass
        k.barrier()

        k.barrier()
    return nc


def host_inputs(inputs, batches, NB):
    f = lambda a: np.ascontiguousarray(a, dtype=np.float32)
    bs = list(batches)
    m = {}
    m["x"] = f(inputs["x"][bs])
    m["ctx"] = f(inputs["ctx"][bs])
    cc = np.zeros((3, D), np.float32)
    for i, b in enumerate(bs):
        cc[i] = inputs["c"][b]
    cc[2] = inputs["c_ctx"]
    m["cc"] = cc
    m["w_ada"] = f(inputs["w_ada"][0])
    m["b_ada"] = f(inputs["b_ada"][0][None, :])
    m["w_in"] = f(inputs["w_in"][0])
    m["conv_w"] = f(inputs["conv_w"][0].reshape(9, 2560))
    bi, bf = inputs["m_bias_i"][0], inputs["m_bias_f"][0]
    m["m_bias"] = f(np.concatenate([bi[0], bf[0], bi[1], bf[1]])[:, None])
    m["ident"] = np.eye(128, dtype=np.float32)
    gm = np.zeros((32, 2), np.float32)
    gm[0:8, 0] = 1; gm[16:24, 0] = 1; gm[8:16, 1] = -1; gm[24:32, 1] = -1
    m["gmask"] = gm
    ii = np.arange(64)
    m["cmask"] = np.stack([(ii[:, None] <= ii[None, :]), (ii[:, None] >= ii[None, :])], axis=1).astype(np.float32)
    m["m_norm_w"] = f(inputs["m_norm_w"][0][None, :])
    hk = lambda v: np.asarray(v, np.float32).reshape(4, 128).T
    m["rp"] = f(np.stack([hk(inputs["r_w0"][0][0]), hk(inputs["r_w0"][0][1]), hk(inputs["r_a0"][0]), hk(inputs["r_kk"][0]),
                          hk(inputs["r_ka"][0]), hk(inputs["r_ka"][0]), hk(inputs["r_bonus"][0].reshape(-1))], axis=2))
    sm = np.ones((128, 1088), np.float32); sm[:, ::64] = 0
    obd = np.zeros((128, 128), np.float32); obd[:64, :64] = 1; obd[64:, 64:] = 1
    m["onesbd"] = obd
    m["smask"] = sm
    jj = np.arange(128) % 64
    tt = np.arange(128)
    rm = np.zeros((128, 2, 128), np.float32)
    for dd in range(2):
        for col in range(128):
            tq = col % 64
            if col < 64:
                rm[:, dd, col] = (jj < tq) if dd == 0 else (jj > tq)
            else:
                rm[:, dd, col] = (jj <= tq) if dd == 0 else (jj >= tq)
    m["rmask"] = rm
    m["nmask"] = np.stack([(ii[None, :] < ii[:, None]), (ii[None, :] > ii[:, None])], axis=1).astype(np.float32)
    m["r_norm_w"] = f(inputs["r_norm_w"][0][None, :])
    m["r_norm_b"] = f(inputs["r_norm_b"][0][None, :])
    m["r_wB"] = f(inputs["r_wB"][0])
    m["r_aB"] = f(inputs["r_aB"][0])
    m["r_gB"] = f(inputs["r_gB"][0])
    m["w_out"] = f(inputs["w_out"][0])
    for nm in ("ln1_g", "ln1_b", "ln2_g", "ln2_b"):
        m[nm] = f(inputs[nm][0][None, :])
    m["rt"] = f(np.concatenate([inputs["rt_g"][0], inputs["rt_e"][0]], axis=1))
    m["rtb"] = f(np.concatenate([inputs["rt_g_b"][0], inputs["rt_e_b"][0]])[None, :])
    m["ex_gate"] = f(inputs["ex_gate"][0].reshape(8192, 2048))
    m["ex_up"] = f(inputs["ex_up"][0].reshape(8192, 2048))
    m["ex_down"] = f(inputs["ex_down"][0].reshape(8192, 2048))
    pp = np.arange(128)
    m["tris"] = (pp[:, None] < pp[None, :]).astype(np.float32)
    m["thr"] = np.broadcast_to((512.0 * pp)[None, :], (128, 128)).astype(np.float32).copy()
    m["blki"] = np.broadcast_to(np.arange(160, dtype=np.float32)[None, :], (128, 160)).copy()
    m["kcp"] = (2 * pp[:, None] + (np.arange(12) % 2)[None, :]).astype(np.float32)
    m["tokid"] = np.broadcast_to((np.arange(64)[None, :] * 128 + pp[:, None])[:, :, None], (128, 64, 16)).astype(np.int32).copy()
    return m


_NC_CACHE = {}


def kernel(**inputs):
    inputs = {k_: np.asarray(v) for k_, v in inputs.items()}
    NB = 2
    n_cores = 8
    if NB not in _NC_CACHE:
        _NC_CACHE[NB] = build(NB=NB)
    nc = _NC_CACHE[NB]
    in_maps = [host_inputs(inputs, [NB * c + j for j in range(NB)], NB) for c in range(n_cores)]
    res = run_bass_kernel_spmd(nc, in_maps, core_ids=list(range(n_cores)))
    out = np.concatenate([np.asarray(r["out"]) for r in res.results], axis=0)
    return np.ascontiguousarray(out, dtype=np.float32)
```
